# Optimizing a Trainium2 kernel written in Bass

```python
import jax
import jax.numpy as jnp
from jax import lax
import numpy as np

D_MODEL = 1024
BATCH = 16
SEQ = 2048
DEPTH = 2

GRID_W = 64
CTX_LEN = 256
HEAD_DIM = 64
N_BRANCH = 4
BRANCH_W = 256
MLA_HEADS = 4
MLA_NOPE = 64
MLA_ROPE = 32
MLA_V = 64
MLA_Q_RANK = 256
MLA_KV_RANK = 256
ML_HEADS = 4
ML_HEAD_DIM = 64
ML_CHUNK = 128
WG_HEADS = 4
WG_KV_HEADS = 2
WINDOW = 128
Q_BLOCK = 128
FN_GROUPS = 4
FN_GROUP_W = 64
D_FF = 2816
ROPE_BASE = 10000.0
EPS = 1e-6
IN_SPLITS = (MLA_Q_RANK, MLA_KV_RANK, MLA_ROPE,
             ML_HEADS * ML_HEAD_DIM, ML_HEADS * ML_HEAD_DIM, ML_HEADS * ML_HEAD_DIM, ML_HEADS * ML_HEAD_DIM, 4 * ML_HEADS,
             WG_HEADS * HEAD_DIM, WG_KV_HEADS * HEAD_DIM, WG_KV_HEADS * HEAD_DIM,
             FN_GROUPS * FN_GROUP_W)
D_IN = sum(IN_SPLITS)

kernel_name = "hybrid_dit_mla_mlstm_swa_fnet"


def rmsnorm(x, g):
    x32 = x.astype(jnp.float32)
    y = x32 * lax.rsqrt(jnp.mean(x32 * x32, axis=-1, keepdims=True) + EPS)
    return (y * g.astype(jnp.float32)).astype(x.dtype)


def split_cols(u, sizes):
    idx = [int(i) for i in np.cumsum(sizes)[:-1]]
    return jnp.split(u, idx, axis=-1)


def to_heads(u, n_heads):
    b, n, _ = u.shape
    return u.reshape(b, n, n_heads, -1).transpose(0, 2, 1, 3)


def from_heads(y):
    b, h, n, d = y.shape
    return y.transpose(0, 2, 1, 3).reshape(b, n, h * d)


def axial_rope(x, row, col):
    d = x.shape[-1]
    half = d // 2
    nf = half // 2
    inv = ROPE_BASE ** (-jnp.arange(nf, dtype=jnp.float32) / nf)
    ang_r = row.astype(jnp.float32)[:, None] * inv
    ang_c = col.astype(jnp.float32)[:, None] * inv

    def rot(xh, ang):
        x1, x2 = xh[..., :nf], xh[..., nf:]
        cos, sin = jnp.cos(ang), jnp.sin(ang)
        return jnp.concatenate([x1 * cos - x2 * sin, x1 * sin + x2 * cos], axis=-1)

    x32 = x.astype(jnp.float32)
    out = jnp.concatenate([rot(x32[..., :half], ang_r), rot(x32[..., half:], ang_c)], axis=-1)
    return out.astype(x.dtype)


def dwconv3(x, w):
    xp = jnp.pad(x, ((0, 0), (1, 1), (0, 0)))
    return xp[:, :-2] * w[0] + xp[:, 1:-1] * w[1] + xp[:, 2:] * w[2]


def softmax_attend(q, k, v, scale):
    s = jnp.einsum('bhqd,bhkd->bhqk', q, k).astype(jnp.float32) * scale
    p = jax.nn.softmax(s, axis=-1).astype(v.dtype)
    return jnp.einsum('bhqk,bhkd->bhqd', p, v)


def blocked_dense_attend(q, k, v, scale):
    b, h, n, d = q.shape
    nb = n // Q_BLOCK
    qb = q.reshape(b, h, nb, Q_BLOCK, d).transpose(2, 0, 1, 3, 4)
    out = lax.map(lambda qi: softmax_attend(qi, k, v, scale), qb)
    return out.transpose(1, 2, 0, 3, 4).reshape(b, h, n, -1)


def window_attend(q, k, v, kc, vc, sink):
    b, hq, n, d = q.shape
    hkv = k.shape[1]
    g = hq // hkv
    nb = n // Q_BLOCK
    scale = d ** -0.5
    qb = q.reshape(b, hkv, g, nb, Q_BLOCK, d)

    def band(t):
        tp = jnp.pad(t, ((0, 0), (0, 0), (WINDOW, WINDOW), (0, 0))).reshape(b, hkv, nb + 2, Q_BLOCK, d)
        return jnp.concatenate([tp[:, :, :-2], tp[:, :, 1:-1], tp[:, :, 2:]], axis=3)

    kb, vb = band(k), band(v)
    blk = jnp.arange(nb)[:, None, None]
    qpos = blk * Q_BLOCK + jnp.arange(Q_BLOCK)[None, :, None]
    kpos = blk * Q_BLOCK - WINDOW + jnp.arange(3 * Q_BLOCK)[None, None, :]
    valid = (jnp.abs(kpos - qpos) <= WINDOW) & (kpos >= 0) & (kpos < n)
    s_loc = jnp.einsum('bhgnqd,bhnkd->bhgnqk', qb, kb).astype(jnp.float32) * scale
    s_loc = jnp.where(valid, s_loc, -jnp.inf)
    s_ctx = jnp.einsum('bhgnqd,bhcd->bhgnqc', qb, kc).astype(jnp.float32) * scale
    c_len = kc.shape[2]
    s_sink = jnp.broadcast_to(sink.astype(jnp.float32).reshape(1, hkv, g, 1, 1, 1), (b, hkv, g, nb, Q_BLOCK, 1))
    p = jax.nn.softmax(jnp.concatenate([s_sink, s_ctx, s_loc], axis=-1), axis=-1).astype(v.dtype)
    out = (jnp.einsum('bhgnqc,bhcd->bhgnqd', p[..., 1:1 + c_len], vc)
           + jnp.einsum('bhgnqk,bhnkd->bhgnqd', p[..., 1 + c_len:], vb))
    return out.reshape(b, hq, n, d)


def ctx_sink_attend(q, kc, vc, sink):
    b, hq, c_len, d = q.shape
    hkv = kc.shape[1]
    g = hq // hkv
    qg = q.reshape(b, hkv, g, c_len, d)
    s = jnp.einsum('bhgqd,bhkd->bhgqk', qg, kc).astype(jnp.float32) * d ** -0.5
    s_sink = jnp.broadcast_to(sink.astype(jnp.float32).reshape(1, hkv, g, 1, 1), (b, hkv, g, c_len, 1))
    p = jax.nn.softmax(jnp.concatenate([s_sink, s], axis=-1), axis=-1)[..., 1:].astype(vc.dtype)
    return jnp.einsum('bhgqk,bhkd->bhgqd', p, vc).reshape(b, hq, c_len, d)


def mla_q(cq, g_qa, w_uq, pos):
    q = to_heads(rmsnorm(cq, g_qa) @ w_uq, MLA_HEADS)
    if pos is not None:
        q = jnp.concatenate([q[..., :MLA_NOPE], axial_rope(q[..., MLA_NOPE:], *pos)], axis=-1)
    return q


def mla_kv(ckv, kr, g_kva, w_ukv, pos):
    kv = to_heads(rmsnorm(ckv, g_kva) @ w_ukv, MLA_HEADS)
    k_nope, v = kv[..., :MLA_NOPE], kv[..., MLA_NOPE:]
    kr = kr[:, None]
    if pos is not None:
        kr = axial_rope(kr, *pos)
    k = jnp.concatenate([k_nope, jnp.broadcast_to(kr, k_nope.shape[:3] + (MLA_ROPE,))], axis=-1)
    return k, v


def mlstm_scan(q, k, v, log_i, log_f, state):
    bsz, nh, n, d = q.shape
    nc = n // ML_CHUNK

    def chunks(t):
        t = t.astype(jnp.float32)
        return jnp.moveaxis(t.reshape(t.shape[:2] + (nc, ML_CHUNK) + t.shape[3:]), 2, 0)

    tril = jnp.tril(jnp.ones((ML_CHUNK, ML_CHUNK), dtype=bool))

    def step(carry, xs):
        c_st, n_st, m_st = carry
        qc, kc, vc, li, lf = xs
        cum = jnp.cumsum(lf, axis=-1)
        log_d = jnp.where(tril, cum[..., :, None] - cum[..., None, :] + li[..., None, :], -jnp.inf)
        log_inter = cum + m_st[..., None]
        m_t = jnp.maximum(log_inter, jnp.max(log_d, axis=-1))
        w_intra = jnp.exp(log_d - m_t[..., None])
        w_inter = jnp.exp(log_inter - m_t)
        s = jnp.einsum('bhtd,bhsd->bhts', qc, kc) * w_intra
        num = jnp.einsum('bhts,bhse->bhte', s, vc) + w_inter[..., None] * jnp.einsum('bhtd,bhde->bhte', qc, c_st)
        den = jnp.sum(s, axis=-1) + w_inter * jnp.einsum('bhtd,bhd->bht', qc, n_st)
        h = num / jnp.maximum(jnp.abs(den), jnp.exp(-m_t))[..., None]
        c_last = cum[..., -1]
        log_w = c_last[..., None] - cum + li
        m_new = jnp.maximum(c_last + m_st, jnp.max(log_w, axis=-1))
        w = jnp.exp(log_w - m_new[..., None])
        decay = jnp.exp(c_last + m_st - m_new)
        c_new = decay[..., None, None] * c_st + jnp.einsum('bhs,bhsd,bhse->bhde', w, kc, vc)
        n_new = decay[..., None] * n_st + jnp.einsum('bhs,bhsd->bhd', w, kc)
        return (c_new, n_new, m_new), h

    xs = (chunks(q), chunks(k), chunks(v), chunks(log_i), chunks(log_f))
    state, h = lax.scan(step, state, xs)
    return jnp.moveaxis(h, 0, 2).reshape(bsz, nh, n, d).astype(q.dtype), state


def mlstm_prep(uq, uk, uv, ug, w_conv, b_gates):
    qk = jax.nn.silu(dwconv3(jnp.concatenate([uq, uk], axis=-1), w_conv))
    q, k = jnp.split(qk, 2, axis=-1)
    q = to_heads(q, ML_HEADS)
    k = to_heads(k, ML_HEADS) * ML_HEAD_DIM ** -0.5
    v = to_heads(uv, ML_HEADS)
    bsz, n, _ = ug.shape
    gt = (ug.astype(jnp.float32) + b_gates.astype(jnp.float32)).reshape(bsz, n, 4, ML_HEADS).transpose(2, 0, 3, 1)
    return q, k, v, gt[0], jax.nn.log_sigmoid(gt[1]), gt[2], jax.nn.log_sigmoid(gt[3])


def mlstm_branch(lat, ctx, w_conv, b_gates, ctx_out):
    uq, uk, uv, uo, ug = lat
    cq, ck, cv, co, cg = ctx
    q, k, v, i_f, f_f, i_b, f_b = mlstm_prep(uq, uk, uv, ug, w_conv, b_gates)
    qc, kc, vc, ic_f, fc_f, ic_b, fc_b = mlstm_prep(cq, ck, cv, cg, w_conv, b_gates)
    bsz = q.shape[0]
    zero = (jnp.zeros((bsz, ML_HEADS, ML_HEAD_DIM, ML_HEAD_DIM), jnp.float32),
            jnp.zeros((bsz, ML_HEADS, ML_HEAD_DIM), jnp.float32),
            jnp.zeros((bsz, ML_HEADS), jnp.float32))
    flip = lambda t: jnp.flip(t, axis=2)
    hc_f, st_f = mlstm_scan(qc, kc, vc, ic_f, fc_f, zero)
    h_f, _ = mlstm_scan(q, k, v, i_f, f_f, st_f)
    hc_b, st_b = mlstm_scan(flip(qc), flip(kc), flip(vc), flip(ic_b), flip(fc_b), zero)
    h_b, _ = mlstm_scan(flip(q), flip(k), flip(v), flip(i_b), flip(f_b), st_b)
    y = jax.nn.sigmoid(uo) * from_heads(h_f + flip(h_b))
    if not ctx_out:
        return y, None
    yc = jax.nn.sigmoid(co) * from_heads(hc_f + flip(hc_b))
    return y, yc


def fourier_mix(u):
    bsz, n, _ = u.shape
    z = jnp.fft.fftn(u.astype(jnp.float32).reshape(bsz, n, FN_GROUPS, FN_GROUP_W), axes=(1, 3), norm="ortho")
    return jnp.real(z).reshape(bsz, n, FN_GROUPS * FN_GROUP_W).astype(u.dtype)


def merge(h, ys, w_gate, b_gate, w_branch, w_out):
    acc = jax.nn.sigmoid(h @ w_gate[0] + b_gate[0]) * (ys[0] @ w_branch[0])
    for s in range(1, N_BRANCH):
        acc = acc + jax.nn.sigmoid(h @ w_gate[s] + b_gate[s]) * (ys[s] @ w_branch[s])
    return acc @ w_out


def token_mixer(h, hc, lp, row, col, ctx_out):
    u = split_cols(h @ lp["w_in"], IN_SPLITS)
    uc = split_cols(hc @ lp["w_in"], IN_SPLITS)
    pos = (row, col)
    scale_a = (MLA_NOPE + MLA_ROPE) ** -0.5
    qa = mla_q(u[0], lp["g_qa"], lp["w_uq"], pos)
    ka, va = mla_kv(u[1], u[2], lp["g_kva"], lp["w_ukv"], pos)
    kca, vca = mla_kv(uc[1], uc[2], lp["g_kva"], lp["w_ukv"], None)
    ya = from_heads(blocked_dense_attend(qa, jnp.concatenate([kca, ka], axis=2),
                                         jnp.concatenate([vca, va], axis=2), scale_a))
    yb, ybc = mlstm_branch(tuple(u[3:8]), tuple(uc[3:8]), lp["w_ml_conv"], lp["b_ml_gates"], ctx_out)
    qw = axial_rope(to_heads(u[8], WG_HEADS), row, col)
    kw = axial_rope(to_heads(u[9], WG_KV_HEADS), row, col)
    vw = to_heads(u[10], WG_KV_HEADS)
    kcw, vcw = to_heads(uc[9], WG_KV_HEADS), to_heads(uc[10], WG_KV_HEADS)
    yc = from_heads(window_attend(qw, kw, vw, kcw, vcw, lp["wg_sink"]))
    yd = fourier_mix(u[11])
    y = merge(h, [ya, yb, yc, yd], lp["w_gate"], lp["b_gate"], lp["w_branch"], lp["w_out"])
    if not ctx_out:
        return y, None
    yca = from_heads(softmax_attend(mla_q(uc[0], lp["g_qa"], lp["w_uq"], None), kca, vca, scale_a))
    ycc = from_heads(ctx_sink_attend(to_heads(uc[8], WG_HEADS), kcw, vcw, lp["wg_sink"]))
    ycd = fourier_mix(uc[11])
    y_ctx = merge(hc, [yca, ybc, ycc, ycd], lp["w_gate"], lp["b_gate"], lp["w_branch"], lp["w_out"])
    return y, y_ctx


def conv_ffn(h, w_up, w_conv, b_conv, w_down):
    a, v = jnp.split(h @ w_up, 2, axis=-1)
    a = dwconv3(a, w_conv) + b_conv
    return (jax.nn.silu(a) * v) @ w_down


def setup_inputs(seed: int = 0) -> dict:
    key = jax.random.key(seed)
    ks = jax.random.split(key, 32)
    f32 = jnp.float32
    L, D = DEPTH, D_MODEL

    def nrm(k, shape, scale=1.0):
        return jax.random.normal(k, shape, f32) * scale

    def gain(k, shape):
        return 1.0 + nrm(k, shape, 0.05)

    f_bias = jnp.linspace(3.0, 6.0, ML_HEADS, dtype=f32)
    b_ml_gates = jnp.concatenate([
        nrm(ks[14], (L, ML_HEADS), 0.1),
        f_bias + nrm(ks[15], (L, ML_HEADS), 0.1),
        nrm(ks[16], (L, ML_HEADS), 0.1),
        f_bias + nrm(ks[17], (L, ML_HEADS), 0.1)], axis=-1)
    return {
        "x": nrm(ks[0], (BATCH, SEQ, D)),
        "c": nrm(ks[1], (BATCH, D)),
        "ctx": nrm(ks[2], (BATCH, CTX_LEN, D)),
        "c_ctx": nrm(ks[3], (D,)),
        "w_mod": nrm(ks[4], (L, D, 6 * D), 0.5 * D ** -0.5),
        "b_mod": nrm(ks[5], (L, 6 * D), 0.02),
        "g_pre_mix": gain(ks[6], (L, D)),
        "g_post_mix": gain(ks[7], (L, D)),
        "g_pre_ffn": gain(ks[8], (L, D)),
        "g_post_ffn": gain(ks[9], (L, D)),
        "w_in": nrm(ks[10], (L, D, D_IN), D ** -0.5),
        "g_qa": gain(ks[11], (L, MLA_Q_RANK)),
        "w_uq": nrm(ks[12], (L, MLA_Q_RANK, MLA_HEADS * (MLA_NOPE + MLA_ROPE)), MLA_Q_RANK ** -0.5),
        "g_kva": gain(ks[13], (L, MLA_KV_RANK)),
        "w_ukv": nrm(ks[18], (L, MLA_KV_RANK, MLA_HEADS * (MLA_NOPE + MLA_V)), MLA_KV_RANK ** -0.5),
        "w_ml_conv": nrm(ks[19], (L, 3, 2 * ML_HEADS * ML_HEAD_DIM), 3 ** -0.5),
        "b_ml_gates": b_ml_gates,
        "wg_sink": nrm(ks[20], (L, WG_HEADS), 0.5),
        "w_gate": nrm(ks[21], (L, N_BRANCH, D, D), D ** -0.5),
        "b_gate": nrm(ks[22], (L, N_BRANCH, D), 0.02),
        "w_branch": nrm(ks[23], (L, N_BRANCH, BRANCH_W, D), BRANCH_W ** -0.5),
        "w_out": nrm(ks[24], (L, D, D), D ** -0.5),
        "w_up": nrm(ks[25], (L, D, 2 * D_FF), D ** -0.5),
        "w_ffn_conv": nrm(ks[26], (L, 3, D_FF), 3 ** -0.5),
        "b_ffn_conv": nrm(ks[27], (L, D_FF), 0.02),
        "w_down": nrm(ks[28], (L, D_FF, D), D_FF ** -0.5),
    }


def reference(x, c, ctx, c_ctx, w_mod, b_mod, g_pre_mix, g_post_mix, g_pre_ffn, g_post_ffn,
              w_in, g_qa, w_uq, g_kva, w_ukv, w_ml_conv, b_ml_gates, wg_sink,
              w_gate, b_gate, w_branch, w_out, w_up, w_ffn_conv, b_ffn_conv, w_down):
    n = x.shape[1]
    rows = n // GRID_W
    row = jnp.repeat(jnp.arange(rows, dtype=jnp.int32), GRID_W)
    col = jnp.tile(jnp.arange(GRID_W, dtype=jnp.int32), rows)
    xc = ctx
    for l in range(DEPTH):
        ctx_out = l < DEPTH - 1
        mod = jax.nn.silu(c) @ w_mod[l] + b_mod[l]
        mod_c = jax.nn.silu(c_ctx) @ w_mod[l] + b_mod[l]
        sh1, sc1, g1, sh2, sc2, g2 = jnp.split(mod[:, None, :], 6, axis=-1)
        shc1, scc1, gc1, shc2, scc2, gc2 = jnp.split(mod_c, 6, axis=-1)
        lp = {"w_in": w_in[l], "g_qa": g_qa[l], "w_uq": w_uq[l], "g_kva": g_kva[l], "w_ukv": w_ukv[l],
              "w_ml_conv": w_ml_conv[l], "b_ml_gates": b_ml_gates[l], "wg_sink": wg_sink[l],
              "w_gate": w_gate[l], "b_gate": b_gate[l], "w_branch": w_branch[l], "w_out": w_out[l]}
        h = rmsnorm(x, g_pre_mix[l]) * (1 + sc1) + sh1
        hc = rmsnorm(xc, g_pre_mix[l]) * (1 + scc1) + shc1
        y, y_ctx = token_mixer(h, hc, lp, row, col, ctx_out)
        x = x + g1 * rmsnorm(y, g_post_mix[l])
        h2 = rmsnorm(x, g_pre_ffn[l]) * (1 + sc2) + sh2
        x = x + g2 * rmsnorm(conv_ffn(h2, w_up[l], w_ffn_conv[l], b_ffn_conv[l], w_down[l]), g_post_ffn[l])
        if ctx_out:
            xc = xc + gc1 * rmsnorm(y_ctx, g_post_mix[l])
            hc2 = rmsnorm(xc, g_pre_ffn[l]) * (1 + scc2) + shc2
            xc = xc + gc2 * rmsnorm(conv_ffn(hc2, w_up[l], w_ffn_conv[l], b_ffn_conv[l], w_down[l]), g_post_ffn[l])
    return x
```

```python
from contextlib import ExitStack
import numpy as np
import ml_dtypes
import concourse.bass as bass
import concourse.mybir as mybir
from concourse.bass_utils import run_bass_kernel_spmd

F32 = mybir.dt.float32
BF16 = mybir.dt.bfloat16
AF = mybir.ActivationFunctionType
ALU = mybir.AluOpType
AX = mybir.AxisListType
ENG = ("pe", "act", "dve", "pool", "sp")
NBIG = -30000.0
D = 1024
KC = 8
DFF = 2816
FC = 22
EPS = 1e-6


class Res:
    __slots__ = ("name", "w", "re", "rd")

    def __init__(self, name=""):
        self.name = name
        self.w = None
        self.re = {}
        self.rd = []


class Prog:
    N_DMA_SEMS = 80

    def __init__(self, nc, stack):
        self.nc = nc
        self.esem = {e: stack.enter_context(nc.semaphore("es_" + e)) for e in ENG}
        self.dsem = [stack.enter_context(nc.semaphore("ds%d" % i)) for i in range(self.N_DMA_SEMS)]
        self.dval = [0] * self.N_DMA_SEMS
        self.dnext = 0
        self.cnt = {e: 0 for e in ENG}
        self.seen = {e: {} for e in ENG}
        self.q = {e: [] for e in ENG}
        self.n_ops = 0

    def _deps(self, reads, writes):
        deps = []
        for r in reads:
            if r.w is not None:
                deps.append(r.w)
        for w in writes:
            if w.w is not None:
                deps.append(w.w)
            for e, c in w.re.items():
                deps.append(("E", e, c))
            deps.extend(w.rd)
        return deps

    def _waits(self, eng, deps):
        seen = self.seen[eng]
        need = {}
        for kind, key, val in deps:
            k = (kind, key)
            if seen.get(k, 0) >= val:
                continue
            if kind == "E" and key == "pe" and eng == "pe":
                continue
            if need.get(k, 0) < val:
                need[k] = val
        out = []
        for k, val in need.items():
            seen[k] = val
            sem = self.esem[k[1]] if k[0] == "E" else self.dsem[k[1]]
            out.append((sem, val))
        return out

    def op(self, eng, fn, reads=(), writes=()):
        waits = self._waits(eng, self._deps(reads, writes))
        self.cnt[eng] += 1
        c = self.cnt[eng]
        tok = ("E", eng, c)
        self.q[eng].append((waits, fn, (self.esem[eng], 1)))
        for r in reads:
            if r.re.get(eng, 0) < c:
                r.re[eng] = c
        for w in writes:
            w.w = tok
            w.re = {}
            w.rd = []
        self.n_ops += 1
        return tok

    def dma(self, qeng, out, in_, reads=(), writes=(), **kw):
        deps = self._deps(reads, writes)
        i = self.dnext
        self.dnext = (self.dnext + 1) % self.N_DMA_SEMS
        prev = self.dval[i]
        if prev:
            deps.append(("D", i, prev))
        waits = self._waits(qeng, deps)
        self.dval[i] = prev + 16
        tok = ("D", i, prev + 16)
        self.q[qeng].append((waits, (lambda e, o=out, s=in_, k=kw: e.dma_start(out=o, in_=s, **k)),
                             (self.dsem[i], 16)))
        for r in reads:
            r.rd.append(tok)
        for w in writes:
            w.w = tok
            w.re = {}
            w.rd = []
        self.n_ops += 1
        return tok

    def barrier(self):
        deps = [("E", e, self.cnt[e]) for e in ENG if self.cnt[e]]
        deps += [("D", i, v) for i, v in enumerate(self.dval) if v]
        for e in ENG:
            waits = self._waits(e, deps)
            if waits:
                self.q[e].append((waits, None, None))

    def emit(self):
        nc = self.nc
        allsems = [self.esem[e] for e in ENG] + self.dsem
        with nc.Block("init") as b0:
            @b0.vector
            def _(v):
                for s in allsems:
                    v.sem_clear(s)
        with nc.Block("main") as blk:
            def run(e, name):
                for waits, fn, inc in self.q[name]:
                    for sem, val in waits:
                        e.wait_ge(sem, val)
                    if fn is not None:
                        fn(e).then_inc(inc[0], inc[1])

            @blk.tensor
            def _(e):
                run(e, "pe")

            @blk.scalar
            def _(e):
                run(e, "act")

            @blk.vector
            def _(e):
                run(e, "dve")

            @blk.gpsimd
            def _(e):
                run(e, "pool")

            @blk.sync
            def _(e):
                run(e, "sp")


def _rope_tab(rd, pos_row, pos_col, n_ctx):
    half = rd // 2
    nf = half // 2
    inv = 10000.0 ** (-np.arange(nf, dtype=np.float64) / nf)
    n = len(pos_row)
    cos = np.ones((rd, n_ctx + n), np.float64)
    sin = np.zeros((rd, n_ctx + n), np.float64)
    for r in range(rd):
        hh, rr = r // half, r % half
        idx = rr % nf
        pos = pos_row if hh == 0 else pos_col
        ang = pos.astype(np.float64) * inv[idx]
        ang = (pos.astype(np.float32) * np.float32(inv[idx]).astype(np.float32)).astype(np.float64)
        cos[r, n_ctx:] = np.cos(ang)
        sin[r, n_ctx:] = np.sin(ang)
    return cos, sin


def make_consts(NLAT, NCTX):
    bf = ml_dtypes.bfloat16
    T = NLAT + NCTX
    c = {}
    c["ident_bf"] = np.eye(128).astype(bf)
    c["ones_bf"] = np.ones((128, 128)).astype(bf)
    c["ident_f"] = np.eye(128, dtype=np.float32)
    c["ones_f"] = np.ones((128, 128), np.float32)
    s = np.arange(128)[:, None]
    t = np.arange(128)[None, :]
    c["tri_f"] = (s <= t).astype(np.float32)
    c["tri_b"] = (s >= t).astype(np.float32)
    mf = np.where(t <= s, 0.0, NBIG).astype(np.float32)
    mb = np.where(t >= s, 0.0, NBIG).astype(np.float32)
    c["mneg_f"] = np.repeat(mf[:, None, :], 4, axis=1).copy()
    c["mneg_b"] = np.repeat(mb[:, None, :], 4, axis=1).copy()
    el = np.zeros((128, 128), np.float32); el[127, :] = 1
    ef = np.zeros((128, 128), np.float32); ef[0, :] = 1
    c["e_last"] = el
    c["e_first"] = ef
    c["maskA"] = np.where(t >= s, 0.0, NBIG).astype(bf)
    c["maskB"] = np.where(t <= s, 0.0, NBIG).astype(bf)
    c["maskN"] = np.full((128, 128), NBIG).astype(bf)
    rows = NLAT // 64
    pr = np.repeat(np.arange(rows), 64)
    pc = np.tile(np.arange(64), rows)
    ca, sa = _rope_tab(32, pr, pc, NCTX)
    sc_a = 96.0 ** -0.5
    cosq = np.ones((96, T)); sinq = np.zeros((96, T))
    cosq[64:] = ca; sinq[64:] = sa
    c["cosq_a"] = (cosq * sc_a).astype(np.float32)
    c["sinq_a"] = (sinq * sc_a).astype(np.float32)
    c["cosk_a"] = ca.astype(np.float32)
    c["sink_a"] = sa.astype(np.float32)
    cc, sc = _rope_tab(64, pr, pc, NCTX)
    c["cos_c"] = cc.astype(np.float32)
    c["sin_c"] = sc.astype(np.float32)
    j = np.arange(64)
    Cc = np.cos(2 * np.pi * np.outer(j, j) / 64)
    Sc = np.sin(2 * np.pi * np.outer(j, j) / 64)
    z = np.zeros((64, 64))
    c["bdc"] = np.block([[Cc, z], [z, Cc]]).astype(bf)
    c["bds"] = (-np.block([[Sc, z], [z, Sc]])).astype(bf)
    for nm, N in (("lat", NLAT), ("ctx", NCTX)):
        n = np.arange(N)
        ph = (np.outer(n, n) % N).astype(np.float64) * (2 * np.pi / N)
        nrm = 1.0 / np.sqrt(N * 64.0)
        c["cn_" + nm] = (np.cos(ph) * nrm).astype(bf)
        c["sn_" + nm] = (np.sin(ph) * nrm).astype(bf)
    return c


CONST_DT = {"ident_bf": BF16, "ones_bf": BF16, "maskA": BF16, "maskB": BF16, "maskN": BF16, "bdc": BF16, "bds": BF16,
            "cn_lat": BF16, "sn_lat": BF16, "cn_ctx": BF16, "sn_ctx": BF16}

W_SHAPES = {
    "w_mod": (D, 6 * D), "w_in": (D, 2352), "w_uq": (256, 384), "w_ukv": (256, 512),
    "w_gate": (4, D, D), "w_branch": (4, 256, D), "w_out": (D, D), "w_up": (D, 2 * DFF), "w_down": (DFF, D),
}


def layout_params(inp, L):
    o = {}

    def fm(v, k):
        v = np.asarray(v, np.float32)
        lead = v.shape[:-1]
        return np.ascontiguousarray(np.moveaxis(v.reshape(lead + (k, 128)), -1, 0))

    o["b_mod"] = np.stack([fm(inp["b_mod"][l], 48) for l in range(L)])
    for nm in ("g_pre_mix", "g_post_mix", "g_pre_ffn", "g_post_ffn"):
        o[nm] = np.stack([fm(inp[nm][l], 8) for l in range(L)])
    o["g_qa"] = np.stack([fm(inp["g_qa"][l], 2) for l in range(L)])
    o["g_kva"] = np.stack([fm(inp["g_kva"][l], 2) for l in range(L)])
    o["b_gate"] = np.stack([fm(inp["b_gate"][l], 8) for l in range(L)])
    o["w_ffn_conv"] = np.stack([fm(inp["w_ffn_conv"][l], FC) for l in range(L)])
    o["b_ffn_conv"] = np.stack([fm(inp["b_ffn_conv"][l], FC) for l in range(L)])
    o["w_ml_conv"] = np.stack([fm(inp["w_ml_conv"][l], 4) for l in range(L)])
    o["b_ml_gates"] = np.asarray(inp["b_ml_gates"], np.float32).reshape(L, 1, 16)
    o["wg_sink"] = np.asarray(inp["wg_sink"], np.float32).reshape(L, 1, 4)
    return o


PARAM_SHAPES = lambda L: {
    "b_mod": (L, 128, 48), "g_pre_mix": (L, 128, 8), "g_post_mix": (L, 128, 8), "g_pre_ffn": (L, 128, 8),
    "g_post_ffn": (L, 128, 8), "g_qa": (L, 128, 2), "g_kva": (L, 128, 2), "b_gate": (L, 128, 4, 8),
    "w_ffn_conv": (L, 128, 3, FC), "b_ffn_conv": (L, 128, FC), "w_ml_conv": (L, 128, 3, 4),
    "b_ml_gates": (L, 1, 16), "wg_sink": (L, 1, 4), "wconvB": (L, 64, 8, 3),
}


class Builder:
    def __init__(self, NLAT, NCTX, NSEQ, L, consts, dbg=None):
        self.NLAT, self.NCTX, self.NSEQ, self.L = NLAT, NCTX, NSEQ, L
        self.T = T = NLAT + NCTX
        self.TT = T // 128
        self.CT = NCTX // 128
        self.blocks = [(0, NCTX)] + [(NCTX + i, min(NCTX + i + 512, T)) for i in range(0, NLAT, 512)]
        self.dbg = dbg
        nc = self.nc = bass.Bass("TRN2", target_bir_lowering=False)
        self.st = ExitStack()
        self.P = Prog(nc, self.st)
        di = lambda n, s, dt=F32: nc.dram_tensor(n, list(s), dt, kind="ExternalInput").ap()
        self.xT_in = di("xT", (NSEQ, D, NLAT))
        self.cT_in = di("ctxT", (NSEQ, D, NCTX))
        self.ccT = di("ccT", (128, KC, NSEQ + 1))
        self.W = {k: di(k, (L,) + v) for k, v in W_SHAPES.items()}
        self.PR = {k: di(k, v) for k, v in PARAM_SHAPES(L).items()}
        self.CD = {k: di(k, v.shape, CONST_DT.get(k, F32)) for k, v in consts.items()}
        self.out = nc.dram_tensor("outT", [NSEQ, D, NLAT], F32, kind="ExternalOutput").ap()
        ds = lambda n, s, dt: nc.dram_tensor(n, list(s), dt, kind="Internal").ap()
        self.XRES = ds("xres", (NSEQ, D, T), F32)
        self.YS = ds("ys", (NSEQ, D, T), BF16)
        self.WI = ds("wi_bf", (L, 128, KC * 2352), BF16)
        self.WG = ds("wg_bf", (L, 4, KC, 128, KC * 128), BF16)
        self.WBR = ds("wbr_bf", (L, 4, 128, 2 * D), BF16)
        self.WO = ds("wo_bf", (L, 128, KC * D), BF16)
        self.WU = ds("wu_bf", (L, FC, 128, KC * 256), BF16)
        self.WD = ds("wd_bf", (L, 128, FC * D), BF16)
        self.r_wbf = Res("wbf")
        self.r_xres = [Res("xres%d" % i) for i in range(NSEQ)]
        self.r_ys = [Res("ys%d" % i) for i in range(NSEQ)]
        self.r_out = Res("out")
        if dbg:
            self.dbg_out = {k: nc.dram_tensor("dbg_" + k, list(s), dt, kind="ExternalOutput").ap() for k, (s, dt) in dbg.items()}
        self.PS = nc.alloc_psum_tensor("PS", [128, 4096], F32)
        self.r_ps = [Res("ps%d" % i) for i in range(8)]
        self.ARENA_E = 98000
        self.arena = nc.alloc_sbuf_tensor("arena", [128, self.ARENA_E], BF16)
        self.a_off = 0
        self.a_base = 0
        self.phase_log = []

    def tile(self, shape, dt, name=""):
        n = int(np.prod(shape[1:]))
        ne = n * (2 if dt == F32 else 1)
        off = (self.a_off + 15) // 16 * 16
        assert off + ne <= self.ARENA_E, ("arena overflow", name, off, ne)
        self.a_off = off + ne
        ap = self.arena[0:shape[0], off:off + ne]
        if dt == F32:
            ap = ap.bitcast(F32)
        if len(shape) == 3:
            ap = ap.rearrange("p (a b) -> p a b", a=shape[1])
        elif len(shape) == 4:
            ap = ap.rearrange("p (a b c) -> p a b c", a=shape[1], b=shape[2])
        return ap, Res(name)

    def phase(self):
        self.P.barrier()
        self.a_off = self.a_base
        import sys
        self.phase_log.append((sys._getframe(1).f_code.co_name, dict(self.P.cnt)))

    def bank(self, b, n=512, lo=0):
        return self.PS[:, b * 512 + lo:b * 512 + lo + n]

    def bank_bf(self, b):
        return self.PS[:, b * 512:(b + 1) * 512].bitcast(BF16)

    def mm(self, out, lhsT, rhs, start, stop, reads, writes):
        self.P.op("pe", lambda e: e.matmul(out, lhsT, rhs, start=start, stop=stop, skip_group_check=True), reads, writes)

    def tr(self, out, in_, ident, reads, writes):
        self.P.op("pe", lambda e: e.transpose(out, in_, ident), reads, writes)

    def act(self, out, in_, func, reads, writes, bias=None, scale=None):
        kw = {}
        if bias is not None:
            kw["bias"] = bias
        if scale is not None:
            kw["scale"] = scale
        self.P.op("act", lambda e: e.activation(out=out, in_=in_, func=func, **kw), reads, writes)

    def tt(self, out, a, b, op, reads, writes, eng="dve"):
        self.P.op(eng, lambda e: e.tensor_tensor(out=out, in0=a, in1=b, op=op), reads, writes)

    def ts(self, out, a, s1, op0, reads, writes, s2=None, op1=None, eng="dve"):
        if op1 is None:
            self.P.op(eng, lambda e: e.tensor_scalar(out=out, in0=a, scalar1=s1, scalar2=None, op0=op0), reads, writes)
        else:
            self.P.op(eng, lambda e: e.tensor_scalar(out=out, in0=a, scalar1=s1, scalar2=s2, op0=op0, op1=op1), reads, writes)

    def stt(self, out, a, s, b, op0, op1, reads, writes):
        self.P.op("dve", lambda e: e.scalar_tensor_tensor(out=out, in0=a, scalar=s, in1=b, op0=op0, op1=op1), reads, writes)

    def cp(self, out, in_, reads, writes, eng="dve"):
        if eng == "act":
            self.P.op("act", lambda e: e.copy(out=out, in_=in_), reads, writes)
        else:
            self.P.op(eng, lambda e: e.tensor_copy(out=out, in_=in_), reads, writes)

    def red(self, out, in_, op, reads, writes):
        self.P.op("dve", lambda e: e.tensor_reduce(out=out, in_=in_, axis=AX.X, op=op), reads, writes)

    def recip(self, out, in_, reads, writes):
        self.P.op("dve", lambda e: e.reciprocal(out=out, in_=in_), reads, writes)

    def ld(self, out, in_, reads, writes, q="sp"):
        self.P.dma(q, out, in_, reads, writes)

    def prepass(self):
        self.phase()
        SZ = 2560
        stf = [self.tile([128, SZ], F32, "stf%d" % i) for i in range(3)]
        stb = [self.tile([128, SZ], BF16, "stb%d" % i) for i in range(3)]
        cnt = [0]

        def piece(srcs, dst, a, b):
            i = cnt[0] % 3
            cnt[0] += 1
            (f, r_f), (bt, r_b) = stf[i], stb[i]
            fv = f[:, 0:a * b].rearrange("p (a b) -> p a b", a=a)
            bv = bt[:, 0:a * b].rearrange("p (a b) -> p a b", a=a)
            for (c0, c1, src) in srcs:
                self.ld(fv[:, :, c0:c1], src, [], [r_f])
            eng = ("act", "dve", "pool")[i]
            self.cp(bv, fv, [r_f], [r_b], eng=eng)
            self.ld(dst, bv, [r_b], [self.r_wbf])

        for l in range(self.L):
            wi = self.W["w_in"][l].rearrange("(k p) n -> p k n", p=128)
            wid = self.WI[l].rearrange("p (k n) -> p k n", k=KC)
            for k in range(KC):
                piece([(0, 2352, wi[:, k:k + 1, :])], wid[:, k:k + 1, :], 1, 2352)
            for br in range(4):
                for dc in range(KC):
                    src = self.W["w_gate"][l][br][:, dc * 128:(dc + 1) * 128].rearrange("(k p) n -> p k n", p=128)
                    piece([(0, 128, src)], self.WG[l, br, dc].rearrange("p (k n) -> p k n", k=KC), KC, 128)
                src = self.W["w_branch"][l][br].rearrange("(k p) n -> p k n", p=128)
                piece([(0, D, src)], self.WBR[l, br].rearrange("p (k n) -> p k n", k=2), 2, D)
            wo = self.W["w_out"][l].rearrange("(k p) n -> p k n", p=128)
            wod = self.WO[l].rearrange("p (k n) -> p k n", k=KC)
            for k in range(0, KC, 2):
                piece([(0, D, wo[:, k:k + 2, :])], wod[:, k:k + 2, :], 2, D)
            for fc in range(FC):
                sa = self.W["w_up"][l][:, fc * 128:(fc + 1) * 128].rearrange("(k p) n -> p k n", p=128)
                sv = self.W["w_up"][l][:, DFF + fc * 128:DFF + (fc + 1) * 128].rearrange("(k p) n -> p k n", p=128)
                piece([(0, 128, sa), (128, 256, sv)], self.WU[l, fc].rearrange("p (k n) -> p k n", k=KC), KC, 256)
            wdn = self.W["w_down"][l].rearrange("(f p) n -> p f n", p=128)
            wdd = self.WD[l].rearrange("p (f n) -> p f n", f=FC)
            for f0 in range(0, FC, 2):
                piece([(0, D, wdn[:, f0:f0 + 2, :])], wdd[:, f0:f0 + 2, :], 2, D)

    def build(self):
        P = self.P
        NSEQ, L, T = self.NSEQ, self.L, self.T
        C = {}
        self.C = C
        r_c = self.r_c = Res("consts")
        for k in ("ident_bf", "ones_bf", "ident_f", "ones_f", "tri_f", "tri_b", "e_last", "e_first", "maskA", "maskB", "maskN", "bdc", "bds"):
            C[k], _ = self.tile([128, 128], CONST_DT.get(k, F32), k)
            self.ld(C[k], self.CD[k], [], [r_c])
        for k in ("mneg_f", "mneg_b"):
            C[k], _ = self.tile([128, 4, 128], F32, k)
            self.ld(C[k], self.CD[k], [], [r_c])
        self.hT, self.r_hT = self.tile([128, KC, T], BF16, "hT")
        self.MOD, self.r_mod = self.tile([128, L, 48, NSEQ + 1], F32, "MOD")
        self.pv = {}
        self.r_pv = Res("pvec")
        for k, shp in PARAM_SHAPES(L).items():
            if shp[1] == 128:
                self.pv[k], _ = self.tile([128, L] + list(shp[2:]) if len(shp) > 2 else [128, L], F32, k)
                src = self.PR[k]
                if len(shp) == 3:
                    self.ld(self.pv[k], src.rearrange("l p a -> p l a"), [], [self.r_pv])
                else:
                    for l in range(L):
                        self.ld(self.pv[k][:, l], src[l], [], [self.r_pv])
        self.bg_b, _ = self.tile([128, L, 16], F32, "bgb")
        self.sink_b, _ = self.tile([128, L, 4], F32, "sinkb")
        for l in range(L):
            self.ld(self.bg_b[:, l, :], self.PR["b_ml_gates"][l].partition_broadcast(128), [], [self.r_pv])
            self.ld(self.sink_b[:, l, :], self.PR["wg_sink"][l].partition_broadcast(128), [], [self.r_pv])
        self.wconvB, _ = self.tile([128, 8, 3], F32, "wconvB")
        self.r_wcb = Res("wcb")
        self.dv, self.r_dv = self.tile([128, 6, KC], F32, "derived")
        self.dvc, self.r_dvc = self.tile([128, 6, KC], F32, "derivedc")
        self.a_base = self.a_off
        for s in range(NSEQ):
            self.ld(self.XRES[s][:, 0:self.NCTX], self.cT_in[s], [], [self.r_xres[s]])
            self.ld(self.XRES[s][:, self.NCTX:T], self.xT_in[s], [], [self.r_xres[s]])
        self.prepass()
        self.mod_phase()
        import os
        stop = int(os.environ.get("KSTOP", "99"))
        for s in range(NSEQ):
            for l in range(L):
                last = (l == L - 1)
                steps = [lambda: self.derive(l, s), lambda: self.norm_mod(s, 0), lambda: self.branch_a(s, l, last),
                         lambda: self.branch_c(s, l, last), lambda: self.branch_d(s, l, last), lambda: self.branch_b(s, l, last),
                         lambda: self.merge(s, l, last), lambda: self.norm_mod(s, 3, skip_ctx=last), lambda: self.ffn(s, l, last)]
                for i, f in enumerate(steps):
                    if i < stop:
                        f()
            self.phase()
            self.ld(self.out[s], self.XRES[s][:, self.NCTX:T], [self.r_xres[s]], [self.r_out])
        P.barrier()
        P.emit()
        self.st.close()
        return self.nc

    def mod_phase(self):
        self.phase()
        NJ = self.NSEQ + 1
        cc, r_cc = self.tile([128, KC, NJ], F32, "cc")
        sg, r_sg = self.tile([128, KC, NJ], F32, "sg")
        self.ld(cc, self.ccT, [], [r_cc])
        self.act(sg, cc, AF.Sigmoid, [r_cc], [r_sg])
        self.tt(cc, cc, sg, ALU.mult, [r_sg, r_cc], [r_cc])
        wm = [self.tile([128, KC, 512], F32, "wm%d" % i) for i in range(2)]
        n = 0
        for l in range(self.L):
            for g in range(12):
                w, r_w = wm[n % 2]
                n += 1
                self.ld(w, self.W["w_mod"][l][:, g * 512:(g + 1) * 512].rearrange("(k p) n -> p k n", p=128), [], [r_w])
                b = n % 2
                for j4 in range(4):
                    for k in range(KC):
                        self.mm(self.bank(b, NJ, j4 * 8), w[:, k, j4 * 128:(j4 + 1) * 128], cc[:, k, :], k == 0 and j4 == 0, k == KC - 1,
                                [r_w, r_cc], [self.r_ps[b]])
                for j4 in range(4):
                    ch = g * 4 + j4
                    self.ts(self.MOD[:, l, ch, :], self.bank(b, NJ, j4 * 8), self.pv["b_mod"][:, l, ch:ch + 1], ALU.add,
                            [self.r_ps[b], self.r_pv], [self.r_mod])

    def derive(self, l, s):
        for (dst, r_dst, j) in ((self.dv, self.r_dv, s), (self.dvc, self.r_dvc, self.NSEQ)):
            for half, gpre, gpost in ((0, "g_pre_mix", "g_post_mix"), (1, "g_pre_ffn", "g_post_ffn")):
                sh = self.MOD[:, l, half * 24 + 0:half * 24 + 8, j]
                sc = self.MOD[:, l, half * 24 + 8:half * 24 + 16, j]
                g = self.MOD[:, l, half * 24 + 16:half * 24 + 24, j]
                self.stt(dst[:, half * 3 + 0, :], sc, 1.0, self.pv[gpre][:, l, :], ALU.add, ALU.mult, [self.r_mod, self.r_pv], [r_dst])
                self.cp(dst[:, half * 3 + 1, :], sh, [self.r_mod], [r_dst])
                self.tt(dst[:, half * 3 + 2, :], g, self.pv[gpost][:, l, :], ALU.mult, [self.r_mod, self.r_pv], [r_dst])

    def seg_dv(self, lo):
        return (self.dvc, self.r_dvc) if lo < self.NCTX else (self.dv, self.r_dv)

    def rstd_block(self, src, r_src, n, nk, dim, sq, r_sq, rs, r_rs, bank):
        for k in range(nk):
            self.act(sq[:, k, 0:n], src[:, k, 0:n], AF.Square, [r_src], [r_sq])
        for k in range(nk):
            self.mm(self.bank(bank, n), self.C["ones_bf"], sq[:, k, 0:n], k == 0, k == nk - 1, [r_sq, self.r_c], [self.r_ps[bank]])
        self.ts(rs[:, 0:n], self.bank(bank, n), 1.0 / dim, ALU.mult, [self.r_ps[bank]], [r_rs], s2=EPS, op1=ALU.add)
        self.act(rs[:, 0:n], rs[:, 0:n], AF.Ln, [r_rs], [r_rs])
        self.act(rs[:, 0:n], rs[:, 0:n], AF.Exp, [r_rs], [r_rs], scale=-0.5)

    def norm_mod(self, s, base, skip_ctx=False):
        self.phase()
        xb = [self.tile([128, KC, 512], F32, "xb%d" % i) for i in range(2)]
        sq, r_sq = self.tile([128, KC, 512], BF16, "sq")
        rs, r_rs = self.tile([128, 512], F32, "rs")
        tmp, r_tmp = self.tile([128, 512], F32, "tmp")
        for bi, (lo, hi) in enumerate(self.blocks):
            if skip_ctx and lo < self.NCTX:
                continue
            n = hi - lo
            x, r_x = xb[bi % 2]
            self.ld(x[:, :, 0:n], self.XRES[s][:, lo:hi].rearrange("(k p) n -> p k n", p=128), [self.r_xres[s]], [r_x])
            self.rstd_block(x, r_x, n, KC, D, sq, r_sq, rs, r_rs, bi % 2)
            dv, r_dv = self.seg_dv(lo)
            for k in range(KC):
                self.tt(tmp[:, 0:n], x[:, k, 0:n], rs[:, 0:n], ALU.mult, [r_x, r_rs], [r_tmp])
                self.act(self.hT[:, k, lo:hi], tmp[:, 0:n], AF.Identity, [r_tmp, r_dv], [self.r_hT],
                         bias=dv[:, base + 1, k:k + 1], scale=dv[:, base + 0, k:k + 1])

    def load_w_cols(self, dst, r_dst, l, c0, c1):
        self.ld(dst, self.WI[l].rearrange("p (k n) -> p k n", k=KC)[:, :, c0:c1], [self.r_wbf], [r_dst])

    def make_perm(self, dst, src, nheads, hd, r0, rd, r_dst, r_src, nk):
        nf = rd // 4
        self.P.op("dve", lambda e: e.memset(dst, 0.0), [], [r_dst])
        for k in range(nk):
            for h in range(nheads):
                b = h * hd + r0
                for hh in range(2):
                    o = b + hh * 2 * nf
                    self.ts(dst[:, k, o:o + nf], src[:, k, o + nf:o + 2 * nf], -1.0, ALU.mult, [r_src], [r_dst])
                    self.cp(dst[:, k, o + nf:o + 2 * nf], src[:, k, o:o + nf], [r_src], [r_dst])

    def attn_scores(self, it, buf):
        Pm, r_Pm, sm, r_sm = buf
        kparts, sink, scale = it["kparts"], it["sink"], it["scale"]
        ncols = max(c0 + k.shape[-1] for (k, _, c0, _) in kparts)
        started = set()
        for (kT, r_k, c0, mask) in kparts:
            n = kT.shape[-1]
            b = c0 // 512
            assert (c0 + n - 1) // 512 == b
            self.mm(self.PS[:, c0:c0 + n], it["q"], kT, b not in started, mask is None, [it["r_q"], r_k], [self.r_ps[b]])
            started.add(b)
            if mask is not None:
                self.mm(self.PS[:, c0:c0 + n], self.C["ident_bf"], mask, False, True, [self.r_c], [self.r_ps[b]])
        tot = ncols
        if sink is not None:
            b = ncols // 512
            self.mm(self.PS[:, ncols:ncols + 1], self.C["ones_f"][0:1, :], sink, b not in started, True, [self.r_c, self.r_pv], [self.r_ps[b]])
            started.add(b)
            tot = ncols + 1
        banks = [self.r_ps[b] for b in sorted(started)]
        self.red(sm[:, 0:1], self.PS[:, 0:tot], ALU.max, banks, [r_sm])
        self.ts(sm[:, 1:2], sm[:, 0:1], -scale, ALU.mult, [r_sm], [r_sm])
        self.act(Pm[:, 0:tot], self.PS[:, 0:tot], AF.Exp, banks + [r_sm], [r_Pm], bias=sm[:, 1:2], scale=scale)
        it["ncols"] = ncols

    def attn_transposes(self, it, buf, PTt, r_PT):
        Pm, r_Pm, sm, r_sm = buf
        vparts = it["vparts"]
        nv = len(vparts)
        for i, (V, r_v, c0) in enumerate(vparts):
            tb = 5 + (i // 8) % 2
            slot = i % 8
            pt_ps = self.bank_bf(tb)[:, slot * 128:(slot + 1) * 128]
            self.tr(pt_ps, Pm[:, c0:c0 + 128], self.C["ident_bf"], [r_Pm, self.r_c], [self.r_ps[tb]])
            if slot == 7 or i == nv - 1:
                g0 = i - slot
                self.cp(PTt[:, g0:i + 1, :], self.bank_bf(tb)[:, 0:(slot + 1) * 128].rearrange("p (a b) -> p a b", b=128),
                        [self.r_ps[tb]], [r_PT], eng="dve" if (i // 8) % 2 == 0 else "act")

    def attn_pv(self, it, buf, PTt, r_PT):
        Pm, r_Pm, sm, r_sm = buf
        vparts, sink, ncols = it["vparts"], it["sink"], it["ncols"]
        nv = len(vparts)
        for i, (V, r_v, c0) in enumerate(vparts):
            self.mm(self.bank(7, 65), PTt[:, i, :], V, i == 0, i == nv - 1, [r_PT, r_v], [self.r_ps[7]])
        if sink is not None:
            self.tt(sm[:, 2:3], self.bank(7, 1, 64), Pm[:, ncols:ncols + 1], ALU.add, [self.r_ps[7], r_Pm], [r_sm])
            self.recip(sm[:, 3:4], sm[:, 2:3], [r_sm], [r_sm])
        else:
            self.recip(sm[:, 3:4], self.bank(7, 1, 64), [self.r_ps[7]], [r_sm])
        self.ts(it["out"], self.bank(7, 64), sm[:, 3:4], ALU.mult, [self.r_ps[7], r_sm], [it["r_out"]])
        if it.get("after"):
            it["after"]()

    def attn_run(self, items):
        bufs = []
        for j in range(2):
            Pm, r_Pm = self.tile([128, self.T + 128], BF16, "Pm%d" % j)
            sm, r_sm = self.tile([128, 4], F32, "sm%d" % j)
            bufs.append((Pm, r_Pm, sm, r_sm))
        PTt, r_PT = self.tile([128, self.TT, 128], BF16, "PT")
        n = len(items)
        if n == 0:
            return
        self.attn_scores(items[0], bufs[0])
        for i in range(n):
            self.attn_transposes(items[i], bufs[i % 2], PTt, r_PT)
            if i + 1 < n:
                self.attn_scores(items[i + 1], bufs[(i + 1) % 2])
            self.attn_pv(items[i], bufs[i % 2], PTt, r_PT)

    def store_y_tm(self, ytm, r_ytm, s, br, tile_i, ybuf):
        yT, r_yT = ybuf
        for c in range(2):
            self.tr(self.bank_bf(6)[:, c * 128:(c + 1) * 128], ytm[:, c * 128:(c + 1) * 128], self.C["ident_bf"], [r_ytm, self.r_c], [self.r_ps[6]])
        self.cp(yT, self.bank_bf(6)[:, 0:256].rearrange("p (a b) -> p a b", b=128), [self.r_ps[6]], [r_yT])
        self.ld(self.YS[s][br * 256:(br + 1) * 256, tile_i * 128:(tile_i + 1) * 128].rearrange("(c p) n -> p c n", p=128), yT,
                [r_yT], [self.r_ys[s]])

    def branch_a(self, s, l, last):
        self.phase()
        T, TT, CT = self.T, self.TT, self.CT
        wA, r_wA = self.tile([128, KC, 544], BF16, "wA")
        wAp, r_wAp = self.tile([128, KC, 32], BF16, "wAp")
        wq, r_wq = self.tile([128, 2, 384], BF16, "wq")
        wqf, r_wqf = self.tile([128, 2, 384], F32, "wqf")
        wqp, r_wqp = self.tile([128, 2, 384], BF16, "wqp")
        wkv, r_wkv = self.tile([128, 2, 512], BF16, "wkv")
        wkvf, r_wkvf = self.tile([128, 2, 512], F32, "wkvf")
        raw, r_raw = self.tile([128, 4, 512], F32, "raw")
        sq, r_sq = self.tile([128, 4, 512], BF16, "sqA")
        rs, r_rs = self.tile([128, 2, 512], F32, "rsA")
        cqn, r_cqn = self.tile([128, 4, 512], BF16, "cqn")
        tab, r_tab = self.tile([128, 4, 512], F32, "tabA")
        t1, r_t1 = self.tile([128, 512], F32, "t1A")
        t2, r_t2 = self.tile([128, 512], F32, "t2A")
        qT, r_qT = self.tile([128, 4, T], BF16, "qTA")
        kT, r_kT = self.tile([128, 4, T], BF16, "kTA")
        Va, r_Va = self.tile([128, TT, 4, 65], BF16, "VaA")
        ytms = [self.tile([128, 256], BF16, "ytmA%d" % i) for i in range(2)]
        ybuf = self.tile([128, 2, 128], BF16, "yTA")
        self.load_w_cols(wA, r_wA, l, 0, 544)
        self.ld(wqf, self.W["w_uq"][l].rearrange("(k p) n -> p k n", p=128), [], [r_wqf])
        self.ld(wkvf, self.W["w_ukv"][l].rearrange("(k p) n -> p k n", p=128), [], [r_wkvf])
        for k in range(2):
            self.ts(wq[:, k, :], wqf[:, k, :], self.pv["g_qa"][:, l, k:k + 1], ALU.mult, [r_wqf, self.r_pv], [r_wq])
            self.ts(wkv[:, k, :], wkvf[:, k, :], self.pv["g_kva"][:, l, k:k + 1], ALU.mult, [r_wkvf, self.r_pv], [r_wkv])
        self.make_perm(wqp, wq, 4, 96, 64, 32, r_wqp, r_wq, 2)
        wkr = wA[:, :, 512:544]
        self.make_perm(wAp, wkr, 1, 32, 0, 32, r_wAp, r_wA, KC)
        self.P.op("dve", lambda e: e.memset(Va, 1.0), [], [r_Va])
        for bi, (lo, hi) in enumerate(self.blocks):
            n = hi - lo
            for c in range(4):
                b = c % 2
                for k in range(KC):
                    self.mm(self.bank(b, n), wA[:, k, c * 128:(c + 1) * 128], self.hT[:, k, lo:hi], k == 0, k == KC - 1,
                            [r_wA, self.r_hT], [self.r_ps[b]])
                self.cp(raw[:, c, 0:n], self.bank(b, n), [self.r_ps[b]], [r_raw], eng="act")
            self.rstd_block(raw[:, 0:2], r_raw, n, 2, 256, sq[:, 0:2], r_sq, rs[:, 0], r_rs, 2)
            self.rstd_block(raw[:, 2:4], r_raw, n, 2, 256, sq[:, 2:4], r_sq, rs[:, 1], r_rs, 3)
            for c in range(4):
                self.tt(cqn[:, c, 0:n], raw[:, c, 0:n], rs[:, c // 2, 0:n], ALU.mult, [r_raw, r_rs], [r_cqn])
            self.ld(tab[0:96, 0, 0:n], self.CD["cosq_a"][:, lo:hi], [], [r_tab])
            self.ld(tab[0:96, 1, 0:n], self.CD["sinq_a"][:, lo:hi], [], [r_tab])
            self.ld(tab[0:32, 2, 0:n], self.CD["cosk_a"][:, lo:hi], [], [r_tab])
            self.ld(tab[0:32, 3, 0:n], self.CD["sink_a"][:, lo:hi], [], [r_tab])
            for h in range(4):
                for (w_, r_w_, b) in ((wq, r_wq, 0), (wqp, r_wqp, 1)):
                    for k in range(2):
                        self.mm(self.bank(b, n)[0:96], w_[:, k, h * 96:(h + 1) * 96], cqn[:, k, 0:n], k == 0, k == 1, [r_w_, r_cqn], [self.r_ps[b]])
                self.tt(t1[0:96, 0:n], self.bank(0, n)[0:96], tab[0:96, 0, 0:n], ALU.mult, [self.r_ps[0], r_tab], [r_t1])
                self.tt(t2[0:96, 0:n], self.bank(1, n)[0:96], tab[0:96, 1, 0:n], ALU.mult, [self.r_ps[1], r_tab], [r_t2])
                self.tt(qT[0:96, h, lo:hi], t1[0:96, 0:n], t2[0:96, 0:n], ALU.add, [r_t1, r_t2], [r_qT])
                for k in range(2):
                    self.mm(self.bank(2, n)[0:64], wkv[:, k, h * 128:h * 128 + 64], cqn[:, 2 + k, 0:n], k == 0, k == 1, [r_wkv, r_cqn], [self.r_ps[2]])
                self.cp(kT[0:64, h, lo:hi], self.bank(2, n)[0:64], [self.r_ps[2]], [r_kT], eng="act")
            for (w_, r_w_, b) in ((wkr, r_wA, 3), (wAp, r_wAp, 4)):
                for k in range(KC):
                    self.mm(self.bank(b, n)[0:32], w_[:, k, :], self.hT[:, k, lo:hi], k == 0, k == KC - 1, [r_w_, self.r_hT], [self.r_ps[b]])
            self.tt(t1[0:32, 0:n], self.bank(3, n)[0:32], tab[0:32, 2, 0:n], ALU.mult, [self.r_ps[3], r_tab], [r_t1])
            self.tt(t2[0:32, 0:n], self.bank(4, n)[0:32], tab[0:32, 3, 0:n], ALU.mult, [self.r_ps[4], r_tab], [r_t2])
            self.tt(t1[0:32, 0:n], t1[0:32, 0:n], t2[0:32, 0:n], ALU.add, [r_t1, r_t2], [r_t1])
            for h in range(4):
                self.cp(kT[64:96, h, lo:hi], t1[0:32, 0:n], [r_t1], [r_kT])
            for ti in range(lo // 128, hi // 128):
                o = ti * 128 - lo
                for k in range(2):
                    self.mm(self.bank(5, 256).rearrange("p (h d) -> p h d", d=64), cqn[:, 2 + k, o:o + 128],
                            wkv[:, k, :].rearrange("p (h x) -> p h x", x=128)[:, :, 64:128], k == 0, k == 1, [r_cqn, r_wkv], [self.r_ps[5]])
                self.cp(Va[:, ti, :, 0:64], self.bank(5, 256).rearrange("p (h d) -> p h d", d=64), [self.r_ps[5]], [r_Va])
        q_tiles = list(range(CT, TT)) + ([] if last else list(range(CT)))
        items = []
        for n_, qi in enumerate(q_tiles):
            is_ctx = qi < CT
            nk = self.NCTX if is_ctx else T
            yt, r_yt = ytms[n_ % 2]
            for h in range(4):
                kparts = []
                c0 = 0
                while c0 < nk:
                    n = min(512, nk - c0)
                    kparts.append((kT[0:96, h, c0:c0 + n], r_kT, c0, None))
                    c0 += n
                vparts = [(Va[:, i, h, :], r_Va, i * 128) for i in range(nk // 128)]
                it = dict(q=qT[0:96, h, qi * 128:(qi + 1) * 128], r_q=r_qT, kparts=kparts, sink=None, vparts=vparts, scale=1.0,
                          out=yt[:, h * 64:(h + 1) * 64], r_out=r_yt)
                if h == 3:
                    it["after"] = (lambda yt=yt, r_yt=r_yt, qi=qi: self.store_y_tm(yt, r_yt, s, 0, qi, ybuf))
                items.append(it)
        self.attn_run(items)

    def branch_c(self, s, l, last):
        self.phase()
        T, TT, CT = self.T, self.TT, self.CT
        wC, r_wC = self.tile([128, KC, 512], BF16, "wC")
        wCp, r_wCp = self.tile([128, KC, 384], BF16, "wCp")
        tab, r_tab = self.tile([128, 2, 512], F32, "tabC")
        t1, r_t1 = self.tile([128, 512], F32, "t1C")
        t2, r_t2 = self.tile([128, 512], F32, "t2C")
        qT, r_qT = self.tile([128, 4, T], BF16, "qTC")
        kT, r_kT = self.tile([128, 2, T], BF16, "kTC")
        Va, r_Va = self.tile([128, TT, 2, 65], BF16, "VaC")
        sk8, r_sk8 = self.tile([128, 4], F32, "sk8")
        ytms = [self.tile([128, 256], BF16, "ytmC%d" % i) for i in range(2)]
        ybuf = self.tile([128, 2, 128], BF16, "yTC")
        self.load_w_cols(wC, r_wC, l, 1584, 2096)
        self.make_perm(wCp, wC[:, :, 0:384], 6, 64, 0, 64, r_wCp, r_wC, KC)
        self.ts(sk8, self.sink_b[:, l, :], 8.0, ALU.mult, [self.r_pv], [r_sk8])
        self.P.op("dve", lambda e: e.memset(Va, 1.0), [], [r_Va])
        for bi, (lo, hi) in enumerate(self.blocks):
            n = hi - lo
            self.ld(tab[0:64, 0, 0:n], self.CD["cos_c"][:, lo:hi], [], [r_tab])
            self.ld(tab[0:64, 1, 0:n], self.CD["sin_c"][:, lo:hi], [], [r_tab])
            for hh in range(6):
                for (w_, r_w_, b) in ((wC, r_wC, 0), (wCp, r_wCp, 1)):
                    for k in range(KC):
                        self.mm(self.bank(b, n)[0:64], w_[:, k, hh * 64:(hh + 1) * 64], self.hT[:, k, lo:hi], k == 0, k == KC - 1,
                                [r_w_, self.r_hT], [self.r_ps[b]])
                self.tt(t1[0:64, 0:n], self.bank(0, n)[0:64], tab[0:64, 0, 0:n], ALU.mult, [self.r_ps[0], r_tab], [r_t1])
                self.tt(t2[0:64, 0:n], self.bank(1, n)[0:64], tab[0:64, 1, 0:n], ALU.mult, [self.r_ps[1], r_tab], [r_t2])
                dst = qT[0:64, hh, lo:hi] if hh < 4 else kT[0:64, hh - 4, lo:hi]
                self.tt(dst, t1[0:64, 0:n], t2[0:64, 0:n], ALU.add, [r_t1, r_t2], [r_qT if hh < 4 else r_kT])
            for ti in range(lo // 128, hi // 128):
                for k in range(KC):
                    self.mm(self.bank(5, 128), self.hT[:, k, ti * 128:(ti + 1) * 128], wC[:, k, 384:512], k == 0, k == KC - 1,
                            [self.r_hT, r_wC], [self.r_ps[5]])
                self.cp(Va[:, ti, :, 0:64], self.bank(5, 128).rearrange("p (h d) -> p h d", d=64), [self.r_ps[5]], [r_Va])
        NQ = TT - CT
        q_tiles = list(range(CT, TT)) + ([] if last else list(range(CT)))
        NC_ = self.NCTX
        items = []
        for n_, qi in enumerate(q_tiles):
            is_ctx = qi < CT
            yt, r_yt = ytms[n_ % 2]
            for h in range(4):
                g = h // 2
                kparts = [(kT[0:64, g, 0:NC_], r_kT, 0, None)]
                vparts = [(Va[:, i, g, :], r_Va, i * 128) for i in range(CT)]
                if not is_ctx:
                    i = qi - CT
                    col = NC_
                    for (j, mk) in ((i - 1, "maskA"), (i, None), (i + 1, "maskB")):
                        if 0 <= j < NQ:
                            kparts.append((kT[0:64, g, NC_ + j * 128:NC_ + (j + 1) * 128], r_kT, col, self.C[mk] if mk else None))
                            vparts.append((Va[:, CT + j, g, :], r_Va, col))
                            col += 128
                it = dict(q=qT[0:64, h, qi * 128:(qi + 1) * 128], r_q=r_qT, kparts=kparts, sink=sk8[0:1, h:h + 1], vparts=vparts, scale=0.125,
                          out=yt[:, h * 64:(h + 1) * 64], r_out=r_yt)
                if h == 3:
                    it["after"] = (lambda yt=yt, r_yt=r_yt, qi=qi: self.store_y_tm(yt, r_yt, s, 2, qi, ybuf))
                items.append(it)
        self.attn_run(items)

    def branch_d(self, s, l, last):
        self.phase()
        T, TT, CT = self.T, self.TT, self.CT
        wD, r_wD = self.tile([128, KC, 256], BF16, "wD")
        ud, r_ud = self.tile([128, 2, T], BF16, "udT")
        uc, r_uc = self.tile([128, TT, 2, 256], BF16, "uc_tm")
        cn, r_cn = self.tile([128, 16, 512], BF16, "cn")
        sn, r_sn = self.tile([128, 16, 512], BF16, "sn")
        yo, r_yo = self.tile([128, 2, 512], BF16, "yoD")
        self.load_w_cols(wD, r_wD, l, 2096, 2352)
        for (lo, hi) in self.blocks:
            n = hi - lo
            for c in range(2):
                for k in range(KC):
                    self.mm(self.bank(c, n), wD[:, k, c * 128:(c + 1) * 128], self.hT[:, k, lo:hi], k == 0, k == KC - 1,
                            [r_wD, self.r_hT], [self.r_ps[c]])
                self.cp(ud[:, c, lo:hi], self.bank(c, n), [self.r_ps[c]], [r_ud], eng="act" if c else "dve")
        for ti in range(TT):
            for j, m in enumerate(("bdc", "bds")):
                for c in range(2):
                    self.mm(self.bank(2, 128, j * 256 + c * 128), ud[:, c, ti * 128:(ti + 1) * 128], self.C[m], c == 0 and j == 0, True,
                            [r_ud, self.r_c], [self.r_ps[2]])
            self.cp(uc[:, ti], self.bank(2).rearrange("p (a b) -> p a b", a=2), [self.r_ps[2]], [r_uc])
        segs = [("lat", CT, TT, self.NCTX)] + ([] if last else [("ctx", 0, CT, 0)])
        for nm, t0, t1_, col0 in segs:
            N = (t1_ - t0) * 128
            ntl = t1_ - t0
            for kb in range(0, N, 512):
                n = min(512, N - kb)
                self.ld(cn[:, 0:ntl, 0:n], self.CD["cn_" + nm][:, kb:kb + n].rearrange("(a p) k -> p a k", p=128), [], [r_cn])
                self.ld(sn[:, 0:ntl, 0:n], self.CD["sn_" + nm][:, kb:kb + n].rearrange("(a p) k -> p a k", p=128), [], [r_sn])
                for c in range(2):
                    b = 3 + c
                    for a in range(ntl):
                        self.mm(self.bank(b, n), uc[:, t0 + a, 0, c * 128:(c + 1) * 128], cn[:, a, 0:n], a == 0, False, [r_uc, r_cn], [self.r_ps[b]])
                        self.mm(self.bank(b, n), uc[:, t0 + a, 1, c * 128:(c + 1) * 128], sn[:, a, 0:n], False, a == ntl - 1, [r_uc, r_sn], [self.r_ps[b]])
                    self.cp(yo[:, c, 0:n], self.bank(b, n), [self.r_ps[b]], [r_yo], eng="act" if c else "dve")
                self.ld(self.YS[s][768:1024, col0 + kb:col0 + kb + n].rearrange("(c p) n -> p c n", p=128), yo[:, :, 0:n], [r_yo], [self.r_ys[s]])

    def branch_b(self, s, l, last):
        self.phase()
        T, TT, CT = self.T, self.TT, self.CT
        wB, r_wB = self.tile([128, KC, 1040], BF16, "wB")
        araw, r_araw = self.tile([128, 514], F32, "arawB")
        c1, r_c1 = self.tile([128, 512], F32, "c1B")
        qk, r_qk = self.tile([128, 8, T], BF16, "qkB")
        ktm, r_ktm = self.tile([128, TT, 256], BF16, "ktmB")
        Va, r_Va = self.tile([128, TT, 4, 65], BF16, "VaB")
        og, r_og = self.tile([128, 256], F32, "ogB")
        G, r_G = self.tile([128, TT, 16], F32, "GB")
        hs, r_hs = self.tile([128, TT, 256], F32, "hsB")
        self.load_w_cols(wB, r_wB, l, 544, 1584)
        self.ld(self.wconvB[0:64], self.PR["wconvB"][l], [], [self.r_wcb])
        self.P.op("dve", lambda e: e.memset(Va, 1.0), [], [r_Va])
        self.P.op("dve", lambda e: e.memset(hs, 0.0), [], [r_hs])
        for (lo, hi) in self.blocks:
            n = hi - lo
            seg_lo, seg_hi = (0, self.NCTX) if lo < self.NCTX else (self.NCTX, T)
            for hh in range(8):
                ch, half = hh // 2, hh % 2
                c0 = hh * 64
                for k in range(KC):
                    self.mm(self.bank(0, n)[0:64], wB[:, k, c0:c0 + 64], self.hT[:, k, lo:hi], k == 0, k == KC - 1, [r_wB, self.r_hT], [self.r_ps[0]])
                self.P.op("dve", lambda e: e.memset(araw[0:64, :], 0.0), [], [r_araw])
                if lo > seg_lo:
                    for k in range(KC):
                        self.mm(self.bank(1, 1)[0:64], wB[:, k, c0:c0 + 64], self.hT[:, k, lo - 1:lo], k == 0, k == KC - 1, [r_wB, self.r_hT], [self.r_ps[1]])
                    self.cp(araw[0:64, 0:1], self.bank(1, 1)[0:64], [self.r_ps[1]], [r_araw])
                if hi < seg_hi:
                    for k in range(KC):
                        self.mm(self.bank(1, 1, 8)[0:64], wB[:, k, c0:c0 + 64], self.hT[:, k, hi:hi + 1], k == 0, k == KC - 1, [r_wB, self.r_hT], [self.r_ps[1]])
                    self.cp(araw[0:64, n + 1:n + 2], self.bank(1, 1, 8)[0:64], [self.r_ps[1]], [r_araw])
                self.cp(araw[0:64, 1:n + 1], self.bank(0, n)[0:64], [self.r_ps[0]], [r_araw], eng="act")
                wsl = self.wconvB[:, hh, :]
                self.ts(c1[0:64, 0:n], araw[0:64, 0:n], wsl[0:64, 0:1], ALU.mult, [r_araw, self.r_wcb], [r_c1])
                self.stt(c1[0:64, 0:n], araw[0:64, 1:n + 1], wsl[0:64, 1:2], c1[0:64, 0:n], ALU.mult, ALU.add, [r_araw, self.r_wcb, r_c1], [r_c1])
                self.stt(c1[0:64, 0:n], araw[0:64, 2:n + 2], wsl[0:64, 2:3], c1[0:64, 0:n], ALU.mult, ALU.add, [r_araw, self.r_wcb, r_c1], [r_c1])
                self.act(c1[0:64, 0:n], c1[0:64, 0:n], AF.Silu, [r_c1], [r_c1])
                self.ts(qk[0:64, hh, lo:hi], c1[0:64, 0:n], 1.0 if hh < 4 else 0.125, ALU.mult, [r_c1], [r_qk])
        Gt, r_Gt = self.tile([128, TT, 2, 4], F32, "GtB")
        for ti in range(TT):
            tsl = slice(ti * 128, (ti + 1) * 128)
            for k in range(KC):
                self.mm(self.bank(2, 16), self.hT[:, k, tsl], wB[:, k, 1024:1040], k == 0, k == KC - 1, [self.r_hT, r_wB], [self.r_ps[2]])
            self.tt(G[:, ti, :], self.bank(2, 16), self.bg_b[:, l, :], ALU.add, [self.r_ps[2], self.r_pv], [r_G])
            for k in range(KC):
                self.mm(self.bank(3, 256), self.hT[:, k, tsl], wB[:, k, 512:768], k == 0, k == KC - 1, [self.r_hT, r_wB], [self.r_ps[3]])
            self.cp(Va[:, ti, :, 0:64], self.bank(3, 256).rearrange("p (h d) -> p h d", d=64), [self.r_ps[3]], [r_Va])
            for h in range(4):
                self.tr(self.bank_bf(5)[:, h * 64:(h + 1) * 64], qk[0:64, 4 + h, tsl], self.C["ident_bf"][0:64, 0:64], [r_qk, self.r_c], [self.r_ps[5]])
            self.cp(ktm[:, ti, :], self.bank_bf(5)[:, 0:256], [self.r_ps[5]], [r_ktm])
        G5 = G.rearrange("p t (a b c) -> p t a b c", a=2, b=2)
        for d_ in range(2):
            fv = G5[:, :, d_, 1, :]
            self.act(Gt[:, :, d_, :], fv, AF.Exp, [r_G], [r_Gt], scale=-1.0)
            self.act(Gt[:, :, d_, :], Gt[:, :, d_, :], AF.Ln, [r_Gt], [r_Gt], bias=1.0)
            self.ts(fv, Gt[:, :, d_, :], -1.0, ALU.mult, [r_Gt], [r_G])
        diag, r_diag = self.tile([128, 4, 128], F32, "diagB")
        bBm, r_bBm = self.tile([128, 4, 128], F32, "bBmB")
        Wt, r_Wt = self.tile([128, 4, 128], F32, "WtB")
        Sb, r_Sb = self.tile([128, 4, 128], BF16, "SbB")
        ST, r_ST = self.tile([128, 4, 128], BF16, "STB")
        kw, r_kw = self.tile([128, 4, 64], BF16, "kwB")
        sv, r_sv = self.tile([128, 16, 4], F32, "svB")
        cm, r_cm = self.tile([128, 8], F32, "cmB")
        mst, r_mst = self.tile([128, 4], F32, "mstB")
        tmpi, r_tmpi = self.tile([128, 4, 65], F32, "tmpiB")
        numh, r_numh = self.tile([128, 4, 64], F32, "numhB")
        Cst, r_Cst = self.tile([128, 4, 65], F32, "CstB")
        Cbf, r_Cbf = self.tile([128, 4, 65], BF16, "CbfB")
        B_TM, M_, NEGM, WIN, EMT, DEN, DAB, RR, WTM, DEC, MX, DENI = range(12)
        for d_ in range(2):
            tri = self.C["tri_f" if d_ == 0 else "tri_b"]
            mneg = self.C["mneg_f" if d_ == 0 else "mneg_b"]
            esel = self.C["e_last" if d_ == 0 else "e_first"]
            order = list(range(TT)) if d_ == 0 else (list(range(CT - 1, -1, -1)) + list(range(TT - 1, CT - 1, -1)))
            self.P.op("dve", lambda e: e.memset(Cst, 0.0), [], [r_Cst])
            self.P.op("dve", lambda e: e.memset(Cbf, 0.0), [], [r_Cbf])
            self.P.op("dve", lambda e: e.memset(mst, 0.0), [], [r_mst])
            for ti in order:
                tsl = slice(ti * 128, (ti + 1) * 128)
                li = G[:, ti, d_ * 8:d_ * 8 + 4]
                lf = G[:, ti, d_ * 8 + 4:d_ * 8 + 8]
                self.mm(self.bank(0, 4), tri, lf, True, True, [self.r_c, r_G], [self.r_ps[0]])
                self.tt(sv[:, B_TM, :], li, self.bank(0, 4), ALU.subtract, [r_G, self.r_ps[0]], [r_sv])
                for h in range(4):
                    self.ts(diag[:, h, :], self.C["ident_f"], sv[:, B_TM, h:h + 1], ALU.mult, [self.r_c, r_sv], [r_diag])
                for h in range(4):
                    self.mm(self.bank(1, 128, h * 128), self.C["ones_f"], diag[:, h, :], h == 0, True, [self.r_c, r_diag], [self.r_ps[1]])
                self.tt(bBm, self.bank(1).rearrange("p (h s) -> p h s", h=4), mneg, ALU.add, [self.r_ps[1], self.r_c], [r_bBm])
                self.red(sv[:, MX, :], bBm, ALU.max, [r_bBm], [r_sv])
                self.tt(sv[:, M_, :], sv[:, MX, :], mst, ALU.max, [r_sv, r_mst], [r_sv])
                self.ts(sv[:, NEGM, :], sv[:, M_, :], -1.0, ALU.mult, [r_sv], [r_sv])
                for h in range(4):
                    self.act(Wt[:, h, :], bBm[:, h, :], AF.Exp, [r_bBm, r_sv], [r_Wt], bias=sv[:, NEGM, h:h + 1], scale=1.0)
                for h in range(4):
                    self.mm(self.bank(2, 128, h * 128), qk[0:64, h, tsl], qk[0:64, 4 + h, tsl], h == 0, True, [r_qk], [self.r_ps[2]])
                self.tt(Sb, self.bank(2).rearrange("p (h s) -> p h s", h=4), Wt, ALU.mult, [self.r_ps[2], r_Wt], [r_Sb])
                self.red(sv[:, DENI, :], Sb, ALU.add, [r_Sb], [r_sv])
                for h in range(4):
                    self.tr(self.bank_bf(3)[:, h * 128:(h + 1) * 128], Sb[:, h, :], self.C["ident_bf"], [r_Sb, self.r_c], [self.r_ps[3]])
                self.cp(ST, self.bank_bf(3)[:, 0:512].rearrange("p (h s) -> p h s", h=4), [self.r_ps[3]], [r_ST])
                for h in range(4):
                    self.mm(self.bank(4, 64, h * 64), ST[:, h, :], Va[:, ti, h, 0:64], h == 0, True, [r_ST, r_Va], [self.r_ps[4]])
                for h in range(4):
                    self.mm(self.bank(5, 65, h * 65), qk[0:64, h, tsl], Cbf[0:64, h, :], h == 0, True, [r_qk, r_Cbf], [self.r_ps[5]])
                self.tt(sv[:, WIN, :], mst, sv[:, M_, :], ALU.subtract, [r_mst, r_sv], [r_sv])
                self.act(sv[:, WIN, :], sv[:, WIN, :], AF.Exp, [r_sv], [r_sv])
                self.tt(cm[:, 0:4], self.bank(0, 4), sv[:, M_, :], ALU.add, [self.r_ps[0], r_sv], [r_cm])
                self.cp(cm[:, 4:8], sv[:, M_, :], [r_sv], [r_cm])
                self.act(sv[:, EMT, :], cm[:, 0:4], AF.Exp, [r_cm], [r_sv], scale=-1.0)
                self.tt(tmpi, self.bank(5, 260).rearrange("p (h e) -> p h e", h=4), sv[:, WIN, :].unsqueeze(2).to_broadcast([128, 4, 65]), ALU.mult,
                        [self.r_ps[5], r_sv], [r_tmpi])
                self.tt(numh, tmpi[:, :, 0:64], self.bank(4, 256).rearrange("p (h e) -> p h e", h=4), ALU.add, [r_tmpi, self.r_ps[4]], [r_numh])
                self.tt(sv[:, DEN, :], tmpi[:, :, 64], sv[:, DENI, :], ALU.add, [r_tmpi, r_sv], [r_sv])
                self.ts(sv[:, DAB, :], sv[:, DEN, :], -1.0, ALU.mult, [r_sv], [r_sv])
                self.tt(sv[:, DAB, :], sv[:, DAB, :], sv[:, DEN, :], ALU.max, [r_sv], [r_sv])
                self.tt(sv[:, DAB, :], sv[:, DAB, :], sv[:, EMT, :], ALU.max, [r_sv], [r_sv])
                self.recip(sv[:, RR, :], sv[:, DAB, :], [r_sv], [r_sv])
                self.tt(numh, numh, sv[:, RR, :].unsqueeze(2).to_broadcast([128, 4, 64]), ALU.mult, [r_numh, r_sv], [r_numh])
                hsv = hs[:, ti, :].rearrange("p (h e) -> p h e", h=4)
                self.tt(hsv, hsv, numh, ALU.add, [r_hs, r_numh], [r_hs])
                self.mm(self.bank(6, 8), esel, cm, True, True, [self.r_c, r_cm], [self.r_ps[6]])
                self.tt(sv[:, WTM, :], sv[:, B_TM, :], self.bank(6, 4, 4), ALU.subtract, [r_sv, self.r_ps[6]], [r_sv])
                self.act(sv[:, WTM, :], sv[:, WTM, :], AF.Exp, [r_sv], [r_sv])
                self.tt(sv[:, DEC, :], mst, self.bank(6, 4, 4), ALU.subtract, [r_mst, self.r_ps[6]], [r_sv])
                self.act(sv[:, DEC, :], sv[:, DEC, :], AF.Exp, [r_sv], [r_sv])
                self.cp(mst, self.bank(6, 4), [self.r_ps[6]], [r_mst])
                self.tt(kw, ktm[:, ti, :].rearrange("p (h e) -> p h e", h=4), sv[:, WTM, :].unsqueeze(2).to_broadcast([128, 4, 64]), ALU.mult,
                        [r_ktm, r_sv], [r_kw])
                for h in range(4):
                    self.mm(self.bank(7, 65, h * 65)[0:64], kw[:, h, :], Va[:, ti, h, :], h == 0, True, [r_kw, r_Va], [self.r_ps[7]])
                self.tt(Cst[0:64], Cst[0:64], sv[0:64, DEC, :].unsqueeze(2).to_broadcast([64, 4, 65]), ALU.mult, [r_Cst, r_sv], [r_Cst])
                self.tt(Cst[0:64], Cst[0:64], self.bank(7, 260)[0:64].rearrange("p (h e) -> p h e", h=4), ALU.add, [r_Cst, self.r_ps[7]], [r_Cst])
                self.cp(Cbf[0:64], Cst[0:64], [r_Cst], [r_Cbf])
        ytm, r_ytm = self.tile([128, 256], BF16, "ytmB")
        ybuf = self.tile([128, 2, 128], BF16, "yTB")
        for ti in range(CT if last else 0, TT):
            tsl = slice(ti * 128, (ti + 1) * 128)
            for k in range(KC):
                self.mm(self.bank(4, 256), self.hT[:, k, tsl], wB[:, k, 768:1024], k == 0, k == KC - 1, [self.r_hT, r_wB], [self.r_ps[4]])
            self.act(og, self.bank(4, 256), AF.Sigmoid, [self.r_ps[4]], [r_og])
            self.tt(ytm, og, hs[:, ti, :], ALU.mult, [r_og, r_hs], [r_ytm])
            self.store_y_tm(ytm, r_ytm, s, 1, ti, ybuf)

    def post_norm_res(self, s, lo, hi, yT, r_yT, gp_idx, tl):
        sq, r_sq, rs, r_rs, tmp, r_tmp, xb, r_xb = tl
        n = hi - lo
        self.rstd_block(yT, r_yT, n, KC, D, sq, r_sq, rs, r_rs, 7)
        self.ld(xb[:, :, 0:n], self.XRES[s][:, lo:hi].rearrange("(k p) n -> p k n", p=128), [self.r_xres[s]], [r_xb])
        dv, r_dv = self.seg_dv(lo)
        for k in range(KC):
            self.tt(tmp[:, 0:n], yT[:, k, 0:n], rs[:, 0:n], ALU.mult, [r_yT, r_rs], [r_tmp])
            self.stt(xb[:, k, 0:n], tmp[:, 0:n], dv[:, gp_idx, k:k + 1], xb[:, k, 0:n], ALU.mult, ALU.add, [r_tmp, r_dv, r_xb], [r_xb])
        self.ld(self.XRES[s][:, lo:hi].rearrange("(k p) n -> p k n", p=128), xb[:, :, 0:n], [r_xb], [self.r_xres[s]])

    def pn_tiles(self):
        sq, r_sq = self.tile([128, KC, 512], BF16, "sqP")
        rs, r_rs = self.tile([128, 512], F32, "rsP")
        tmp, r_tmp = self.tile([128, 512], F32, "tmpP")
        xb, r_xb = self.tile([128, KC, 512], F32, "xbP")
        return (sq, r_sq, rs, r_rs, tmp, r_tmp, xb, r_xb)

    def merge(self, s, l, last):
        self.phase()
        ysb, r_ysb = self.tile([128, 8, 512], BF16, "ysb")
        wg = [self.tile([128, 4, KC, 128], BF16, "wg%d" % i) for i in range(2)]
        wbr = [self.tile([128, 4, 2, 128], BF16, "wbr%d" % i) for i in range(2)]
        sig, r_sig = self.tile([128, 512], F32, "sig")
        accf, r_accf = self.tile([128, 512], F32, "accf")
        tmpm, r_tmpm = self.tile([128, 512], F32, "tmpm")
        acc, r_acc = self.tile([128, KC, 512], BF16, "acc")
        wo, r_wo = self.tile([128, KC, D], BF16, "wo")
        yT, r_yT = self.tile([128, KC, 512], F32, "yTm")
        tl = self.pn_tiles()
        self.ld(wo, self.WO[l].rearrange("p (k n) -> p k n", k=KC), [self.r_wbf], [r_wo])
        it = 0
        for (lo, hi) in self.blocks:
            if last and lo < self.NCTX:
                continue
            n = hi - lo
            self.ld(ysb[:, :, 0:n], self.YS[s][:, lo:hi].rearrange("(k p) n -> p k n", p=128), [self.r_ys[s]], [r_ysb])
            for dc in range(KC):
                (wg_, r_wg), (wb_, r_wb) = wg[it % 2], wbr[it % 2]
                it += 1
                for br in range(4):
                    self.ld(wg_[:, br], self.WG[l, br, dc].rearrange("p (k n) -> p k n", k=KC), [self.r_wbf], [r_wg])
                    self.ld(wb_[:, br], self.WBR[l, br].rearrange("p (k n) -> p k n", k=2)[:, :, dc * 128:(dc + 1) * 128], [self.r_wbf], [r_wb])
                for br in range(4):
                    bg, bp = br % 2, 2 + br % 2
                    for k in range(KC):
                        self.mm(self.bank(bg, n), wg_[:, br, k, :], self.hT[:, k, lo:hi], k == 0, k == KC - 1, [r_wg, self.r_hT], [self.r_ps[bg]])
                    self.act(sig[:, 0:n], self.bank(bg, n), AF.Sigmoid, [self.r_ps[bg], self.r_pv], [r_sig], bias=self.pv["b_gate"][:, l, br, dc:dc + 1], scale=1.0)
                    for k in range(2):
                        self.mm(self.bank(bp, n), wb_[:, br, k, :], ysb[:, br * 2 + k, 0:n], k == 0, k == 1, [r_wb, r_ysb], [self.r_ps[bp]])
                    if br == 0:
                        self.tt(accf[:, 0:n], sig[:, 0:n], self.bank(bp, n), ALU.mult, [r_sig, self.r_ps[bp]], [r_accf])
                    else:
                        self.tt(tmpm[:, 0:n], sig[:, 0:n], self.bank(bp, n), ALU.mult, [r_sig, self.r_ps[bp]], [r_tmpm])
                        if br < 3:
                            self.tt(accf[:, 0:n], accf[:, 0:n], tmpm[:, 0:n], ALU.add, [r_accf, r_tmpm], [r_accf])
                        else:
                            self.tt(acc[:, dc, 0:n], accf[:, 0:n], tmpm[:, 0:n], ALU.add, [r_accf, r_tmpm], [r_acc])
            for d2 in range(KC):
                b = 4 + d2 % 2
                for k in range(KC):
                    self.mm(self.bank(b, n), wo[:, k, d2 * 128:(d2 + 1) * 128], acc[:, k, 0:n], k == 0, k == KC - 1, [r_wo, r_acc], [self.r_ps[b]])
                self.cp(yT[:, d2, 0:n], self.bank(b, n), [self.r_ps[b]], [r_yT], eng="act" if d2 % 2 else "dve")
            self.post_norm_res(s, lo, hi, yT, r_yT, 2, tl)

    def ffn(self, s, l, last):
        self.phase()
        T = self.T
        wd, r_wd = self.tile([128, FC, D], BF16, "wd")
        wu = [self.tile([128, KC, 256], BF16, "wu%d" % i) for i in range(2)]
        gT, r_gT = self.tile([128, FC, 512], BF16, "gT")
        a_sb, r_a = self.tile([128, 514], F32, "a_sb")
        c1, r_c1 = self.tile([128, 512], F32, "c1F")
        yT, r_yT = self.tile([128, KC, 512], F32, "yTf")
        tl = self.pn_tiles()
        self.ld(wd, self.WD[l].rearrange("p (f n) -> p f n", f=FC), [self.r_wbf], [r_wd])
        wcv, bcv = self.pv["w_ffn_conv"], self.pv["b_ffn_conv"]
        it = 0
        for (lo, hi) in self.blocks:
            if last and lo < self.NCTX:
                continue
            n = hi - lo
            seg_lo, seg_hi = (0, self.NCTX) if lo < self.NCTX else (self.NCTX, T)
            for fc in range(FC):
                w_, r_w = wu[it % 2]
                it += 1
                self.ld(w_, self.WU[l, fc].rearrange("p (k n) -> p k n", k=KC), [self.r_wbf], [r_w])
                ba, bv = fc % 2, 2 + fc % 2
                for k in range(KC):
                    self.mm(self.bank(ba, n), w_[:, k, 0:128], self.hT[:, k, lo:hi], k == 0, k == KC - 1, [r_w, self.r_hT], [self.r_ps[ba]])
                for k in range(KC):
                    self.mm(self.bank(bv, n), w_[:, k, 128:256], self.hT[:, k, lo:hi], k == 0, k == KC - 1, [r_w, self.r_hT], [self.r_ps[bv]])
                self.P.op("dve", lambda e: e.memset(a_sb[:, 0:1], 0.0), [], [r_a])
                self.P.op("dve", lambda e: e.memset(a_sb[:, n + 1:n + 2], 0.0), [], [r_a])
                if lo > seg_lo:
                    for k in range(KC):
                        self.mm(self.bank(6, 1), w_[:, k, 0:128], self.hT[:, k, lo - 1:lo], k == 0, k == KC - 1, [r_w, self.r_hT], [self.r_ps[6]])
                    self.cp(a_sb[:, 0:1], self.bank(6, 1), [self.r_ps[6]], [r_a])
                if hi < seg_hi:
                    for k in range(KC):
                        self.mm(self.bank(6, 1, 8), w_[:, k, 0:128], self.hT[:, k, hi:hi + 1], k == 0, k == KC - 1, [r_w, self.r_hT], [self.r_ps[6]])
                    self.cp(a_sb[:, n + 1:n + 2], self.bank(6, 1, 8), [self.r_ps[6]], [r_a])
                self.cp(a_sb[:, 1:n + 1], self.bank(ba, n), [self.r_ps[ba]], [r_a], eng="act")
                self.act(self.bank(ba, n), self.bank(ba, n), AF.Copy, [self.r_ps[ba], self.r_pv], [self.r_ps[ba]], scale=wcv[:, l, 1, fc:fc + 1])
                self.stt(self.bank(ba, n), a_sb[:, 0:n], wcv[:, l, 0, fc:fc + 1], self.bank(ba, n), ALU.mult, ALU.add, [r_a, self.r_pv, self.r_ps[ba]], [self.r_ps[ba]])
                self.stt(c1[:, 0:n], a_sb[:, 2:n + 2], wcv[:, l, 2, fc:fc + 1], self.bank(ba, n), ALU.mult, ALU.add, [r_a, self.r_pv, self.r_ps[ba]], [r_c1])
                self.act(c1[:, 0:n], c1[:, 0:n], AF.Silu, [r_c1, self.r_pv], [r_c1], bias=bcv[:, l, fc:fc + 1], scale=1.0)
                self.tt(gT[:, fc, 0:n], c1[:, 0:n], self.bank(bv, n), ALU.mult, [r_c1, self.r_ps[bv]], [r_gT])
            for d2 in range(KC):
                b = 4 + d2 % 2
                for fc in range(FC):
                    self.mm(self.bank(b, n), wd[:, fc, d2 * 128:(d2 + 1) * 128], gT[:, fc, 0:n], fc == 0, fc == FC - 1, [r_wd, r_gT], [self.r_ps[b]])
                self.cp(yT[:, d2, 0:n], self.bank(b, n), [self.r_ps[b]], [r_yT], eng="act" if d2 % 2 else "dve")
            self.post_norm_res(s, lo, hi, yT, r_yT, 5, tl)


NLAT_FULL, NCTX_FULL, DEPTH = 2048, 256, 2
_cache = {}


def run_device(inp, NLAT, NCTX, B, L, n_cores):
    NSEQ = B // n_cores
    consts = make_consts(NLAT, NCTX)
    key = (NLAT, NCTX, NSEQ, L)
    bld = Builder(NLAT, NCTX, NSEQ, L, consts)
    nc = bld.build()
    x = np.asarray(inp["x"], np.float32)
    ctx = np.asarray(inp["ctx"], np.float32)
    c = np.asarray(inp["c"], np.float32)
    c_ctx = np.asarray(inp["c_ctx"], np.float32)
    params = layout_params(inp, L)
    wml = np.asarray(inp["w_ml_conv"], np.float32)
    params["wconvB"] = np.ascontiguousarray(wml.reshape(L, 3, 8, 64).transpose(0, 3, 2, 1))
    shared = {k: np.ascontiguousarray(np.asarray(inp[k], np.float32)) for k in W_SHAPES}
    shared.update(params)
    shared.update(consts)
    in_maps = []
    for ci in range(n_cores):
        sl = slice(ci * NSEQ, (ci + 1) * NSEQ)
        cc = np.concatenate([c[sl], c_ctx[None]], 0)
        m = dict(shared)
        m["xT"] = np.ascontiguousarray(x[sl].transpose(0, 2, 1))
        m["ctxT"] = np.ascontiguousarray(ctx[sl].transpose(0, 2, 1))
        m["ccT"] = np.ascontiguousarray(cc.T.reshape(KC, 128, NSEQ + 1).transpose(1, 0, 2))
        in_maps.append(m)
    return nc, in_maps


def kernel(**inp):
    n_cores = 8
    nc, in_maps = run_device(inp, NLAT_FULL, NCTX_FULL, 16, DEPTH, n_cores)
    res = run_bass_kernel_spmd(nc, in_maps, core_ids=list(range(n_cores)))
    outs = [np.asarray(r["outT"]).transpose(0, 2, 1) for r in res.results]
    return np.ascontiguousarray(np.concatenate(outs, 0).astype(np.float32))
```

```python
from contextlib import ExitStack
import numpy as np
import ml_dtypes
import concourse.bass as bass
import concourse.mybir as mybir
from concourse.bass_utils import run_bass_kernel_spmd

F32 = mybir.dt.float32
BF16 = mybir.dt.bfloat16
AF = mybir.ActivationFunctionType
ALU = mybir.AluOpType
AX = mybir.AxisListType
ENG = ("pe", "act", "dve", "pool", "sp")
NBIG = -30000.0
D = 1024
KC = 8
DFF = 2816
FC = 22
EPS = 1e-6


class Res:
    __slots__ = ("name", "w", "re", "rd")

    def __init__(self, name=""):
        self.name = name
        self.w = None
        self.re = {}
        self.rd = []


class Prog:
    N_DMA_SEMS = 80

    def __init__(self, nc, stack):
        self.nc = nc
        self.esem = {e: stack.enter_context(nc.semaphore("es_" + e)) for e in ENG}
        self.dsem = [stack.enter_context(nc.semaphore("ds%d" % i)) for i in range(self.N_DMA_SEMS)]
        self.dval = [0] * self.N_DMA_SEMS
        self.dnext = 0
        self.cnt = {e: 0 for e in ENG}
        self.seen = {e: {} for e in ENG}
        self.q = {e: [] for e in ENG}
        self.n_ops = 0

    def _deps(self, reads, writes):
        deps = []
        for r in reads:
            if r.w is not None:
                deps.append(r.w)
        for w in writes:
            if w.w is not None:
                deps.append(w.w)
            for e, c in w.re.items():
                deps.append(("E", e, c))
            deps.extend(w.rd)
        return deps

    def _waits(self, eng, deps):
        seen = self.seen[eng]
        need = {}
        for kind, key, val in deps:
            k = (kind, key)
            if seen.get(k, 0) >= val:
                continue
            if kind == "E" and key == "pe" and eng == "pe":
                continue
            if need.get(k, 0) < val:
                need[k] = val
        out = []
        for k, val in need.items():
            seen[k] = val
            sem = self.esem[k[1]] if k[0] == "E" else self.dsem[k[1]]
            out.append((sem, val))
        return out

    def op(self, eng, fn, reads=(), writes=()):
        waits = self._waits(eng, self._deps(reads, writes))
        self.cnt[eng] += 1
        c = self.cnt[eng]
        tok = ("E", eng, c)
        self.q[eng].append((waits, fn, (self.esem[eng], 1)))
        for r in reads:
            if r.re.get(eng, 0) < c:
                r.re[eng] = c
        for w in writes:
            w.w = tok
            w.re = {}
            w.rd = []
        self.n_ops += 1
        return tok

    def dma(self, qeng, out, in_, reads=(), writes=(), **kw):
        deps = self._deps(reads, writes)
        i = self.dnext
        self.dnext = (self.dnext + 1) % self.N_DMA_SEMS
        prev = self.dval[i]
        if prev:
            deps.append(("D", i, prev))
        waits = self._waits(qeng, deps)
        self.dval[i] = prev + 16
        tok = ("D", i, prev + 16)
        self.q[qeng].append((waits, (lambda e, o=out, s=in_, k=kw: e.dma_start(out=o, in_=s, **k)),
                             (self.dsem[i], 16)))
        for r in reads:
            r.rd.append(tok)
        for w in writes:
            w.w = tok
            w.re = {}
            w.rd = []
        self.n_ops += 1
        return tok

    def barrier(self):
        deps = [("E", e, self.cnt[e]) for e in ENG if self.cnt[e]]
        deps += [("D", i, v) for i, v in enumerate(self.dval) if v]
        for e in ENG:
            waits = self._waits(e, deps)
            if waits:
                self.q[e].append((waits, None, None))

    def emit(self):
        nc = self.nc
        allsems = [self.esem[e] for e in ENG] + self.dsem
        with nc.Block("init") as b0:
            @b0.vector
            def _(v):
                for s in allsems:
                    v.sem_clear(s)
        with nc.Block("main") as blk:
            def run(e, name):
                for waits, fn, inc in self.q[name]:
                    for sem, val in waits:
                        e.wait_ge(sem, val)
                    if fn is not None:
                        fn(e).then_inc(inc[0], inc[1])

            @blk.tensor
            def _(e):
                run(e, "pe")

            @blk.scalar
            def _(e):
                run(e, "act")

            @blk.vector
            def _(e):
                run(e, "dve")

            @blk.gpsimd
            def _(e):
                run(e, "pool")

            @blk.sync
            def _(e):
                run(e, "sp")


def _rope_tab(rd, pos_row, pos_col, n_ctx):
    half = rd // 2
    nf = half // 2
    inv = 10000.0 ** (-np.arange(nf, dtype=np.float64) / nf)
    n = len(pos_row)
    cos = np.ones((rd, n_ctx + n), np.float64)
    sin = np.zeros((rd, n_ctx + n), np.float64)
    for r in range(rd):
        hh, rr = r // half, r % half
        idx = rr % nf
        pos = pos_row if hh == 0 else pos_col
        ang = pos.astype(np.float64) * inv[idx]
        ang = (pos.astype(np.float32) * np.float32(inv[idx]).astype(np.float32)).astype(np.float64)
        cos[r, n_ctx:] = np.cos(ang)
        sin[r, n_ctx:] = np.sin(ang)
    return cos, sin


def make_consts(NLAT, NCTX):
    bf = ml_dtypes.bfloat16
    T = NLAT + NCTX
    c = {}
    c["ident_bf"] = np.eye(128).astype(bf)
    c["ones_bf"] = np.ones((128, 128)).astype(bf)
    c["ident_f"] = np.eye(128, dtype=np.float32)
    c["ones_f"] = np.ones((128, 128), np.float32)
    s = np.arange(128)[:, None]
    t = np.arange(128)[None, :]
    c["tri_f"] = (s <= t).astype(np.float32)
    c["tri_b"] = (s >= t).astype(np.float32)
    mf = np.where(t <= s, 0.0, NBIG).astype(np.float32)
    mb = np.where(t >= s, 0.0, NBIG).astype(np.float32)
    c["mneg_f"] = np.repeat(mf[:, None, :], 4, axis=1).copy()
    c["mneg_b"] = np.repeat(mb[:, None, :], 4, axis=1).copy()
    el = np.zeros((128, 128), np.float32); el[127, :] = 1
    ef = np.zeros((128, 128), np.float32); ef[0, :] = 1
    c["e_last"] = el
    c["e_first"] = ef
    c["maskA"] = np.where(t >= s, 0.0, NBIG).astype(bf)
    c["maskB"] = np.where(t <= s, 0.0, NBIG).astype(bf)
    c["maskN"] = np.full((128, 128), NBIG).astype(bf)
    rows = NLAT // 64
    pr = np.repeat(np.arange(rows), 64)
    pc = np.tile(np.arange(64), rows)
    ca, sa = _rope_tab(32, pr, pc, NCTX)
    sc_a = 96.0 ** -0.5
    cosq = np.ones((96, T)); sinq = np.zeros((96, T))
    cosq[64:] = ca; sinq[64:] = sa
    c["cosq_a"] = (cosq * sc_a).astype(np.float32)
    c["sinq_a"] = (sinq * sc_a).astype(np.float32)
    c["cosk_a"] = ca.astype(np.float32)
    c["sink_a"] = sa.astype(np.float32)
    cc, sc = _rope_tab(64, pr, pc, NCTX)
    c["cos_c"] = cc.astype(np.float32)
    c["sin_c"] = sc.astype(np.float32)
    j = np.arange(64)
    Cc = np.cos(2 * np.pi * np.outer(j, j) / 64)
    Sc = np.sin(2 * np.pi * np.outer(j, j) / 64)
    z = np.zeros((64, 64))
    c["bdc"] = np.block([[Cc, z], [z, Cc]]).astype(bf)
    c["bds"] = (-np.block([[Sc, z], [z, Sc]])).astype(bf)
    for nm, N in (("lat", NLAT), ("ctx", NCTX)):
        n = np.arange(N)
        ph = (np.outer(n, n) % N).astype(np.float64) * (2 * np.pi / N)
        nrm = 1.0 / np.sqrt(N * 64.0)
        c["cn_" + nm] = (np.cos(ph) * nrm).astype(bf)
        c["sn_" + nm] = (np.sin(ph) * nrm).astype(bf)
    return c


CONST_DT = {"ident_bf": BF16, "ones_bf": BF16, "maskA": BF16, "maskB": BF16, "maskN": BF16, "bdc": BF16, "bds": BF16,
            "cn_lat": BF16, "sn_lat": BF16, "cn_ctx": BF16, "sn_ctx": BF16}

W_SHAPES = {
    "w_mod": (D, 6 * D), "w_in": (D, 2352), "w_uq": (256, 384), "w_ukv": (256, 512),
    "w_gate": (4, D, D), "w_branch": (4, 256, D), "w_out": (D, D), "w_up": (D, 2 * DFF), "w_down": (DFF, D),
}


def layout_params(inp, L):
    o = {}

    def fm(v, k):
        v = np.asarray(v, np.float32)
        lead = v.shape[:-1]
        return np.ascontiguousarray(np.moveaxis(v.reshape(lead + (k, 128)), -1, 0))

    o["b_mod"] = np.stack([fm(inp["b_mod"][l], 48) for l in range(L)])
    for nm in ("g_pre_mix", "g_post_mix", "g_pre_ffn", "g_post_ffn"):
        o[nm] = np.stack([fm(inp[nm][l], 8) for l in range(L)])
    o["g_qa"] = np.stack([fm(inp["g_qa"][l], 2) for l in range(L)])
    o["g_kva"] = np.stack([fm(inp["g_kva"][l], 2) for l in range(L)])
    o["b_gate"] = np.stack([fm(inp["b_gate"][l], 8) for l in range(L)])
    o["w_ffn_conv"] = np.stack([fm(inp["w_ffn_conv"][l], FC) for l in range(L)])
    o["b_ffn_conv"] = np.stack([fm(inp["b_ffn_conv"][l], FC) for l in range(L)])
    o["w_ml_conv"] = np.stack([fm(inp["w_ml_conv"][l], 4) for l in range(L)])
    o["b_ml_gates"] = np.asarray(inp["b_ml_gates"], np.float32).reshape(L, 1, 16)
    o["wg_sink"] = np.asarray(inp["wg_sink"], np.float32).reshape(L, 1, 4)
    return o


PARAM_SHAPES = lambda L: {
    "b_mod": (L, 128, 48), "g_pre_mix": (L, 128, 8), "g_post_mix": (L, 128, 8), "g_pre_ffn": (L, 128, 8),
    "g_post_ffn": (L, 128, 8), "g_qa": (L, 128, 2), "g_kva": (L, 128, 2), "b_gate": (L, 128, 4, 8),
    "w_ffn_conv": (L, 128, 3, FC), "b_ffn_conv": (L, 128, FC), "w_ml_conv": (L, 128, 3, 4),
    "b_ml_gates": (L, 1, 16), "wg_sink": (L, 1, 4), "wconvB": (L, 64, 8, 3),
}


class Builder:
    def __init__(self, NLAT, NCTX, NSEQ, L, consts, dbg=None):
        self.NLAT, self.NCTX, self.NSEQ, self.L = NLAT, NCTX, NSEQ, L
        self.T = T = NLAT + NCTX
        self.TT = T // 128
        self.CT = NCTX // 128
        self.blocks = [(0, NCTX)] + [(NCTX + i, min(NCTX + i + 512, T)) for i in range(0, NLAT, 512)]
        self.dbg = dbg
        nc = self.nc = bass.Bass("TRN2", target_bir_lowering=False)
        self.st = ExitStack()
        self.P = Prog(nc, self.st)
        di = lambda n, s, dt=F32: nc.dram_tensor(n, list(s), dt, kind="ExternalInput").ap()
        self.xT_in = di("xT", (NSEQ, D, NLAT))
        self.cT_in = di("ctxT", (NSEQ, D, NCTX))
        self.ccT = di("ccT", (128, KC, NSEQ + 1))
        self.W = {k: di(k, (L,) + v) for k, v in W_SHAPES.items()}
        self.PR = {k: di(k, v) for k, v in PARAM_SHAPES(L).items()}
        self.CD = {k: di(k, v.shape, CONST_DT.get(k, F32)) for k, v in consts.items()}
        self.out = nc.dram_tensor("outT", [NSEQ, D, NLAT], F32, kind="ExternalOutput").ap()
        ds = lambda n, s, dt: nc.dram_tensor(n, list(s), dt, kind="Internal").ap()
        self.XRES = ds("xres", (NSEQ, D, T), F32)
        self.YS = ds("ys", (NSEQ, D, T), BF16)
        self.WI = ds("wi_bf", (L, 128, KC * 2352), BF16)
        self.WG = ds("wg_bf", (L, 4, KC, 128, KC * 128), BF16)
        self.WBR = ds("wbr_bf", (L, 4, 128, 2 * D), BF16)
        self.WO = ds("wo_bf", (L, 128, KC * D), BF16)
        self.WU = ds("wu_bf", (L, FC, 128, KC * 256), BF16)
        self.WD = ds("wd_bf", (L, 128, FC * D), BF16)
        self.r_wbf = Res("wbf")
        self.r_xres = [Res("xres%d" % i) for i in range(NSEQ)]
        self.r_ys = [Res("ys%d" % i) for i in range(NSEQ)]
        self.r_out = Res("out")
        if dbg:
            self.dbg_out = {k: nc.dram_tensor("dbg_" + k, list(s), dt, kind="ExternalOutput").ap() for k, (s, dt) in dbg.items()}
        self.PS = nc.alloc_psum_tensor("PS", [128, 4096], F32)
        self.r_ps = [Res("ps%d" % i) for i in range(8)]
        self.ARENA_E = 98000
        self.arena = nc.alloc_sbuf_tensor("arena", [128, self.ARENA_E], BF16)
        self.a_off = 0
        self.a_base = 0
        self.phase_log = []

    def tile(self, shape, dt, name=""):
        n = int(np.prod(shape[1:]))
        ne = n * (2 if dt == F32 else 1)
        off = (self.a_off + 15) // 16 * 16
        assert off + ne <= self.ARENA_E, ("arena overflow", name, off, ne)
        self.a_off = off + ne
        ap = self.arena[0:shape[0], off:off + ne]
        if dt == F32:
            ap = ap.bitcast(F32)
        if len(shape) == 3:
            ap = ap.rearrange("p (a b) -> p a b", a=shape[1])
        elif len(shape) == 4:
            ap = ap.rearrange("p (a b c) -> p a b c", a=shape[1], b=shape[2])
        return ap, Res(name)

    def phase(self):
        self.P.barrier()
        self.a_off = self.a_base
        import sys
        self.phase_log.append((sys._getframe(1).f_code.co_name, dict(self.P.cnt)))

    def bank(self, b, n=512, lo=0):
        return self.PS[:, b * 512 + lo:b * 512 + lo + n]

    def bank_bf(self, b):
        return self.PS[:, b * 512:(b + 1) * 512].bitcast(BF16)

    def mm(self, out, lhsT, rhs, start, stop, reads, writes):
        self.P.op("pe", lambda e: e.matmul(out, lhsT, rhs, start=start, stop=stop, skip_group_check=True), reads, writes)

    def tr(self, out, in_, ident, reads, writes):
        self.P.op("pe", lambda e: e.transpose(out, in_, ident), reads, writes)

    def act(self, out, in_, func, reads, writes, bias=None, scale=None):
        kw = {}
        if bias is not None:
            kw["bias"] = bias
        if scale is not None:
            kw["scale"] = scale
        self.P.op("act", lambda e: e.activation(out=out, in_=in_, func=func, **kw), reads, writes)

    def tt(self, out, a, b, op, reads, writes, eng="dve"):
        self.P.op(eng, lambda e: e.tensor_tensor(out=out, in0=a, in1=b, op=op), reads, writes)

    def ts(self, out, a, s1, op0, reads, writes, s2=None, op1=None, eng="dve"):
        if op1 is None:
            self.P.op(eng, lambda e: e.tensor_scalar(out=out, in0=a, scalar1=s1, scalar2=None, op0=op0), reads, writes)
        else:
            self.P.op(eng, lambda e: e.tensor_scalar(out=out, in0=a, scalar1=s1, scalar2=s2, op0=op0, op1=op1), reads, writes)

    def stt(self, out, a, s, b, op0, op1, reads, writes):
        self.P.op("dve", lambda e: e.scalar_tensor_tensor(out=out, in0=a, scalar=s, in1=b, op0=op0, op1=op1), reads, writes)

    def cp(self, out, in_, reads, writes, eng="dve"):
        if eng == "act":
            self.P.op("act", lambda e: e.copy(out=out, in_=in_), reads, writes)
        else:
            self.P.op(eng, lambda e: e.tensor_copy(out=out, in_=in_), reads, writes)

    def red(self, out, in_, op, reads, writes):
        self.P.op("dve", lambda e: e.tensor_reduce(out=out, in_=in_, axis=AX.X, op=op), reads, writes)

    def recip(self, out, in_, reads, writes):
        self.P.op("dve", lambda e: e.reciprocal(out=out, in_=in_), reads, writes)

    def ld(self, out, in_, reads, writes, q="sp"):
        self.P.dma(q, out, in_, reads, writes)

    def prepass(self):
        self.phase()
        SZ = 2560
        stf = [self.tile([128, SZ], F32, "stf%d" % i) for i in range(3)]
        stb = [self.tile([128, SZ], BF16, "stb%d" % i) for i in range(3)]
        pieces = []

        def piece(srcs, dst, a, b):
            pieces.append((srcs, dst, a, b))

        def views(j):
            srcs, dst, a, b = pieces[j]
            (f, r_f), (bt, r_b) = stf[j % 3], stb[j % 3]
            fv = f[:, 0:a * b].rearrange("p (a b) -> p a b", a=a)
            bv = bt[:, 0:a * b].rearrange("p (a b) -> p a b", a=a)
            return srcs, dst, fv, bv, r_f, r_b

        def emit_in(j):
            srcs, dst, fv, bv, r_f, r_b = views(j)
            for (c0, c1, src) in srcs:
                self.ld(fv[:, :, c0:c1], src, [], [r_f])

        def emit_rest(j):
            srcs, dst, fv, bv, r_f, r_b = views(j)
            self.cp(bv, fv, [r_f], [r_b], eng=("act", "dve", "pool")[j % 3])
            self.ld(dst, bv, [r_b], [self.r_wbf])

        for l in range(self.L):
            wi = self.W["w_in"][l].rearrange("(k p) n -> p k n", p=128)
            wid = self.WI[l].rearrange("p (k n) -> p k n", k=KC)
            for k in range(KC):
                piece([(0, 2352, wi[:, k:k + 1, :])], wid[:, k:k + 1, :], 1, 2352)
            for br in range(4):
                for dc in range(KC):
                    src = self.W["w_gate"][l][br][:, dc * 128:(dc + 1) * 128].rearrange("(k p) n -> p k n", p=128)
                    piece([(0, 128, src)], self.WG[l, br, dc].rearrange("p (k n) -> p k n", k=KC), KC, 128)
                src = self.W["w_branch"][l][br].rearrange("(k p) n -> p k n", p=128)
                piece([(0, D, src)], self.WBR[l, br].rearrange("p (k n) -> p k n", k=2), 2, D)
            wo = self.W["w_out"][l].rearrange("(k p) n -> p k n", p=128)
            wod = self.WO[l].rearrange("p (k n) -> p k n", k=KC)
            for k in range(0, KC, 2):
                piece([(0, D, wo[:, k:k + 2, :])], wod[:, k:k + 2, :], 2, D)
            for fc in range(FC):
                sa = self.W["w_up"][l][:, fc * 128:(fc + 1) * 128].rearrange("(k p) n -> p k n", p=128)
                sv = self.W["w_up"][l][:, DFF + fc * 128:DFF + (fc + 1) * 128].rearrange("(k p) n -> p k n", p=128)
                piece([(0, 128, sa), (128, 256, sv)], self.WU[l, fc].rearrange("p (k n) -> p k n", k=KC), KC, 256)
            wdn = self.W["w_down"][l].rearrange("(f p) n -> p f n", p=128)
            wdd = self.WD[l].rearrange("p (f n) -> p f n", f=FC)
            for f0 in range(0, FC, 2):
                piece([(0, D, wdn[:, f0:f0 + 2, :])], wdd[:, f0:f0 + 2, :], 2, D)
        npc = len(pieces)
        for j in range(npc + 2):
            if j < npc:
                emit_in(j)
            if j - 2 >= 0:
                emit_rest(j - 2)

    def build(self):
        P = self.P
        NSEQ, L, T = self.NSEQ, self.L, self.T
        C = {}
        self.C = C
        r_c = self.r_c = Res("consts")
        for k in ("ident_bf", "ones_bf", "ident_f", "ones_f", "tri_f", "tri_b", "e_last", "e_first", "maskA", "maskB", "maskN", "bdc", "bds"):
            C[k], _ = self.tile([128, 128], CONST_DT.get(k, F32), k)
            self.ld(C[k], self.CD[k], [], [r_c])
        for k in ("mneg_f", "mneg_b"):
            C[k], _ = self.tile([128, 4, 128], F32, k)
            self.ld(C[k], self.CD[k], [], [r_c])
        self.hT, self.r_hT = self.tile([128, KC, T], BF16, "hT")
        self.MOD, self.r_mod = self.tile([128, L, 48, NSEQ + 1], F32, "MOD")
        self.pv = {}
        self.r_pv = Res("pvec")
        for k, shp in PARAM_SHAPES(L).items():
            if shp[1] == 128:
                self.pv[k], _ = self.tile([128, L] + list(shp[2:]) if len(shp) > 2 else [128, L], F32, k)
                src = self.PR[k]
                if len(shp) == 3:
                    self.ld(self.pv[k], src.rearrange("l p a -> p l a"), [], [self.r_pv])
                else:
                    for l in range(L):
                        self.ld(self.pv[k][:, l], src[l], [], [self.r_pv])
        self.bg_b, _ = self.tile([128, L, 16], F32, "bgb")
        self.sink_b, _ = self.tile([128, L, 4], F32, "sinkb")
        for l in range(L):
            self.ld(self.bg_b[:, l, :], self.PR["b_ml_gates"][l].partition_broadcast(128), [], [self.r_pv])
            self.ld(self.sink_b[:, l, :], self.PR["wg_sink"][l].partition_broadcast(128), [], [self.r_pv])
        self.wconvB, _ = self.tile([128, 8, 3], F32, "wconvB")
        self.r_wcb = Res("wcb")
        self.dv, self.r_dv = self.tile([128, 6, KC], F32, "derived")
        self.dvc, self.r_dvc = self.tile([128, 6, KC], F32, "derivedc")
        self.a_base = self.a_off
        for s in range(NSEQ):
            self.ld(self.XRES[s][:, 0:self.NCTX], self.cT_in[s], [], [self.r_xres[s]])
            self.ld(self.XRES[s][:, self.NCTX:T], self.xT_in[s], [], [self.r_xres[s]])
        self.prepass()
        self.mod_phase()
        import os
        stop = int(os.environ.get("KSTOP", "99"))
        for s in range(NSEQ):
            for l in range(L):
                last = (l == L - 1)
                steps = [lambda: self.derive(l, s), lambda: self.norm_mod(s, 0), lambda: self.branch_a(s, l, last),
                         lambda: self.branch_c(s, l, last), lambda: self.branch_d(s, l, last), lambda: self.branch_b(s, l, last),
                         lambda: self.merge(s, l, last), lambda: self.norm_mod(s, 3, skip_ctx=last), lambda: self.ffn(s, l, last)]
                for i, f in enumerate(steps):
                    if i < stop:
                        f()
            self.phase()
            self.ld(self.out[s], self.XRES[s][:, self.NCTX:T], [self.r_xres[s]], [self.r_out])
        P.barrier()
        P.emit()
        self.st.close()
        return self.nc

    def mod_phase(self):
        self.phase()
        NJ = self.NSEQ + 1
        cc, r_cc = self.tile([128, KC, NJ], F32, "cc")
        sg, r_sg = self.tile([128, KC, NJ], F32, "sg")
        self.ld(cc, self.ccT, [], [r_cc])
        self.act(sg, cc, AF.Sigmoid, [r_cc], [r_sg])
        self.tt(cc, cc, sg, ALU.mult, [r_sg, r_cc], [r_cc])
        wm = [self.tile([128, KC, 512], F32, "wm%d" % i) for i in range(2)]
        n = 0
        for l in range(self.L):
            for g in range(12):
                w, r_w = wm[n % 2]
                n += 1
                self.ld(w, self.W["w_mod"][l][:, g * 512:(g + 1) * 512].rearrange("(k p) n -> p k n", p=128), [], [r_w])
                b = n % 2
                for j4 in range(4):
                    for k in range(KC):
                        self.mm(self.bank(b, NJ, j4 * 8), w[:, k, j4 * 128:(j4 + 1) * 128], cc[:, k, :], k == 0 and j4 == 0, k == KC - 1,
                                [r_w, r_cc], [self.r_ps[b]])
                for j4 in range(4):
                    ch = g * 4 + j4
                    self.ts(self.MOD[:, l, ch, :], self.bank(b, NJ, j4 * 8), self.pv["b_mod"][:, l, ch:ch + 1], ALU.add,
                            [self.r_ps[b], self.r_pv], [self.r_mod])

    def derive(self, l, s):
        for (dst, r_dst, j) in ((self.dv, self.r_dv, s), (self.dvc, self.r_dvc, self.NSEQ)):
            for half, gpre, gpost in ((0, "g_pre_mix", "g_post_mix"), (1, "g_pre_ffn", "g_post_ffn")):
                sh = self.MOD[:, l, half * 24 + 0:half * 24 + 8, j]
                sc = self.MOD[:, l, half * 24 + 8:half * 24 + 16, j]
                g = self.MOD[:, l, half * 24 + 16:half * 24 + 24, j]
                self.stt(dst[:, half * 3 + 0, :], sc, 1.0, self.pv[gpre][:, l, :], ALU.add, ALU.mult, [self.r_mod, self.r_pv], [r_dst])
                self.cp(dst[:, half * 3 + 1, :], sh, [self.r_mod], [r_dst])
                self.tt(dst[:, half * 3 + 2, :], g, self.pv[gpost][:, l, :], ALU.mult, [self.r_mod, self.r_pv], [r_dst])

    def seg_dv(self, lo):
        return (self.dvc, self.r_dvc) if lo < self.NCTX else (self.dv, self.r_dv)

    def rstd_block(self, src, r_src, n, nk, dim, sq, r_sq, rs, r_rs, bank):
        for k in range(nk):
            self.act(sq[:, k, 0:n], src[:, k, 0:n], AF.Square, [r_src], [r_sq])
        for k in range(nk):
            self.mm(self.bank(bank, n), self.C["ones_bf"], sq[:, k, 0:n], k == 0, k == nk - 1, [r_sq, self.r_c], [self.r_ps[bank]])
        self.ts(rs[:, 0:n], self.bank(bank, n), 1.0 / dim, ALU.mult, [self.r_ps[bank]], [r_rs], s2=EPS, op1=ALU.add)
        self.act(rs[:, 0:n], rs[:, 0:n], AF.Ln, [r_rs], [r_rs])
        self.act(rs[:, 0:n], rs[:, 0:n], AF.Exp, [r_rs], [r_rs], scale=-0.5)

    def norm_mod(self, s, base, skip_ctx=False):
        self.phase()
        xb = [self.tile([128, KC, 512], F32, "xb%d" % i) for i in range(2)]
        sq, r_sq = self.tile([128, KC, 512], BF16, "sq")
        rs, r_rs = self.tile([128, 512], F32, "rs")
        tmp, r_tmp = self.tile([128, 512], F32, "tmp")
        for bi, (lo, hi) in enumerate(self.blocks):
            if skip_ctx and lo < self.NCTX:
                continue
            n = hi - lo
            x, r_x = xb[bi % 2]
            self.ld(x[:, :, 0:n], self.XRES[s][:, lo:hi].rearrange("(k p) n -> p k n", p=128), [self.r_xres[s]], [r_x])
            self.rstd_block(x, r_x, n, KC, D, sq, r_sq, rs, r_rs, bi % 2)
            dv, r_dv = self.seg_dv(lo)
            for k in range(KC):
                self.tt(tmp[:, 0:n], x[:, k, 0:n], rs[:, 0:n], ALU.mult, [r_x, r_rs], [r_tmp])
                self.act(self.hT[:, k, lo:hi], tmp[:, 0:n], AF.Identity, [r_tmp, r_dv], [self.r_hT],
                         bias=dv[:, base + 1, k:k + 1], scale=dv[:, base + 0, k:k + 1])

    def load_w_cols(self, dst, r_dst, l, c0, c1):
        self.ld(dst, self.WI[l].rearrange("p (k n) -> p k n", k=KC)[:, :, c0:c1], [self.r_wbf], [r_dst])

    def make_perm(self, dst, src, nheads, hd, r0, rd, r_dst, r_src, nk):
        nf = rd // 4
        self.P.op("dve", lambda e: e.memset(dst, 0.0), [], [r_dst])
        for k in range(nk):
            for h in range(nheads):
                b = h * hd + r0
                for hh in range(2):
                    o = b + hh * 2 * nf
                    self.ts(dst[:, k, o:o + nf], src[:, k, o + nf:o + 2 * nf], -1.0, ALU.mult, [r_src], [r_dst])
                    self.cp(dst[:, k, o + nf:o + 2 * nf], src[:, k, o:o + nf], [r_src], [r_dst])

    def attn_scores(self, it, buf):
        Pm, r_Pm, sm, r_sm = buf
        kparts, sink, scale = it["kparts"], it["sink"], it["scale"]
        ncols = max(c0 + k.shape[-1] for (k, _, c0, _) in kparts)
        started = set()
        for (kT, r_k, c0, mask) in kparts:
            n = kT.shape[-1]
            b = c0 // 512
            assert (c0 + n - 1) // 512 == b
            self.mm(self.PS[:, c0:c0 + n], it["q"], kT, b not in started, mask is None, [it["r_q"], r_k], [self.r_ps[b]])
            started.add(b)
            if mask is not None:
                self.mm(self.PS[:, c0:c0 + n], self.C["ident_bf"], mask, False, True, [self.r_c], [self.r_ps[b]])
        tot = ncols
        if sink is not None:
            b = ncols // 512
            self.mm(self.PS[:, ncols:ncols + 1], self.C["ones_f"][0:1, :], sink, b not in started, True, [self.r_c, self.r_pv], [self.r_ps[b]])
            started.add(b)
            tot = ncols + 1
        banks = [self.r_ps[b] for b in sorted(started)]
        self.red(sm[:, 0:1], self.PS[:, 0:tot], ALU.max, banks, [r_sm])
        self.ts(sm[:, 1:2], sm[:, 0:1], -scale, ALU.mult, [r_sm], [r_sm])
        self.act(Pm[:, 0:tot], self.PS[:, 0:tot], AF.Exp, banks + [r_sm], [r_Pm], bias=sm[:, 1:2], scale=scale)
        it["ncols"] = ncols

    def attn_transposes(self, it, buf, PTt, r_PT):
        Pm, r_Pm, sm, r_sm = buf
        vparts = it["vparts"]
        nv = len(vparts)
        for i, (V, r_v, c0) in enumerate(vparts):
            tb = 5 + (i // 8) % 2
            slot = i % 8
            pt_ps = self.bank_bf(tb)[:, slot * 128:(slot + 1) * 128]
            self.tr(pt_ps, Pm[:, c0:c0 + 128], self.C["ident_bf"], [r_Pm, self.r_c], [self.r_ps[tb]])
            if slot == 7 or i == nv - 1:
                g0 = i - slot
                self.cp(PTt[:, g0:i + 1, :], self.bank_bf(tb)[:, 0:(slot + 1) * 128].rearrange("p (a b) -> p a b", b=128),
                        [self.r_ps[tb]], [r_PT], eng="dve" if (i // 8) % 2 == 0 else "act")

    def attn_pv(self, it, buf, PTt, r_PT):
        Pm, r_Pm, sm, r_sm = buf
        vparts, sink, ncols = it["vparts"], it["sink"], it["ncols"]
        nv = len(vparts)
        for i, (V, r_v, c0) in enumerate(vparts):
            self.mm(self.bank(7, 65), PTt[:, i, :], V, i == 0, i == nv - 1, [r_PT, r_v], [self.r_ps[7]])
        if sink is not None:
            self.tt(sm[:, 2:3], self.bank(7, 1, 64), Pm[:, ncols:ncols + 1], ALU.add, [self.r_ps[7], r_Pm], [r_sm])
            self.recip(sm[:, 3:4], sm[:, 2:3], [r_sm], [r_sm])
        else:
            self.recip(sm[:, 3:4], self.bank(7, 1, 64), [self.r_ps[7]], [r_sm])
        self.ts(it["out"], self.bank(7, 64), sm[:, 3:4], ALU.mult, [self.r_ps[7], r_sm], [it["r_out"]])
        if it.get("after"):
            it["after"]()

    def attn_run(self, items):
        bufs = []
        for j in range(2):
            Pm, r_Pm = self.tile([128, self.T + 128], BF16, "Pm%d" % j)
            sm, r_sm = self.tile([128, 4], F32, "sm%d" % j)
            bufs.append((Pm, r_Pm, sm, r_sm))
        PTt, r_PT = self.tile([128, self.TT, 128], BF16, "PT")
        n = len(items)
        if n == 0:
            return
        self.attn_scores(items[0], bufs[0])
        for i in range(n):
            if i + 1 < n:
                self.attn_scores(items[i + 1], bufs[(i + 1) % 2])
            self.attn_transposes(items[i], bufs[i % 2], PTt, r_PT)
            self.attn_pv(items[i], bufs[i % 2], PTt, r_PT)

    def store_y_tm(self, ytm, r_ytm, s, br, tile_i, ybuf):
        yT, r_yT = ybuf
        for c in range(2):
            self.tr(self.bank_bf(6)[:, c * 128:(c + 1) * 128], ytm[:, c * 128:(c + 1) * 128], self.C["ident_bf"], [r_ytm, self.r_c], [self.r_ps[6]])
        self.cp(yT, self.bank_bf(6)[:, 0:256].rearrange("p (a b) -> p a b", b=128), [self.r_ps[6]], [r_yT])
        self.ld(self.YS[s][br * 256:(br + 1) * 256, tile_i * 128:(tile_i + 1) * 128].rearrange("(c p) n -> p c n", p=128), yT,
                [r_yT], [self.r_ys[s]])

    def branch_a(self, s, l, last):
        self.phase()
        T, TT, CT = self.T, self.TT, self.CT
        wA, r_wA = self.tile([128, KC, 544], BF16, "wA")
        wAp, r_wAp = self.tile([128, KC, 32], BF16, "wAp")
        wq, r_wq = self.tile([128, 2, 384], BF16, "wq")
        wqf, r_wqf = self.tile([128, 2, 384], F32, "wqf")
        wqp, r_wqp = self.tile([128, 2, 384], BF16, "wqp")
        wkv, r_wkv = self.tile([128, 2, 512], BF16, "wkv")
        wkvf, r_wkvf = self.tile([128, 2, 512], F32, "wkvf")
        raw, r_raw = self.tile([128, 4, 512], F32, "raw")
        sq, r_sq = self.tile([128, 4, 512], BF16, "sqA")
        rs, r_rs = self.tile([128, 2, 512], F32, "rsA")
        cqn, r_cqn = self.tile([128, 4, 512], BF16, "cqn")
        tab, r_tab = self.tile([128, 4, 512], F32, "tabA")
        t1, r_t1 = self.tile([128, 512], F32, "t1A")
        t2, r_t2 = self.tile([128, 512], F32, "t2A")
        qT, r_qT = self.tile([128, 4, T], BF16, "qTA")
        kT, r_kT = self.tile([128, 4, T], BF16, "kTA")
        Va, r_Va = self.tile([128, TT, 4, 65], BF16, "VaA")
        ytms = [self.tile([128, 256], BF16, "ytmA%d" % i) for i in range(2)]
        ybuf = self.tile([128, 2, 128], BF16, "yTA")
        self.load_w_cols(wA, r_wA, l, 0, 544)
        self.ld(wqf, self.W["w_uq"][l].rearrange("(k p) n -> p k n", p=128), [], [r_wqf])
        self.ld(wkvf, self.W["w_ukv"][l].rearrange("(k p) n -> p k n", p=128), [], [r_wkvf])
        for k in range(2):
            self.ts(wq[:, k, :], wqf[:, k, :], self.pv["g_qa"][:, l, k:k + 1], ALU.mult, [r_wqf, self.r_pv], [r_wq])
            self.ts(wkv[:, k, :], wkvf[:, k, :], self.pv["g_kva"][:, l, k:k + 1], ALU.mult, [r_wkvf, self.r_pv], [r_wkv])
        self.make_perm(wqp, wq, 4, 96, 64, 32, r_wqp, r_wq, 2)
        wkr = wA[:, :, 512:544]
        self.make_perm(wAp, wkr, 1, 32, 0, 32, r_wAp, r_wA, KC)
        self.P.op("dve", lambda e: e.memset(Va, 1.0), [], [r_Va])
        for bi, (lo, hi) in enumerate(self.blocks):
            n = hi - lo
            for c in range(4):
                b = c % 2
                for k in range(KC):
                    self.mm(self.bank(b, n), wA[:, k, c * 128:(c + 1) * 128], self.hT[:, k, lo:hi], k == 0, k == KC - 1,
                            [r_wA, self.r_hT], [self.r_ps[b]])
                self.cp(raw[:, c, 0:n], self.bank(b, n), [self.r_ps[b]], [r_raw], eng="act")
            self.rstd_block(raw[:, 0:2], r_raw, n, 2, 256, sq[:, 0:2], r_sq, rs[:, 0], r_rs, 2)
            self.rstd_block(raw[:, 2:4], r_raw, n, 2, 256, sq[:, 2:4], r_sq, rs[:, 1], r_rs, 3)
            for c in range(4):
                self.tt(cqn[:, c, 0:n], raw[:, c, 0:n], rs[:, c // 2, 0:n], ALU.mult, [r_raw, r_rs], [r_cqn])
            self.ld(tab[0:96, 0, 0:n], self.CD["cosq_a"][:, lo:hi], [], [r_tab])
            self.ld(tab[0:96, 1, 0:n], self.CD["sinq_a"][:, lo:hi], [], [r_tab])
            self.ld(tab[0:32, 2, 0:n], self.CD["cosk_a"][:, lo:hi], [], [r_tab])
            self.ld(tab[0:32, 3, 0:n], self.CD["sink_a"][:, lo:hi], [], [r_tab])
            for h in range(4):
                for (w_, r_w_, b) in ((wq, r_wq, 0), (wqp, r_wqp, 1)):
                    for k in range(2):
                        self.mm(self.bank(b, n)[0:96], w_[:, k, h * 96:(h + 1) * 96], cqn[:, k, 0:n], k == 0, k == 1, [r_w_, r_cqn], [self.r_ps[b]])
                self.tt(t1[0:96, 0:n], self.bank(0, n)[0:96], tab[0:96, 0, 0:n], ALU.mult, [self.r_ps[0], r_tab], [r_t1])
                self.tt(t2[0:96, 0:n], self.bank(1, n)[0:96], tab[0:96, 1, 0:n], ALU.mult, [self.r_ps[1], r_tab], [r_t2])
                self.tt(qT[0:96, h, lo:hi], t1[0:96, 0:n], t2[0:96, 0:n], ALU.add, [r_t1, r_t2], [r_qT])
                for k in range(2):
                    self.mm(self.bank(2, n)[0:64], wkv[:, k, h * 128:h * 128 + 64], cqn[:, 2 + k, 0:n], k == 0, k == 1, [r_wkv, r_cqn], [self.r_ps[2]])
                self.cp(kT[0:64, h, lo:hi], self.bank(2, n)[0:64], [self.r_ps[2]], [r_kT], eng="act")
            for (w_, r_w_, b) in ((wkr, r_wA, 3), (wAp, r_wAp, 4)):
                for k in range(KC):
                    self.mm(self.bank(b, n)[0:32], w_[:, k, :], self.hT[:, k, lo:hi], k == 0, k == KC - 1, [r_w_, self.r_hT], [self.r_ps[b]])
            self.tt(t1[0:32, 0:n], self.bank(3, n)[0:32], tab[0:32, 2, 0:n], ALU.mult, [self.r_ps[3], r_tab], [r_t1])
            self.tt(t2[0:32, 0:n], self.bank(4, n)[0:32], tab[0:32, 3, 0:n], ALU.mult, [self.r_ps[4], r_tab], [r_t2])
            self.tt(t1[0:32, 0:n], t1[0:32, 0:n], t2[0:32, 0:n], ALU.add, [r_t1, r_t2], [r_t1])
            for h in range(4):
                self.cp(kT[64:96, h, lo:hi], t1[0:32, 0:n], [r_t1], [r_kT])
            for ti in range(lo // 128, hi // 128):
                o = ti * 128 - lo
                for k in range(2):
                    self.mm(self.bank(5, 256).rearrange("p (h d) -> p h d", d=64), cqn[:, 2 + k, o:o + 128],
                            wkv[:, k, :].rearrange("p (h x) -> p h x", x=128)[:, :, 64:128], k == 0, k == 1, [r_cqn, r_wkv], [self.r_ps[5]])
                self.cp(Va[:, ti, :, 0:64], self.bank(5, 256).rearrange("p (h d) -> p h d", d=64), [self.r_ps[5]], [r_Va])
        q_tiles = list(range(CT, TT)) + ([] if last else list(range(CT)))
        items = []
        for n_, qi in enumerate(q_tiles):
            is_ctx = qi < CT
            nk = self.NCTX if is_ctx else T
            yt, r_yt = ytms[n_ % 2]
            for h in range(4):
                kparts = []
                c0 = 0
                while c0 < nk:
                    n = min(512, nk - c0)
                    kparts.append((kT[0:96, h, c0:c0 + n], r_kT, c0, None))
                    c0 += n
                vparts = [(Va[:, i, h, :], r_Va, i * 128) for i in range(nk // 128)]
                it = dict(q=qT[0:96, h, qi * 128:(qi + 1) * 128], r_q=r_qT, kparts=kparts, sink=None, vparts=vparts, scale=1.0,
                          out=yt[:, h * 64:(h + 1) * 64], r_out=r_yt)
                if h == 3:
                    it["after"] = (lambda yt=yt, r_yt=r_yt, qi=qi: self.store_y_tm(yt, r_yt, s, 0, qi, ybuf))
                items.append(it)
        self.attn_run(items)

    def branch_c(self, s, l, last):
        self.phase()
        T, TT, CT = self.T, self.TT, self.CT
        wC, r_wC = self.tile([128, KC, 512], BF16, "wC")
        wCp, r_wCp = self.tile([128, KC, 384], BF16, "wCp")
        tab, r_tab = self.tile([128, 2, 512], F32, "tabC")
        t1, r_t1 = self.tile([128, 512], F32, "t1C")
        t2, r_t2 = self.tile([128, 512], F32, "t2C")
        qT, r_qT = self.tile([128, 4, T], BF16, "qTC")
        kT, r_kT = self.tile([128, 2, T], BF16, "kTC")
        Va, r_Va = self.tile([128, TT, 2, 65], BF16, "VaC")
        sk8, r_sk8 = self.tile([128, 4], F32, "sk8")
        ytms = [self.tile([128, 256], BF16, "ytmC%d" % i) for i in range(2)]
        ybuf = self.tile([128, 2, 128], BF16, "yTC")
        self.load_w_cols(wC, r_wC, l, 1584, 2096)
        self.make_perm(wCp, wC[:, :, 0:384], 6, 64, 0, 64, r_wCp, r_wC, KC)
        self.ts(sk8, self.sink_b[:, l, :], 8.0, ALU.mult, [self.r_pv], [r_sk8])
        self.P.op("dve", lambda e: e.memset(Va, 1.0), [], [r_Va])
        for bi, (lo, hi) in enumerate(self.blocks):
            n = hi - lo
            self.ld(tab[0:64, 0, 0:n], self.CD["cos_c"][:, lo:hi], [], [r_tab])
            self.ld(tab[0:64, 1, 0:n], self.CD["sin_c"][:, lo:hi], [], [r_tab])
            for hh in range(6):
                for (w_, r_w_, b) in ((wC, r_wC, 0), (wCp, r_wCp, 1)):
                    for k in range(KC):
                        self.mm(self.bank(b, n)[0:64], w_[:, k, hh * 64:(hh + 1) * 64], self.hT[:, k, lo:hi], k == 0, k == KC - 1,
                                [r_w_, self.r_hT], [self.r_ps[b]])
                self.tt(t1[0:64, 0:n], self.bank(0, n)[0:64], tab[0:64, 0, 0:n], ALU.mult, [self.r_ps[0], r_tab], [r_t1])
                self.tt(t2[0:64, 0:n], self.bank(1, n)[0:64], tab[0:64, 1, 0:n], ALU.mult, [self.r_ps[1], r_tab], [r_t2])
                dst = qT[0:64, hh, lo:hi] if hh < 4 else kT[0:64, hh - 4, lo:hi]
                self.tt(dst, t1[0:64, 0:n], t2[0:64, 0:n], ALU.add, [r_t1, r_t2], [r_qT if hh < 4 else r_kT])
            for ti in range(lo // 128, hi // 128):
                for k in range(KC):
                    self.mm(self.bank(5, 128), self.hT[:, k, ti * 128:(ti + 1) * 128], wC[:, k, 384:512], k == 0, k == KC - 1,
                            [self.r_hT, r_wC], [self.r_ps[5]])
                self.cp(Va[:, ti, :, 0:64], self.bank(5, 128).rearrange("p (h d) -> p h d", d=64), [self.r_ps[5]], [r_Va])
        NQ = TT - CT
        q_tiles = list(range(CT, TT)) + ([] if last else list(range(CT)))
        NC_ = self.NCTX
        items = []
        for n_, qi in enumerate(q_tiles):
            is_ctx = qi < CT
            yt, r_yt = ytms[n_ % 2]
            for h in range(4):
                g = h // 2
                kparts = [(kT[0:64, g, 0:NC_], r_kT, 0, None)]
                vparts = [(Va[:, i, g, :], r_Va, i * 128) for i in range(CT)]
                if not is_ctx:
                    i = qi - CT
                    col = NC_
                    for (j, mk) in ((i - 1, "maskA"), (i, None), (i + 1, "maskB")):
                        if 0 <= j < NQ:
                            kparts.append((kT[0:64, g, NC_ + j * 128:NC_ + (j + 1) * 128], r_kT, col, self.C[mk] if mk else None))
                            vparts.append((Va[:, CT + j, g, :], r_Va, col))
                            col += 128
                it = dict(q=qT[0:64, h, qi * 128:(qi + 1) * 128], r_q=r_qT, kparts=kparts, sink=sk8[0:1, h:h + 1], vparts=vparts, scale=0.125,
                          out=yt[:, h * 64:(h + 1) * 64], r_out=r_yt)
                if h == 3:
                    it["after"] = (lambda yt=yt, r_yt=r_yt, qi=qi: self.store_y_tm(yt, r_yt, s, 2, qi, ybuf))
                items.append(it)
        self.attn_run(items)

    def branch_d(self, s, l, last):
        self.phase()
        T, TT, CT = self.T, self.TT, self.CT
        wD, r_wD = self.tile([128, KC, 256], BF16, "wD")
        ud, r_ud = self.tile([128, 2, T], BF16, "udT")
        uc, r_uc = self.tile([128, TT, 2, 256], BF16, "uc_tm")
        cn, r_cn = self.tile([128, 16, 512], BF16, "cn")
        sn, r_sn = self.tile([128, 16, 512], BF16, "sn")
        yo, r_yo = self.tile([128, 2, 512], BF16, "yoD")
        self.load_w_cols(wD, r_wD, l, 2096, 2352)
        for (lo, hi) in self.blocks:
            n = hi - lo
            for c in range(2):
                for k in range(KC):
                    self.mm(self.bank(c, n), wD[:, k, c * 128:(c + 1) * 128], self.hT[:, k, lo:hi], k == 0, k == KC - 1,
                            [r_wD, self.r_hT], [self.r_ps[c]])
                self.cp(ud[:, c, lo:hi], self.bank(c, n), [self.r_ps[c]], [r_ud], eng="act" if c else "dve")
        for ti in range(TT):
            for j, m in enumerate(("bdc", "bds")):
                for c in range(2):
                    self.mm(self.bank(2, 128, j * 256 + c * 128), ud[:, c, ti * 128:(ti + 1) * 128], self.C[m], c == 0 and j == 0, True,
                            [r_ud, self.r_c], [self.r_ps[2]])
            self.cp(uc[:, ti], self.bank(2).rearrange("p (a b) -> p a b", a=2), [self.r_ps[2]], [r_uc])
        segs = [("lat", CT, TT, self.NCTX)] + ([] if last else [("ctx", 0, CT, 0)])
        for nm, t0, t1_, col0 in segs:
            N = (t1_ - t0) * 128
            ntl = t1_ - t0
            for kb in range(0, N, 512):
                n = min(512, N - kb)
                self.ld(cn[:, 0:ntl, 0:n], self.CD["cn_" + nm][:, kb:kb + n].rearrange("(a p) k -> p a k", p=128), [], [r_cn])
                self.ld(sn[:, 0:ntl, 0:n], self.CD["sn_" + nm][:, kb:kb + n].rearrange("(a p) k -> p a k", p=128), [], [r_sn])
                for c in range(2):
                    b = 3 + c
                    for a in range(ntl):
                        self.mm(self.bank(b, n), uc[:, t0 + a, 0, c * 128:(c + 1) * 128], cn[:, a, 0:n], a == 0, False, [r_uc, r_cn], [self.r_ps[b]])
                        self.mm(self.bank(b, n), uc[:, t0 + a, 1, c * 128:(c + 1) * 128], sn[:, a, 0:n], False, a == ntl - 1, [r_uc, r_sn], [self.r_ps[b]])
                    self.cp(yo[:, c, 0:n], self.bank(b, n), [self.r_ps[b]], [r_yo], eng="act" if c else "dve")
                self.ld(self.YS[s][768:1024, col0 + kb:col0 + kb + n].rearrange("(c p) n -> p c n", p=128), yo[:, :, 0:n], [r_yo], [self.r_ys[s]])

    def branch_b(self, s, l, last):
        self.phase()
        T, TT, CT = self.T, self.TT, self.CT
        wB, r_wB = self.tile([128, KC, 1040], BF16, "wB")
        araw, r_araw = self.tile([128, 514], F32, "arawB")
        c1, r_c1 = self.tile([128, 512], F32, "c1B")
        qk, r_qk = self.tile([128, 8, T], BF16, "qkB")
        ktm, r_ktm = self.tile([128, TT, 256], BF16, "ktmB")
        Va, r_Va = self.tile([128, TT, 4, 65], BF16, "VaB")
        og, r_og = self.tile([128, 256], F32, "ogB")
        G, r_G = self.tile([128, TT, 16], F32, "GB")
        hs, r_hs = self.tile([128, TT, 256], F32, "hsB")
        self.load_w_cols(wB, r_wB, l, 544, 1584)
        self.ld(self.wconvB[0:64], self.PR["wconvB"][l], [], [self.r_wcb])
        self.P.op("dve", lambda e: e.memset(Va, 1.0), [], [r_Va])
        self.P.op("dve", lambda e: e.memset(hs, 0.0), [], [r_hs])
        for (lo, hi) in self.blocks:
            n = hi - lo
            seg_lo, seg_hi = (0, self.NCTX) if lo < self.NCTX else (self.NCTX, T)
            for hh in range(8):
                ch, half = hh // 2, hh % 2
                c0 = hh * 64
                for k in range(KC):
                    self.mm(self.bank(0, n)[0:64], wB[:, k, c0:c0 + 64], self.hT[:, k, lo:hi], k == 0, k == KC - 1, [r_wB, self.r_hT], [self.r_ps[0]])
                self.P.op("dve", lambda e: e.memset(araw[0:64, :], 0.0), [], [r_araw])
                if lo > seg_lo:
                    for k in range(KC):
                        self.mm(self.bank(1, 1)[0:64], wB[:, k, c0:c0 + 64], self.hT[:, k, lo - 1:lo], k == 0, k == KC - 1, [r_wB, self.r_hT], [self.r_ps[1]])
                    self.cp(araw[0:64, 0:1], self.bank(1, 1)[0:64], [self.r_ps[1]], [r_araw])
                if hi < seg_hi:
                    for k in range(KC):
                        self.mm(self.bank(1, 1, 8)[0:64], wB[:, k, c0:c0 + 64], self.hT[:, k, hi:hi + 1], k == 0, k == KC - 1, [r_wB, self.r_hT], [self.r_ps[1]])
                    self.cp(araw[0:64, n + 1:n + 2], self.bank(1, 1, 8)[0:64], [self.r_ps[1]], [r_araw])
                self.cp(araw[0:64, 1:n + 1], self.bank(0, n)[0:64], [self.r_ps[0]], [r_araw], eng="act")
                wsl = self.wconvB[:, hh, :]
                self.ts(c1[0:64, 0:n], araw[0:64, 0:n], wsl[0:64, 0:1], ALU.mult, [r_araw, self.r_wcb], [r_c1])
                self.stt(c1[0:64, 0:n], araw[0:64, 1:n + 1], wsl[0:64, 1:2], c1[0:64, 0:n], ALU.mult, ALU.add, [r_araw, self.r_wcb, r_c1], [r_c1])
                self.stt(c1[0:64, 0:n], araw[0:64, 2:n + 2], wsl[0:64, 2:3], c1[0:64, 0:n], ALU.mult, ALU.add, [r_araw, self.r_wcb, r_c1], [r_c1])
                self.act(c1[0:64, 0:n], c1[0:64, 0:n], AF.Silu, [r_c1], [r_c1])
                self.ts(qk[0:64, hh, lo:hi], c1[0:64, 0:n], 1.0 if hh < 4 else 0.125, ALU.mult, [r_c1], [r_qk])
        Gt, r_Gt = self.tile([128, TT, 2, 4], F32, "GtB")
        for ti in range(TT):
            tsl = slice(ti * 128, (ti + 1) * 128)
            for k in range(KC):
                self.mm(self.bank(2, 16), self.hT[:, k, tsl], wB[:, k, 1024:1040], k == 0, k == KC - 1, [self.r_hT, r_wB], [self.r_ps[2]])
            self.tt(G[:, ti, :], self.bank(2, 16), self.bg_b[:, l, :], ALU.add, [self.r_ps[2], self.r_pv], [r_G])
            for k in range(KC):
                self.mm(self.bank(3, 256), self.hT[:, k, tsl], wB[:, k, 512:768], k == 0, k == KC - 1, [self.r_hT, r_wB], [self.r_ps[3]])
            self.cp(Va[:, ti, :, 0:64], self.bank(3, 256).rearrange("p (h d) -> p h d", d=64), [self.r_ps[3]], [r_Va])
            for h in range(4):
                self.tr(self.bank_bf(5)[:, h * 64:(h + 1) * 64], qk[0:64, 4 + h, tsl], self.C["ident_bf"][0:64, 0:64], [r_qk, self.r_c], [self.r_ps[5]])
            self.cp(ktm[:, ti, :], self.bank_bf(5)[:, 0:256], [self.r_ps[5]], [r_ktm])
        G5 = G.rearrange("p t (a b c) -> p t a b c", a=2, b=2)
        for d_ in range(2):
            fv = G5[:, :, d_, 1, :]
            self.act(Gt[:, :, d_, :], fv, AF.Exp, [r_G], [r_Gt], scale=-1.0)
            self.act(Gt[:, :, d_, :], Gt[:, :, d_, :], AF.Ln, [r_Gt], [r_Gt], bias=1.0)
            self.ts(fv, Gt[:, :, d_, :], -1.0, ALU.mult, [r_Gt], [r_G])
        B_TM, M_, NEGM, WIN, EMT, DEN, DAB, RR, WTM, DEC, MX, DENI = range(12)
        SX = []
        for d_ in range(2):
            X = {}
            for nm, shp, dt in (("diag", [128, 4, 128], F32), ("bBm", [128, 4, 128], F32), ("Wt", [128, 4, 128], F32),
                                ("Sb", [128, 4, 128], BF16), ("ST", [128, 4, 128], BF16), ("kw", [128, 4, 64], BF16),
                                ("sv", [128, 16, 4], F32), ("cm", [128, 8], F32), ("mst", [128, 4], F32), ("tmpi", [128, 4, 65], F32),
                                ("numh", [128, 4, 64], F32), ("Cst", [128, 4, 65], F32), ("Cbf", [128, 4, 65], BF16)):
                X[nm], X["r_" + nm] = self.tile(shp, dt, nm + "B%d" % d_)
            X["tri"] = self.C["tri_f" if d_ == 0 else "tri_b"]
            X["mneg"] = self.C["mneg_f" if d_ == 0 else "mneg_b"]
            X["esel"] = self.C["e_last" if d_ == 0 else "e_first"]
            X["b0"] = 4 * d_
            SX.append(X)
            self.P.op("dve", lambda e, t=X["Cst"]: e.memset(t, 0.0), [], [X["r_Cst"]])
            self.P.op("dve", lambda e, t=X["Cbf"]: e.memset(t, 0.0), [], [X["r_Cbf"]])
            self.P.op("dve", lambda e, t=X["mst"]: e.memset(t, 0.0), [], [X["r_mst"]])

        def chunk(d_, ti):
            X = SX[d_]
            diag, bBm, Wt, Sb, ST, kw, sv, cm, mst, tmpi, numh, Cst, Cbf = (X[k] for k in (
                "diag", "bBm", "Wt", "Sb", "ST", "kw", "sv", "cm", "mst", "tmpi", "numh", "Cst", "Cbf"))
            r_diag, r_bBm, r_Wt, r_Sb, r_ST, r_kw, r_sv, r_cm, r_mst, r_tmpi, r_numh, r_Cst, r_Cbf = (X["r_" + k] for k in (
                "diag", "bBm", "Wt", "Sb", "ST", "kw", "sv", "cm", "mst", "tmpi", "numh", "Cst", "Cbf"))
            b0 = X["b0"]
            bB_b, qk_b, st_b, ms_b = b0, b0 + 1, b0 + 2, b0 + 3
            r0, r1, r2, r3 = self.r_ps[bB_b], self.r_ps[qk_b], self.r_ps[st_b], self.r_ps[ms_b]
            cum_ps = self.bank(ms_b, 4)
            sel_ps = self.bank(ms_b, 8, 8)
            inter_ps = self.bank(ms_b, 260, 16)
            upd_ps = self.bank(ms_b, 260, 16)
            num_ps = self.bank(st_b, 256, 256)
            tsl = slice(ti * 128, (ti + 1) * 128)
            li = G[:, ti, d_ * 8:d_ * 8 + 4]
            lf = G[:, ti, d_ * 8 + 4:d_ * 8 + 8]
            self.mm(cum_ps, X["tri"], lf, True, True, [self.r_c, r_G], [r3])
            for h in range(4):
                self.mm(self.bank(qk_b, 128, h * 128), qk[0:64, h, tsl], qk[0:64, 4 + h, tsl], True, True, [r_qk], [r1])
            yield
            self.tt(sv[:, B_TM, :], li, cum_ps, ALU.subtract, [r_G, r3], [r_sv])
            for h in range(4):
                self.ts(diag[:, h, :], self.C["ident_f"], sv[:, B_TM, h:h + 1], ALU.mult, [self.r_c, r_sv], [r_diag])
            yield
            for h in range(4):
                self.mm(self.bank(bB_b, 128, h * 128), self.C["ones_f"], diag[:, h, :], True, True, [self.r_c, r_diag], [r0])
            yield
            self.tt(bBm, self.bank(bB_b).rearrange("p (h s) -> p h s", h=4), X["mneg"], ALU.add, [r0, self.r_c], [r_bBm])
            self.red(sv[:, MX, :], bBm, ALU.max, [r_bBm], [r_sv])
            self.tt(sv[:, M_, :], sv[:, MX, :], mst, ALU.max, [r_sv, r_mst], [r_sv])
            self.ts(sv[:, NEGM, :], sv[:, M_, :], -1.0, ALU.mult, [r_sv], [r_sv])
            self.tt(sv[:, WIN, :], mst, sv[:, M_, :], ALU.subtract, [r_mst, r_sv], [r_sv])
            self.tt(cm[:, 0:4], cum_ps, sv[:, M_, :], ALU.add, [r3, r_sv], [r_cm])
            self.cp(cm[:, 4:8], sv[:, M_, :], [r_sv], [r_cm])
            yield
            for h in range(4):
                self.act(Wt[:, h, :], bBm[:, h, :], AF.Exp, [r_bBm, r_sv], [r_Wt], bias=sv[:, NEGM, h:h + 1], scale=1.0)
            self.act(sv[:, WIN, :], sv[:, WIN, :], AF.Exp, [r_sv], [r_sv])
            self.act(sv[:, EMT, :], cm[:, 0:4], AF.Exp, [r_cm], [r_sv], scale=-1.0)
            self.mm(sel_ps, X["esel"], cm, True, True, [self.r_c, r_cm], [r3])
            yield
            self.tt(Sb, self.bank(qk_b).rearrange("p (h s) -> p h s", h=4), Wt, ALU.mult, [r1, r_Wt], [r_Sb])
            self.red(sv[:, DENI, :], Sb, ALU.add, [r_Sb], [r_sv])
            self.tt(sv[:, WTM, :], sv[:, B_TM, :], self.bank(ms_b, 4, 12), ALU.subtract, [r_sv, r3], [r_sv])
            self.tt(sv[:, DEC, :], mst, self.bank(ms_b, 4, 12), ALU.subtract, [r_mst, r3], [r_sv])
            self.cp(mst, self.bank(ms_b, 4, 8), [r3], [r_mst])
            yield
            for h in range(4):
                self.tr(self.bank_bf(st_b)[:, h * 128:(h + 1) * 128], Sb[:, h, :], self.C["ident_bf"], [r_Sb, self.r_c], [r2])
            for h in range(4):
                self.mm(self.bank(ms_b, 65, 16 + h * 65), qk[0:64, h, tsl], Cbf[0:64, h, :], True, True, [r_qk, r_Cbf], [r3])
            self.act(sv[:, WTM, :], sv[:, WTM, :], AF.Exp, [r_sv], [r_sv])
            self.act(sv[:, DEC, :], sv[:, DEC, :], AF.Exp, [r_sv], [r_sv])
            yield
            self.cp(ST, self.bank_bf(st_b)[:, 0:512].rearrange("p (h s) -> p h s", h=4), [r2], [r_ST])
            self.tt(tmpi, inter_ps.rearrange("p (h e) -> p h e", h=4), sv[:, WIN, :].unsqueeze(2).to_broadcast([128, 4, 65]), ALU.mult,
                    [r3, r_sv], [r_tmpi])
            self.tt(kw, ktm[:, ti, :].rearrange("p (h e) -> p h e", h=4), sv[:, WTM, :].unsqueeze(2).to_broadcast([128, 4, 64]), ALU.mult,
                    [r_ktm, r_sv], [r_kw])
            yield
            for h in range(4):
                self.mm(self.bank(st_b, 64, 256 + h * 64), ST[:, h, :], Va[:, ti, h, 0:64], True, True, [r_ST, r_Va], [r2])
            for h in range(4):
                self.mm(self.bank(ms_b, 65, 16 + h * 65)[0:64], kw[:, h, :], Va[:, ti, h, :], True, True, [r_kw, r_Va], [r3])
            yield
            self.tt(numh, tmpi[:, :, 0:64], num_ps.rearrange("p (h e) -> p h e", h=4), ALU.add, [r_tmpi, r2], [r_numh])
            self.tt(sv[:, DEN, :], tmpi[:, :, 64], sv[:, DENI, :], ALU.add, [r_tmpi, r_sv], [r_sv])
            self.ts(sv[:, DAB, :], sv[:, DEN, :], -1.0, ALU.mult, [r_sv], [r_sv])
            self.tt(sv[:, DAB, :], sv[:, DAB, :], sv[:, DEN, :], ALU.max, [r_sv], [r_sv])
            self.tt(sv[:, DAB, :], sv[:, DAB, :], sv[:, EMT, :], ALU.max, [r_sv], [r_sv])
            self.recip(sv[:, RR, :], sv[:, DAB, :], [r_sv], [r_sv])
            self.tt(numh, numh, sv[:, RR, :].unsqueeze(2).to_broadcast([128, 4, 64]), ALU.mult, [r_numh, r_sv], [r_numh])
            hsv = hs[:, ti, :].rearrange("p (h e) -> p h e", h=4)
            self.tt(hsv, hsv, numh, ALU.add, [r_hs, r_numh], [r_hs])
            self.tt(Cst[0:64], Cst[0:64], sv[0:64, DEC, :].unsqueeze(2).to_broadcast([64, 4, 65]), ALU.mult, [r_Cst, r_sv], [r_Cst])
            self.tt(Cst[0:64], Cst[0:64], upd_ps[0:64].rearrange("p (h e) -> p h e", h=4), ALU.add, [r_Cst, r3], [r_Cst])
            self.cp(Cbf[0:64], Cst[0:64], [r_Cst], [r_Cbf])
            yield

        orders = [list(range(TT)), list(range(CT - 1, -1, -1)) + list(range(TT - 1, CT - 1, -1))]
        for i in range(TT):
            gens = [chunk(0, orders[0][i]), chunk(1, orders[1][i])]
            alive = True
            while alive:
                alive = False
                for g in gens:
                    try:
                        next(g)
                        alive = True
                    except StopIteration:
                        pass
        ytm, r_ytm = self.tile([128, 256], BF16, "ytmB")
        ybuf = self.tile([128, 2, 128], BF16, "yTB")
        for ti in range(CT if last else 0, TT):
            tsl = slice(ti * 128, (ti + 1) * 128)
            for k in range(KC):
                self.mm(self.bank(4, 256), self.hT[:, k, tsl], wB[:, k, 768:1024], k == 0, k == KC - 1, [self.r_hT, r_wB], [self.r_ps[4]])
            self.act(og, self.bank(4, 256), AF.Sigmoid, [self.r_ps[4]], [r_og])
            self.tt(ytm, og, hs[:, ti, :], ALU.mult, [r_og, r_hs], [r_ytm])
            self.store_y_tm(ytm, r_ytm, s, 1, ti, ybuf)

    def post_norm_res(self, s, lo, hi, yT, r_yT, gp_idx, tl):
        sq, r_sq, rs, r_rs, tmp, r_tmp, xb, r_xb = tl
        n = hi - lo
        self.rstd_block(yT, r_yT, n, KC, D, sq, r_sq, rs, r_rs, 7)
        self.ld(xb[:, :, 0:n], self.XRES[s][:, lo:hi].rearrange("(k p) n -> p k n", p=128), [self.r_xres[s]], [r_xb])
        dv, r_dv = self.seg_dv(lo)
        for k in range(KC):
            self.tt(tmp[:, 0:n], yT[:, k, 0:n], rs[:, 0:n], ALU.mult, [r_yT, r_rs], [r_tmp])
            self.stt(xb[:, k, 0:n], tmp[:, 0:n], dv[:, gp_idx, k:k + 1], xb[:, k, 0:n], ALU.mult, ALU.add, [r_tmp, r_dv, r_xb], [r_xb])
        self.ld(self.XRES[s][:, lo:hi].rearrange("(k p) n -> p k n", p=128), xb[:, :, 0:n], [r_xb], [self.r_xres[s]])

    def pn_tiles(self):
        sq, r_sq = self.tile([128, KC, 512], BF16, "sqP")
        rs, r_rs = self.tile([128, 512], F32, "rsP")
        tmp, r_tmp = self.tile([128, 512], F32, "tmpP")
        xb, r_xb = self.tile([128, KC, 512], F32, "xbP")
        return (sq, r_sq, rs, r_rs, tmp, r_tmp, xb, r_xb)

    def merge(self, s, l, last):
        self.phase()
        ysb, r_ysb = self.tile([128, 8, 512], BF16, "ysb")
        wg = [self.tile([128, 4, KC, 128], BF16, "wg%d" % i) for i in range(2)]
        wbr = [self.tile([128, 4, 2, 128], BF16, "wbr%d" % i) for i in range(2)]
        sig, r_sig = self.tile([128, 512], F32, "sig")
        accf, r_accf = self.tile([128, 512], F32, "accf")
        tmpm, r_tmpm = self.tile([128, 512], F32, "tmpm")
        acc, r_acc = self.tile([128, KC, 512], BF16, "acc")
        wo, r_wo = self.tile([128, KC, D], BF16, "wo")
        yT, r_yT = self.tile([128, KC, 512], F32, "yTm")
        tl = self.pn_tiles()
        self.ld(wo, self.WO[l].rearrange("p (k n) -> p k n", k=KC), [self.r_wbf], [r_wo])
        it = 0
        for (lo, hi) in self.blocks:
            if last and lo < self.NCTX:
                continue
            n = hi - lo
            self.ld(ysb[:, :, 0:n], self.YS[s][:, lo:hi].rearrange("(k p) n -> p k n", p=128), [self.r_ys[s]], [r_ysb])
            for dc in range(KC):
                (wg_, r_wg), (wb_, r_wb) = wg[it % 2], wbr[it % 2]
                it += 1
                for br in range(4):
                    self.ld(wg_[:, br], self.WG[l, br, dc].rearrange("p (k n) -> p k n", k=KC), [self.r_wbf], [r_wg])
                    self.ld(wb_[:, br], self.WBR[l, br].rearrange("p (k n) -> p k n", k=2)[:, :, dc * 128:(dc + 1) * 128], [self.r_wbf], [r_wb])
                for br in range(4):
                    bg, bp = br % 2, 2 + br % 2
                    for k in range(KC):
                        self.mm(self.bank(bg, n), wg_[:, br, k, :], self.hT[:, k, lo:hi], k == 0, k == KC - 1, [r_wg, self.r_hT], [self.r_ps[bg]])
                    self.act(sig[:, 0:n], self.bank(bg, n), AF.Sigmoid, [self.r_ps[bg], self.r_pv], [r_sig], bias=self.pv["b_gate"][:, l, br, dc:dc + 1], scale=1.0)
                    for k in range(2):
                        self.mm(self.bank(bp, n), wb_[:, br, k, :], ysb[:, br * 2 + k, 0:n], k == 0, k == 1, [r_wb, r_ysb], [self.r_ps[bp]])
                    if br == 0:
                        self.tt(accf[:, 0:n], sig[:, 0:n], self.bank(bp, n), ALU.mult, [r_sig, self.r_ps[bp]], [r_accf])
                    else:
                        self.tt(tmpm[:, 0:n], sig[:, 0:n], self.bank(bp, n), ALU.mult, [r_sig, self.r_ps[bp]], [r_tmpm])
                        if br < 3:
                            self.tt(accf[:, 0:n], accf[:, 0:n], tmpm[:, 0:n], ALU.add, [r_accf, r_tmpm], [r_accf])
                        else:
                            self.tt(acc[:, dc, 0:n], accf[:, 0:n], tmpm[:, 0:n], ALU.add, [r_accf, r_tmpm], [r_acc])
            for d2 in range(KC):
                b = 4 + d2 % 2
                for k in range(KC):
                    self.mm(self.bank(b, n), wo[:, k, d2 * 128:(d2 + 1) * 128], acc[:, k, 0:n], k == 0, k == KC - 1, [r_wo, r_acc], [self.r_ps[b]])
                self.cp(yT[:, d2, 0:n], self.bank(b, n), [self.r_ps[b]], [r_yT], eng="act" if d2 % 2 else "dve")
            self.post_norm_res(s, lo, hi, yT, r_yT, 2, tl)

    def ffn(self, s, l, last):
        self.phase()
        T = self.T
        wd, r_wd = self.tile([128, FC, D], BF16, "wd")
        wu = [self.tile([128, KC, 256], BF16, "wu%d" % i) for i in range(2)]
        gT, r_gT = self.tile([128, FC, 512], BF16, "gT")
        a_sb, r_a = self.tile([128, 514], F32, "a_sb")
        c1, r_c1 = self.tile([128, 512], F32, "c1F")
        yT, r_yT = self.tile([128, KC, 512], F32, "yTf")
        tl = self.pn_tiles()
        self.ld(wd, self.WD[l].rearrange("p (f n) -> p f n", f=FC), [self.r_wbf], [r_wd])
        wcv, bcv = self.pv["w_ffn_conv"], self.pv["b_ffn_conv"]
        it = 0
        for (lo, hi) in self.blocks:
            if last and lo < self.NCTX:
                continue
            n = hi - lo
            seg_lo, seg_hi = (0, self.NCTX) if lo < self.NCTX else (self.NCTX, T)
            for fc in range(FC):
                w_, r_w = wu[it % 2]
                it += 1
                self.ld(w_, self.WU[l, fc].rearrange("p (k n) -> p k n", k=KC), [self.r_wbf], [r_w])
                ba, bv = fc % 2, 2 + fc % 2
                for k in range(KC):
                    self.mm(self.bank(ba, n), w_[:, k, 0:128], self.hT[:, k, lo:hi], k == 0, k == KC - 1, [r_w, self.r_hT], [self.r_ps[ba]])
                for k in range(KC):
                    self.mm(self.bank(bv, n), w_[:, k, 128:256], self.hT[:, k, lo:hi], k == 0, k == KC - 1, [r_w, self.r_hT], [self.r_ps[bv]])
                self.P.op("dve", lambda e: e.memset(a_sb[:, 0:1], 0.0), [], [r_a])
                self.P.op("dve", lambda e: e.memset(a_sb[:, n + 1:n + 2], 0.0), [], [r_a])
                if lo > seg_lo:
                    for k in range(KC):
                        self.mm(self.bank(6, 1), w_[:, k, 0:128], self.hT[:, k, lo - 1:lo], k == 0, k == KC - 1, [r_w, self.r_hT], [self.r_ps[6]])
                    self.cp(a_sb[:, 0:1], self.bank(6, 1), [self.r_ps[6]], [r_a])
                if hi < seg_hi:
                    for k in range(KC):
                        self.mm(self.bank(6, 1, 8), w_[:, k, 0:128], self.hT[:, k, hi:hi + 1], k == 0, k == KC - 1, [r_w, self.r_hT], [self.r_ps[6]])
                    self.cp(a_sb[:, n + 1:n + 2], self.bank(6, 1, 8), [self.r_ps[6]], [r_a])
                self.cp(a_sb[:, 1:n + 1], self.bank(ba, n), [self.r_ps[ba]], [r_a], eng="act")
                self.act(self.bank(ba, n), self.bank(ba, n), AF.Copy, [self.r_ps[ba], self.r_pv], [self.r_ps[ba]], scale=wcv[:, l, 1, fc:fc + 1])
                self.stt(self.bank(ba, n), a_sb[:, 0:n], wcv[:, l, 0, fc:fc + 1], self.bank(ba, n), ALU.mult, ALU.add, [r_a, self.r_pv, self.r_ps[ba]], [self.r_ps[ba]])
                self.stt(c1[:, 0:n], a_sb[:, 2:n + 2], wcv[:, l, 2, fc:fc + 1], self.bank(ba, n), ALU.mult, ALU.add, [r_a, self.r_pv, self.r_ps[ba]], [r_c1])
                self.act(c1[:, 0:n], c1[:, 0:n], AF.Silu, [r_c1, self.r_pv], [r_c1], bias=bcv[:, l, fc:fc + 1], scale=1.0)
                self.tt(gT[:, fc, 0:n], c1[:, 0:n], self.bank(bv, n), ALU.mult, [r_c1, self.r_ps[bv]], [r_gT])
            for d2 in range(KC):
                b = 4 + d2 % 2
                for fc in range(FC):
                    self.mm(self.bank(b, n), wd[:, fc, d2 * 128:(d2 + 1) * 128], gT[:, fc, 0:n], fc == 0, fc == FC - 1, [r_wd, r_gT], [self.r_ps[b]])
                self.cp(yT[:, d2, 0:n], self.bank(b, n), [self.r_ps[b]], [r_yT], eng="act" if d2 % 2 else "dve")
            self.post_norm_res(s, lo, hi, yT, r_yT, 5, tl)


NLAT_FULL, NCTX_FULL, DEPTH = 2048, 256, 2
_cache = {}


def run_device(inp, NLAT, NCTX, B, L, n_cores):
    NSEQ = B // n_cores
    consts = make_consts(NLAT, NCTX)
    key = (NLAT, NCTX, NSEQ, L)
    bld = Builder(NLAT, NCTX, NSEQ, L, consts)
    nc = bld.build()
    x = np.asarray(inp["x"], np.float32)
    ctx = np.asarray(inp["ctx"], np.float32)
    c = np.asarray(inp["c"], np.float32)
    c_ctx = np.asarray(inp["c_ctx"], np.float32)
    params = layout_params(inp, L)
    wml = np.asarray(inp["w_ml_conv"], np.float32)
    params["wconvB"] = np.ascontiguousarray(wml.reshape(L, 3, 8, 64).transpose(0, 3, 2, 1))
    shared = {k: np.ascontiguousarray(np.asarray(inp[k], np.float32)) for k in W_SHAPES}
    shared.update(params)
    shared.update(consts)
    in_maps = []
    for ci in range(n_cores):
        sl = slice(ci * NSEQ, (ci + 1) * NSEQ)
        cc = np.concatenate([c[sl], c_ctx[None]], 0)
        m = dict(shared)
        m["xT"] = np.ascontiguousarray(x[sl].transpose(0, 2, 1))
        m["ctxT"] = np.ascontiguousarray(ctx[sl].transpose(0, 2, 1))
        m["ccT"] = np.ascontiguousarray(cc.T.reshape(KC, 128, NSEQ + 1).transpose(1, 0, 2))
        in_maps.append(m)
    return nc, in_maps


def kernel(**inp):
    n_cores = 8
    nc, in_maps = run_device(inp, NLAT_FULL, NCTX_FULL, 16, DEPTH, n_cores)
    res = run_bass_kernel_spmd(nc, in_maps, core_ids=list(range(n_cores)))
    outs = [np.asarray(r["outT"]).transpose(0, 2, 1) for r in res.results]
    return np.ascontiguousarray(np.concatenate(outs, 0).astype(np.float32))
```

```python
from contextlib import ExitStack
import numpy as np
import ml_dtypes
import concourse.bass as bass
import concourse.mybir as mybir
from concourse.bass_utils import run_bass_kernel_spmd

F32 = mybir.dt.float32
BF16 = mybir.dt.bfloat16
AF = mybir.ActivationFunctionType
ALU = mybir.AluOpType
AX = mybir.AxisListType
ENG = ("pe", "act", "dve", "pool", "sp")
NBIG = -30000.0
D = 1024
KC = 8
DFF = 2816
FC = 22
EPS = 1e-6


class Res:
    __slots__ = ("name", "w", "re", "rd")

    def __init__(self, name=""):
        self.name = name
        self.w = None
        self.re = {}
        self.rd = []


class Prog:
    N_DMA_SEMS = 80

    def __init__(self, nc, stack):
        self.nc = nc
        self.esem = {e: stack.enter_context(nc.semaphore("es_" + e)) for e in ENG}
        self.dsem = [stack.enter_context(nc.semaphore("ds%d" % i)) for i in range(self.N_DMA_SEMS)]
        self.dval = [0] * self.N_DMA_SEMS
        self.dnext = 0
        self.cnt = {e: 0 for e in ENG}
        self.seen = {e: {} for e in ENG}
        self.q = {e: [] for e in ENG}
        self.n_ops = 0

    def _deps(self, reads, writes):
        deps = []
        for r in reads:
            if r.w is not None:
                deps.append(r.w)
        for w in writes:
            if w.w is not None:
                deps.append(w.w)
            for e, c in w.re.items():
                deps.append(("E", e, c))
            deps.extend(w.rd)
        return deps

    def _waits(self, eng, deps):
        seen = self.seen[eng]
        need = {}
        for kind, key, val in deps:
            k = (kind, key)
            if seen.get(k, 0) >= val:
                continue
            if kind == "E" and key == "pe" and eng == "pe":
                continue
            if need.get(k, 0) < val:
                need[k] = val
        out = []
        for k, val in need.items():
            seen[k] = val
            sem = self.esem[k[1]] if k[0] == "E" else self.dsem[k[1]]
            out.append((sem, val))
        return out

    def op(self, eng, fn, reads=(), writes=()):
        waits = self._waits(eng, self._deps(reads, writes))
        self.cnt[eng] += 1
        c = self.cnt[eng]
        tok = ("E", eng, c)
        self.q[eng].append((waits, fn, (self.esem[eng], 1)))
        for r in reads:
            if r.re.get(eng, 0) < c:
                r.re[eng] = c
        for w in writes:
            w.w = tok
            w.re = {}
            w.rd = []
        self.n_ops += 1
        return tok

    def dma(self, qeng, out, in_, reads=(), writes=(), **kw):
        deps = self._deps(reads, writes)
        i = self.dnext
        self.dnext = (self.dnext + 1) % self.N_DMA_SEMS
        prev = self.dval[i]
        if prev:
            deps.append(("D", i, prev))
        waits = self._waits(qeng, deps)
        self.dval[i] = prev + 16
        tok = ("D", i, prev + 16)
        self.q[qeng].append((waits, (lambda e, o=out, s=in_, k=kw: e.dma_start(out=o, in_=s, **k)),
                             (self.dsem[i], 16)))
        for r in reads:
            r.rd.append(tok)
        for w in writes:
            w.w = tok
            w.re = {}
            w.rd = []
        self.n_ops += 1
        return tok

    def barrier(self):
        deps = [("E", e, self.cnt[e]) for e in ENG if self.cnt[e]]
        deps += [("D", i, v) for i, v in enumerate(self.dval) if v]
        for e in ENG:
            waits = self._waits(e, deps)
            if waits:
                self.q[e].append((waits, None, None))

    def emit(self):
        nc = self.nc
        allsems = [self.esem[e] for e in ENG] + self.dsem
        with nc.Block("init") as b0:
            @b0.vector
            def _(v):
                for s in allsems:
                    v.sem_clear(s)
        with nc.Block("main") as blk:
            def run(e, name):
                for waits, fn, inc in self.q[name]:
                    for sem, val in waits:
                        e.wait_ge(sem, val)
                    if fn is not None:
                        fn(e).then_inc(inc[0], inc[1])

            @blk.tensor
            def _(e):
                run(e, "pe")

            @blk.scalar
            def _(e):
                run(e, "act")

            @blk.vector
            def _(e):
                run(e, "dve")

            @blk.gpsimd
            def _(e):
                run(e, "pool")

            @blk.sync
            def _(e):
                run(e, "sp")


def _rope_tab(rd, pos_row, pos_col, n_ctx):
    half = rd // 2
    nf = half // 2
    inv = 10000.0 ** (-np.arange(nf, dtype=np.float64) / nf)
    n = len(pos_row)
    cos = np.ones((rd, n_ctx + n), np.float64)
    sin = np.zeros((rd, n_ctx + n), np.float64)
    for r in range(rd):
        hh, rr = r // half, r % half
        idx = rr % nf
        pos = pos_row if hh == 0 else pos_col
        ang = pos.astype(np.float64) * inv[idx]
        ang = (pos.astype(np.float32) * np.float32(inv[idx]).astype(np.float32)).astype(np.float64)
        cos[r, n_ctx:] = np.cos(ang)
        sin[r, n_ctx:] = np.sin(ang)
    return cos, sin


def make_consts(NLAT, NCTX):
    bf = ml_dtypes.bfloat16
    T = NLAT + NCTX
    c = {}
    c["ident_bf"] = np.eye(128).astype(bf)
    c["ones_bf"] = np.ones((128, 128)).astype(bf)
    c["ident_f"] = np.eye(128, dtype=np.float32)
    c["ones_f"] = np.ones((128, 128), np.float32)
    s = np.arange(128)[:, None]
    t = np.arange(128)[None, :]
    c["tri_f"] = (s <= t).astype(np.float32)
    c["tri_b"] = (s >= t).astype(np.float32)
    mf = np.where(t <= s, 0.0, NBIG).astype(np.float32)
    mb = np.where(t >= s, 0.0, NBIG).astype(np.float32)
    c["mneg_f"] = np.repeat(mf[:, None, :], 4, axis=1).copy()
    c["mneg_b"] = np.repeat(mb[:, None, :], 4, axis=1).copy()
    el = np.zeros((128, 128), np.float32); el[127, :] = 1
    ef = np.zeros((128, 128), np.float32); ef[0, :] = 1
    c["e_last"] = el
    c["e_first"] = ef
    c["maskA"] = np.where(t >= s, 0.0, NBIG).astype(bf)
    c["maskB"] = np.where(t <= s, 0.0, NBIG).astype(bf)
    c["maskN"] = np.full((128, 128), NBIG).astype(bf)
    rows = NLAT // 64
    pr = np.repeat(np.arange(rows), 64)
    pc = np.tile(np.arange(64), rows)
    ca, sa = _rope_tab(32, pr, pc, NCTX)
    sc_a = 96.0 ** -0.5
    cosq = np.ones((96, T)); sinq = np.zeros((96, T))
    cosq[64:] = ca; sinq[64:] = sa
    c["cosq_a"] = (cosq * sc_a).astype(np.float32)
    c["sinq_a"] = (sinq * sc_a).astype(np.float32)
    c["cosk_a"] = ca.astype(np.float32)
    c["sink_a"] = sa.astype(np.float32)
    cc, sc = _rope_tab(64, pr, pc, NCTX)
    c["cos_c"] = cc.astype(np.float32)
    c["sin_c"] = sc.astype(np.float32)
    j = np.arange(64)
    Cc = np.cos(2 * np.pi * np.outer(j, j) / 64)
    Sc = np.sin(2 * np.pi * np.outer(j, j) / 64)
    z = np.zeros((64, 64))
    c["bdc"] = np.block([[Cc, z], [z, Cc]]).astype(bf)
    c["bds"] = (-np.block([[Sc, z], [z, Sc]])).astype(bf)
    for nm, N in (("lat", NLAT), ("ctx", NCTX)):
        n = np.arange(N)
        ph = (np.outer(n, n) % N).astype(np.float64) * (2 * np.pi / N)
        nrm = 1.0 / np.sqrt(N * 64.0)
        c["cn_" + nm] = (np.cos(ph) * nrm).astype(bf)
        c["sn_" + nm] = (np.sin(ph) * nrm).astype(bf)
    return c


CONST_DT = {"ident_bf": BF16, "ones_bf": BF16, "maskA": BF16, "maskB": BF16, "maskN": BF16, "bdc": BF16, "bds": BF16,
            "cn_lat": BF16, "sn_lat": BF16, "cn_ctx": BF16, "sn_ctx": BF16}

W_SHAPES = {
    "w_mod": (D, 6 * D), "w_in": (D, 2352), "w_uq": (256, 384), "w_ukv": (256, 512),
    "w_gate": (4, D, D), "w_branch": (4, 256, D), "w_out": (D, D), "w_up": (D, 2 * DFF), "w_down": (DFF, D),
}


def layout_params(inp, L):
    o = {}

    def fm(v, k):
        v = np.asarray(v, np.float32)
        lead = v.shape[:-1]
        return np.ascontiguousarray(np.moveaxis(v.reshape(lead + (k, 128)), -1, 0))

    o["b_mod"] = np.stack([fm(inp["b_mod"][l], 48) for l in range(L)])
    for nm in ("g_pre_mix", "g_post_mix", "g_pre_ffn", "g_post_ffn"):
        o[nm] = np.stack([fm(inp[nm][l], 8) for l in range(L)])
    o["g_qa"] = np.stack([fm(inp["g_qa"][l], 2) for l in range(L)])
    o["g_kva"] = np.stack([fm(inp["g_kva"][l], 2) for l in range(L)])
    o["b_gate"] = np.stack([fm(inp["b_gate"][l], 8) for l in range(L)])
    o["w_ffn_conv"] = np.stack([fm(inp["w_ffn_conv"][l], FC) for l in range(L)])
    o["b_ffn_conv"] = np.stack([fm(inp["b_ffn_conv"][l], FC) for l in range(L)])
    o["w_ml_conv"] = np.stack([fm(inp["w_ml_conv"][l], 4) for l in range(L)])
    o["b_ml_gates"] = np.asarray(inp["b_ml_gates"], np.float32).reshape(L, 1, 16)
    o["wg_sink"] = np.asarray(inp["wg_sink"], np.float32).reshape(L, 1, 4)
    return o


PARAM_SHAPES = lambda L: {
    "b_mod": (L, 128, 48), "g_pre_mix": (L, 128, 8), "g_post_mix": (L, 128, 8), "g_pre_ffn": (L, 128, 8),
    "g_post_ffn": (L, 128, 8), "g_qa": (L, 128, 2), "g_kva": (L, 128, 2), "b_gate": (L, 128, 4, 8),
    "w_ffn_conv": (L, 128, 3, FC), "b_ffn_conv": (L, 128, FC), "w_ml_conv": (L, 128, 3, 4),
    "b_ml_gates": (L, 1, 16), "wg_sink": (L, 1, 4), "wconvB": (L, 64, 8, 3),
}


class Builder:
    def __init__(self, NLAT, NCTX, NSEQ, L, consts, dbg=None):
        self.NLAT, self.NCTX, self.NSEQ, self.L = NLAT, NCTX, NSEQ, L
        self.T = T = NLAT + NCTX
        self.TT = T // 128
        self.CT = NCTX // 128
        self.blocks = [(0, NCTX)] + [(NCTX + i, min(NCTX + i + 512, T)) for i in range(0, NLAT, 512)]
        self.dbg = dbg
        nc = self.nc = bass.Bass("TRN2", target_bir_lowering=False)
        self.st = ExitStack()
        self.P = Prog(nc, self.st)
        di = lambda n, s, dt=F32: nc.dram_tensor(n, list(s), dt, kind="ExternalInput").ap()
        self.xT_in = di("xT", (NSEQ, D, NLAT))
        self.cT_in = di("ctxT", (NSEQ, D, NCTX))
        self.ccT = di("ccT", (128, KC, NSEQ + 1))
        self.W = {k: di(k, (L,) + v) for k, v in W_SHAPES.items()}
        self.PR = {k: di(k, v) for k, v in PARAM_SHAPES(L).items()}
        self.CD = {k: di(k, v.shape, CONST_DT.get(k, F32)) for k, v in consts.items()}
        self.out = nc.dram_tensor("outT", [NSEQ, D, NLAT], F32, kind="ExternalOutput").ap()
        ds = lambda n, s, dt: nc.dram_tensor(n, list(s), dt, kind="Internal").ap()
        self.XRES = ds("xres", (NSEQ, D, T), F32)
        self.YS = ds("ys", (NSEQ, D, T), BF16)
        self.WI = ds("wi_bf", (L, 128, KC * 2352), BF16)
        self.WG = ds("wg_bf", (L, 4, KC, 128, KC * 128), BF16)
        self.WBR = ds("wbr_bf", (L, 4, 128, 2 * D), BF16)
        self.WO = ds("wo_bf", (L, 128, KC * D), BF16)
        self.WU = ds("wu_bf", (L, FC, 128, KC * 256), BF16)
        self.WD = ds("wd_bf", (L, 128, FC * D), BF16)
        self.r_wbf = Res("wbf")
        self.r_xres = [Res("xres%d" % i) for i in range(NSEQ)]
        self.r_ys = [Res("ys%d" % i) for i in range(NSEQ)]
        self.r_out = Res("out")
        if dbg:
            self.dbg_out = {k: nc.dram_tensor("dbg_" + k, list(s), dt, kind="ExternalOutput").ap() for k, (s, dt) in dbg.items()}
        self.PS = nc.alloc_psum_tensor("PS", [128, 4096], F32)
        self.r_ps = [Res("ps%d" % i) for i in range(8)]
        self.ARENA_E = 98000
        self.arena = nc.alloc_sbuf_tensor("arena", [128, self.ARENA_E], BF16)
        self.a_off = 0
        self.a_base = 0
        self.phase_log = []

    def tile(self, shape, dt, name=""):
        n = int(np.prod(shape[1:]))
        ne = n * (2 if dt == F32 else 1)
        off = (self.a_off + 15) // 16 * 16
        assert off + ne <= self.ARENA_E, ("arena overflow", name, off, ne)
        self.a_off = off + ne
        ap = self.arena[0:shape[0], off:off + ne]
        if dt == F32:
            ap = ap.bitcast(F32)
        if len(shape) == 3:
            ap = ap.rearrange("p (a b) -> p a b", a=shape[1])
        elif len(shape) == 4:
            ap = ap.rearrange("p (a b c) -> p a b c", a=shape[1], b=shape[2])
        return ap, Res(name)

    def phase(self):
        self.P.barrier()
        self.a_off = self.a_base
        import sys
        self.phase_log.append((sys._getframe(1).f_code.co_name, dict(self.P.cnt)))

    def bank(self, b, n=512, lo=0):
        return self.PS[:, b * 512 + lo:b * 512 + lo + n]

    def bank_bf(self, b):
        return self.PS[:, b * 512:(b + 1) * 512].bitcast(BF16)

    def mm(self, out, lhsT, rhs, start, stop, reads, writes):
        self.P.op("pe", lambda e: e.matmul(out, lhsT, rhs, start=start, stop=stop, skip_group_check=True), reads, writes)

    def tr(self, out, in_, ident, reads, writes):
        self.P.op("pe", lambda e: e.transpose(out, in_, ident), reads, writes)

    def act(self, out, in_, func, reads, writes, bias=None, scale=None):
        kw = {}
        if bias is not None:
            kw["bias"] = bias
        if scale is not None:
            kw["scale"] = scale
        self.P.op("act", lambda e: e.activation(out=out, in_=in_, func=func, **kw), reads, writes)

    def tt(self, out, a, b, op, reads, writes, eng="dve"):
        self.P.op(eng, lambda e: e.tensor_tensor(out=out, in0=a, in1=b, op=op), reads, writes)

    def ts(self, out, a, s1, op0, reads, writes, s2=None, op1=None, eng="dve"):
        if op1 is None:
            self.P.op(eng, lambda e: e.tensor_scalar(out=out, in0=a, scalar1=s1, scalar2=None, op0=op0), reads, writes)
        else:
            self.P.op(eng, lambda e: e.tensor_scalar(out=out, in0=a, scalar1=s1, scalar2=s2, op0=op0, op1=op1), reads, writes)

    def stt(self, out, a, s, b, op0, op1, reads, writes):
        self.P.op("dve", lambda e: e.scalar_tensor_tensor(out=out, in0=a, scalar=s, in1=b, op0=op0, op1=op1), reads, writes)

    def cp(self, out, in_, reads, writes, eng="dve"):
        if eng == "act":
            self.P.op("act", lambda e: e.copy(out=out, in_=in_), reads, writes)
        else:
            self.P.op(eng, lambda e: e.tensor_copy(out=out, in_=in_), reads, writes)

    def red(self, out, in_, op, reads, writes):
        self.P.op("dve", lambda e: e.tensor_reduce(out=out, in_=in_, axis=AX.X, op=op), reads, writes)

    def recip(self, out, in_, reads, writes):
        self.P.op("dve", lambda e: e.reciprocal(out=out, in_=in_), reads, writes)

    def ld(self, out, in_, reads, writes, q="sp"):
        self.P.dma(q, out, in_, reads, writes)

    def prepass(self):
        self.phase()
        SZ = 2560
        stf = [self.tile([128, SZ], F32, "stf%d" % i) for i in range(3)]
        stb = [self.tile([128, SZ], BF16, "stb%d" % i) for i in range(3)]
        pieces = []

        def piece(srcs, dst, a, b):
            pieces.append((srcs, dst, a, b))

        def views(j):
            srcs, dst, a, b = pieces[j]
            (f, r_f), (bt, r_b) = stf[j % 3], stb[j % 3]
            fv = f[:, 0:a * b].rearrange("p (a b) -> p a b", a=a)
            bv = bt[:, 0:a * b].rearrange("p (a b) -> p a b", a=a)
            return srcs, dst, fv, bv, r_f, r_b

        def emit_in(j):
            srcs, dst, fv, bv, r_f, r_b = views(j)
            for (c0, c1, src) in srcs:
                self.ld(fv[:, :, c0:c1], src, [], [r_f])

        def emit_rest(j):
            srcs, dst, fv, bv, r_f, r_b = views(j)
            self.cp(bv, fv, [r_f], [r_b], eng=("act", "dve", "pool")[j % 3])
            self.ld(dst, bv, [r_b], [self.r_wbf])

        for l in range(self.L):
            wi = self.W["w_in"][l].rearrange("(k p) n -> p k n", p=128)
            wid = self.WI[l].rearrange("p (k n) -> p k n", k=KC)
            for k in range(KC):
                piece([(0, 2352, wi[:, k:k + 1, :])], wid[:, k:k + 1, :], 1, 2352)
            for br in range(4):
                for dc in range(KC):
                    src = self.W["w_gate"][l][br][:, dc * 128:(dc + 1) * 128].rearrange("(k p) n -> p k n", p=128)
                    piece([(0, 128, src)], self.WG[l, br, dc].rearrange("p (k n) -> p k n", k=KC), KC, 128)
                src = self.W["w_branch"][l][br].rearrange("(k p) n -> p k n", p=128)
                piece([(0, D, src)], self.WBR[l, br].rearrange("p (k n) -> p k n", k=2), 2, D)
            wo = self.W["w_out"][l].rearrange("(k p) n -> p k n", p=128)
            wod = self.WO[l].rearrange("p (k n) -> p k n", k=KC)
            for k in range(0, KC, 2):
                piece([(0, D, wo[:, k:k + 2, :])], wod[:, k:k + 2, :], 2, D)
            for fc in range(FC):
                sa = self.W["w_up"][l][:, fc * 128:(fc + 1) * 128].rearrange("(k p) n -> p k n", p=128)
                sv = self.W["w_up"][l][:, DFF + fc * 128:DFF + (fc + 1) * 128].rearrange("(k p) n -> p k n", p=128)
                piece([(0, 128, sa), (128, 256, sv)], self.WU[l, fc].rearrange("p (k n) -> p k n", k=KC), KC, 256)
            wdn = self.W["w_down"][l].rearrange("(f p) n -> p f n", p=128)
            wdd = self.WD[l].rearrange("p (f n) -> p f n", f=FC)
            for f0 in range(0, FC, 2):
                piece([(0, D, wdn[:, f0:f0 + 2, :])], wdd[:, f0:f0 + 2, :], 2, D)
        npc = len(pieces)
        for j in range(npc + 2):
            if j < npc:
                emit_in(j)
            if j - 2 >= 0:
                emit_rest(j - 2)

    def build(self):
        P = self.P
        NSEQ, L, T = self.NSEQ, self.L, self.T
        C = {}
        self.C = C
        r_c = self.r_c = Res("consts")
        for k in ("ident_bf", "ones_bf", "ident_f", "ones_f", "tri_f", "tri_b", "e_last", "e_first", "maskA", "maskB", "maskN", "bdc", "bds"):
            C[k], _ = self.tile([128, 128], CONST_DT.get(k, F32), k)
            self.ld(C[k], self.CD[k], [], [r_c])
        for k in ("mneg_f", "mneg_b"):
            C[k], _ = self.tile([128, 4, 128], F32, k)
            self.ld(C[k], self.CD[k], [], [r_c])
        self.hT, self.r_hT = self.tile([128, KC, T], BF16, "hT")
        self.MOD, self.r_mod = self.tile([128, L, 48, NSEQ + 1], F32, "MOD")
        self.pv = {}
        self.r_pv = Res("pvec")
        for k, shp in PARAM_SHAPES(L).items():
            if shp[1] == 128:
                self.pv[k], _ = self.tile([128, L] + list(shp[2:]) if len(shp) > 2 else [128, L], F32, k)
                src = self.PR[k]
                if len(shp) == 3:
                    self.ld(self.pv[k], src.rearrange("l p a -> p l a"), [], [self.r_pv])
                else:
                    for l in range(L):
                        self.ld(self.pv[k][:, l], src[l], [], [self.r_pv])
        self.bg_b, _ = self.tile([128, L, 16], F32, "bgb")
        self.sink_b, _ = self.tile([128, L, 4], F32, "sinkb")
        for l in range(L):
            self.ld(self.bg_b[:, l, :], self.PR["b_ml_gates"][l].partition_broadcast(128), [], [self.r_pv])
            self.ld(self.sink_b[:, l, :], self.PR["wg_sink"][l].partition_broadcast(128), [], [self.r_pv])
        self.wconvB, _ = self.tile([128, 8, 3], F32, "wconvB")
        self.r_wcb = Res("wcb")
        self.dv, self.r_dv = self.tile([128, 6, KC], F32, "derived")
        self.dvc, self.r_dvc = self.tile([128, 6, KC], F32, "derivedc")
        self.a_base = self.a_off
        for s in range(NSEQ):
            self.ld(self.XRES[s][:, 0:self.NCTX], self.cT_in[s], [], [self.r_xres[s]])
            self.ld(self.XRES[s][:, self.NCTX:T], self.xT_in[s], [], [self.r_xres[s]])
        self.prepass()
        self.mod_phase()
        import os
        stop = int(os.environ.get("KSTOP", "99"))
        for s in range(NSEQ):
            for l in range(L):
                last = (l == L - 1)
                steps = [lambda: self.derive(l, s), lambda: self.norm_mod(s, 0), lambda: self.branch_a(s, l, last),
                         lambda: self.branch_c(s, l, last), lambda: self.branch_d(s, l, last), lambda: self.branch_b(s, l, last),
                         lambda: self.merge(s, l, last), lambda: self.norm_mod(s, 3, skip_ctx=last), lambda: self.ffn(s, l, last)]
                for i, f in enumerate(steps):
                    if i < stop:
                        f()
            self.phase()
            self.ld(self.out[s], self.XRES[s][:, self.NCTX:T], [self.r_xres[s]], [self.r_out])
        P.barrier()
        P.emit()
        self.st.close()
        return self.nc

    def mod_phase(self):
        self.phase()
        NJ = self.NSEQ + 1
        cc, r_cc = self.tile([128, KC, NJ], F32, "cc")
        sg, r_sg = self.tile([128, KC, NJ], F32, "sg")
        self.ld(cc, self.ccT, [], [r_cc])
        self.act(sg, cc, AF.Sigmoid, [r_cc], [r_sg])
        self.tt(cc, cc, sg, ALU.mult, [r_sg, r_cc], [r_cc])
        wm = [self.tile([128, KC, 512], F32, "wm%d" % i) for i in range(2)]
        n = 0
        for l in range(self.L):
            for g in range(12):
                w, r_w = wm[n % 2]
                n += 1
                self.ld(w, self.W["w_mod"][l][:, g * 512:(g + 1) * 512].rearrange("(k p) n -> p k n", p=128), [], [r_w])
                b = n % 2
                for j4 in range(4):
                    for k in range(KC):
                        self.mm(self.bank(b, NJ, j4 * 8), w[:, k, j4 * 128:(j4 + 1) * 128], cc[:, k, :], k == 0 and j4 == 0, k == KC - 1,
                                [r_w, r_cc], [self.r_ps[b]])
                for j4 in range(4):
                    ch = g * 4 + j4
                    self.ts(self.MOD[:, l, ch, :], self.bank(b, NJ, j4 * 8), self.pv["b_mod"][:, l, ch:ch + 1], ALU.add,
                            [self.r_ps[b], self.r_pv], [self.r_mod])

    def derive(self, l, s):
        for (dst, r_dst, j) in ((self.dv, self.r_dv, s), (self.dvc, self.r_dvc, self.NSEQ)):
            for half, gpre, gpost in ((0, "g_pre_mix", "g_post_mix"), (1, "g_pre_ffn", "g_post_ffn")):
                sh = self.MOD[:, l, half * 24 + 0:half * 24 + 8, j]
                sc = self.MOD[:, l, half * 24 + 8:half * 24 + 16, j]
                g = self.MOD[:, l, half * 24 + 16:half * 24 + 24, j]
                self.stt(dst[:, half * 3 + 0, :], sc, 1.0, self.pv[gpre][:, l, :], ALU.add, ALU.mult, [self.r_mod, self.r_pv], [r_dst])
                self.cp(dst[:, half * 3 + 1, :], sh, [self.r_mod], [r_dst])
                self.tt(dst[:, half * 3 + 2, :], g, self.pv[gpost][:, l, :], ALU.mult, [self.r_mod, self.r_pv], [r_dst])

    def seg_dv(self, lo):
        return (self.dvc, self.r_dvc) if lo < self.NCTX else (self.dv, self.r_dv)

    def rstd_block(self, src, r_src, n, nk, dim, sq, r_sq, rs, r_rs, bank):
        for k in range(nk):
            self.act(sq[:, k, 0:n], src[:, k, 0:n], AF.Square, [r_src], [r_sq])
        for k in range(nk):
            self.mm(self.bank(bank, n), self.C["ones_bf"], sq[:, k, 0:n], k == 0, k == nk - 1, [r_sq, self.r_c], [self.r_ps[bank]])
        self.ts(rs[:, 0:n], self.bank(bank, n), 1.0 / dim, ALU.mult, [self.r_ps[bank]], [r_rs], s2=EPS, op1=ALU.add)
        self.act(rs[:, 0:n], rs[:, 0:n], AF.Ln, [r_rs], [r_rs])
        self.act(self.bank(bank, n), rs[:, 0:n], AF.Exp, [r_rs], [self.r_ps[bank]], scale=-0.5)
        return self.bank(bank, n), self.r_ps[bank]

    def norm_mod(self, s, base, skip_ctx=False):
        self.phase()
        xb = [self.tile([128, KC, 512], F32, "xb%d" % i) for i in range(2)]
        sqs = [self.tile([128, KC, 512], BF16, "sq%d" % i) for i in range(2)]
        rss = [self.tile([128, 512], F32, "rs%d" % i) for i in range(2)]
        tmps = [self.tile([128, 512], F32, "tmp%d" % i) for i in range(2)]
        for bi, (lo, hi) in enumerate(self.blocks):
            if skip_ctx and lo < self.NCTX:
                continue
            n = hi - lo
            x, r_x = xb[bi % 2]
            (sq, r_sq), (rs, r_rs) = sqs[bi % 2], rss[bi % 2]
            self.ld(x[:, :, 0:n], self.XRES[s][:, lo:hi].rearrange("(k p) n -> p k n", p=128), [self.r_xres[s]], [r_x])
            rsp, r_rsp = self.rstd_block(x, r_x, n, KC, D, sq, r_sq, rs, r_rs, bi % 2)
            dv, r_dv = self.seg_dv(lo)
            for k in range(KC):
                tmp, r_tmp = tmps[k % 2]
                self.tt(tmp[:, 0:n], x[:, k, 0:n], rsp, ALU.mult, [r_x, r_rsp], [r_tmp])
                self.act(self.hT[:, k, lo:hi], tmp[:, 0:n], AF.Identity, [r_tmp, r_dv], [self.r_hT],
                         bias=dv[:, base + 1, k:k + 1], scale=dv[:, base + 0, k:k + 1])

    def load_w_cols(self, dst, r_dst, l, c0, c1):
        self.ld(dst, self.WI[l].rearrange("p (k n) -> p k n", k=KC)[:, :, c0:c1], [self.r_wbf], [r_dst])

    def make_perm(self, dst, src, nheads, hd, r0, rd, r_dst, r_src, nk):
        nf = rd // 4
        self.P.op("dve", lambda e: e.memset(dst, 0.0), [], [r_dst])
        for k in range(nk):
            for h in range(nheads):
                b = h * hd + r0
                for hh in range(2):
                    o = b + hh * 2 * nf
                    self.ts(dst[:, k, o:o + nf], src[:, k, o + nf:o + 2 * nf], -1.0, ALU.mult, [r_src], [r_dst])
                    self.cp(dst[:, k, o + nf:o + 2 * nf], src[:, k, o:o + nf], [r_src], [r_dst])

    def attn_scores(self, it, buf):
        Pm, r_Pm, sm, r_sm = buf
        kparts, sink, scale = it["kparts"], it["sink"], it["scale"]
        pc = it.get("pcol", 0)
        ncols = max(c0 + k.shape[-1] for (k, _, c0, _) in kparts)
        started = set()
        for (kT, r_k, c0, mask) in kparts:
            n = kT.shape[-1]
            b = (pc + c0) // 512
            assert (pc + c0 + n - 1) // 512 == b
            self.mm(self.PS[:, pc + c0:pc + c0 + n], it["q"], kT, b not in started, mask is None, [it["r_q"], r_k], [self.r_ps[b]])
            started.add(b)
            if mask is not None:
                self.mm(self.PS[:, pc + c0:pc + c0 + n], self.C["ident_bf"], mask, False, True, [self.r_c], [self.r_ps[b]])
        tot = ncols
        if sink is not None:
            b = (pc + ncols) // 512
            self.mm(self.PS[:, pc + ncols:pc + ncols + 1], self.C["ones_f"][0:1, :], sink, b not in started, True, [self.r_c, self.r_pv], [self.r_ps[b]])
            started.add(b)
            tot = ncols + 1
        banks = [self.r_ps[b] for b in sorted(started)]
        self.red(sm[:, 0:1], self.PS[:, pc:pc + tot], ALU.max, banks, [r_sm])
        self.ts(sm[:, 1:2], sm[:, 0:1], -scale, ALU.mult, [r_sm], [r_sm])
        self.act(Pm[:, 0:tot], self.PS[:, pc:pc + tot], AF.Exp, banks + [r_sm], [r_Pm], bias=sm[:, 1:2], scale=scale)
        it["ncols"] = ncols

    def attn_transposes(self, it, buf, PTt, r_PT):
        Pm, r_Pm, sm, r_sm = buf
        vparts = it["vparts"]
        nv = len(vparts)
        for i, (V, r_v, c0) in enumerate(vparts):
            tb = 5 + (i // 8) % 2
            slot = i % 8
            pt_ps = self.bank_bf(tb)[:, slot * 128:(slot + 1) * 128]
            self.tr(pt_ps, Pm[:, c0:c0 + 128], self.C["ident_bf"], [r_Pm, self.r_c], [self.r_ps[tb]])
            if slot == 7 or i == nv - 1:
                g0 = i - slot
                self.cp(PTt[:, g0:i + 1, :], self.bank_bf(tb)[:, 0:(slot + 1) * 128].rearrange("p (a b) -> p a b", b=128),
                        [self.r_ps[tb]], [r_PT], eng="dve" if (i // 8) % 2 == 0 else "act")

    def attn_pv(self, it, buf, PTt, r_PT):
        Pm, r_Pm, sm, r_sm = buf
        vparts, sink, ncols = it["vparts"], it["sink"], it["ncols"]
        nv = len(vparts)
        for i, (V, r_v, c0) in enumerate(vparts):
            self.mm(self.bank(7, 65), PTt[:, i, :], V, i == 0, i == nv - 1, [r_PT, r_v], [self.r_ps[7]])
        if sink is not None:
            self.tt(sm[:, 2:3], self.bank(7, 1, 64), Pm[:, ncols:ncols + 1], ALU.add, [self.r_ps[7], r_Pm], [r_sm])
            self.recip(sm[:, 3:4], sm[:, 2:3], [r_sm], [r_sm])
        else:
            self.recip(sm[:, 3:4], self.bank(7, 1, 64), [self.r_ps[7]], [r_sm])
        self.ts(it["out"], self.bank(7, 64), sm[:, 3:4], ALU.mult, [self.r_ps[7], r_sm], [it["r_out"]])
        if it.get("after"):
            it["after"]()

    def attn_run(self, items, pingpong=False):
        if pingpong:
            for i, it in enumerate(items):
                it["pcol"] = (i % 2) * 1024
        bufs = []
        for j in range(2):
            Pm, r_Pm = self.tile([128, self.T + 128], BF16, "Pm%d" % j)
            sm, r_sm = self.tile([128, 4], F32, "sm%d" % j)
            bufs.append((Pm, r_Pm, sm, r_sm))
        PTt, r_PT = self.tile([128, self.TT, 128], BF16, "PT")
        n = len(items)
        if n == 0:
            return
        self.attn_scores(items[0], bufs[0])
        for i in range(n):
            if i + 1 < n:
                self.attn_scores(items[i + 1], bufs[(i + 1) % 2])
            self.attn_transposes(items[i], bufs[i % 2], PTt, r_PT)
            self.attn_pv(items[i], bufs[i % 2], PTt, r_PT)

    def store_y_tm(self, ytm, r_ytm, s, br, tile_i, ybuf):
        yT, r_yT = ybuf
        for c in range(2):
            self.tr(self.bank_bf(6)[:, c * 128:(c + 1) * 128], ytm[:, c * 128:(c + 1) * 128], self.C["ident_bf"], [r_ytm, self.r_c], [self.r_ps[6]])
        self.cp(yT, self.bank_bf(6)[:, 0:256].rearrange("p (a b) -> p a b", b=128), [self.r_ps[6]], [r_yT])
        self.ld(self.YS[s][br * 256:(br + 1) * 256, tile_i * 128:(tile_i + 1) * 128].rearrange("(c p) n -> p c n", p=128), yT,
                [r_yT], [self.r_ys[s]])

    def branch_a(self, s, l, last):
        self.phase()
        T, TT, CT = self.T, self.TT, self.CT
        wA, r_wA = self.tile([128, KC, 544], BF16, "wA")
        wAp, r_wAp = self.tile([128, KC, 32], BF16, "wAp")
        wq, r_wq = self.tile([128, 2, 384], BF16, "wq")
        wqf, r_wqf = self.tile([128, 2, 384], F32, "wqf")
        wqp, r_wqp = self.tile([128, 2, 384], BF16, "wqp")
        wkv, r_wkv = self.tile([128, 2, 512], BF16, "wkv")
        wkvf, r_wkvf = self.tile([128, 2, 512], F32, "wkvf")
        raw, r_raw = self.tile([128, 4, 512], F32, "raw")
        sq, r_sq = self.tile([128, 4, 512], BF16, "sqA")
        rs, r_rs = self.tile([128, 2, 512], F32, "rsA")
        cqn, r_cqn = self.tile([128, 4, 512], BF16, "cqn")
        tab, r_tab = self.tile([128, 4, 512], F32, "tabA")
        t1, r_t1 = self.tile([128, 512], F32, "t1A")
        t2, r_t2 = self.tile([128, 512], F32, "t2A")
        qT, r_qT = self.tile([128, 4, T], BF16, "qTA")
        kT, r_kT = self.tile([128, 4, T], BF16, "kTA")
        Va, r_Va = self.tile([128, TT, 4, 65], BF16, "VaA")
        ytms = [self.tile([128, 256], BF16, "ytmA%d" % i) for i in range(2)]
        ybuf = self.tile([128, 2, 128], BF16, "yTA")
        self.load_w_cols(wA, r_wA, l, 0, 544)
        self.ld(wqf, self.W["w_uq"][l].rearrange("(k p) n -> p k n", p=128), [], [r_wqf])
        self.ld(wkvf, self.W["w_ukv"][l].rearrange("(k p) n -> p k n", p=128), [], [r_wkvf])
        for k in range(2):
            self.ts(wq[:, k, :], wqf[:, k, :], self.pv["g_qa"][:, l, k:k + 1], ALU.mult, [r_wqf, self.r_pv], [r_wq])
            self.ts(wkv[:, k, :], wkvf[:, k, :], self.pv["g_kva"][:, l, k:k + 1], ALU.mult, [r_wkvf, self.r_pv], [r_wkv])
        self.make_perm(wqp, wq, 4, 96, 64, 32, r_wqp, r_wq, 2)
        wkr = wA[:, :, 512:544]
        self.make_perm(wAp, wkr, 1, 32, 0, 32, r_wAp, r_wA, KC)
        self.P.op("dve", lambda e: e.memset(Va, 1.0), [], [r_Va])
        for bi, (lo, hi) in enumerate(self.blocks):
            n = hi - lo
            for c in range(4):
                b = c % 2
                for k in range(KC):
                    self.mm(self.bank(b, n), wA[:, k, c * 128:(c + 1) * 128], self.hT[:, k, lo:hi], k == 0, k == KC - 1,
                            [r_wA, self.r_hT], [self.r_ps[b]])
                self.cp(raw[:, c, 0:n], self.bank(b, n), [self.r_ps[b]], [r_raw], eng="act")
            rp0 = self.rstd_block(raw[:, 0:2], r_raw, n, 2, 256, sq[:, 0:2], r_sq, rs[:, 0], r_rs, 6)
            rp1 = self.rstd_block(raw[:, 2:4], r_raw, n, 2, 256, sq[:, 2:4], r_sq, rs[:, 1], r_rs, 7)
            for c in range(4):
                rp, r_rp = (rp0, rp1)[c // 2]
                self.tt(cqn[:, c, 0:n], raw[:, c, 0:n], rp, ALU.mult, [r_raw, r_rp], [r_cqn])
            self.ld(tab[0:96, 0, 0:n], self.CD["cosq_a"][:, lo:hi], [], [r_tab])
            self.ld(tab[0:96, 1, 0:n], self.CD["sinq_a"][:, lo:hi], [], [r_tab])
            self.ld(tab[0:32, 2, 0:n], self.CD["cosk_a"][:, lo:hi], [], [r_tab])
            self.ld(tab[0:32, 3, 0:n], self.CD["sink_a"][:, lo:hi], [], [r_tab])
            for h in range(4):
                for (w_, r_w_, b) in ((wq, r_wq, 0), (wqp, r_wqp, 1)):
                    for k in range(2):
                        self.mm(self.bank(b, n)[0:96], w_[:, k, h * 96:(h + 1) * 96], cqn[:, k, 0:n], k == 0, k == 1, [r_w_, r_cqn], [self.r_ps[b]])
                self.tt(t1[0:96, 0:n], self.bank(0, n)[0:96], tab[0:96, 0, 0:n], ALU.mult, [self.r_ps[0], r_tab], [r_t1])
                self.tt(t2[0:96, 0:n], self.bank(1, n)[0:96], tab[0:96, 1, 0:n], ALU.mult, [self.r_ps[1], r_tab], [r_t2])
                self.tt(qT[0:96, h, lo:hi], t1[0:96, 0:n], t2[0:96, 0:n], ALU.add, [r_t1, r_t2], [r_qT])
                for k in range(2):
                    self.mm(self.bank(2, n)[0:64], wkv[:, k, h * 128:h * 128 + 64], cqn[:, 2 + k, 0:n], k == 0, k == 1, [r_wkv, r_cqn], [self.r_ps[2]])
                self.cp(kT[0:64, h, lo:hi], self.bank(2, n)[0:64], [self.r_ps[2]], [r_kT], eng="act")
            for (w_, r_w_, b) in ((wkr, r_wA, 3), (wAp, r_wAp, 4)):
                for k in range(KC):
                    self.mm(self.bank(b, n)[0:32], w_[:, k, :], self.hT[:, k, lo:hi], k == 0, k == KC - 1, [r_w_, self.r_hT], [self.r_ps[b]])
            self.tt(t1[0:32, 0:n], self.bank(3, n)[0:32], tab[0:32, 2, 0:n], ALU.mult, [self.r_ps[3], r_tab], [r_t1])
            self.tt(t2[0:32, 0:n], self.bank(4, n)[0:32], tab[0:32, 3, 0:n], ALU.mult, [self.r_ps[4], r_tab], [r_t2])
            self.tt(t1[0:32, 0:n], t1[0:32, 0:n], t2[0:32, 0:n], ALU.add, [r_t1, r_t2], [r_t1])
            for h in range(4):
                self.cp(kT[64:96, h, lo:hi], t1[0:32, 0:n], [r_t1], [r_kT])
            for ti in range(lo // 128, hi // 128):
                o = ti * 128 - lo
                for k in range(2):
                    self.mm(self.bank(5, 256).rearrange("p (h d) -> p h d", d=64), cqn[:, 2 + k, o:o + 128],
                            wkv[:, k, :].rearrange("p (h x) -> p h x", x=128)[:, :, 64:128], k == 0, k == 1, [r_cqn, r_wkv], [self.r_ps[5]])
                self.cp(Va[:, ti, :, 0:64], self.bank(5, 256).rearrange("p (h d) -> p h d", d=64), [self.r_ps[5]], [r_Va])
        q_tiles = list(range(CT, TT)) + ([] if last else list(range(CT)))
        items = []
        for n_, qi in enumerate(q_tiles):
            is_ctx = qi < CT
            nk = self.NCTX if is_ctx else T
            yt, r_yt = ytms[n_ % 2]
            for h in range(4):
                kparts = []
                c0 = 0
                while c0 < nk:
                    n = min(512, nk - c0)
                    kparts.append((kT[0:96, h, c0:c0 + n], r_kT, c0, None))
                    c0 += n
                vparts = [(Va[:, i, h, :], r_Va, i * 128) for i in range(nk // 128)]
                it = dict(q=qT[0:96, h, qi * 128:(qi + 1) * 128], r_q=r_qT, kparts=kparts, sink=None, vparts=vparts, scale=1.0,
                          out=yt[:, h * 64:(h + 1) * 64], r_out=r_yt)
                if h == 3:
                    it["after"] = (lambda yt=yt, r_yt=r_yt, qi=qi: self.store_y_tm(yt, r_yt, s, 0, qi, ybuf))
                items.append(it)
        self.attn_run(items)

    def branch_c(self, s, l, last):
        self.phase()
        T, TT, CT = self.T, self.TT, self.CT
        wC, r_wC = self.tile([128, KC, 512], BF16, "wC")
        wCp, r_wCp = self.tile([128, KC, 384], BF16, "wCp")
        tab, r_tab = self.tile([128, 2, 512], F32, "tabC")
        t1, r_t1 = self.tile([128, 512], F32, "t1C")
        t2, r_t2 = self.tile([128, 512], F32, "t2C")
        qT, r_qT = self.tile([128, 4, T], BF16, "qTC")
        kT, r_kT = self.tile([128, 2, T], BF16, "kTC")
        Va, r_Va = self.tile([128, TT, 2, 65], BF16, "VaC")
        sk8, r_sk8 = self.tile([128, 4], F32, "sk8")
        ytms = [self.tile([128, 256], BF16, "ytmC%d" % i) for i in range(2)]
        ybuf = self.tile([128, 2, 128], BF16, "yTC")
        self.load_w_cols(wC, r_wC, l, 1584, 2096)
        self.make_perm(wCp, wC[:, :, 0:384], 6, 64, 0, 64, r_wCp, r_wC, KC)
        self.ts(sk8, self.sink_b[:, l, :], 8.0, ALU.mult, [self.r_pv], [r_sk8])
        self.P.op("dve", lambda e: e.memset(Va, 1.0), [], [r_Va])
        for bi, (lo, hi) in enumerate(self.blocks):
            n = hi - lo
            self.ld(tab[0:64, 0, 0:n], self.CD["cos_c"][:, lo:hi], [], [r_tab])
            self.ld(tab[0:64, 1, 0:n], self.CD["sin_c"][:, lo:hi], [], [r_tab])
            for hh in range(6):
                for (w_, r_w_, b) in ((wC, r_wC, 0), (wCp, r_wCp, 1)):
                    for k in range(KC):
                        self.mm(self.bank(b, n)[0:64], w_[:, k, hh * 64:(hh + 1) * 64], self.hT[:, k, lo:hi], k == 0, k == KC - 1,
                                [r_w_, self.r_hT], [self.r_ps[b]])
                self.tt(t1[0:64, 0:n], self.bank(0, n)[0:64], tab[0:64, 0, 0:n], ALU.mult, [self.r_ps[0], r_tab], [r_t1])
                self.tt(t2[0:64, 0:n], self.bank(1, n)[0:64], tab[0:64, 1, 0:n], ALU.mult, [self.r_ps[1], r_tab], [r_t2])
                dst = qT[0:64, hh, lo:hi] if hh < 4 else kT[0:64, hh - 4, lo:hi]
                self.tt(dst, t1[0:64, 0:n], t2[0:64, 0:n], ALU.add, [r_t1, r_t2], [r_qT if hh < 4 else r_kT])
            for ti in range(lo // 128, hi // 128):
                for k in range(KC):
                    self.mm(self.bank(5, 128), self.hT[:, k, ti * 128:(ti + 1) * 128], wC[:, k, 384:512], k == 0, k == KC - 1,
                            [self.r_hT, r_wC], [self.r_ps[5]])
                self.cp(Va[:, ti, :, 0:64], self.bank(5, 128).rearrange("p (h d) -> p h d", d=64), [self.r_ps[5]], [r_Va])
        NQ = TT - CT
        q_tiles = list(range(CT, TT)) + ([] if last else list(range(CT)))
        NC_ = self.NCTX
        items = []
        for n_, qi in enumerate(q_tiles):
            is_ctx = qi < CT
            yt, r_yt = ytms[n_ % 2]
            for h in range(4):
                g = h // 2
                kparts = [(kT[0:64, g, 0:NC_], r_kT, 0, None)]
                vparts = [(Va[:, i, g, :], r_Va, i * 128) for i in range(CT)]
                if not is_ctx:
                    i = qi - CT
                    col = NC_
                    for (j, mk) in ((i - 1, "maskA"), (i, None), (i + 1, "maskB")):
                        if 0 <= j < NQ:
                            kparts.append((kT[0:64, g, NC_ + j * 128:NC_ + (j + 1) * 128], r_kT, col, self.C[mk] if mk else None))
                            vparts.append((Va[:, CT + j, g, :], r_Va, col))
                            col += 128
                it = dict(q=qT[0:64, h, qi * 128:(qi + 1) * 128], r_q=r_qT, kparts=kparts, sink=sk8[0:1, h:h + 1], vparts=vparts, scale=0.125,
                          out=yt[:, h * 64:(h + 1) * 64], r_out=r_yt)
                if h == 3:
                    it["after"] = (lambda yt=yt, r_yt=r_yt, qi=qi: self.store_y_tm(yt, r_yt, s, 2, qi, ybuf))
                items.append(it)
        self.attn_run(items, pingpong=True)

    def branch_d(self, s, l, last):
        self.phase()
        T, TT, CT = self.T, self.TT, self.CT
        wD, r_wD = self.tile([128, KC, 256], BF16, "wD")
        ud, r_ud = self.tile([128, 2, T], BF16, "udT")
        uc, r_uc = self.tile([128, TT, 2, 256], BF16, "uc_tm")
        cn, r_cn = self.tile([128, 16, 512], BF16, "cn")
        sn, r_sn = self.tile([128, 16, 512], BF16, "sn")
        yo, r_yo = self.tile([128, 2, 512], BF16, "yoD")
        self.load_w_cols(wD, r_wD, l, 2096, 2352)
        for (lo, hi) in self.blocks:
            n = hi - lo
            for c in range(2):
                for k in range(KC):
                    self.mm(self.bank(c, n), wD[:, k, c * 128:(c + 1) * 128], self.hT[:, k, lo:hi], k == 0, k == KC - 1,
                            [r_wD, self.r_hT], [self.r_ps[c]])
                self.cp(ud[:, c, lo:hi], self.bank(c, n), [self.r_ps[c]], [r_ud], eng="act" if c else "dve")
        for ti in range(TT):
            for j, m in enumerate(("bdc", "bds")):
                for c in range(2):
                    self.mm(self.bank(2, 128, j * 256 + c * 128), ud[:, c, ti * 128:(ti + 1) * 128], self.C[m], c == 0 and j == 0, True,
                            [r_ud, self.r_c], [self.r_ps[2]])
            self.cp(uc[:, ti], self.bank(2).rearrange("p (a b) -> p a b", a=2), [self.r_ps[2]], [r_uc])
        segs = [("lat", CT, TT, self.NCTX)] + ([] if last else [("ctx", 0, CT, 0)])
        for nm, t0, t1_, col0 in segs:
            N = (t1_ - t0) * 128
            ntl = t1_ - t0
            for kb in range(0, N, 512):
                n = min(512, N - kb)
                self.ld(cn[:, 0:ntl, 0:n], self.CD["cn_" + nm][:, kb:kb + n].rearrange("(a p) k -> p a k", p=128), [], [r_cn])
                self.ld(sn[:, 0:ntl, 0:n], self.CD["sn_" + nm][:, kb:kb + n].rearrange("(a p) k -> p a k", p=128), [], [r_sn])
                for c in range(2):
                    b = 3 + c
                    for a in range(ntl):
                        self.mm(self.bank(b, n), uc[:, t0 + a, 0, c * 128:(c + 1) * 128], cn[:, a, 0:n], a == 0, False, [r_uc, r_cn], [self.r_ps[b]])
                        self.mm(self.bank(b, n), uc[:, t0 + a, 1, c * 128:(c + 1) * 128], sn[:, a, 0:n], False, a == ntl - 1, [r_uc, r_sn], [self.r_ps[b]])
                    self.cp(yo[:, c, 0:n], self.bank(b, n), [self.r_ps[b]], [r_yo], eng="act" if c else "dve")
                self.ld(self.YS[s][768:1024, col0 + kb:col0 + kb + n].rearrange("(c p) n -> p c n", p=128), yo[:, :, 0:n], [r_yo], [self.r_ys[s]])

    def branch_b(self, s, l, last):
        self.phase()
        T, TT, CT = self.T, self.TT, self.CT
        wB, r_wB = self.tile([128, KC, 1040], BF16, "wB")
        araw, r_araw = self.tile([128, 514], F32, "arawB")
        c1, r_c1 = self.tile([128, 512], F32, "c1B")
        qk, r_qk = self.tile([128, 8, T], BF16, "qkB")
        ktm, r_ktm = self.tile([128, TT, 256], BF16, "ktmB")
        Va, r_Va = self.tile([128, TT, 4, 65], BF16, "VaB")
        og, r_og = self.tile([128, 256], F32, "ogB")
        G, r_G = self.tile([128, TT, 16], F32, "GB")
        hs, r_hs = self.tile([128, TT, 256], F32, "hsB")
        self.load_w_cols(wB, r_wB, l, 544, 1584)
        self.ld(self.wconvB[0:64], self.PR["wconvB"][l], [], [self.r_wcb])
        self.P.op("dve", lambda e: e.memset(Va, 1.0), [], [r_Va])
        self.P.op("dve", lambda e: e.memset(hs, 0.0), [], [r_hs])
        for (lo, hi) in self.blocks:
            n = hi - lo
            seg_lo, seg_hi = (0, self.NCTX) if lo < self.NCTX else (self.NCTX, T)
            for hh in range(8):
                ch, half = hh // 2, hh % 2
                c0 = hh * 64
                for k in range(KC):
                    self.mm(self.bank(0, n)[0:64], wB[:, k, c0:c0 + 64], self.hT[:, k, lo:hi], k == 0, k == KC - 1, [r_wB, self.r_hT], [self.r_ps[0]])
                self.P.op("dve", lambda e: e.memset(araw[0:64, :], 0.0), [], [r_araw])
                if lo > seg_lo:
                    for k in range(KC):
                        self.mm(self.bank(1, 1)[0:64], wB[:, k, c0:c0 + 64], self.hT[:, k, lo - 1:lo], k == 0, k == KC - 1, [r_wB, self.r_hT], [self.r_ps[1]])
                    self.cp(araw[0:64, 0:1], self.bank(1, 1)[0:64], [self.r_ps[1]], [r_araw])
                if hi < seg_hi:
                    for k in range(KC):
                        self.mm(self.bank(1, 1, 8)[0:64], wB[:, k, c0:c0 + 64], self.hT[:, k, hi:hi + 1], k == 0, k == KC - 1, [r_wB, self.r_hT], [self.r_ps[1]])
                    self.cp(araw[0:64, n + 1:n + 2], self.bank(1, 1, 8)[0:64], [self.r_ps[1]], [r_araw])
                self.cp(araw[0:64, 1:n + 1], self.bank(0, n)[0:64], [self.r_ps[0]], [r_araw], eng="act")
                wsl = self.wconvB[:, hh, :]
                self.ts(c1[0:64, 0:n], araw[0:64, 0:n], wsl[0:64, 0:1], ALU.mult, [r_araw, self.r_wcb], [r_c1])
                self.stt(c1[0:64, 0:n], araw[0:64, 1:n + 1], wsl[0:64, 1:2], c1[0:64, 0:n], ALU.mult, ALU.add, [r_araw, self.r_wcb, r_c1], [r_c1])
                self.stt(c1[0:64, 0:n], araw[0:64, 2:n + 2], wsl[0:64, 2:3], c1[0:64, 0:n], ALU.mult, ALU.add, [r_araw, self.r_wcb, r_c1], [r_c1])
                self.act(c1[0:64, 0:n], c1[0:64, 0:n], AF.Silu, [r_c1], [r_c1])
                self.ts(qk[0:64, hh, lo:hi], c1[0:64, 0:n], 1.0 if hh < 4 else 0.125, ALU.mult, [r_c1], [r_qk])
        Gt, r_Gt = self.tile([128, TT, 2, 4], F32, "GtB")
        for ti in range(TT):
            tsl = slice(ti * 128, (ti + 1) * 128)
            for k in range(KC):
                self.mm(self.bank(2, 16), self.hT[:, k, tsl], wB[:, k, 1024:1040], k == 0, k == KC - 1, [self.r_hT, r_wB], [self.r_ps[2]])
            self.tt(G[:, ti, :], self.bank(2, 16), self.bg_b[:, l, :], ALU.add, [self.r_ps[2], self.r_pv], [r_G])
            for k in range(KC):
                self.mm(self.bank(3, 256), self.hT[:, k, tsl], wB[:, k, 512:768], k == 0, k == KC - 1, [self.r_hT, r_wB], [self.r_ps[3]])
            self.cp(Va[:, ti, :, 0:64], self.bank(3, 256).rearrange("p (h d) -> p h d", d=64), [self.r_ps[3]], [r_Va])
            for h in range(4):
                self.tr(self.bank_bf(5)[:, h * 64:(h + 1) * 64], qk[0:64, 4 + h, tsl], self.C["ident_bf"][0:64, 0:64], [r_qk, self.r_c], [self.r_ps[5]])
            self.cp(ktm[:, ti, :], self.bank_bf(5)[:, 0:256], [self.r_ps[5]], [r_ktm])
        G5 = G.rearrange("p t (a b c) -> p t a b c", a=2, b=2)
        for d_ in range(2):
            fv = G5[:, :, d_, 1, :]
            self.act(Gt[:, :, d_, :], fv, AF.Exp, [r_G], [r_Gt], scale=-1.0)
            self.act(Gt[:, :, d_, :], Gt[:, :, d_, :], AF.Ln, [r_Gt], [r_Gt], bias=1.0)
            self.ts(fv, Gt[:, :, d_, :], -1.0, ALU.mult, [r_Gt], [r_G])
        B_TM, M_, NEGM, WIN, EMT, DEN, DAB, RR, WTM, DEC, MX, DENI = range(12)
        SX = []
        for d_ in range(2):
            X = {}
            for nm, shp, dt in (("diag", [128, 4, 128], F32), ("bBm", [128, 4, 128], F32), ("Wt", [128, 4, 128], F32),
                                ("Sb", [128, 4, 128], BF16), ("ST", [128, 4, 128], BF16), ("kw", [128, 4, 64], BF16),
                                ("sv", [128, 16, 4], F32), ("cm", [128, 8], F32), ("mst", [128, 4], F32), ("tmpi", [128, 4, 65], F32),
                                ("numh", [128, 4, 64], F32), ("Cst", [128, 4, 65], F32), ("Cbf", [128, 4, 65], BF16)):
                X[nm], X["r_" + nm] = self.tile(shp, dt, nm + "B%d" % d_)
            X["tri"] = self.C["tri_f" if d_ == 0 else "tri_b"]
            X["mneg"] = self.C["mneg_f" if d_ == 0 else "mneg_b"]
            X["esel"] = self.C["e_last" if d_ == 0 else "e_first"]
            X["b0"] = 4 * d_
            SX.append(X)
            self.P.op("dve", lambda e, t=X["Cst"]: e.memset(t, 0.0), [], [X["r_Cst"]])
            self.P.op("dve", lambda e, t=X["Cbf"]: e.memset(t, 0.0), [], [X["r_Cbf"]])
            self.P.op("dve", lambda e, t=X["mst"]: e.memset(t, 0.0), [], [X["r_mst"]])

        def chunk(d_, ti):
            X = SX[d_]
            diag, bBm, Wt, Sb, ST, kw, sv, cm, mst, tmpi, numh, Cst, Cbf = (X[k] for k in (
                "diag", "bBm", "Wt", "Sb", "ST", "kw", "sv", "cm", "mst", "tmpi", "numh", "Cst", "Cbf"))
            r_diag, r_bBm, r_Wt, r_Sb, r_ST, r_kw, r_sv, r_cm, r_mst, r_tmpi, r_numh, r_Cst, r_Cbf = (X["r_" + k] for k in (
                "diag", "bBm", "Wt", "Sb", "ST", "kw", "sv", "cm", "mst", "tmpi", "numh", "Cst", "Cbf"))
            b0 = X["b0"]
            bB_b, qk_b, st_b, ms_b = b0, b0 + 1, b0 + 2, b0 + 3
            r0, r1, r2, r3 = self.r_ps[bB_b], self.r_ps[qk_b], self.r_ps[st_b], self.r_ps[ms_b]
            cum_ps = self.bank(ms_b, 4)
            sel_ps = self.bank(ms_b, 8, 8)
            inter_ps = self.bank(ms_b, 260, 16)
            upd_ps = self.bank(ms_b, 260, 16)
            num_ps = self.bank(st_b, 256, 256)
            tsl = slice(ti * 128, (ti + 1) * 128)
            li = G[:, ti, d_ * 8:d_ * 8 + 4]
            lf = G[:, ti, d_ * 8 + 4:d_ * 8 + 8]
            self.mm(cum_ps, X["tri"], lf, True, True, [self.r_c, r_G], [r3])
            for h in range(4):
                self.mm(self.bank(qk_b, 128, h * 128), qk[0:64, h, tsl], qk[0:64, 4 + h, tsl], True, True, [r_qk], [r1])
            yield
            self.tt(sv[:, B_TM, :], li, cum_ps, ALU.subtract, [r_G, r3], [r_sv])
            for h in range(4):
                self.ts(diag[:, h, :], self.C["ident_f"], sv[:, B_TM, h:h + 1], ALU.mult, [self.r_c, r_sv], [r_diag])
            yield
            for h in range(4):
                self.mm(self.bank(bB_b, 128, h * 128), self.C["ones_f"], diag[:, h, :], True, True, [self.r_c, r_diag], [r0])
            yield
            self.tt(bBm, self.bank(bB_b).rearrange("p (h s) -> p h s", h=4), X["mneg"], ALU.add, [r0, self.r_c], [r_bBm])
            self.red(sv[:, MX, :], bBm, ALU.max, [r_bBm], [r_sv])
            self.tt(sv[:, M_, :], sv[:, MX, :], mst, ALU.max, [r_sv, r_mst], [r_sv])
            self.ts(sv[:, NEGM, :], sv[:, M_, :], -1.0, ALU.mult, [r_sv], [r_sv])
            self.tt(sv[:, WIN, :], mst, sv[:, M_, :], ALU.subtract, [r_mst, r_sv], [r_sv])
            self.tt(cm[:, 0:4], cum_ps, sv[:, M_, :], ALU.add, [r3, r_sv], [r_cm])
            self.cp(cm[:, 4:8], sv[:, M_, :], [r_sv], [r_cm])
            yield
            for h in range(4):
                self.act(Wt[:, h, :], bBm[:, h, :], AF.Exp, [r_bBm, r_sv], [r_Wt], bias=sv[:, NEGM, h:h + 1], scale=1.0)
            self.act(sv[:, WIN, :], sv[:, WIN, :], AF.Exp, [r_sv], [r_sv])
            self.act(sv[:, EMT, :], cm[:, 0:4], AF.Exp, [r_cm], [r_sv], scale=-1.0)
            self.mm(sel_ps, X["esel"], cm, True, True, [self.r_c, r_cm], [r3])
            yield
            self.tt(Sb, self.bank(qk_b).rearrange("p (h s) -> p h s", h=4), Wt, ALU.mult, [r1, r_Wt], [r_Sb])
            self.red(sv[:, DENI, :], Sb, ALU.add, [r_Sb], [r_sv])
            self.tt(sv[:, WTM, :], sv[:, B_TM, :], self.bank(ms_b, 4, 12), ALU.subtract, [r_sv, r3], [r_sv])
            self.tt(sv[:, DEC, :], mst, self.bank(ms_b, 4, 12), ALU.subtract, [r_mst, r3], [r_sv])
            self.cp(mst, self.bank(ms_b, 4, 8), [r3], [r_mst])
            yield
            for h in range(4):
                self.tr(self.bank_bf(st_b)[:, h * 128:(h + 1) * 128], Sb[:, h, :], self.C["ident_bf"], [r_Sb, self.r_c], [r2])
            for h in range(4):
                self.mm(self.bank(ms_b, 65, 16 + h * 65), qk[0:64, h, tsl], Cbf[0:64, h, :], True, True, [r_qk, r_Cbf], [r3])
            self.act(sv[:, WTM, :], sv[:, WTM, :], AF.Exp, [r_sv], [r_sv])
            self.act(sv[:, DEC, :], sv[:, DEC, :], AF.Exp, [r_sv], [r_sv])
            yield
            self.cp(ST, self.bank_bf(st_b)[:, 0:512].rearrange("p (h s) -> p h s", h=4), [r2], [r_ST])
            self.tt(tmpi, inter_ps.rearrange("p (h e) -> p h e", h=4), sv[:, WIN, :].unsqueeze(2).to_broadcast([128, 4, 65]), ALU.mult,
                    [r3, r_sv], [r_tmpi])
            self.tt(kw, ktm[:, ti, :].rearrange("p (h e) -> p h e", h=4), sv[:, WTM, :].unsqueeze(2).to_broadcast([128, 4, 64]), ALU.mult,
                    [r_ktm, r_sv], [r_kw])
            yield
            for h in range(4):
                self.mm(self.bank(st_b, 64, 256 + h * 64), ST[:, h, :], Va[:, ti, h, 0:64], True, True, [r_ST, r_Va], [r2])
            for h in range(4):
                self.mm(self.bank(ms_b, 65, 16 + h * 65)[0:64], kw[:, h, :], Va[:, ti, h, :], True, True, [r_kw, r_Va], [r3])
            yield
            self.tt(numh, tmpi[:, :, 0:64], num_ps.rearrange("p (h e) -> p h e", h=4), ALU.add, [r_tmpi, r2], [r_numh])
            self.tt(sv[:, DEN, :], tmpi[:, :, 64], sv[:, DENI, :], ALU.add, [r_tmpi, r_sv], [r_sv])
            self.ts(sv[:, DAB, :], sv[:, DEN, :], -1.0, ALU.mult, [r_sv], [r_sv])
            self.tt(sv[:, DAB, :], sv[:, DAB, :], sv[:, DEN, :], ALU.max, [r_sv], [r_sv])
            self.tt(sv[:, DAB, :], sv[:, DAB, :], sv[:, EMT, :], ALU.max, [r_sv], [r_sv])
            self.recip(sv[:, RR, :], sv[:, DAB, :], [r_sv], [r_sv])
            self.tt(numh, numh, sv[:, RR, :].unsqueeze(2).to_broadcast([128, 4, 64]), ALU.mult, [r_numh, r_sv], [r_numh])
            hsv = hs[:, ti, :].rearrange("p (h e) -> p h e", h=4)
            self.tt(hsv, hsv, numh, ALU.add, [r_hs, r_numh], [r_hs])
            self.tt(Cst[0:64], Cst[0:64], sv[0:64, DEC, :].unsqueeze(2).to_broadcast([64, 4, 65]), ALU.mult, [r_Cst, r_sv], [r_Cst])
            self.tt(Cst[0:64], Cst[0:64], upd_ps[0:64].rearrange("p (h e) -> p h e", h=4), ALU.add, [r_Cst, r3], [r_Cst])
            self.cp(Cbf[0:64], Cst[0:64], [r_Cst], [r_Cbf])
            yield

        orders = [list(range(TT)), list(range(CT - 1, -1, -1)) + list(range(TT - 1, CT - 1, -1))]
        for i in range(TT):
            gens = [chunk(0, orders[0][i]), chunk(1, orders[1][i])]
            alive = True
            while alive:
                alive = False
                for g in gens:
                    try:
                        next(g)
                        alive = True
                    except StopIteration:
                        pass
        ytm, r_ytm = self.tile([128, 256], BF16, "ytmB")
        ybuf = self.tile([128, 2, 128], BF16, "yTB")
        for ti in range(CT if last else 0, TT):
            tsl = slice(ti * 128, (ti + 1) * 128)
            for k in range(KC):
                self.mm(self.bank(4, 256), self.hT[:, k, tsl], wB[:, k, 768:1024], k == 0, k == KC - 1, [self.r_hT, r_wB], [self.r_ps[4]])
            self.act(og, self.bank(4, 256), AF.Sigmoid, [self.r_ps[4]], [r_og])
            self.tt(ytm, og, hs[:, ti, :], ALU.mult, [r_og, r_hs], [r_ytm])
            self.store_y_tm(ytm, r_ytm, s, 1, ti, ybuf)

    def post_norm_res(self, s, lo, hi, yT, r_yT, gp_idx, tl):
        sq, r_sq, rs, r_rs, tmp, r_tmp, xb, r_xb = tl
        n = hi - lo
        rsp, r_rsp = self.rstd_block(yT, r_yT, n, KC, D, sq, r_sq, rs, r_rs, 7)
        self.ld(xb[:, :, 0:n], self.XRES[s][:, lo:hi].rearrange("(k p) n -> p k n", p=128), [self.r_xres[s]], [r_xb])
        dv, r_dv = self.seg_dv(lo)
        for k in range(KC):
            self.tt(tmp[:, 0:n], yT[:, k, 0:n], rsp, ALU.mult, [r_yT, r_rsp], [r_tmp])
            self.stt(xb[:, k, 0:n], tmp[:, 0:n], dv[:, gp_idx, k:k + 1], xb[:, k, 0:n], ALU.mult, ALU.add, [r_tmp, r_dv, r_xb], [r_xb])
        self.ld(self.XRES[s][:, lo:hi].rearrange("(k p) n -> p k n", p=128), xb[:, :, 0:n], [r_xb], [self.r_xres[s]])

    def pn_tiles(self):
        sq, r_sq = self.tile([128, KC, 512], BF16, "sqP")
        rs, r_rs = self.tile([128, 512], F32, "rsP")
        tmp, r_tmp = self.tile([128, 512], F32, "tmpP")
        xb, r_xb = self.tile([128, KC, 512], F32, "xbP")
        return (sq, r_sq, rs, r_rs, tmp, r_tmp, xb, r_xb)

    def merge(self, s, l, last):
        self.phase()
        ysb, r_ysb = self.tile([128, 8, 512], BF16, "ysb")
        wg = [self.tile([128, 4, KC, 128], BF16, "wg%d" % i) for i in range(2)]
        wbr = [self.tile([128, 4, 2, 128], BF16, "wbr%d" % i) for i in range(2)]
        sig, r_sig = self.tile([128, 512], F32, "sig")
        accf, r_accf = self.tile([128, 512], F32, "accf")
        tmpm, r_tmpm = self.tile([128, 512], F32, "tmpm")
        acc, r_acc = self.tile([128, KC, 512], BF16, "acc")
        wo, r_wo = self.tile([128, KC, D], BF16, "wo")
        yT, r_yT = self.tile([128, KC, 512], F32, "yTm")
        tl = self.pn_tiles()
        self.ld(wo, self.WO[l].rearrange("p (k n) -> p k n", k=KC), [self.r_wbf], [r_wo])
        it = 0
        for (lo, hi) in self.blocks:
            if last and lo < self.NCTX:
                continue
            n = hi - lo
            self.ld(ysb[:, :, 0:n], self.YS[s][:, lo:hi].rearrange("(k p) n -> p k n", p=128), [self.r_ys[s]], [r_ysb])
            for dc in range(KC):
                (wg_, r_wg), (wb_, r_wb) = wg[it % 2], wbr[it % 2]
                it += 1
                for br in range(4):
                    self.ld(wg_[:, br], self.WG[l, br, dc].rearrange("p (k n) -> p k n", k=KC), [self.r_wbf], [r_wg])
                    self.ld(wb_[:, br], self.WBR[l, br].rearrange("p (k n) -> p k n", k=2)[:, :, dc * 128:(dc + 1) * 128], [self.r_wbf], [r_wb])
                for br in range(4):
                    bg, bp = br % 2, 2 + br % 2
                    for k in range(KC):
                        self.mm(self.bank(bg, n), wg_[:, br, k, :], self.hT[:, k, lo:hi], k == 0, k == KC - 1, [r_wg, self.r_hT], [self.r_ps[bg]])
                    self.act(sig[:, 0:n], self.bank(bg, n), AF.Sigmoid, [self.r_ps[bg], self.r_pv], [r_sig], bias=self.pv["b_gate"][:, l, br, dc:dc + 1], scale=1.0)
                    for k in range(2):
                        self.mm(self.bank(bp, n), wb_[:, br, k, :], ysb[:, br * 2 + k, 0:n], k == 0, k == 1, [r_wb, r_ysb], [self.r_ps[bp]])
                    if br == 0:
                        self.tt(accf[:, 0:n], sig[:, 0:n], self.bank(bp, n), ALU.mult, [r_sig, self.r_ps[bp]], [r_accf])
                    else:
                        self.tt(tmpm[:, 0:n], sig[:, 0:n], self.bank(bp, n), ALU.mult, [r_sig, self.r_ps[bp]], [r_tmpm])
                        if br < 3:
                            self.tt(accf[:, 0:n], accf[:, 0:n], tmpm[:, 0:n], ALU.add, [r_accf, r_tmpm], [r_accf])
                        else:
                            self.tt(acc[:, dc, 0:n], accf[:, 0:n], tmpm[:, 0:n], ALU.add, [r_accf, r_tmpm], [r_acc])
            for d2 in range(KC):
                b = 4 + d2 % 2
                for k in range(KC):
                    self.mm(self.bank(b, n), wo[:, k, d2 * 128:(d2 + 1) * 128], acc[:, k, 0:n], k == 0, k == KC - 1, [r_wo, r_acc], [self.r_ps[b]])
                self.cp(yT[:, d2, 0:n], self.bank(b, n), [self.r_ps[b]], [r_yT], eng="act" if d2 % 2 else "dve")
            self.post_norm_res(s, lo, hi, yT, r_yT, 2, tl)

    def ffn(self, s, l, last):
        self.phase()
        T = self.T
        wd, r_wd = self.tile([128, FC, D], BF16, "wd")
        wu = [self.tile([128, KC, 256], BF16, "wu%d" % i) for i in range(2)]
        gT, r_gT = self.tile([128, FC, 512], BF16, "gT")
        a_sb, r_a = self.tile([128, 514], F32, "a_sb")
        c1, r_c1 = self.tile([128, 512], F32, "c1F")
        yT, r_yT = self.tile([128, KC, 512], F32, "yTf")
        tl = self.pn_tiles()
        self.ld(wd, self.WD[l].rearrange("p (f n) -> p f n", f=FC), [self.r_wbf], [r_wd])
        wcv, bcv = self.pv["w_ffn_conv"], self.pv["b_ffn_conv"]
        it = 0
        for (lo, hi) in self.blocks:
            if last and lo < self.NCTX:
                continue
            n = hi - lo
            seg_lo, seg_hi = (0, self.NCTX) if lo < self.NCTX else (self.NCTX, T)
            for fc in range(FC):
                w_, r_w = wu[it % 2]
                it += 1
                self.ld(w_, self.WU[l, fc].rearrange("p (k n) -> p k n", k=KC), [self.r_wbf], [r_w])
                ba, bv = fc % 2, 2 + fc % 2
                for k in range(KC):
                    self.mm(self.bank(ba, n), w_[:, k, 0:128], self.hT[:, k, lo:hi], k == 0, k == KC - 1, [r_w, self.r_hT], [self.r_ps[ba]])
                for k in range(KC):
                    self.mm(self.bank(bv, n), w_[:, k, 128:256], self.hT[:, k, lo:hi], k == 0, k == KC - 1, [r_w, self.r_hT], [self.r_ps[bv]])
                self.P.op("dve", lambda e: e.memset(a_sb[:, 0:1], 0.0), [], [r_a])
                self.P.op("dve", lambda e: e.memset(a_sb[:, n + 1:n + 2], 0.0), [], [r_a])
                if lo > seg_lo:
                    for k in range(KC):
                        self.mm(self.bank(6, 1), w_[:, k, 0:128], self.hT[:, k, lo - 1:lo], k == 0, k == KC - 1, [r_w, self.r_hT], [self.r_ps[6]])
                    self.cp(a_sb[:, 0:1], self.bank(6, 1), [self.r_ps[6]], [r_a])
                if hi < seg_hi:
                    for k in range(KC):
                        self.mm(self.bank(6, 1, 8), w_[:, k, 0:128], self.hT[:, k, hi:hi + 1], k == 0, k == KC - 1, [r_w, self.r_hT], [self.r_ps[6]])
                    self.cp(a_sb[:, n + 1:n + 2], self.bank(6, 1, 8), [self.r_ps[6]], [r_a])
                self.cp(a_sb[:, 1:n + 1], self.bank(ba, n), [self.r_ps[ba]], [r_a], eng="act")
                self.act(self.bank(ba, n), self.bank(ba, n), AF.Copy, [self.r_ps[ba], self.r_pv], [self.r_ps[ba]], scale=wcv[:, l, 1, fc:fc + 1])
                self.stt(self.bank(ba, n), a_sb[:, 0:n], wcv[:, l, 0, fc:fc + 1], self.bank(ba, n), ALU.mult, ALU.add, [r_a, self.r_pv, self.r_ps[ba]], [self.r_ps[ba]])
                self.stt(c1[:, 0:n], a_sb[:, 2:n + 2], wcv[:, l, 2, fc:fc + 1], self.bank(ba, n), ALU.mult, ALU.add, [r_a, self.r_pv, self.r_ps[ba]], [r_c1])
                self.act(c1[:, 0:n], c1[:, 0:n], AF.Silu, [r_c1, self.r_pv], [r_c1], bias=bcv[:, l, fc:fc + 1], scale=1.0)
                self.tt(gT[:, fc, 0:n], c1[:, 0:n], self.bank(bv, n), ALU.mult, [r_c1, self.r_ps[bv]], [r_gT])
            for d2 in range(KC):
                b = 4 + d2 % 2
                for fc in range(FC):
                    self.mm(self.bank(b, n), wd[:, fc, d2 * 128:(d2 + 1) * 128], gT[:, fc, 0:n], fc == 0, fc == FC - 1, [r_wd, r_gT], [self.r_ps[b]])
                self.cp(yT[:, d2, 0:n], self.bank(b, n), [self.r_ps[b]], [r_yT], eng="act" if d2 % 2 else "dve")
            self.post_norm_res(s, lo, hi, yT, r_yT, 5, tl)


NLAT_FULL, NCTX_FULL, DEPTH = 2048, 256, 2
_cache = {}


def run_device(inp, NLAT, NCTX, B, L, n_cores):
    NSEQ = B // n_cores
    consts = make_consts(NLAT, NCTX)
    key = (NLAT, NCTX, NSEQ, L)
    bld = Builder(NLAT, NCTX, NSEQ, L, consts)
    nc = bld.build()
    x = np.asarray(inp["x"], np.float32)
    ctx = np.asarray(inp["ctx"], np.float32)
    c = np.asarray(inp["c"], np.float32)
    c_ctx = np.asarray(inp["c_ctx"], np.float32)
    params = layout_params(inp, L)
    wml = np.asarray(inp["w_ml_conv"], np.float32)
    params["wconvB"] = np.ascontiguousarray(wml.reshape(L, 3, 8, 64).transpose(0, 3, 2, 1))
    shared = {k: np.ascontiguousarray(np.asarray(inp[k], np.float32)) for k in W_SHAPES}
    shared.update(params)
    shared.update(consts)
    in_maps = []
    for ci in range(n_cores):
        sl = slice(ci * NSEQ, (ci + 1) * NSEQ)
        cc = np.concatenate([c[sl], c_ctx[None]], 0)
        m = dict(shared)
        m["xT"] = np.ascontiguousarray(x[sl].transpose(0, 2, 1))
        m["ctxT"] = np.ascontiguousarray(ctx[sl].transpose(0, 2, 1))
        m["ccT"] = np.ascontiguousarray(cc.T.reshape(KC, 128, NSEQ + 1).transpose(1, 0, 2))
        in_maps.append(m)
    return nc, in_maps


def kernel(**inp):
    n_cores = 8
    nc, in_maps = run_device(inp, NLAT_FULL, NCTX_FULL, 16, DEPTH, n_cores)
    res = run_bass_kernel_spmd(nc, in_maps, core_ids=list(range(n_cores)))
    outs = [np.asarray(r["outT"]).transpose(0, 2, 1) for r in res.results]
    return np.ascontiguousarray(np.concatenate(outs, 0).astype(np.float32))
```

```python
from contextlib import ExitStack
import numpy as np
import ml_dtypes
import concourse.bass as bass
import concourse.mybir as mybir
from concourse.bass_utils import run_bass_kernel_spmd

F32 = mybir.dt.float32
BF16 = mybir.dt.bfloat16
AF = mybir.ActivationFunctionType
ALU = mybir.AluOpType
AX = mybir.AxisListType
ENG = ("pe", "act", "dve", "pool", "sp")
NBIG = -30000.0
D = 1024
KC = 8
DFF = 2816
FC = 22
EPS = 1e-6


class Res:
    __slots__ = ("name", "w", "re", "rd")

    def __init__(self, name=""):
        self.name = name
        self.w = None
        self.re = {}
        self.rd = []


class Prog:
    N_DMA_SEMS = 80

    def __init__(self, nc, stack):
        self.nc = nc
        self.esem = {e: stack.enter_context(nc.semaphore("es_" + e)) for e in ENG}
        self.dsem = [stack.enter_context(nc.semaphore("ds%d" % i)) for i in range(self.N_DMA_SEMS)]
        self.dval = [0] * self.N_DMA_SEMS
        self.dnext = 0
        self.cnt = {e: 0 for e in ENG}
        self.seen = {e: {} for e in ENG}
        self.q = {e: [] for e in ENG}
        self.n_ops = 0

    def _deps(self, reads, writes):
        deps = []
        for r in reads:
            if r.w is not None:
                deps.append(r.w)
        for w in writes:
            if w.w is not None:
                deps.append(w.w)
            for e, c in w.re.items():
                deps.append(("E", e, c))
            deps.extend(w.rd)
        return deps

    def _waits(self, eng, deps):
        seen = self.seen[eng]
        need = {}
        for kind, key, val in deps:
            k = (kind, key)
            if seen.get(k, 0) >= val:
                continue
            if kind == "E" and key == "pe" and eng == "pe":
                continue
            if need.get(k, 0) < val:
                need[k] = val
        out = []
        for k, val in need.items():
            seen[k] = val
            sem = self.esem[k[1]] if k[0] == "E" else self.dsem[k[1]]
            out.append((sem, val))
        return out

    def op(self, eng, fn, reads=(), writes=()):
        waits = self._waits(eng, self._deps(reads, writes))
        self.cnt[eng] += 1
        c = self.cnt[eng]
        tok = ("E", eng, c)
        self.q[eng].append((waits, fn, (self.esem[eng], 1)))
        for r in reads:
            if r.re.get(eng, 0) < c:
                r.re[eng] = c
        for w in writes:
            w.w = tok
            w.re = {}
            w.rd = []
        self.n_ops += 1
        return tok

    def dma(self, qeng, out, in_, reads=(), writes=(), **kw):
        deps = self._deps(reads, writes)
        i = self.dnext
        self.dnext = (self.dnext + 1) % self.N_DMA_SEMS
        prev = self.dval[i]
        if prev:
            deps.append(("D", i, prev))
        waits = self._waits(qeng, deps)
        self.dval[i] = prev + 16
        tok = ("D", i, prev + 16)
        self.q[qeng].append((waits, (lambda e, o=out, s=in_, k=kw: e.dma_start(out=o, in_=s, **k)),
                             (self.dsem[i], 16)))
        for r in reads:
            r.rd.append(tok)
        for w in writes:
            w.w = tok
            w.re = {}
            w.rd = []
        self.n_ops += 1
        return tok

    def barrier(self):
        deps = [("E", e, self.cnt[e]) for e in ENG if self.cnt[e]]
        deps += [("D", i, v) for i, v in enumerate(self.dval) if v]
        for e in ENG:
            waits = self._waits(e, deps)
            if waits:
                self.q[e].append((waits, None, None))

    def emit(self):
        nc = self.nc
        allsems = [self.esem[e] for e in ENG] + self.dsem
        with nc.Block("init") as b0:
            @b0.vector
            def _(v):
                for s in allsems:
                    v.sem_clear(s)
        with nc.Block("main") as blk:
            def run(e, name):
                for waits, fn, inc in self.q[name]:
                    for sem, val in waits:
                        e.wait_ge(sem, val)
                    if fn is not None:
                        fn(e).then_inc(inc[0], inc[1])

            @blk.tensor
            def _(e):
                run(e, "pe")

            @blk.scalar
            def _(e):
                run(e, "act")

            @blk.vector
            def _(e):
                run(e, "dve")

            @blk.gpsimd
            def _(e):
                run(e, "pool")

            @blk.sync
            def _(e):
                run(e, "sp")


def _rope_tab(rd, pos_row, pos_col, n_ctx):
    half = rd // 2
    nf = half // 2
    inv = 10000.0 ** (-np.arange(nf, dtype=np.float64) / nf)
    n = len(pos_row)
    cos = np.ones((rd, n_ctx + n), np.float64)
    sin = np.zeros((rd, n_ctx + n), np.float64)
    for r in range(rd):
        hh, rr = r // half, r % half
        idx = rr % nf
        pos = pos_row if hh == 0 else pos_col
        ang = pos.astype(np.float64) * inv[idx]
        ang = (pos.astype(np.float32) * np.float32(inv[idx]).astype(np.float32)).astype(np.float64)
        cos[r, n_ctx:] = np.cos(ang)
        sin[r, n_ctx:] = np.sin(ang)
    return cos, sin


def make_consts(NLAT, NCTX):
    bf = ml_dtypes.bfloat16
    T = NLAT + NCTX
    c = {}
    c["ident_bf"] = np.eye(128).astype(bf)
    c["ones_bf"] = np.ones((128, 128)).astype(bf)
    c["ident_f"] = np.eye(128, dtype=np.float32)
    c["ones_f"] = np.ones((128, 128), np.float32)
    s = np.arange(128)[:, None]
    t = np.arange(128)[None, :]
    c["tri_f"] = (s <= t).astype(np.float32)
    c["tri_b"] = (s >= t).astype(np.float32)
    mf = np.where(t <= s, 0.0, NBIG).astype(np.float32)
    mb = np.where(t >= s, 0.0, NBIG).astype(np.float32)
    c["mneg_f"] = np.repeat(mf[:, None, :], 4, axis=1).copy()
    c["mneg_b"] = np.repeat(mb[:, None, :], 4, axis=1).copy()
    el = np.zeros((128, 128), np.float32); el[127, :] = 1
    ef = np.zeros((128, 128), np.float32); ef[0, :] = 1
    c["e_last"] = el
    c["e_first"] = ef
    c["maskA"] = np.where(t >= s, 0.0, NBIG).astype(bf)
    c["maskB"] = np.where(t <= s, 0.0, NBIG).astype(bf)
    c["maskN"] = np.full((128, 128), NBIG).astype(bf)
    rows = NLAT // 64
    pr = np.repeat(np.arange(rows), 64)
    pc = np.tile(np.arange(64), rows)
    ca, sa = _rope_tab(32, pr, pc, NCTX)
    sc_a = 96.0 ** -0.5
    cosq = np.ones((96, T)); sinq = np.zeros((96, T))
    cosq[64:] = ca; sinq[64:] = sa
    c["cosq_a"] = (cosq * sc_a).astype(np.float32)
    c["sinq_a"] = (sinq * sc_a).astype(np.float32)
    c["cosk_a"] = ca.astype(np.float32)
    c["sink_a"] = sa.astype(np.float32)
    cc, sc = _rope_tab(64, pr, pc, NCTX)
    c["cos_c"] = cc.astype(np.float32)
    c["sin_c"] = sc.astype(np.float32)
    j = np.arange(64)
    Cc = np.cos(2 * np.pi * np.outer(j, j) / 64)
    Sc = np.sin(2 * np.pi * np.outer(j, j) / 64)
    z = np.zeros((64, 64))
    c["bdc"] = np.block([[Cc, z], [z, Cc]]).astype(bf)
    c["bds"] = (-np.block([[Sc, z], [z, Sc]])).astype(bf)
    for nm, N in (("lat", NLAT), ("ctx", NCTX)):
        n = np.arange(N)
        ph = (np.outer(n, n) % N).astype(np.float64) * (2 * np.pi / N)
        nrm = 1.0 / np.sqrt(N * 64.0)
        c["cn_" + nm] = (np.cos(ph) * nrm).astype(bf)
        c["sn_" + nm] = (np.sin(ph) * nrm).astype(bf)
    return c


CONST_DT = {"ident_bf": BF16, "ones_bf": BF16, "maskA": BF16, "maskB": BF16, "maskN": BF16, "bdc": BF16, "bds": BF16,
            "cn_lat": BF16, "sn_lat": BF16, "cn_ctx": BF16, "sn_ctx": BF16}

W_SHAPES = {
    "w_mod": (D, 6 * D), "w_in": (D, 2352), "w_uq": (256, 384), "w_ukv": (256, 512),
    "w_gate": (4, D, D), "w_branch": (4, 256, D), "w_out": (D, D), "w_up": (D, 2 * DFF), "w_down": (DFF, D),
}


def layout_params(inp, L):
    o = {}

    def fm(v, k):
        v = np.asarray(v, np.float32)
        lead = v.shape[:-1]
        return np.ascontiguousarray(np.moveaxis(v.reshape(lead + (k, 128)), -1, 0))

    o["b_mod"] = np.stack([fm(inp["b_mod"][l], 48) for l in range(L)])
    for nm in ("g_pre_mix", "g_post_mix", "g_pre_ffn", "g_post_ffn"):
        o[nm] = np.stack([fm(inp[nm][l], 8) for l in range(L)])
    o["g_qa"] = np.stack([fm(inp["g_qa"][l], 2) for l in range(L)])
    o["g_kva"] = np.stack([fm(inp["g_kva"][l], 2) for l in range(L)])
    o["b_gate"] = np.stack([fm(inp["b_gate"][l], 8) for l in range(L)])
    o["w_ffn_conv"] = np.stack([fm(inp["w_ffn_conv"][l], FC) for l in range(L)])
    o["b_ffn_conv"] = np.stack([fm(inp["b_ffn_conv"][l], FC) for l in range(L)])
    o["w_ml_conv"] = np.stack([fm(inp["w_ml_conv"][l], 4) for l in range(L)])
    o["b_ml_gates"] = np.asarray(inp["b_ml_gates"], np.float32).reshape(L, 1, 16)
    o["wg_sink"] = np.asarray(inp["wg_sink"], np.float32).reshape(L, 1, 4)
    return o


PARAM_SHAPES = lambda L: {
    "b_mod": (L, 128, 48), "g_pre_mix": (L, 128, 8), "g_post_mix": (L, 128, 8), "g_pre_ffn": (L, 128, 8),
    "g_post_ffn": (L, 128, 8), "g_qa": (L, 128, 2), "g_kva": (L, 128, 2), "b_gate": (L, 128, 4, 8),
    "w_ffn_conv": (L, 128, 3, FC), "b_ffn_conv": (L, 128, FC), "w_ml_conv": (L, 128, 3, 4),
    "b_ml_gates": (L, 1, 16), "wg_sink": (L, 1, 4), "wconvB": (L, 64, 8, 3),
}


class Builder:
    def __init__(self, NLAT, NCTX, NSEQ, L, consts, dbg=None):
        self.NLAT, self.NCTX, self.NSEQ, self.L = NLAT, NCTX, NSEQ, L
        self.T = T = NLAT + NCTX
        self.TT = T // 128
        self.CT = NCTX // 128
        self.blocks = [(0, NCTX)] + [(NCTX + i, min(NCTX + i + 512, T)) for i in range(0, NLAT, 512)]
        self.dbg = dbg
        nc = self.nc = bass.Bass("TRN2", target_bir_lowering=False)
        self.st = ExitStack()
        self.P = Prog(nc, self.st)
        di = lambda n, s, dt=F32: nc.dram_tensor(n, list(s), dt, kind="ExternalInput").ap()
        self.xT_in = di("xT", (NSEQ, D, NLAT))
        self.cT_in = di("ctxT", (NSEQ, D, NCTX))
        self.ccT = di("ccT", (128, KC, NSEQ + 1))
        self.W = {k: di(k, (L,) + v) for k, v in W_SHAPES.items()}
        self.PR = {k: di(k, v) for k, v in PARAM_SHAPES(L).items()}
        self.CD = {k: di(k, v.shape, CONST_DT.get(k, F32)) for k, v in consts.items()}
        self.out = nc.dram_tensor("outT", [NSEQ, D, NLAT], F32, kind="ExternalOutput").ap()
        ds = lambda n, s, dt: nc.dram_tensor(n, list(s), dt, kind="Internal").ap()
        self.XRES = ds("xres", (NSEQ, D, T), F32)
        self.YS = ds("ys", (NSEQ, D, T), BF16)
        self.WI = ds("wi_bf", (L, 128, KC * 2352), BF16)
        self.WG = ds("wg_bf", (L, 4, KC, 128, KC * 128), BF16)
        self.WBR = ds("wbr_bf", (L, 4, 128, 2 * D), BF16)
        self.WO = ds("wo_bf", (L, 128, KC * D), BF16)
        self.WU = ds("wu_bf", (L, FC, 128, KC * 256), BF16)
        self.WD = ds("wd_bf", (L, 128, FC * D), BF16)
        self.r_wbf = Res("wbf")
        self.r_xres = [Res("xres%d" % i) for i in range(NSEQ)]
        self.r_ys = [Res("ys%d" % i) for i in range(NSEQ)]
        self.r_out = Res("out")
        if dbg:
            self.dbg_out = {k: nc.dram_tensor("dbg_" + k, list(s), dt, kind="ExternalOutput").ap() for k, (s, dt) in dbg.items()}
        self.PS = nc.alloc_psum_tensor("PS", [128, 4096], F32)
        self.r_ps = [Res("ps%d" % i) for i in range(8)]
        self.ARENA_E = 98000
        self.arena = nc.alloc_sbuf_tensor("arena", [128, self.ARENA_E], BF16)
        self.a_off = 0
        self.a_base = 0
        self.phase_log = []

    def tile(self, shape, dt, name=""):
        n = int(np.prod(shape[1:]))
        ne = n * (2 if dt == F32 else 1)
        off = (self.a_off + 15) // 16 * 16
        assert off + ne <= self.ARENA_E, ("arena overflow", name, off, ne)
        self.a_off = off + ne
        ap = self.arena[0:shape[0], off:off + ne]
        if dt == F32:
            ap = ap.bitcast(F32)
        if len(shape) == 3:
            ap = ap.rearrange("p (a b) -> p a b", a=shape[1])
        elif len(shape) == 4:
            ap = ap.rearrange("p (a b c) -> p a b c", a=shape[1], b=shape[2])
        return ap, Res(name)

    def phase(self):
        self.P.barrier()
        self.a_off = self.a_base
        import sys
        self.phase_log.append((sys._getframe(1).f_code.co_name, dict(self.P.cnt)))

    def bank(self, b, n=512, lo=0):
        return self.PS[:, b * 512 + lo:b * 512 + lo + n]

    def bank_bf(self, b):
        return self.PS[:, b * 512:(b + 1) * 512].bitcast(BF16)

    def mm(self, out, lhsT, rhs, start, stop, reads, writes):
        self.P.op("pe", lambda e: e.matmul(out, lhsT, rhs, start=start, stop=stop, skip_group_check=True), reads, writes)

    def tr(self, out, in_, ident, reads, writes):
        self.P.op("pe", lambda e: e.transpose(out, in_, ident), reads, writes)

    def act(self, out, in_, func, reads, writes, bias=None, scale=None):
        kw = {}
        if bias is not None:
            kw["bias"] = bias
        if scale is not None:
            kw["scale"] = scale
        self.P.op("act", lambda e: e.activation(out=out, in_=in_, func=func, **kw), reads, writes)

    def tt(self, out, a, b, op, reads, writes, eng="dve"):
        self.P.op(eng, lambda e: e.tensor_tensor(out=out, in0=a, in1=b, op=op), reads, writes)

    def ts(self, out, a, s1, op0, reads, writes, s2=None, op1=None, eng="dve"):
        if op1 is None:
            self.P.op(eng, lambda e: e.tensor_scalar(out=out, in0=a, scalar1=s1, scalar2=None, op0=op0), reads, writes)
        else:
            self.P.op(eng, lambda e: e.tensor_scalar(out=out, in0=a, scalar1=s1, scalar2=s2, op0=op0, op1=op1), reads, writes)

    def stt(self, out, a, s, b, op0, op1, reads, writes):
        self.P.op("dve", lambda e: e.scalar_tensor_tensor(out=out, in0=a, scalar=s, in1=b, op0=op0, op1=op1), reads, writes)

    def cp(self, out, in_, reads, writes, eng="dve"):
        if eng == "act":
            self.P.op("act", lambda e: e.copy(out=out, in_=in_), reads, writes)
        else:
            self.P.op(eng, lambda e: e.tensor_copy(out=out, in_=in_), reads, writes)

    def red(self, out, in_, op, reads, writes):
        self.P.op("dve", lambda e: e.tensor_reduce(out=out, in_=in_, axis=AX.X, op=op), reads, writes)

    def recip(self, out, in_, reads, writes):
        self.P.op("dve", lambda e: e.reciprocal(out=out, in_=in_), reads, writes)

    def ld(self, out, in_, reads, writes, q="sp"):
        self.P.dma(q, out, in_, reads, writes)

    def prepass(self):
        SZ = 2560
        stf = [self.tile([128, SZ], F32, "stf%d" % i) for i in range(3)]
        stb = [self.tile([128, SZ], BF16, "stb%d" % i) for i in range(3)]
        pieces = []

        def piece(srcs, dst, a, b):
            pieces.append((srcs, dst, a, b))

        def views(j):
            srcs, dst, a, b = pieces[j]
            (f, r_f), (bt, r_b) = stf[j % 3], stb[j % 3]
            fv = f[:, 0:a * b].rearrange("p (a b) -> p a b", a=a)
            bv = bt[:, 0:a * b].rearrange("p (a b) -> p a b", a=a)
            return srcs, dst, fv, bv, r_f, r_b

        def emit_in(j):
            srcs, dst, fv, bv, r_f, r_b = views(j)
            for (c0, c1, src) in srcs:
                self.ld(fv[:, :, c0:c1], src, [], [r_f])

        def emit_rest(j):
            srcs, dst, fv, bv, r_f, r_b = views(j)
            self.cp(bv, fv, [r_f], [r_b], eng=("act", "dve", "pool")[j % 3])
            self.ld(dst, bv, [r_b], [self.r_wbf])

        for l in range(self.L):
            wi = self.W["w_in"][l].rearrange("(k p) n -> p k n", p=128)
            wid = self.WI[l].rearrange("p (k n) -> p k n", k=KC)
            for k in range(KC):
                piece([(0, 2352, wi[:, k:k + 1, :])], wid[:, k:k + 1, :], 1, 2352)
            for br in range(4):
                for dc in range(KC):
                    src = self.W["w_gate"][l][br][:, dc * 128:(dc + 1) * 128].rearrange("(k p) n -> p k n", p=128)
                    piece([(0, 128, src)], self.WG[l, br, dc].rearrange("p (k n) -> p k n", k=KC), KC, 128)
                src = self.W["w_branch"][l][br].rearrange("(k p) n -> p k n", p=128)
                piece([(0, D, src)], self.WBR[l, br].rearrange("p (k n) -> p k n", k=2), 2, D)
            wo = self.W["w_out"][l].rearrange("(k p) n -> p k n", p=128)
            wod = self.WO[l].rearrange("p (k n) -> p k n", k=KC)
            for k in range(0, KC, 2):
                piece([(0, D, wo[:, k:k + 2, :])], wod[:, k:k + 2, :], 2, D)
            for fc in range(FC):
                sa = self.W["w_up"][l][:, fc * 128:(fc + 1) * 128].rearrange("(k p) n -> p k n", p=128)
                sv = self.W["w_up"][l][:, DFF + fc * 128:DFF + (fc + 1) * 128].rearrange("(k p) n -> p k n", p=128)
                piece([(0, 128, sa), (128, 256, sv)], self.WU[l, fc].rearrange("p (k n) -> p k n", k=KC), KC, 256)
            wdn = self.W["w_down"][l].rearrange("(f p) n -> p f n", p=128)
            wdd = self.WD[l].rearrange("p (f n) -> p f n", f=FC)
            for f0 in range(0, FC, 2):
                piece([(0, D, wdn[:, f0:f0 + 2, :])], wdd[:, f0:f0 + 2, :], 2, D)
        npc = len(pieces)
        for j in range(npc + 2):
            if j < npc:
                emit_in(j)
            if j - 2 >= 0:
                emit_rest(j - 2)
            yield

    def build(self):
        P = self.P
        NSEQ, L, T = self.NSEQ, self.L, self.T
        C = {}
        self.C = C
        r_c = self.r_c = Res("consts")
        for k in ("ident_bf", "ones_bf", "ident_f", "ones_f", "tri_f", "tri_b", "e_last", "e_first", "maskA", "maskB", "maskN", "bdc", "bds"):
            C[k], _ = self.tile([128, 128], CONST_DT.get(k, F32), k)
            self.ld(C[k], self.CD[k], [], [r_c])
        for k in ("mneg_f", "mneg_b"):
            C[k], _ = self.tile([128, 4, 128], F32, k)
            self.ld(C[k], self.CD[k], [], [r_c])
        self.hT, self.r_hT = self.tile([128, KC, T], BF16, "hT")
        self.MOD, self.r_mod = self.tile([128, L, 48, NSEQ + 1], F32, "MOD")
        self.pv = {}
        self.r_pv = Res("pvec")
        for k, shp in PARAM_SHAPES(L).items():
            if shp[1] == 128:
                self.pv[k], _ = self.tile([128, L] + list(shp[2:]) if len(shp) > 2 else [128, L], F32, k)
                src = self.PR[k]
                if len(shp) == 3:
                    self.ld(self.pv[k], src.rearrange("l p a -> p l a"), [], [self.r_pv])
                else:
                    for l in range(L):
                        self.ld(self.pv[k][:, l], src[l], [], [self.r_pv])
        self.bg_b, _ = self.tile([128, L, 16], F32, "bgb")
        self.sink_b, _ = self.tile([128, L, 4], F32, "sinkb")
        for l in range(L):
            self.ld(self.bg_b[:, l, :], self.PR["b_ml_gates"][l].partition_broadcast(128), [], [self.r_pv])
            self.ld(self.sink_b[:, l, :], self.PR["wg_sink"][l].partition_broadcast(128), [], [self.r_pv])
        self.wconvB, _ = self.tile([128, 8, 3], F32, "wconvB")
        self.r_wcb = Res("wcb")
        self.dv, self.r_dv = self.tile([128, 6, KC], F32, "derived")
        self.dvc, self.r_dvc = self.tile([128, 6, KC], F32, "derivedc")
        self.a_base = self.a_off
        for s in range(NSEQ):
            self.ld(self.XRES[s][:, 0:self.NCTX], self.cT_in[s], [], [self.r_xres[s]])
            self.ld(self.XRES[s][:, self.NCTX:T], self.xT_in[s], [], [self.r_xres[s]])
        self.phase()
        pg = self.prepass()
        self.mod_phase(pg)
        for _ in pg:
            pass
        import os
        stop = int(os.environ.get("KSTOP", "99"))
        for s in range(NSEQ):
            for l in range(L):
                last = (l == L - 1)
                steps = [lambda: self.derive(l, s), lambda: self.norm_mod(s, 0), lambda: self.branch_a(s, l, last),
                         lambda: self.branch_c(s, l, last), lambda: None, lambda: self.branch_b(s, l, last),
                         lambda: self.merge(s, l, last), lambda: self.norm_mod(s, 3, skip_ctx=last), lambda: self.ffn(s, l, last)]
                for i, f in enumerate(steps):
                    if i < stop:
                        f()
            self.phase()
            self.ld(self.out[s], self.XRES[s][:, self.NCTX:T], [self.r_xres[s]], [self.r_out])
        P.barrier()
        P.emit()
        self.st.close()
        return self.nc

    def mod_phase(self, pg):
        next(pg, None)
        NJ = self.NSEQ + 1
        cc, r_cc = self.tile([128, KC, NJ], F32, "cc")
        sg, r_sg = self.tile([128, KC, NJ], F32, "sg")
        self.ld(cc, self.ccT, [], [r_cc])
        self.act(sg, cc, AF.Sigmoid, [r_cc], [r_sg])
        self.tt(cc, cc, sg, ALU.mult, [r_sg, r_cc], [r_cc])
        wm = [self.tile([128, KC, 512], F32, "wm%d" % i) for i in range(2)]
        n = 0
        for l in range(self.L):
            for g in range(12):
                w, r_w = wm[n % 2]
                n += 1
                self.ld(w, self.W["w_mod"][l][:, g * 512:(g + 1) * 512].rearrange("(k p) n -> p k n", p=128), [], [r_w])
                for _ in range(7):
                    next(pg, None)
                b = n % 2
                for j4 in range(4):
                    for k in range(KC):
                        self.mm(self.bank(b, NJ, j4 * 8), w[:, k, j4 * 128:(j4 + 1) * 128], cc[:, k, :], k == 0 and j4 == 0, k == KC - 1,
                                [r_w, r_cc], [self.r_ps[b]])
                for j4 in range(4):
                    ch = g * 4 + j4
                    self.ts(self.MOD[:, l, ch, :], self.bank(b, NJ, j4 * 8), self.pv["b_mod"][:, l, ch:ch + 1], ALU.add,
                            [self.r_ps[b], self.r_pv], [self.r_mod])

    def derive(self, l, s):
        for (dst, r_dst, j) in ((self.dv, self.r_dv, s), (self.dvc, self.r_dvc, self.NSEQ)):
            for half, gpre, gpost in ((0, "g_pre_mix", "g_post_mix"), (1, "g_pre_ffn", "g_post_ffn")):
                sh = self.MOD[:, l, half * 24 + 0:half * 24 + 8, j]
                sc = self.MOD[:, l, half * 24 + 8:half * 24 + 16, j]
                g = self.MOD[:, l, half * 24 + 16:half * 24 + 24, j]
                self.stt(dst[:, half * 3 + 0, :], sc, 1.0, self.pv[gpre][:, l, :], ALU.add, ALU.mult, [self.r_mod, self.r_pv], [r_dst])
                self.cp(dst[:, half * 3 + 1, :], sh, [self.r_mod], [r_dst])
                self.tt(dst[:, half * 3 + 2, :], g, self.pv[gpost][:, l, :], ALU.mult, [self.r_mod, self.r_pv], [r_dst])

    def seg_dv(self, lo):
        return (self.dvc, self.r_dvc) if lo < self.NCTX else (self.dv, self.r_dv)

    def rstd_block(self, src, r_src, n, nk, dim, sq, r_sq, rs, r_rs, bank):
        for k in range(nk):
            self.act(sq[:, k, 0:n], src[:, k, 0:n], AF.Square, [r_src], [r_sq])
        for k in range(nk):
            self.mm(self.bank(bank, n), self.C["ones_bf"], sq[:, k, 0:n], k == 0, k == nk - 1, [r_sq, self.r_c], [self.r_ps[bank]])
        self.ts(rs[:, 0:n], self.bank(bank, n), 1.0 / dim, ALU.mult, [self.r_ps[bank]], [r_rs], s2=EPS, op1=ALU.add)
        self.act(rs[:, 0:n], rs[:, 0:n], AF.Ln, [r_rs], [r_rs])
        self.act(self.bank(bank, n), rs[:, 0:n], AF.Exp, [r_rs], [self.r_ps[bank]], scale=-0.5)
        return self.bank(bank, n), self.r_ps[bank]

    def norm_mod(self, s, base, skip_ctx=False):
        self.phase()
        xb = [self.tile([128, KC, 512], F32, "xb%d" % i) for i in range(2)]
        sqs = [self.tile([128, KC, 512], BF16, "sq%d" % i) for i in range(2)]
        rss = [self.tile([128, 512], F32, "rs%d" % i) for i in range(2)]
        tmps = [self.tile([128, 512], F32, "tmp%d" % i) for i in range(2)]
        for bi, (lo, hi) in enumerate(self.blocks):
            if skip_ctx and lo < self.NCTX:
                continue
            n = hi - lo
            x, r_x = xb[bi % 2]
            (sq, r_sq), (rs, r_rs) = sqs[bi % 2], rss[bi % 2]
            self.ld(x[:, :, 0:n], self.XRES[s][:, lo:hi].rearrange("(k p) n -> p k n", p=128), [self.r_xres[s]], [r_x])
            rsp, r_rsp = self.rstd_block(x, r_x, n, KC, D, sq, r_sq, rs, r_rs, bi % 2)
            dv, r_dv = self.seg_dv(lo)
            for k in range(KC):
                tmp, r_tmp = tmps[k % 2]
                self.tt(tmp[:, 0:n], x[:, k, 0:n], rsp, ALU.mult, [r_x, r_rsp], [r_tmp])
                self.act(self.hT[:, k, lo:hi], tmp[:, 0:n], AF.Identity, [r_tmp, r_dv], [self.r_hT],
                         bias=dv[:, base + 1, k:k + 1], scale=dv[:, base + 0, k:k + 1])

    def load_w_cols(self, dst, r_dst, l, c0, c1):
        self.ld(dst, self.WI[l].rearrange("p (k n) -> p k n", k=KC)[:, :, c0:c1], [self.r_wbf], [r_dst])

    def make_perm(self, dst, src, nheads, hd, r0, rd, r_dst, r_src, nk):
        nf = rd // 4
        self.P.op("dve", lambda e: e.memset(dst, 0.0), [], [r_dst])
        for k in range(nk):
            for h in range(nheads):
                b = h * hd + r0
                for hh in range(2):
                    o = b + hh * 2 * nf
                    self.ts(dst[:, k, o:o + nf], src[:, k, o + nf:o + 2 * nf], -1.0, ALU.mult, [r_src], [r_dst])
                    self.cp(dst[:, k, o + nf:o + 2 * nf], src[:, k, o:o + nf], [r_src], [r_dst])

    def attn_scores(self, it, buf):
        Pm, r_Pm, sm, r_sm = buf
        kparts, sink, scale = it["kparts"], it["sink"], it["scale"]
        pc = it.get("pcol", 0)
        ncols = max(c0 + k.shape[-1] for (k, _, c0, _) in kparts)
        started = set()
        for (kT, r_k, c0, mask) in kparts:
            n = kT.shape[-1]
            b = (pc + c0) // 512
            assert (pc + c0 + n - 1) // 512 == b
            self.mm(self.PS[:, pc + c0:pc + c0 + n], it["q"], kT, b not in started, mask is None, [it["r_q"], r_k], [self.r_ps[b]])
            started.add(b)
            if mask is not None:
                self.mm(self.PS[:, pc + c0:pc + c0 + n], self.C["ident_bf"], mask, False, True, [self.r_c], [self.r_ps[b]])
        tot = ncols
        if sink is not None:
            b = (pc + ncols) // 512
            self.mm(self.PS[:, pc + ncols:pc + ncols + 1], self.C["ones_f"][0:1, :], sink, b not in started, True, [self.r_c, self.r_pv], [self.r_ps[b]])
            started.add(b)
            tot = ncols + 1
        banks = [self.r_ps[b] for b in sorted(started)]
        self.red(sm[:, 0:1], self.PS[:, pc:pc + tot], ALU.max, banks, [r_sm])
        self.ts(sm[:, 1:2], sm[:, 0:1], -scale, ALU.mult, [r_sm], [r_sm])
        self.act(Pm[:, 0:tot], self.PS[:, pc:pc + tot], AF.Exp, banks + [r_sm], [r_Pm], bias=sm[:, 1:2], scale=scale)
        it["ncols"] = ncols

    def attn_transposes(self, it, buf, PTt, r_PT):
        Pm, r_Pm, sm, r_sm = buf
        vparts = it["vparts"]
        nv = len(vparts)
        for i, (V, r_v, c0) in enumerate(vparts):
            tb = 5 + (i // 8) % 2
            slot = i % 8
            pt_ps = self.bank_bf(tb)[:, slot * 128:(slot + 1) * 128]
            self.tr(pt_ps, Pm[:, c0:c0 + 128], self.C["ident_bf"], [r_Pm, self.r_c], [self.r_ps[tb]])
            if slot == 7 or i == nv - 1:
                g0 = i - slot
                self.cp(PTt[:, g0:i + 1, :], self.bank_bf(tb)[:, 0:(slot + 1) * 128].rearrange("p (a b) -> p a b", b=128),
                        [self.r_ps[tb]], [r_PT], eng="dve" if (i // 8) % 2 == 0 else "act")

    def attn_pv(self, it, buf, PTt, r_PT):
        Pm, r_Pm, sm, r_sm = buf
        vparts, sink, ncols = it["vparts"], it["sink"], it["ncols"]
        nv = len(vparts)
        for i, (V, r_v, c0) in enumerate(vparts):
            self.mm(self.bank(7, 65), PTt[:, i, :], V, i == 0, i == nv - 1, [r_PT, r_v], [self.r_ps[7]])
        if sink is not None:
            self.tt(sm[:, 2:3], self.bank(7, 1, 64), Pm[:, ncols:ncols + 1], ALU.add, [self.r_ps[7], r_Pm], [r_sm])
            self.recip(sm[:, 3:4], sm[:, 2:3], [r_sm], [r_sm])
        else:
            self.recip(sm[:, 3:4], self.bank(7, 1, 64), [self.r_ps[7]], [r_sm])
        self.ts(it["out"], self.bank(7, 64), sm[:, 3:4], ALU.mult, [self.r_ps[7], r_sm], [it["r_out"]])
        if it.get("after"):
            it["after"]()

    def attn_run(self, items, pingpong=False, hook=None):
        if pingpong:
            for i, it in enumerate(items):
                it["pcol"] = (i % 2) * 1024
        bufs = []
        for j in range(2):
            Pm, r_Pm = self.tile([128, self.T + 128], BF16, "Pm%d" % j)
            sm, r_sm = self.tile([128, 4], F32, "sm%d" % j)
            bufs.append((Pm, r_Pm, sm, r_sm))
        PTt, r_PT = self.tile([128, self.TT, 128], BF16, "PT")
        n = len(items)
        if n == 0:
            return
        self.attn_scores(items[0], bufs[0])
        for i in range(n):
            if i + 1 < n:
                self.attn_scores(items[i + 1], bufs[(i + 1) % 2])
            self.attn_transposes(items[i], bufs[i % 2], PTt, r_PT)
            if hook is not None:
                hook()
            self.attn_pv(items[i], bufs[i % 2], PTt, r_PT)

    def store_y_tm(self, ytm, r_ytm, s, br, tile_i, ybuf):
        yT, r_yT = ybuf
        for c in range(2):
            self.tr(self.bank_bf(6)[:, c * 128:(c + 1) * 128], ytm[:, c * 128:(c + 1) * 128], self.C["ident_bf"], [r_ytm, self.r_c], [self.r_ps[6]])
        self.cp(yT, self.bank_bf(6)[:, 0:256].rearrange("p (a b) -> p a b", b=128), [self.r_ps[6]], [r_yT])
        self.ld(self.YS[s][br * 256:(br + 1) * 256, tile_i * 128:(tile_i + 1) * 128].rearrange("(c p) n -> p c n", p=128), yT,
                [r_yT], [self.r_ys[s]])

    def branch_a(self, s, l, last):
        self.phase()
        T, TT, CT = self.T, self.TT, self.CT
        wA, r_wA = self.tile([128, KC, 544], BF16, "wA")
        wAp, r_wAp = self.tile([128, KC, 32], BF16, "wAp")
        wq, r_wq = self.tile([128, 2, 384], BF16, "wq")
        wqf, r_wqf = self.tile([128, 2, 384], F32, "wqf")
        wqp, r_wqp = self.tile([128, 2, 384], BF16, "wqp")
        wkv, r_wkv = self.tile([128, 2, 512], BF16, "wkv")
        wkvf, r_wkvf = self.tile([128, 2, 512], F32, "wkvf")
        raw, r_raw = self.tile([128, 4, 512], F32, "raw")
        sq, r_sq = self.tile([128, 4, 512], BF16, "sqA")
        rs, r_rs = self.tile([128, 2, 512], F32, "rsA")
        cqn, r_cqn = self.tile([128, 4, 512], BF16, "cqn")
        tab, r_tab = self.tile([128, 4, 512], F32, "tabA")
        t1, r_t1 = self.tile([128, 512], F32, "t1A")
        t2, r_t2 = self.tile([128, 512], F32, "t2A")
        qT, r_qT = self.tile([128, 4, T], BF16, "qTA")
        kT, r_kT = self.tile([128, 4, T], BF16, "kTA")
        Va, r_Va = self.tile([128, TT, 4, 65], BF16, "VaA")
        ytms = [self.tile([128, 256], BF16, "ytmA%d" % i) for i in range(2)]
        ybuf = self.tile([128, 2, 128], BF16, "yTA")
        self.load_w_cols(wA, r_wA, l, 0, 544)
        self.ld(wqf, self.W["w_uq"][l].rearrange("(k p) n -> p k n", p=128), [], [r_wqf])
        self.ld(wkvf, self.W["w_ukv"][l].rearrange("(k p) n -> p k n", p=128), [], [r_wkvf])
        for k in range(2):
            self.ts(wq[:, k, :], wqf[:, k, :], self.pv["g_qa"][:, l, k:k + 1], ALU.mult, [r_wqf, self.r_pv], [r_wq])
            self.ts(wkv[:, k, :], wkvf[:, k, :], self.pv["g_kva"][:, l, k:k + 1], ALU.mult, [r_wkvf, self.r_pv], [r_wkv])
        self.make_perm(wqp, wq, 4, 96, 64, 32, r_wqp, r_wq, 2)
        wkr = wA[:, :, 512:544]
        self.make_perm(wAp, wkr, 1, 32, 0, 32, r_wAp, r_wA, KC)
        self.P.op("dve", lambda e: e.memset(Va, 1.0), [], [r_Va])
        for bi, (lo, hi) in enumerate(self.blocks):
            n = hi - lo
            for c in range(4):
                b = c % 2
                for k in range(KC):
                    self.mm(self.bank(b, n), wA[:, k, c * 128:(c + 1) * 128], self.hT[:, k, lo:hi], k == 0, k == KC - 1,
                            [r_wA, self.r_hT], [self.r_ps[b]])
                self.cp(raw[:, c, 0:n], self.bank(b, n), [self.r_ps[b]], [r_raw], eng="act")
            rp0 = self.rstd_block(raw[:, 0:2], r_raw, n, 2, 256, sq[:, 0:2], r_sq, rs[:, 0], r_rs, 6)
            rp1 = self.rstd_block(raw[:, 2:4], r_raw, n, 2, 256, sq[:, 2:4], r_sq, rs[:, 1], r_rs, 7)
            for c in range(4):
                rp, r_rp = (rp0, rp1)[c // 2]
                self.tt(cqn[:, c, 0:n], raw[:, c, 0:n], rp, ALU.mult, [r_raw, r_rp], [r_cqn])
            self.ld(tab[0:96, 0, 0:n], self.CD["cosq_a"][:, lo:hi], [], [r_tab])
            self.ld(tab[0:96, 1, 0:n], self.CD["sinq_a"][:, lo:hi], [], [r_tab])
            self.ld(tab[0:32, 2, 0:n], self.CD["cosk_a"][:, lo:hi], [], [r_tab])
            self.ld(tab[0:32, 3, 0:n], self.CD["sink_a"][:, lo:hi], [], [r_tab])
            for h in range(4):
                for (w_, r_w_, b) in ((wq, r_wq, 0), (wqp, r_wqp, 1)):
                    for k in range(2):
                        self.mm(self.bank(b, n)[0:96], w_[:, k, h * 96:(h + 1) * 96], cqn[:, k, 0:n], k == 0, k == 1, [r_w_, r_cqn], [self.r_ps[b]])
                self.tt(t1[0:96, 0:n], self.bank(0, n)[0:96], tab[0:96, 0, 0:n], ALU.mult, [self.r_ps[0], r_tab], [r_t1])
                self.tt(t2[0:96, 0:n], self.bank(1, n)[0:96], tab[0:96, 1, 0:n], ALU.mult, [self.r_ps[1], r_tab], [r_t2])
                self.tt(qT[0:96, h, lo:hi], t1[0:96, 0:n], t2[0:96, 0:n], ALU.add, [r_t1, r_t2], [r_qT])
                for k in range(2):
                    self.mm(self.bank(2, n)[0:64], wkv[:, k, h * 128:h * 128 + 64], cqn[:, 2 + k, 0:n], k == 0, k == 1, [r_wkv, r_cqn], [self.r_ps[2]])
                self.cp(kT[0:64, h, lo:hi], self.bank(2, n)[0:64], [self.r_ps[2]], [r_kT], eng="act")
            for (w_, r_w_, b) in ((wkr, r_wA, 3), (wAp, r_wAp, 4)):
                for k in range(KC):
                    self.mm(self.bank(b, n)[0:32], w_[:, k, :], self.hT[:, k, lo:hi], k == 0, k == KC - 1, [r_w_, self.r_hT], [self.r_ps[b]])
            self.tt(t1[0:32, 0:n], self.bank(3, n)[0:32], tab[0:32, 2, 0:n], ALU.mult, [self.r_ps[3], r_tab], [r_t1])
            self.tt(t2[0:32, 0:n], self.bank(4, n)[0:32], tab[0:32, 3, 0:n], ALU.mult, [self.r_ps[4], r_tab], [r_t2])
            self.tt(t1[0:32, 0:n], t1[0:32, 0:n], t2[0:32, 0:n], ALU.add, [r_t1, r_t2], [r_t1])
            for h in range(4):
                self.cp(kT[64:96, h, lo:hi], t1[0:32, 0:n], [r_t1], [r_kT])
            for ti in range(lo // 128, hi // 128):
                o = ti * 128 - lo
                for k in range(2):
                    self.mm(self.bank(5, 256).rearrange("p (h d) -> p h d", d=64), cqn[:, 2 + k, o:o + 128],
                            wkv[:, k, :].rearrange("p (h x) -> p h x", x=128)[:, :, 64:128], k == 0, k == 1, [r_cqn, r_wkv], [self.r_ps[5]])
                self.cp(Va[:, ti, :, 0:64], self.bank(5, 256).rearrange("p (h d) -> p h d", d=64), [self.r_ps[5]], [r_Va])
        q_tiles = list(range(CT, TT)) + ([] if last else list(range(CT)))
        items = []
        for n_, qi in enumerate(q_tiles):
            is_ctx = qi < CT
            nk = self.NCTX if is_ctx else T
            yt, r_yt = ytms[n_ % 2]
            for h in range(4):
                kparts = []
                c0 = 0
                while c0 < nk:
                    n = min(512, nk - c0)
                    kparts.append((kT[0:96, h, c0:c0 + n], r_kT, c0, None))
                    c0 += n
                vparts = [(Va[:, i, h, :], r_Va, i * 128) for i in range(nk // 128)]
                it = dict(q=qT[0:96, h, qi * 128:(qi + 1) * 128], r_q=r_qT, kparts=kparts, sink=None, vparts=vparts, scale=1.0,
                          out=yt[:, h * 64:(h + 1) * 64], r_out=r_yt)
                if h == 3:
                    it["after"] = (lambda yt=yt, r_yt=r_yt, qi=qi: self.store_y_tm(yt, r_yt, s, 0, qi, ybuf))
                items.append(it)
        self.attn_run(items)

    def branch_c(self, s, l, last):
        self.phase()
        T, TT, CT = self.T, self.TT, self.CT
        wC, r_wC = self.tile([128, KC, 512], BF16, "wC")
        wCp, r_wCp = self.tile([128, KC, 384], BF16, "wCp")
        tab, r_tab = self.tile([128, 2, 512], F32, "tabC")
        t1, r_t1 = self.tile([128, 512], F32, "t1C")
        t2, r_t2 = self.tile([128, 512], F32, "t2C")
        qT, r_qT = self.tile([128, 4, T], BF16, "qTC")
        kT, r_kT = self.tile([128, 2, T], BF16, "kTC")
        Va, r_Va = self.tile([128, TT, 2, 65], BF16, "VaC")
        sk8, r_sk8 = self.tile([128, 4], F32, "sk8")
        ytms = [self.tile([128, 256], BF16, "ytmC%d" % i) for i in range(2)]
        ybuf = self.tile([128, 2, 128], BF16, "yTC")
        self.load_w_cols(wC, r_wC, l, 1584, 2096)
        self.make_perm(wCp, wC[:, :, 0:384], 6, 64, 0, 64, r_wCp, r_wC, KC)
        self.ts(sk8, self.sink_b[:, l, :], 8.0, ALU.mult, [self.r_pv], [r_sk8])
        self.P.op("dve", lambda e: e.memset(Va, 1.0), [], [r_Va])
        for bi, (lo, hi) in enumerate(self.blocks):
            n = hi - lo
            self.ld(tab[0:64, 0, 0:n], self.CD["cos_c"][:, lo:hi], [], [r_tab])
            self.ld(tab[0:64, 1, 0:n], self.CD["sin_c"][:, lo:hi], [], [r_tab])
            for hh in range(6):
                for (w_, r_w_, b) in ((wC, r_wC, 0), (wCp, r_wCp, 1)):
                    for k in range(KC):
                        self.mm(self.bank(b, n)[0:64], w_[:, k, hh * 64:(hh + 1) * 64], self.hT[:, k, lo:hi], k == 0, k == KC - 1,
                                [r_w_, self.r_hT], [self.r_ps[b]])
                self.tt(t1[0:64, 0:n], self.bank(0, n)[0:64], tab[0:64, 0, 0:n], ALU.mult, [self.r_ps[0], r_tab], [r_t1])
                self.tt(t2[0:64, 0:n], self.bank(1, n)[0:64], tab[0:64, 1, 0:n], ALU.mult, [self.r_ps[1], r_tab], [r_t2])
                dst = qT[0:64, hh, lo:hi] if hh < 4 else kT[0:64, hh - 4, lo:hi]
                self.tt(dst, t1[0:64, 0:n], t2[0:64, 0:n], ALU.add, [r_t1, r_t2], [r_qT if hh < 4 else r_kT])
            for ti in range(lo // 128, hi // 128):
                for k in range(KC):
                    self.mm(self.bank(5, 128), self.hT[:, k, ti * 128:(ti + 1) * 128], wC[:, k, 384:512], k == 0, k == KC - 1,
                            [self.r_hT, r_wC], [self.r_ps[5]])
                self.cp(Va[:, ti, :, 0:64], self.bank(5, 128).rearrange("p (h d) -> p h d", d=64), [self.r_ps[5]], [r_Va])
        NQ = TT - CT
        q_tiles = list(range(CT, TT)) + ([] if last else list(range(CT)))
        NC_ = self.NCTX
        items = []
        for n_, qi in enumerate(q_tiles):
            is_ctx = qi < CT
            yt, r_yt = ytms[n_ % 2]
            for h in range(4):
                g = h // 2
                kparts = [(kT[0:64, g, 0:NC_], r_kT, 0, None)]
                vparts = [(Va[:, i, g, :], r_Va, i * 128) for i in range(CT)]
                if not is_ctx:
                    i = qi - CT
                    col = NC_
                    for (j, mk) in ((i - 1, "maskA"), (i, None), (i + 1, "maskB")):
                        if 0 <= j < NQ:
                            kparts.append((kT[0:64, g, NC_ + j * 128:NC_ + (j + 1) * 128], r_kT, col, self.C[mk] if mk else None))
                            vparts.append((Va[:, CT + j, g, :], r_Va, col))
                            col += 128
                it = dict(q=qT[0:64, h, qi * 128:(qi + 1) * 128], r_q=r_qT, kparts=kparts, sink=sk8[0:1, h:h + 1], vparts=vparts, scale=0.125,
                          out=yt[:, h * 64:(h + 1) * 64], r_out=r_yt)
                if h == 3:
                    it["after"] = (lambda yt=yt, r_yt=r_yt, qi=qi: self.store_y_tm(yt, r_yt, s, 2, qi, ybuf))
                items.append(it)
        dg = self.branch_d(s, l, last)
        self.attn_run(items, hook=lambda: next(dg, None))
        for _ in dg:
            pass

    def branch_d(self, s, l, last):
        T, TT, CT = self.T, self.TT, self.CT
        wD, r_wD = self.tile([128, KC, 256], BF16, "wD")
        ud, r_ud = self.tile([128, 2, T], BF16, "udT")
        uc, r_uc = self.tile([128, TT, 2, 256], BF16, "uc_tm")
        cn, r_cn = self.tile([128, 16, 512], BF16, "cn")
        sn, r_sn = self.tile([128, 16, 512], BF16, "sn")
        yo, r_yo = self.tile([128, 2, 512], BF16, "yoD")
        self.load_w_cols(wD, r_wD, l, 2096, 2352)
        yield
        for (lo, hi) in self.blocks:
            n = hi - lo
            for c in range(2):
                b = 2 + c
                for k in range(KC):
                    self.mm(self.bank(b, n), wD[:, k, c * 128:(c + 1) * 128], self.hT[:, k, lo:hi], k == 0, k == KC - 1,
                            [r_wD, self.r_hT], [self.r_ps[b]])
                self.cp(ud[:, c, lo:hi], self.bank(b, n), [self.r_ps[b]], [r_ud], eng="act" if c else "dve")
                yield
        for ti in range(TT):
            for j, m in enumerate(("bdc", "bds")):
                for c in range(2):
                    self.mm(self.bank(4, 128, j * 256 + c * 128), ud[:, c, ti * 128:(ti + 1) * 128], self.C[m], c == 0 and j == 0, True,
                            [r_ud, self.r_c], [self.r_ps[4]])
            self.cp(uc[:, ti], self.bank(4).rearrange("p (a b) -> p a b", a=2), [self.r_ps[4]], [r_uc])
            yield
        segs = [("lat", CT, TT, self.NCTX)] + ([] if last else [("ctx", 0, CT, 0)])
        for nm, t0, t1_, col0 in segs:
            N = (t1_ - t0) * 128
            ntl = t1_ - t0
            for kb in range(0, N, 512):
                n = min(512, N - kb)
                self.ld(cn[:, 0:ntl, 0:n], self.CD["cn_" + nm][:, kb:kb + n].rearrange("(a p) k -> p a k", p=128), [], [r_cn])
                self.ld(sn[:, 0:ntl, 0:n], self.CD["sn_" + nm][:, kb:kb + n].rearrange("(a p) k -> p a k", p=128), [], [r_sn])
                for c in range(2):
                    b = 2 + c
                    for a in range(ntl):
                        self.mm(self.bank(b, n), uc[:, t0 + a, 0, c * 128:(c + 1) * 128], cn[:, a, 0:n], a == 0, False, [r_uc, r_cn], [self.r_ps[b]])
                        self.mm(self.bank(b, n), uc[:, t0 + a, 1, c * 128:(c + 1) * 128], sn[:, a, 0:n], False, a == ntl - 1, [r_uc, r_sn], [self.r_ps[b]])
                        if a % 4 == 3:
                            yield
                    self.cp(yo[:, c, 0:n], self.bank(b, n), [self.r_ps[b]], [r_yo], eng="act" if c else "dve")
                self.ld(self.YS[s][768:1024, col0 + kb:col0 + kb + n].rearrange("(c p) n -> p c n", p=128), yo[:, :, 0:n], [r_yo], [self.r_ys[s]])
                yield

    def branch_b(self, s, l, last):
        self.phase()
        T, TT, CT = self.T, self.TT, self.CT
        wB, r_wB = self.tile([128, KC, 1040], BF16, "wB")
        araw, r_araw = self.tile([128, 514], F32, "arawB")
        c1, r_c1 = self.tile([128, 512], F32, "c1B")
        qk, r_qk = self.tile([128, 8, T], BF16, "qkB")
        ktm, r_ktm = self.tile([128, TT, 256], BF16, "ktmB")
        Va, r_Va = self.tile([128, TT, 4, 65], BF16, "VaB")
        og, r_og = self.tile([128, 256], F32, "ogB")
        G, r_G = self.tile([128, TT, 16], F32, "GB")
        hs, r_hs = self.tile([128, TT, 256], F32, "hsB")
        self.load_w_cols(wB, r_wB, l, 544, 1584)
        self.ld(self.wconvB[0:64], self.PR["wconvB"][l], [], [self.r_wcb])
        self.P.op("dve", lambda e: e.memset(Va, 1.0), [], [r_Va])
        self.P.op("dve", lambda e: e.memset(hs, 0.0), [], [r_hs])
        for (lo, hi) in self.blocks:
            n = hi - lo
            seg_lo, seg_hi = (0, self.NCTX) if lo < self.NCTX else (self.NCTX, T)
            for hh in range(8):
                ch, half = hh // 2, hh % 2
                c0 = hh * 64
                for k in range(KC):
                    self.mm(self.bank(0, n)[0:64], wB[:, k, c0:c0 + 64], self.hT[:, k, lo:hi], k == 0, k == KC - 1, [r_wB, self.r_hT], [self.r_ps[0]])
                self.P.op("dve", lambda e: e.memset(araw[0:64, :], 0.0), [], [r_araw])
                if lo > seg_lo:
                    for k in range(KC):
                        self.mm(self.bank(1, 1)[0:64], wB[:, k, c0:c0 + 64], self.hT[:, k, lo - 1:lo], k == 0, k == KC - 1, [r_wB, self.r_hT], [self.r_ps[1]])
                    self.cp(araw[0:64, 0:1], self.bank(1, 1)[0:64], [self.r_ps[1]], [r_araw])
                if hi < seg_hi:
                    for k in range(KC):
                        self.mm(self.bank(1, 1, 8)[0:64], wB[:, k, c0:c0 + 64], self.hT[:, k, hi:hi + 1], k == 0, k == KC - 1, [r_wB, self.r_hT], [self.r_ps[1]])
                    self.cp(araw[0:64, n + 1:n + 2], self.bank(1, 1, 8)[0:64], [self.r_ps[1]], [r_araw])
                self.cp(araw[0:64, 1:n + 1], self.bank(0, n)[0:64], [self.r_ps[0]], [r_araw], eng="act")
                wsl = self.wconvB[:, hh, :]
                self.ts(c1[0:64, 0:n], araw[0:64, 0:n], wsl[0:64, 0:1], ALU.mult, [r_araw, self.r_wcb], [r_c1])
                self.stt(c1[0:64, 0:n], araw[0:64, 1:n + 1], wsl[0:64, 1:2], c1[0:64, 0:n], ALU.mult, ALU.add, [r_araw, self.r_wcb, r_c1], [r_c1])
                self.stt(c1[0:64, 0:n], araw[0:64, 2:n + 2], wsl[0:64, 2:3], c1[0:64, 0:n], ALU.mult, ALU.add, [r_araw, self.r_wcb, r_c1], [r_c1])
                self.act(c1[0:64, 0:n], c1[0:64, 0:n], AF.Silu, [r_c1], [r_c1])
                self.ts(qk[0:64, hh, lo:hi], c1[0:64, 0:n], 1.0 if hh < 4 else 0.125, ALU.mult, [r_c1], [r_qk])
        Gt, r_Gt = self.tile([128, TT, 2, 4], F32, "GtB")
        for ti in range(TT):
            tsl = slice(ti * 128, (ti + 1) * 128)
            for k in range(KC):
                self.mm(self.bank(2, 16), self.hT[:, k, tsl], wB[:, k, 1024:1040], k == 0, k == KC - 1, [self.r_hT, r_wB], [self.r_ps[2]])
            self.tt(G[:, ti, :], self.bank(2, 16), self.bg_b[:, l, :], ALU.add, [self.r_ps[2], self.r_pv], [r_G])
            for k in range(KC):
                self.mm(self.bank(3, 256), self.hT[:, k, tsl], wB[:, k, 512:768], k == 0, k == KC - 1, [self.r_hT, r_wB], [self.r_ps[3]])
            self.cp(Va[:, ti, :, 0:64], self.bank(3, 256).rearrange("p (h d) -> p h d", d=64), [self.r_ps[3]], [r_Va])
            for h in range(4):
                self.tr(self.bank_bf(5)[:, h * 64:(h + 1) * 64], qk[0:64, 4 + h, tsl], self.C["ident_bf"][0:64, 0:64], [r_qk, self.r_c], [self.r_ps[5]])
            self.cp(ktm[:, ti, :], self.bank_bf(5)[:, 0:256], [self.r_ps[5]], [r_ktm])
        G5 = G.rearrange("p t (a b c) -> p t a b c", a=2, b=2)
        for d_ in range(2):
            fv = G5[:, :, d_, 1, :]
            self.act(Gt[:, :, d_, :], fv, AF.Exp, [r_G], [r_Gt], scale=-1.0)
            self.act(Gt[:, :, d_, :], Gt[:, :, d_, :], AF.Ln, [r_Gt], [r_Gt], bias=1.0)
            self.ts(fv, Gt[:, :, d_, :], -1.0, ALU.mult, [r_Gt], [r_G])
        B_TM, M_, NEGM, WIN, EMT, DEN, DAB, RR, WTM, DEC, MX, DENI = range(12)
        SX = []
        for d_ in range(2):
            X = {}
            for nm, shp, dt in (("diag", [128, 4, 128], F32), ("bBm", [128, 4, 128], F32), ("Wt", [128, 4, 128], F32),
                                ("Sb", [128, 4, 128], BF16), ("ST", [128, 4, 128], BF16), ("kw", [128, 4, 64], BF16),
                                ("sv", [128, 16, 4], F32), ("cm", [128, 8], F32), ("mst", [128, 4], F32), ("tmpi", [128, 4, 65], F32),
                                ("numh", [128, 4, 64], F32), ("Cst", [128, 4, 65], F32), ("Cbf", [128, 4, 65], BF16)):
                X[nm], X["r_" + nm] = self.tile(shp, dt, nm + "B%d" % d_)
            X["tri"] = self.C["tri_f" if d_ == 0 else "tri_b"]
            X["mneg"] = self.C["mneg_f" if d_ == 0 else "mneg_b"]
            X["esel"] = self.C["e_last" if d_ == 0 else "e_first"]
            X["b0"] = 4 * d_
            SX.append(X)
            self.P.op("dve", lambda e, t=X["Cst"]: e.memset(t, 0.0), [], [X["r_Cst"]])
            self.P.op("dve", lambda e, t=X["Cbf"]: e.memset(t, 0.0), [], [X["r_Cbf"]])
            self.P.op("dve", lambda e, t=X["mst"]: e.memset(t, 0.0), [], [X["r_mst"]])

        def chunk(d_, ti):
            X = SX[d_]
            diag, bBm, Wt, Sb, ST, kw, sv, cm, mst, tmpi, numh, Cst, Cbf = (X[k] for k in (
                "diag", "bBm", "Wt", "Sb", "ST", "kw", "sv", "cm", "mst", "tmpi", "numh", "Cst", "Cbf"))
            r_diag, r_bBm, r_Wt, r_Sb, r_ST, r_kw, r_sv, r_cm, r_mst, r_tmpi, r_numh, r_Cst, r_Cbf = (X["r_" + k] for k in (
                "diag", "bBm", "Wt", "Sb", "ST", "kw", "sv", "cm", "mst", "tmpi", "numh", "Cst", "Cbf"))
            b0 = X["b0"]
            bB_b, qk_b, st_b, ms_b = b0, b0 + 1, b0 + 2, b0 + 3
            r0, r1, r2, r3 = self.r_ps[bB_b], self.r_ps[qk_b], self.r_ps[st_b], self.r_ps[ms_b]
            cum_ps = self.bank(ms_b, 4)
            sel_ps = self.bank(ms_b, 8, 8)
            inter_ps = self.bank(ms_b, 260, 16)
            upd_ps = self.bank(ms_b, 260, 16)
            num_ps = self.bank(st_b, 256, 256)
            tsl = slice(ti * 128, (ti + 1) * 128)
            li = G[:, ti, d_ * 8:d_ * 8 + 4]
            lf = G[:, ti, d_ * 8 + 4:d_ * 8 + 8]
            self.mm(cum_ps, X["tri"], lf, True, True, [self.r_c, r_G], [r3])
            for h in range(4):
                self.mm(self.bank(qk_b, 128, h * 128), qk[0:64, h, tsl], qk[0:64, 4 + h, tsl], True, True, [r_qk], [r1])
            yield
            self.tt(sv[:, B_TM, :], li, cum_ps, ALU.subtract, [r_G, r3], [r_sv])
            for h in range(4):
                self.ts(diag[:, h, :], self.C["ident_f"], sv[:, B_TM, h:h + 1], ALU.mult, [self.r_c, r_sv], [r_diag])
            yield
            for h in range(4):
                self.mm(self.bank(bB_b, 128, h * 128), self.C["ones_f"], diag[:, h, :], True, True, [self.r_c, r_diag], [r0])
            yield
            self.tt(bBm, self.bank(bB_b).rearrange("p (h s) -> p h s", h=4), X["mneg"], ALU.add, [r0, self.r_c], [r_bBm])
            self.red(sv[:, MX, :], bBm, ALU.max, [r_bBm], [r_sv])
            self.tt(sv[:, M_, :], sv[:, MX, :], mst, ALU.max, [r_sv, r_mst], [r_sv])
            self.ts(sv[:, NEGM, :], sv[:, M_, :], -1.0, ALU.mult, [r_sv], [r_sv])
            self.tt(sv[:, WIN, :], mst, sv[:, M_, :], ALU.subtract, [r_mst, r_sv], [r_sv])
            self.tt(cm[:, 0:4], cum_ps, sv[:, M_, :], ALU.add, [r3, r_sv], [r_cm])
            self.cp(cm[:, 4:8], sv[:, M_, :], [r_sv], [r_cm])
            yield
            for h in range(4):
                self.act(Wt[:, h, :], bBm[:, h, :], AF.Exp, [r_bBm, r_sv], [r_Wt], bias=sv[:, NEGM, h:h + 1], scale=1.0)
            self.act(sv[:, WIN, :], sv[:, WIN, :], AF.Exp, [r_sv], [r_sv])
            self.act(sv[:, EMT, :], cm[:, 0:4], AF.Exp, [r_cm], [r_sv], scale=-1.0)
            self.mm(sel_ps, X["esel"], cm, True, True, [self.r_c, r_cm], [r3])
            yield
            self.tt(Sb, self.bank(qk_b).rearrange("p (h s) -> p h s", h=4), Wt, ALU.mult, [r1, r_Wt], [r_Sb])
            self.red(sv[:, DENI, :], Sb, ALU.add, [r_Sb], [r_sv])
            self.tt(sv[:, WTM, :], sv[:, B_TM, :], self.bank(ms_b, 4, 12), ALU.subtract, [r_sv, r3], [r_sv])
            self.tt(sv[:, DEC, :], mst, self.bank(ms_b, 4, 12), ALU.subtract, [r_mst, r3], [r_sv])
            self.cp(mst, self.bank(ms_b, 4, 8), [r3], [r_mst])
            yield
            for h in range(4):
                self.tr(self.bank_bf(st_b)[:, h * 128:(h + 1) * 128], Sb[:, h, :], self.C["ident_bf"], [r_Sb, self.r_c], [r2])
            for h in range(4):
                self.mm(self.bank(ms_b, 65, 16 + h * 65), qk[0:64, h, tsl], Cbf[0:64, h, :], True, True, [r_qk, r_Cbf], [r3])
            self.act(sv[:, WTM, :], sv[:, WTM, :], AF.Exp, [r_sv], [r_sv])
            self.act(sv[:, DEC, :], sv[:, DEC, :], AF.Exp, [r_sv], [r_sv])
            yield
            self.cp(ST, self.bank_bf(st_b)[:, 0:512].rearrange("p (h s) -> p h s", h=4), [r2], [r_ST])
            self.tt(tmpi, inter_ps.rearrange("p (h e) -> p h e", h=4), sv[:, WIN, :].unsqueeze(2).to_broadcast([128, 4, 65]), ALU.mult,
                    [r3, r_sv], [r_tmpi])
            self.tt(kw, ktm[:, ti, :].rearrange("p (h e) -> p h e", h=4), sv[:, WTM, :].unsqueeze(2).to_broadcast([128, 4, 64]), ALU.mult,
                    [r_ktm, r_sv], [r_kw])
            yield
            for h in range(4):
                self.mm(self.bank(st_b, 64, 256 + h * 64), ST[:, h, :], Va[:, ti, h, 0:64], True, True, [r_ST, r_Va], [r2])
            for h in range(4):
                self.mm(self.bank(ms_b, 65, 16 + h * 65)[0:64], kw[:, h, :], Va[:, ti, h, :], True, True, [r_kw, r_Va], [r3])
            yield
            self.tt(numh, tmpi[:, :, 0:64], num_ps.rearrange("p (h e) -> p h e", h=4), ALU.add, [r_tmpi, r2], [r_numh])
            self.tt(sv[:, DEN, :], tmpi[:, :, 64], sv[:, DENI, :], ALU.add, [r_tmpi, r_sv], [r_sv])
            self.ts(sv[:, DAB, :], sv[:, DEN, :], -1.0, ALU.mult, [r_sv], [r_sv])
            self.tt(sv[:, DAB, :], sv[:, DAB, :], sv[:, DEN, :], ALU.max, [r_sv], [r_sv])
            self.tt(sv[:, DAB, :], sv[:, DAB, :], sv[:, EMT, :], ALU.max, [r_sv], [r_sv])
            self.recip(sv[:, RR, :], sv[:, DAB, :], [r_sv], [r_sv])
            self.tt(numh, numh, sv[:, RR, :].unsqueeze(2).to_broadcast([128, 4, 64]), ALU.mult, [r_numh, r_sv], [r_numh])
            hsv = hs[:, ti, :].rearrange("p (h e) -> p h e", h=4)
            self.tt(hsv, hsv, numh, ALU.add, [r_hs, r_numh], [r_hs])
            self.tt(Cst[0:64], Cst[0:64], sv[0:64, DEC, :].unsqueeze(2).to_broadcast([64, 4, 65]), ALU.mult, [r_Cst, r_sv], [r_Cst])
            self.tt(Cst[0:64], Cst[0:64], upd_ps[0:64].rearrange("p (h e) -> p h e", h=4), ALU.add, [r_Cst, r3], [r_Cst])
            self.cp(Cbf[0:64], Cst[0:64], [r_Cst], [r_Cbf])
            yield

        orders = [list(range(TT)), list(range(CT - 1, -1, -1)) + list(range(TT - 1, CT - 1, -1))]
        for i in range(TT):
            gens = [chunk(0, orders[0][i]), chunk(1, orders[1][i])]
            alive = True
            while alive:
                alive = False
                for g in gens:
                    try:
                        next(g)
                        alive = True
                    except StopIteration:
                        pass
        ytm, r_ytm = self.tile([128, 256], BF16, "ytmB")
        ybuf = self.tile([128, 2, 128], BF16, "yTB")
        for ti in range(CT if last else 0, TT):
            tsl = slice(ti * 128, (ti + 1) * 128)
            for k in range(KC):
                self.mm(self.bank(4, 256), self.hT[:, k, tsl], wB[:, k, 768:1024], k == 0, k == KC - 1, [self.r_hT, r_wB], [self.r_ps[4]])
            self.act(og, self.bank(4, 256), AF.Sigmoid, [self.r_ps[4]], [r_og])
            self.tt(ytm, og, hs[:, ti, :], ALU.mult, [r_og, r_hs], [r_ytm])
            self.store_y_tm(ytm, r_ytm, s, 1, ti, ybuf)

    def post_norm_res(self, s, lo, hi, yT, r_yT, gp_idx, tl):
        sq, r_sq, rs, r_rs, tmp, r_tmp, xb, r_xb = tl
        n = hi - lo
        rsp, r_rsp = self.rstd_block(yT, r_yT, n, KC, D, sq, r_sq, rs, r_rs, 7)
        self.ld(xb[:, :, 0:n], self.XRES[s][:, lo:hi].rearrange("(k p) n -> p k n", p=128), [self.r_xres[s]], [r_xb])
        dv, r_dv = self.seg_dv(lo)
        for k in range(KC):
            self.tt(tmp[:, 0:n], yT[:, k, 0:n], rsp, ALU.mult, [r_yT, r_rsp], [r_tmp])
            self.stt(xb[:, k, 0:n], tmp[:, 0:n], dv[:, gp_idx, k:k + 1], xb[:, k, 0:n], ALU.mult, ALU.add, [r_tmp, r_dv, r_xb], [r_xb])
        self.ld(self.XRES[s][:, lo:hi].rearrange("(k p) n -> p k n", p=128), xb[:, :, 0:n], [r_xb], [self.r_xres[s]])

    def pn_tiles(self):
        sq, r_sq = self.tile([128, KC, 512], BF16, "sqP")
        rs, r_rs = self.tile([128, 512], F32, "rsP")
        tmp, r_tmp = self.tile([128, 512], F32, "tmpP")
        xb, r_xb = self.tile([128, KC, 512], F32, "xbP")
        return (sq, r_sq, rs, r_rs, tmp, r_tmp, xb, r_xb)

    def merge(self, s, l, last):
        self.phase()
        ysb, r_ysb = self.tile([128, 8, 512], BF16, "ysb")
        wg = [self.tile([128, 4, KC, 128], BF16, "wg%d" % i) for i in range(2)]
        wbr = [self.tile([128, 4, 2, 128], BF16, "wbr%d" % i) for i in range(2)]
        sig, r_sig = self.tile([128, 512], F32, "sig")
        accf, r_accf = self.tile([128, 512], F32, "accf")
        tmpm, r_tmpm = self.tile([128, 512], F32, "tmpm")
        acc, r_acc = self.tile([128, KC, 512], BF16, "acc")
        wo, r_wo = self.tile([128, KC, D], BF16, "wo")
        yT, r_yT = self.tile([128, KC, 512], F32, "yTm")
        tl = self.pn_tiles()
        self.ld(wo, self.WO[l].rearrange("p (k n) -> p k n", k=KC), [self.r_wbf], [r_wo])
        it = 0
        for (lo, hi) in self.blocks:
            if last and lo < self.NCTX:
                continue
            n = hi - lo
            self.ld(ysb[:, :, 0:n], self.YS[s][:, lo:hi].rearrange("(k p) n -> p k n", p=128), [self.r_ys[s]], [r_ysb])
            for dc in range(KC):
                (wg_, r_wg), (wb_, r_wb) = wg[it % 2], wbr[it % 2]
                it += 1
                for br in range(4):
                    self.ld(wg_[:, br], self.WG[l, br, dc].rearrange("p (k n) -> p k n", k=KC), [self.r_wbf], [r_wg])
                    self.ld(wb_[:, br], self.WBR[l, br].rearrange("p (k n) -> p k n", k=2)[:, :, dc * 128:(dc + 1) * 128], [self.r_wbf], [r_wb])
                for br in range(4):
                    bg, bp = br % 2, 2 + br % 2
                    for k in range(KC):
                        self.mm(self.bank(bg, n), wg_[:, br, k, :], self.hT[:, k, lo:hi], k == 0, k == KC - 1, [r_wg, self.r_hT], [self.r_ps[bg]])
                    self.act(sig[:, 0:n], self.bank(bg, n), AF.Sigmoid, [self.r_ps[bg], self.r_pv], [r_sig], bias=self.pv["b_gate"][:, l, br, dc:dc + 1], scale=1.0)
                    for k in range(2):
                        self.mm(self.bank(bp, n), wb_[:, br, k, :], ysb[:, br * 2 + k, 0:n], k == 0, k == 1, [r_wb, r_ysb], [self.r_ps[bp]])
                    if br == 0:
                        self.tt(accf[:, 0:n], sig[:, 0:n], self.bank(bp, n), ALU.mult, [r_sig, self.r_ps[bp]], [r_accf])
                    else:
                        self.tt(tmpm[:, 0:n], sig[:, 0:n], self.bank(bp, n), ALU.mult, [r_sig, self.r_ps[bp]], [r_tmpm])
                        if br < 3:
                            self.tt(accf[:, 0:n], accf[:, 0:n], tmpm[:, 0:n], ALU.add, [r_accf, r_tmpm], [r_accf])
                        else:
                            self.tt(acc[:, dc, 0:n], accf[:, 0:n], tmpm[:, 0:n], ALU.add, [r_accf, r_tmpm], [r_acc])
            for d2 in range(KC):
                b = 4 + d2 % 2
                for k in range(KC):
                    self.mm(self.bank(b, n), wo[:, k, d2 * 128:(d2 + 1) * 128], acc[:, k, 0:n], k == 0, k == KC - 1, [r_wo, r_acc], [self.r_ps[b]])
                self.cp(yT[:, d2, 0:n], self.bank(b, n), [self.r_ps[b]], [r_yT], eng="act" if d2 % 2 else "dve")
            self.post_norm_res(s, lo, hi, yT, r_yT, 2, tl)

    def ffn(self, s, l, last):
        self.phase()
        T = self.T
        wd, r_wd = self.tile([128, FC, D], BF16, "wd")
        wu = [self.tile([128, KC, 256], BF16, "wu%d" % i) for i in range(2)]
        gT, r_gT = self.tile([128, FC, 512], BF16, "gT")
        a_sb, r_a = self.tile([128, 514], F32, "a_sb")
        c1, r_c1 = self.tile([128, 512], F32, "c1F")
        yT, r_yT = self.tile([128, KC, 512], F32, "yTf")
        tl = self.pn_tiles()
        self.ld(wd, self.WD[l].rearrange("p (f n) -> p f n", f=FC), [self.r_wbf], [r_wd])
        wcv, bcv = self.pv["w_ffn_conv"], self.pv["b_ffn_conv"]
        it = 0
        for (lo, hi) in self.blocks:
            if last and lo < self.NCTX:
                continue
            n = hi - lo
            seg_lo, seg_hi = (0, self.NCTX) if lo < self.NCTX else (self.NCTX, T)
            for fc in range(FC):
                w_, r_w = wu[it % 2]
                it += 1
                self.ld(w_, self.WU[l, fc].rearrange("p (k n) -> p k n", k=KC), [self.r_wbf], [r_w])
                ba, bv = fc % 2, 2 + fc % 2
                for k in range(KC):
                    self.mm(self.bank(ba, n), w_[:, k, 0:128], self.hT[:, k, lo:hi], k == 0, k == KC - 1, [r_w, self.r_hT], [self.r_ps[ba]])
                for k in range(KC):
                    self.mm(self.bank(bv, n), w_[:, k, 128:256], self.hT[:, k, lo:hi], k == 0, k == KC - 1, [r_w, self.r_hT], [self.r_ps[bv]])
                self.P.op("dve", lambda e: e.memset(a_sb[:, 0:1], 0.0), [], [r_a])
                self.P.op("dve", lambda e: e.memset(a_sb[:, n + 1:n + 2], 0.0), [], [r_a])
                if lo > seg_lo:
                    for k in range(KC):
                        self.mm(self.bank(6, 1), w_[:, k, 0:128], self.hT[:, k, lo - 1:lo], k == 0, k == KC - 1, [r_w, self.r_hT], [self.r_ps[6]])
                    self.cp(a_sb[:, 0:1], self.bank(6, 1), [self.r_ps[6]], [r_a])
                if hi < seg_hi:
                    for k in range(KC):
                        self.mm(self.bank(6, 1, 8), w_[:, k, 0:128], self.hT[:, k, hi:hi + 1], k == 0, k == KC - 1, [r_w, self.r_hT], [self.r_ps[6]])
                    self.cp(a_sb[:, n + 1:n + 2], self.bank(6, 1, 8), [self.r_ps[6]], [r_a])
                self.cp(a_sb[:, 1:n + 1], self.bank(ba, n), [self.r_ps[ba]], [r_a], eng="act")
                self.act(self.bank(ba, n), self.bank(ba, n), AF.Copy, [self.r_ps[ba], self.r_pv], [self.r_ps[ba]], scale=wcv[:, l, 1, fc:fc + 1])
                self.stt(self.bank(ba, n), a_sb[:, 0:n], wcv[:, l, 0, fc:fc + 1], self.bank(ba, n), ALU.mult, ALU.add, [r_a, self.r_pv, self.r_ps[ba]], [self.r_ps[ba]])
                self.stt(c1[:, 0:n], a_sb[:, 2:n + 2], wcv[:, l, 2, fc:fc + 1], self.bank(ba, n), ALU.mult, ALU.add, [r_a, self.r_pv, self.r_ps[ba]], [r_c1])
                self.act(c1[:, 0:n], c1[:, 0:n], AF.Silu, [r_c1, self.r_pv], [r_c1], bias=bcv[:, l, fc:fc + 1], scale=1.0)
                self.tt(gT[:, fc, 0:n], c1[:, 0:n], self.bank(bv, n), ALU.mult, [r_c1, self.r_ps[bv]], [r_gT])
            for d2 in range(KC):
                b = 4 + d2 % 2
                for fc in range(FC):
                    self.mm(self.bank(b, n), wd[:, fc, d2 * 128:(d2 + 1) * 128], gT[:, fc, 0:n], fc == 0, fc == FC - 1, [r_wd, r_gT], [self.r_ps[b]])
                self.cp(yT[:, d2, 0:n], self.bank(b, n), [self.r_ps[b]], [r_yT], eng="act" if d2 % 2 else "dve")
            self.post_norm_res(s, lo, hi, yT, r_yT, 5, tl)


NLAT_FULL, NCTX_FULL, DEPTH = 2048, 256, 2
_cache = {}


def run_device(inp, NLAT, NCTX, B, L, n_cores):
    NSEQ = B // n_cores
    consts = make_consts(NLAT, NCTX)
    key = (NLAT, NCTX, NSEQ, L)
    bld = Builder(NLAT, NCTX, NSEQ, L, consts)
    nc = bld.build()
    x = np.asarray(inp["x"], np.float32)
    ctx = np.asarray(inp["ctx"], np.float32)
    c = np.asarray(inp["c"], np.float32)
    c_ctx = np.asarray(inp["c_ctx"], np.float32)
    params = layout_params(inp, L)
    wml = np.asarray(inp["w_ml_conv"], np.float32)
    params["wconvB"] = np.ascontiguousarray(wml.reshape(L, 3, 8, 64).transpose(0, 3, 2, 1))
    shared = {k: np.ascontiguousarray(np.asarray(inp[k], np.float32)) for k in W_SHAPES}
    shared.update(params)
    shared.update(consts)
    in_maps = []
    for ci in range(n_cores):
        sl = slice(ci * NSEQ, (ci + 1) * NSEQ)
        cc = np.concatenate([c[sl], c_ctx[None]], 0)
        m = dict(shared)
        m["xT"] = np.ascontiguousarray(x[sl].transpose(0, 2, 1))
        m["ctxT"] = np.ascontiguousarray(ctx[sl].transpose(0, 2, 1))
        m["ccT"] = np.ascontiguousarray(cc.T.reshape(KC, 128, NSEQ + 1).transpose(1, 0, 2))
        in_maps.append(m)
    return nc, in_maps


def kernel(**inp):
    n_cores = 8
    nc, in_maps = run_device(inp, NLAT_FULL, NCTX_FULL, 16, DEPTH, n_cores)
    res = run_bass_kernel_spmd(nc, in_maps, core_ids=list(range(n_cores)))
    outs = [np.asarray(r["outT"]).transpose(0, 2, 1) for r in res.results]
    return np.ascontiguousarray(np.concatenate(outs, 0).astype(np.float32))
```

```python
from contextlib import ExitStack
import numpy as np
import ml_dtypes
import concourse.bass as bass
import concourse.mybir as mybir
from concourse.bass_utils import run_bass_kernel_spmd

F32 = mybir.dt.float32
BF16 = mybir.dt.bfloat16
AF = mybir.ActivationFunctionType
ALU = mybir.AluOpType
AX = mybir.AxisListType
ENG = ("pe", "act", "dve", "pool", "sp")
NBIG = -30000.0
D = 1024
KC = 8
DFF = 2816
FC = 22
EPS = 1e-6


class Res:
    __slots__ = ("name", "w", "re", "rd")

    def __init__(self, name=""):
        self.name = name
        self.w = None
        self.re = {}
        self.rd = []


class Prog:
    N_DMA_SEMS = 80

    def __init__(self, nc, stack):
        self.nc = nc
        self.esem = {e: stack.enter_context(nc.semaphore("es_" + e)) for e in ENG}
        self.dsem = [stack.enter_context(nc.semaphore("ds%d" % i)) for i in range(self.N_DMA_SEMS)]
        self.dval = [0] * self.N_DMA_SEMS
        self.dnext = 0
        self.cnt = {e: 0 for e in ENG}
        self.seen = {e: {} for e in ENG}
        self.q = {e: [] for e in ENG}
        self.n_ops = 0

    def _deps(self, reads, writes):
        deps = []
        for r in reads:
            if r.w is not None:
                deps.append(r.w)
        for w in writes:
            if w.w is not None:
                deps.append(w.w)
            for e, c in w.re.items():
                deps.append(("E", e, c))
            deps.extend(w.rd)
        return deps

    def _waits(self, eng, deps):
        seen = self.seen[eng]
        need = {}
        for kind, key, val in deps:
            k = (kind, key)
            if seen.get(k, 0) >= val:
                continue
            if kind == "E" and key == "pe" and eng == "pe":
                continue
            if need.get(k, 0) < val:
                need[k] = val
        out = []
        for k, val in need.items():
            seen[k] = val
            sem = self.esem[k[1]] if k[0] == "E" else self.dsem[k[1]]
            out.append((sem, val))
        return out

    def op(self, eng, fn, reads=(), writes=()):
        waits = self._waits(eng, self._deps(reads, writes))
        self.cnt[eng] += 1
        c = self.cnt[eng]
        tok = ("E", eng, c)
        self.q[eng].append((waits, fn, (self.esem[eng], 1)))
        for r in reads:
            if r.re.get(eng, 0) < c:
                r.re[eng] = c
        for w in writes:
            w.w = tok
            w.re = {}
            w.rd = []
        self.n_ops += 1
        return tok

    def dma(self, qeng, out, in_, reads=(), writes=(), **kw):
        deps = self._deps(reads, writes)
        i = self.dnext
        self.dnext = (self.dnext + 1) % self.N_DMA_SEMS
        prev = self.dval[i]
        if prev:
            deps.append(("D", i, prev))
        waits = self._waits(qeng, deps)
        self.dval[i] = prev + 16
        tok = ("D", i, prev + 16)
        self.q[qeng].append((waits, (lambda e, o=out, s=in_, k=kw: e.dma_start(out=o, in_=s, **k)),
                             (self.dsem[i], 16)))
        for r in reads:
            r.rd.append(tok)
        for w in writes:
            w.w = tok
            w.re = {}
            w.rd = []
        self.n_ops += 1
        return tok

    def barrier(self):
        deps = [("E", e, self.cnt[e]) for e in ENG if self.cnt[e]]
        deps += [("D", i, v) for i, v in enumerate(self.dval) if v]
        for e in ENG:
            waits = self._waits(e, deps)
            if waits:
                self.q[e].append((waits, None, None))

    def emit(self):
        nc = self.nc
        allsems = [self.esem[e] for e in ENG] + self.dsem
        with nc.Block("init") as b0:
            @b0.vector
            def _(v):
                for s in allsems:
                    v.sem_clear(s)
        with nc.Block("main") as blk:
            def run(e, name):
                for waits, fn, inc in self.q[name]:
                    for sem, val in waits:
                        e.wait_ge(sem, val)
                    if fn is not None:
                        fn(e).then_inc(inc[0], inc[1])

            @blk.tensor
            def _(e):
                run(e, "pe")

            @blk.scalar
            def _(e):
                run(e, "act")

            @blk.vector
            def _(e):
                run(e, "dve")

            @blk.gpsimd
            def _(e):
                run(e, "pool")

            @blk.sync
            def _(e):
                run(e, "sp")


def _rope_tab(rd, pos_row, pos_col, n_ctx):
    half = rd // 2
    nf = half // 2
    inv = 10000.0 ** (-np.arange(nf, dtype=np.float64) / nf)
    n = len(pos_row)
    cos = np.ones((rd, n_ctx + n), np.float64)
    sin = np.zeros((rd, n_ctx + n), np.float64)
    for r in range(rd):
        hh, rr = r // half, r % half
        idx = rr % nf
        pos = pos_row if hh == 0 else pos_col
        ang = pos.astype(np.float64) * inv[idx]
        ang = (pos.astype(np.float32) * np.float32(inv[idx]).astype(np.float32)).astype(np.float64)
        cos[r, n_ctx:] = np.cos(ang)
        sin[r, n_ctx:] = np.sin(ang)
    return cos, sin


def make_consts(NLAT, NCTX):
    bf = ml_dtypes.bfloat16
    T = NLAT + NCTX
    c = {}
    c["ident_bf"] = np.eye(128).astype(bf)
    c["ones_bf"] = np.ones((128, 128)).astype(bf)
    c["ident_f"] = np.eye(128, dtype=np.float32)
    c["ones_f"] = np.ones((128, 128), np.float32)
    s = np.arange(128)[:, None]
    t = np.arange(128)[None, :]
    c["tri_f"] = (s <= t).astype(np.float32)
    c["tri_b"] = (s >= t).astype(np.float32)
    mf = np.where(t <= s, 0.0, NBIG).astype(np.float32)
    mb = np.where(t >= s, 0.0, NBIG).astype(np.float32)
    c["mneg_f"] = np.repeat(mf[:, None, :], 4, axis=1).copy()
    c["mneg_b"] = np.repeat(mb[:, None, :], 4, axis=1).copy()
    el = np.zeros((128, 128), np.float32); el[127, :] = 1
    ef = np.zeros((128, 128), np.float32); ef[0, :] = 1
    c["e_last"] = el
    c["e_first"] = ef
    c["maskA"] = np.where(t >= s, 0.0, NBIG).astype(bf)
    c["maskB"] = np.where(t <= s, 0.0, NBIG).astype(bf)
    c["maskN"] = np.full((128, 128), NBIG).astype(bf)
    rows = NLAT // 64
    pr = np.repeat(np.arange(rows), 64)
    pc = np.tile(np.arange(64), rows)
    ca, sa = _rope_tab(32, pr, pc, NCTX)
    sc_a = 96.0 ** -0.5
    cosq = np.ones((96, T)); sinq = np.zeros((96, T))
    cosq[64:] = ca; sinq[64:] = sa
    c["cosq_a"] = (cosq * sc_a).astype(np.float32)
    c["sinq_a"] = (sinq * sc_a).astype(np.float32)
    c["cosk_a"] = ca.astype(np.float32)
    c["sink_a"] = sa.astype(np.float32)
    cc, sc = _rope_tab(64, pr, pc, NCTX)
    c["cos_c"] = cc.astype(np.float32)
    c["sin_c"] = sc.astype(np.float32)
    j = np.arange(64)
    Cc = np.cos(2 * np.pi * np.outer(j, j) / 64)
    Sc = np.sin(2 * np.pi * np.outer(j, j) / 64)
    z = np.zeros((64, 64))
    c["bdc"] = np.block([[Cc, z], [z, Cc]]).astype(bf)
    c["bds"] = (-np.block([[Sc, z], [z, Sc]])).astype(bf)
    for nm, N in (("lat", NLAT), ("ctx", NCTX)):
        n = np.arange(N)
        ph = (np.outer(n, n) % N).astype(np.float64) * (2 * np.pi / N)
        nrm = 1.0 / np.sqrt(N * 64.0)
        c["cn_" + nm] = (np.cos(ph) * nrm).astype(bf)
        c["sn_" + nm] = (np.sin(ph) * nrm).astype(bf)
    return c


CONST_DT = {"ident_bf": BF16, "ones_bf": BF16, "maskA": BF16, "maskB": BF16, "maskN": BF16, "bdc": BF16, "bds": BF16,
            "cn_lat": BF16, "sn_lat": BF16, "cn_ctx": BF16, "sn_ctx": BF16}

W_SHAPES = {
    "w_mod": (D, 6 * D), "w_in": (D, 2352), "w_uq": (256, 384), "w_ukv": (256, 512),
    "w_gate": (4, D, D), "w_branch": (4, 256, D), "w_out": (D, D), "w_up": (D, 2 * DFF), "w_down": (DFF, D),
}


def layout_params(inp, L):
    o = {}

    def fm(v, k):
        v = np.asarray(v, np.float32)
        lead = v.shape[:-1]
        return np.ascontiguousarray(np.moveaxis(v.reshape(lead + (k, 128)), -1, 0))

    o["b_mod"] = np.stack([fm(inp["b_mod"][l], 48) for l in range(L)])
    for nm in ("g_pre_mix", "g_post_mix", "g_pre_ffn", "g_post_ffn"):
        o[nm] = np.stack([fm(inp[nm][l], 8) for l in range(L)])
    o["g_qa"] = np.stack([fm(inp["g_qa"][l], 2) for l in range(L)])
    o["g_kva"] = np.stack([fm(inp["g_kva"][l], 2) for l in range(L)])
    o["b_gate"] = np.stack([fm(inp["b_gate"][l], 8) for l in range(L)])
    o["w_ffn_conv"] = np.stack([fm(inp["w_ffn_conv"][l], FC) for l in range(L)])
    o["b_ffn_conv"] = np.stack([fm(inp["b_ffn_conv"][l], FC) for l in range(L)])
    o["w_ml_conv"] = np.stack([fm(inp["w_ml_conv"][l], 4) for l in range(L)])
    o["b_ml_gates"] = np.asarray(inp["b_ml_gates"], np.float32).reshape(L, 1, 16)
    o["wg_sink"] = np.asarray(inp["wg_sink"], np.float32).reshape(L, 1, 4)
    return o


PARAM_SHAPES = lambda L: {
    "b_mod": (L, 128, 48), "g_pre_mix": (L, 128, 8), "g_post_mix": (L, 128, 8), "g_pre_ffn": (L, 128, 8),
    "g_post_ffn": (L, 128, 8), "g_qa": (L, 128, 2), "g_kva": (L, 128, 2), "b_gate": (L, 128, 4, 8),
    "w_ffn_conv": (L, 128, 3, FC), "b_ffn_conv": (L, 128, FC), "w_ml_conv": (L, 128, 3, 4),
    "b_ml_gates": (L, 1, 16), "wg_sink": (L, 1, 4), "wconvB": (L, 64, 8, 3),
}


class Builder:
    def __init__(self, NLAT, NCTX, NSEQ, L, consts, dbg=None):
        self.NLAT, self.NCTX, self.NSEQ, self.L = NLAT, NCTX, NSEQ, L
        self.T = T = NLAT + NCTX
        self.TT = T // 128
        self.CT = NCTX // 128
        self.blocks = [(0, NCTX)] + [(NCTX + i, min(NCTX + i + 512, T)) for i in range(0, NLAT, 512)]
        self.dbg = dbg
        nc = self.nc = bass.Bass("TRN2", target_bir_lowering=False)
        self.st = ExitStack()
        self.P = Prog(nc, self.st)
        di = lambda n, s, dt=F32: nc.dram_tensor(n, list(s), dt, kind="ExternalInput").ap()
        self.xT_in = di("xT", (NSEQ, D, NLAT))
        self.cT_in = di("ctxT", (NSEQ, D, NCTX))
        self.ccT = di("ccT", (128, KC, NSEQ + 1))
        self.W = {k: di(k, (L,) + v) for k, v in W_SHAPES.items()}
        self.PR = {k: di(k, v) for k, v in PARAM_SHAPES(L).items()}
        self.CD = {k: di(k, v.shape, CONST_DT.get(k, F32)) for k, v in consts.items()}
        self.out = nc.dram_tensor("outT", [NSEQ, D, NLAT], F32, kind="ExternalOutput").ap()
        ds = lambda n, s, dt: nc.dram_tensor(n, list(s), dt, kind="Internal").ap()
        self.XRES = ds("xres", (NSEQ, D, T), F32)
        self.YS = ds("ys", (NSEQ, D, T), BF16)
        self.WI = ds("wi_bf", (L, 128, KC * 2352), BF16)
        self.WG = ds("wg_bf", (L, 4, KC, 128, KC * 128), BF16)
        self.WBR = ds("wbr_bf", (L, 4, 128, 2 * D), BF16)
        self.WO = ds("wo_bf", (L, 128, KC * D), BF16)
        self.WU = ds("wu_bf", (L, FC, 128, KC * 256), BF16)
        self.WD = ds("wd_bf", (L, 128, FC * D), BF16)
        self.r_wbf = Res("wbf")
        self.r_xres = [Res("xres%d" % i) for i in range(NSEQ)]
        self.r_ys = [Res("ys%d" % i) for i in range(NSEQ)]
        self.r_out = Res("out")
        if dbg:
            self.dbg_out = {k: nc.dram_tensor("dbg_" + k, list(s), dt, kind="ExternalOutput").ap() for k, (s, dt) in dbg.items()}
        self.PS = nc.alloc_psum_tensor("PS", [128, 4096], F32)
        self.r_ps = [Res("ps%d" % i) for i in range(8)]
        self.ARENA_E = 98000
        self.arena = nc.alloc_sbuf_tensor("arena", [128, self.ARENA_E], BF16)
        self.a_off = 0
        self.a_base = 0
        self.phase_log = []

    def tile(self, shape, dt, name=""):
        n = int(np.prod(shape[1:]))
        ne = n * (2 if dt == F32 else 1)
        off = (self.a_off + 15) // 16 * 16
        assert off + ne <= self.ARENA_E, ("arena overflow", name, off, ne)
        self.a_off = off + ne
        ap = self.arena[0:shape[0], off:off + ne]
        if dt == F32:
            ap = ap.bitcast(F32)
        if len(shape) == 3:
            ap = ap.rearrange("p (a b) -> p a b", a=shape[1])
        elif len(shape) == 4:
            ap = ap.rearrange("p (a b c) -> p a b c", a=shape[1], b=shape[2])
        return ap, Res(name)

    def phase(self):
        self.P.barrier()
        self.a_off = self.a_base
        import sys
        self.phase_log.append((sys._getframe(1).f_code.co_name, dict(self.P.cnt)))

    def bank(self, b, n=512, lo=0):
        return self.PS[:, b * 512 + lo:b * 512 + lo + n]

    def bank_bf(self, b):
        return self.PS[:, b * 512:(b + 1) * 512].bitcast(BF16)

    def mm(self, out, lhsT, rhs, start, stop, reads, writes):
        self.P.op("pe", lambda e: e.matmul(out, lhsT, rhs, start=start, stop=stop, skip_group_check=True), reads, writes)

    def tr(self, out, in_, ident, reads, writes):
        self.P.op("pe", lambda e: e.transpose(out, in_, ident), reads, writes)

    def act(self, out, in_, func, reads, writes, bias=None, scale=None):
        kw = {}
        if bias is not None:
            kw["bias"] = bias
        if scale is not None:
            kw["scale"] = scale
        self.P.op("act", lambda e: e.activation(out=out, in_=in_, func=func, **kw), reads, writes)

    def tt(self, out, a, b, op, reads, writes, eng="dve"):
        self.P.op(eng, lambda e: e.tensor_tensor(out=out, in0=a, in1=b, op=op), reads, writes)

    def ts(self, out, a, s1, op0, reads, writes, s2=None, op1=None, eng="dve"):
        if op1 is None:
            self.P.op(eng, lambda e: e.tensor_scalar(out=out, in0=a, scalar1=s1, scalar2=None, op0=op0), reads, writes)
        else:
            self.P.op(eng, lambda e: e.tensor_scalar(out=out, in0=a, scalar1=s1, scalar2=s2, op0=op0, op1=op1), reads, writes)

    def stt(self, out, a, s, b, op0, op1, reads, writes):
        self.P.op("dve", lambda e: e.scalar_tensor_tensor(out=out, in0=a, scalar=s, in1=b, op0=op0, op1=op1), reads, writes)

    def cp(self, out, in_, reads, writes, eng="dve"):
        if eng == "act":
            self.P.op("act", lambda e: e.copy(out=out, in_=in_), reads, writes)
        else:
            self.P.op(eng, lambda e: e.tensor_copy(out=out, in_=in_), reads, writes)

    def red(self, out, in_, op, reads, writes):
        self.P.op("dve", lambda e: e.tensor_reduce(out=out, in_=in_, axis=AX.X, op=op), reads, writes)

    def recip(self, out, in_, reads, writes):
        self.P.op("dve", lambda e: e.reciprocal(out=out, in_=in_), reads, writes)

    def ld(self, out, in_, reads, writes, q="sp"):
        self.P.dma(q, out, in_, reads, writes)

    def prepass(self):
        SZ = 2560
        stf = [self.tile([128, SZ], F32, "stf%d" % i) for i in range(3)]
        stb = [self.tile([128, SZ], BF16, "stb%d" % i) for i in range(3)]
        pieces = []

        def piece(srcs, dst, a, b):
            pieces.append((srcs, dst, a, b))

        def views(j):
            srcs, dst, a, b = pieces[j]
            (f, r_f), (bt, r_b) = stf[j % 3], stb[j % 3]
            fv = f[:, 0:a * b].rearrange("p (a b) -> p a b", a=a)
            bv = bt[:, 0:a * b].rearrange("p (a b) -> p a b", a=a)
            return srcs, dst, fv, bv, r_f, r_b

        def emit_in(j):
            srcs, dst, fv, bv, r_f, r_b = views(j)
            for (c0, c1, src) in srcs:
                self.ld(fv[:, :, c0:c1], src, [], [r_f])

        def emit_rest(j):
            srcs, dst, fv, bv, r_f, r_b = views(j)
            self.cp(bv, fv, [r_f], [r_b], eng=("act", "dve", "pool")[j % 3])
            self.ld(dst, bv, [r_b], [self.r_wbf])

        for l in range(self.L):
            wi = self.W["w_in"][l].rearrange("(k p) n -> p k n", p=128)
            wid = self.WI[l].rearrange("p (k n) -> p k n", k=KC)
            for k in range(KC):
                piece([(0, 2352, wi[:, k:k + 1, :])], wid[:, k:k + 1, :], 1, 2352)
            for br in range(4):
                for dc in range(KC):
                    src = self.W["w_gate"][l][br][:, dc * 128:(dc + 1) * 128].rearrange("(k p) n -> p k n", p=128)
                    piece([(0, 128, src)], self.WG[l, br, dc].rearrange("p (k n) -> p k n", k=KC), KC, 128)
                src = self.W["w_branch"][l][br].rearrange("(k p) n -> p k n", p=128)
                piece([(0, D, src)], self.WBR[l, br].rearrange("p (k n) -> p k n", k=2), 2, D)
            wo = self.W["w_out"][l].rearrange("(k p) n -> p k n", p=128)
            wod = self.WO[l].rearrange("p (k n) -> p k n", k=KC)
            for k in range(0, KC, 2):
                piece([(0, D, wo[:, k:k + 2, :])], wod[:, k:k + 2, :], 2, D)
            for fc in range(FC):
                sa = self.W["w_up"][l][:, fc * 128:(fc + 1) * 128].rearrange("(k p) n -> p k n", p=128)
                sv = self.W["w_up"][l][:, DFF + fc * 128:DFF + (fc + 1) * 128].rearrange("(k p) n -> p k n", p=128)
                piece([(0, 128, sa), (128, 256, sv)], self.WU[l, fc].rearrange("p (k n) -> p k n", k=KC), KC, 256)
            wdn = self.W["w_down"][l].rearrange("(f p) n -> p f n", p=128)
            wdd = self.WD[l].rearrange("p (f n) -> p f n", f=FC)
            for f0 in range(0, FC, 2):
                piece([(0, D, wdn[:, f0:f0 + 2, :])], wdd[:, f0:f0 + 2, :], 2, D)
        npc = len(pieces)
        for j in range(npc + 2):
            if j < npc:
                emit_in(j)
            if j - 2 >= 0:
                emit_rest(j - 2)
            yield

    def build(self):
        P = self.P
        NSEQ, L, T = self.NSEQ, self.L, self.T
        C = {}
        self.C = C
        r_c = self.r_c = Res("consts")
        for k in ("ident_bf", "ones_bf", "ident_f", "ones_f", "tri_f", "tri_b", "e_last", "e_first", "maskA", "maskB", "maskN", "bdc", "bds"):
            C[k], _ = self.tile([128, 128], CONST_DT.get(k, F32), k)
            self.ld(C[k], self.CD[k], [], [r_c])
        for k in ("mneg_f", "mneg_b"):
            C[k], _ = self.tile([128, 4, 128], F32, k)
            self.ld(C[k], self.CD[k], [], [r_c])
        self.hT, self.r_hT = self.tile([128, KC, T], BF16, "hT")
        self.MOD, self.r_mod = self.tile([128, L, 48, NSEQ + 1], F32, "MOD")
        self.pv = {}
        self.r_pv = Res("pvec")
        for k, shp in PARAM_SHAPES(L).items():
            if shp[1] == 128:
                self.pv[k], _ = self.tile([128, L] + list(shp[2:]) if len(shp) > 2 else [128, L], F32, k)
                src = self.PR[k]
                if len(shp) == 3:
                    self.ld(self.pv[k], src.rearrange("l p a -> p l a"), [], [self.r_pv])
                else:
                    for l in range(L):
                        self.ld(self.pv[k][:, l], src[l], [], [self.r_pv])
        self.bg_b, _ = self.tile([128, L, 16], F32, "bgb")
        self.sink_b, _ = self.tile([128, L, 4], F32, "sinkb")
        for l in range(L):
            self.ld(self.bg_b[:, l, :], self.PR["b_ml_gates"][l].partition_broadcast(128), [], [self.r_pv])
            self.ld(self.sink_b[:, l, :], self.PR["wg_sink"][l].partition_broadcast(128), [], [self.r_pv])
        self.wconvB, _ = self.tile([128, 8, 3], F32, "wconvB")
        self.r_wcb = Res("wcb")
        self.dv, self.r_dv = self.tile([128, 6, KC], F32, "derived")
        self.dvc, self.r_dvc = self.tile([128, 6, KC], F32, "derivedc")
        self.a_base = self.a_off
        for s in range(NSEQ):
            self.ld(self.XRES[s][:, 0:self.NCTX], self.cT_in[s], [], [self.r_xres[s]])
            self.ld(self.XRES[s][:, self.NCTX:T], self.xT_in[s], [], [self.r_xres[s]])
        self.phase()
        pg = self.prepass()
        self.mod_phase(pg)
        for _ in pg:
            pass
        import os
        stop = int(os.environ.get("KSTOP", "99"))
        for s in range(NSEQ):
            for l in range(L):
                last = (l == L - 1)
                steps = [lambda: self.derive(l, s), lambda: self.norm_mod(s, 0), lambda: self.branch_a(s, l, last),
                         lambda: self.branch_c(s, l, last), lambda: None, lambda: self.branch_b(s, l, last),
                         lambda: self.merge(s, l, last), lambda: self.norm_mod(s, 3, skip_ctx=last), lambda: self.ffn(s, l, last)]
                for i, f in enumerate(steps):
                    if i < stop:
                        f()
            self.phase()
            self.ld(self.out[s], self.XRES[s][:, self.NCTX:T], [self.r_xres[s]], [self.r_out])
        P.barrier()
        P.emit()
        self.st.close()
        return self.nc

    def mod_phase(self, pg):
        next(pg, None)
        NJ = self.NSEQ + 1
        cc, r_cc = self.tile([128, KC, NJ], F32, "cc")
        sg, r_sg = self.tile([128, KC, NJ], F32, "sg")
        self.ld(cc, self.ccT, [], [r_cc])
        self.act(sg, cc, AF.Sigmoid, [r_cc], [r_sg])
        self.tt(cc, cc, sg, ALU.mult, [r_sg, r_cc], [r_cc])
        wm = [self.tile([128, KC, 512], F32, "wm%d" % i) for i in range(2)]
        n = 0
        for l in range(self.L):
            for g in range(12):
                w, r_w = wm[n % 2]
                n += 1
                self.ld(w, self.W["w_mod"][l][:, g * 512:(g + 1) * 512].rearrange("(k p) n -> p k n", p=128), [], [r_w])
                for _ in range(7):
                    next(pg, None)
                b = n % 2
                for j4 in range(4):
                    for k in range(KC):
                        self.mm(self.bank(b, NJ, j4 * 8), w[:, k, j4 * 128:(j4 + 1) * 128], cc[:, k, :], k == 0 and j4 == 0, k == KC - 1,
                                [r_w, r_cc], [self.r_ps[b]])
                for j4 in range(4):
                    ch = g * 4 + j4
                    self.ts(self.MOD[:, l, ch, :], self.bank(b, NJ, j4 * 8), self.pv["b_mod"][:, l, ch:ch + 1], ALU.add,
                            [self.r_ps[b], self.r_pv], [self.r_mod])

    def derive(self, l, s):
        for (dst, r_dst, j) in ((self.dv, self.r_dv, s), (self.dvc, self.r_dvc, self.NSEQ)):
            for half, gpre, gpost in ((0, "g_pre_mix", "g_post_mix"), (1, "g_pre_ffn", "g_post_ffn")):
                sh = self.MOD[:, l, half * 24 + 0:half * 24 + 8, j]
                sc = self.MOD[:, l, half * 24 + 8:half * 24 + 16, j]
                g = self.MOD[:, l, half * 24 + 16:half * 24 + 24, j]
                self.stt(dst[:, half * 3 + 0, :], sc, 1.0, self.pv[gpre][:, l, :], ALU.add, ALU.mult, [self.r_mod, self.r_pv], [r_dst])
                self.cp(dst[:, half * 3 + 1, :], sh, [self.r_mod], [r_dst])
                self.tt(dst[:, half * 3 + 2, :], g, self.pv[gpost][:, l, :], ALU.mult, [self.r_mod, self.r_pv], [r_dst])

    def seg_dv(self, lo):
        return (self.dvc, self.r_dvc) if lo < self.NCTX else (self.dv, self.r_dv)

    def rstd_block(self, src, r_src, n, nk, dim, sq, r_sq, rs, r_rs, bank):
        for k in range(nk):
            self.act(sq[:, k, 0:n], src[:, k, 0:n], AF.Square, [r_src], [r_sq])
        for k in range(nk):
            self.mm(self.bank(bank, n), self.C["ones_bf"], sq[:, k, 0:n], k == 0, k == nk - 1, [r_sq, self.r_c], [self.r_ps[bank]])
        self.ts(rs[:, 0:n], self.bank(bank, n), 1.0 / dim, ALU.mult, [self.r_ps[bank]], [r_rs], s2=EPS, op1=ALU.add)
        self.act(rs[:, 0:n], rs[:, 0:n], AF.Ln, [r_rs], [r_rs])
        self.act(self.bank(bank, n), rs[:, 0:n], AF.Exp, [r_rs], [self.r_ps[bank]], scale=-0.5)
        return self.bank(bank, n), self.r_ps[bank]

    def norm_mod(self, s, base, skip_ctx=False):
        self.phase()
        xb = [self.tile([128, KC, 512], F32, "xb%d" % i) for i in range(2)]
        sqs = [self.tile([128, KC, 512], BF16, "sq%d" % i) for i in range(2)]
        rss = [self.tile([128, 512], F32, "rs%d" % i) for i in range(2)]
        tmps = [self.tile([128, 512], F32, "tmp%d" % i) for i in range(2)]
        for bi, (lo, hi) in enumerate(self.blocks):
            if skip_ctx and lo < self.NCTX:
                continue
            n = hi - lo
            x, r_x = xb[bi % 2]
            (sq, r_sq), (rs, r_rs) = sqs[bi % 2], rss[bi % 2]
            self.ld(x[:, :, 0:n], self.XRES[s][:, lo:hi].rearrange("(k p) n -> p k n", p=128), [self.r_xres[s]], [r_x])
            rsp, r_rsp = self.rstd_block(x, r_x, n, KC, D, sq, r_sq, rs, r_rs, bi % 2)
            dv, r_dv = self.seg_dv(lo)
            for k in range(KC):
                tmp, r_tmp = tmps[k % 2]
                self.tt(tmp[:, 0:n], x[:, k, 0:n], rsp, ALU.mult, [r_x, r_rsp], [r_tmp])
                self.act(self.hT[:, k, lo:hi], tmp[:, 0:n], AF.Identity, [r_tmp, r_dv], [self.r_hT],
                         bias=dv[:, base + 1, k:k + 1], scale=dv[:, base + 0, k:k + 1])

    def load_w_cols(self, dst, r_dst, l, c0, c1):
        self.ld(dst, self.WI[l].rearrange("p (k n) -> p k n", k=KC)[:, :, c0:c1], [self.r_wbf], [r_dst])

    def make_perm(self, dst, src, nheads, hd, r0, rd, r_dst, r_src, nk):
        nf = rd // 4
        self.P.op("dve", lambda e: e.memset(dst, 0.0), [], [r_dst])
        for k in range(nk):
            for h in range(nheads):
                b = h * hd + r0
                for hh in range(2):
                    o = b + hh * 2 * nf
                    self.ts(dst[:, k, o:o + nf], src[:, k, o + nf:o + 2 * nf], -1.0, ALU.mult, [r_src], [r_dst])
                    self.cp(dst[:, k, o + nf:o + 2 * nf], src[:, k, o:o + nf], [r_src], [r_dst])

    def attn_scores(self, it, buf):
        Pm, r_Pm, sm, r_sm = buf
        kparts, sink, scale = it["kparts"], it["sink"], it["scale"]
        pc = it.get("pcol", 0)
        ncols = max(c0 + k.shape[-1] for (k, _, c0, _) in kparts)
        started = set()
        for (kT, r_k, c0, mask) in kparts:
            n = kT.shape[-1]
            b = (pc + c0) // 512
            assert (pc + c0 + n - 1) // 512 == b
            self.mm(self.PS[:, pc + c0:pc + c0 + n], it["q"], kT, b not in started, mask is None, [it["r_q"], r_k], [self.r_ps[b]])
            started.add(b)
            if mask is not None:
                self.mm(self.PS[:, pc + c0:pc + c0 + n], self.C["ident_bf"], mask, False, True, [self.r_c], [self.r_ps[b]])
        tot = ncols
        if sink is not None:
            b = (pc + ncols) // 512
            self.mm(self.PS[:, pc + ncols:pc + ncols + 1], self.C["ones_f"][0:1, :], sink, b not in started, True, [self.r_c, self.r_pv], [self.r_ps[b]])
            started.add(b)
            tot = ncols + 1
        banks = [self.r_ps[b] for b in sorted(started)]
        self.red(sm[:, 0:1], self.PS[:, pc:pc + tot], ALU.max, banks, [r_sm])
        self.ts(sm[:, 1:2], sm[:, 0:1], -scale, ALU.mult, [r_sm], [r_sm])
        it["ncols"] = ncols
        it["exp"] = (lambda: self.act(Pm[:, 0:tot], self.PS[:, pc:pc + tot], AF.Exp, banks + [r_sm], [r_Pm], bias=sm[:, 1:2], scale=scale))

    def attn_transposes(self, it, buf, PTt, r_PT):
        Pm, r_Pm, sm, r_sm = buf
        vparts = it["vparts"]
        nv = len(vparts)
        for i, (V, r_v, c0) in enumerate(vparts):
            tb = 5 + (i // 8) % 2
            slot = i % 8
            pt_ps = self.bank_bf(tb)[:, slot * 128:(slot + 1) * 128]
            self.tr(pt_ps, Pm[:, c0:c0 + 128], self.C["ident_bf"], [r_Pm, self.r_c], [self.r_ps[tb]])
            if slot == 7 or i == nv - 1:
                g0 = i - slot
                self.cp(PTt[:, g0:i + 1, :], self.bank_bf(tb)[:, 0:(slot + 1) * 128].rearrange("p (a b) -> p a b", b=128),
                        [self.r_ps[tb]], [r_PT], eng="act")

    def attn_pv(self, it, buf, PTt, r_PT):
        Pm, r_Pm, sm, r_sm = buf
        vparts, sink, ncols = it["vparts"], it["sink"], it["ncols"]
        nv = len(vparts)
        for i, (V, r_v, c0) in enumerate(vparts):
            self.mm(self.bank(7, 65), PTt[:, i, :], V, i == 0, i == nv - 1, [r_PT, r_v], [self.r_ps[7]])
        if sink is not None:
            self.tt(sm[:, 2:3], self.bank(7, 1, 64), Pm[:, ncols:ncols + 1], ALU.add, [self.r_ps[7], r_Pm], [r_sm])
            self.recip(sm[:, 3:4], sm[:, 2:3], [r_sm], [r_sm])
        else:
            self.recip(sm[:, 3:4], self.bank(7, 1, 64), [self.r_ps[7]], [r_sm])
        self.ts(it["out"], self.bank(7, 64), sm[:, 3:4], ALU.mult, [self.r_ps[7], r_sm], [it["r_out"]])
        if it.get("after"):
            it["after"]()

    def attn_run(self, items, pingpong=False, hook=None):
        if pingpong:
            for i, it in enumerate(items):
                it["pcol"] = (i % 2) * 1024
        bufs = []
        for j in range(2):
            Pm, r_Pm = self.tile([128, self.T + 128], BF16, "Pm%d" % j)
            sm, r_sm = self.tile([128, 4], F32, "sm%d" % j)
            bufs.append((Pm, r_Pm, sm, r_sm))
        PTt, r_PT = self.tile([128, self.TT, 128], BF16, "PT")
        n = len(items)
        if n == 0:
            return
        self.attn_scores(items[0], bufs[0])
        items[0]["exp"]()
        for i in range(n):
            if i + 1 < n:
                self.attn_scores(items[i + 1], bufs[(i + 1) % 2])
            self.attn_transposes(items[i], bufs[i % 2], PTt, r_PT)
            if i + 1 < n:
                items[i + 1]["exp"]()
            if hook is not None:
                hook()
            self.attn_pv(items[i], bufs[i % 2], PTt, r_PT)

    def store_y_tm(self, ytm, r_ytm, s, br, tile_i, ybuf):
        yT, r_yT = ybuf
        for c in range(2):
            self.tr(self.bank_bf(6)[:, c * 128:(c + 1) * 128], ytm[:, c * 128:(c + 1) * 128], self.C["ident_bf"], [r_ytm, self.r_c], [self.r_ps[6]])
        self.cp(yT, self.bank_bf(6)[:, 0:256].rearrange("p (a b) -> p a b", b=128), [self.r_ps[6]], [r_yT])
        self.ld(self.YS[s][br * 256:(br + 1) * 256, tile_i * 128:(tile_i + 1) * 128].rearrange("(c p) n -> p c n", p=128), yT,
                [r_yT], [self.r_ys[s]])

    def branch_a(self, s, l, last):
        self.phase()
        T, TT, CT = self.T, self.TT, self.CT
        wA, r_wA = self.tile([128, KC, 544], BF16, "wA")
        wAp, r_wAp = self.tile([128, KC, 32], BF16, "wAp")
        wq, r_wq = self.tile([128, 2, 384], BF16, "wq")
        wqf, r_wqf = self.tile([128, 2, 384], F32, "wqf")
        wqp, r_wqp = self.tile([128, 2, 384], BF16, "wqp")
        wkv, r_wkv = self.tile([128, 2, 512], BF16, "wkv")
        wkvf, r_wkvf = self.tile([128, 2, 512], F32, "wkvf")
        raw, r_raw = self.tile([128, 4, 512], F32, "raw")
        sq, r_sq = self.tile([128, 4, 512], BF16, "sqA")
        rs, r_rs = self.tile([128, 2, 512], F32, "rsA")
        cqn, r_cqn = self.tile([128, 4, 512], BF16, "cqn")
        tab, r_tab = self.tile([128, 4, 512], F32, "tabA")
        t1, r_t1 = self.tile([128, 512], F32, "t1A")
        t2, r_t2 = self.tile([128, 512], F32, "t2A")
        qT, r_qT = self.tile([128, 4, T], BF16, "qTA")
        kT, r_kT = self.tile([128, 4, T], BF16, "kTA")
        Va, r_Va = self.tile([128, TT, 4, 65], BF16, "VaA")
        ytms = [self.tile([128, 256], BF16, "ytmA%d" % i) for i in range(2)]
        ybuf = self.tile([128, 2, 128], BF16, "yTA")
        self.load_w_cols(wA, r_wA, l, 0, 544)
        self.ld(wqf, self.W["w_uq"][l].rearrange("(k p) n -> p k n", p=128), [], [r_wqf])
        self.ld(wkvf, self.W["w_ukv"][l].rearrange("(k p) n -> p k n", p=128), [], [r_wkvf])
        for k in range(2):
            self.ts(wq[:, k, :], wqf[:, k, :], self.pv["g_qa"][:, l, k:k + 1], ALU.mult, [r_wqf, self.r_pv], [r_wq])
            self.ts(wkv[:, k, :], wkvf[:, k, :], self.pv["g_kva"][:, l, k:k + 1], ALU.mult, [r_wkvf, self.r_pv], [r_wkv])
        self.make_perm(wqp, wq, 4, 96, 64, 32, r_wqp, r_wq, 2)
        wkr = wA[:, :, 512:544]
        self.make_perm(wAp, wkr, 1, 32, 0, 32, r_wAp, r_wA, KC)
        self.P.op("dve", lambda e: e.memset(Va, 1.0), [], [r_Va])
        for bi, (lo, hi) in enumerate(self.blocks):
            n = hi - lo
            for c in range(4):
                b = c % 2
                for k in range(KC):
                    self.mm(self.bank(b, n), wA[:, k, c * 128:(c + 1) * 128], self.hT[:, k, lo:hi], k == 0, k == KC - 1,
                            [r_wA, self.r_hT], [self.r_ps[b]])
                self.cp(raw[:, c, 0:n], self.bank(b, n), [self.r_ps[b]], [r_raw], eng="act")
            rp0 = self.rstd_block(raw[:, 0:2], r_raw, n, 2, 256, sq[:, 0:2], r_sq, rs[:, 0], r_rs, 6)
            rp1 = self.rstd_block(raw[:, 2:4], r_raw, n, 2, 256, sq[:, 2:4], r_sq, rs[:, 1], r_rs, 7)
            for c in range(4):
                rp, r_rp = (rp0, rp1)[c // 2]
                self.tt(cqn[:, c, 0:n], raw[:, c, 0:n], rp, ALU.mult, [r_raw, r_rp], [r_cqn])
            self.ld(tab[0:96, 0, 0:n], self.CD["cosq_a"][:, lo:hi], [], [r_tab])
            self.ld(tab[0:96, 1, 0:n], self.CD["sinq_a"][:, lo:hi], [], [r_tab])
            self.ld(tab[0:32, 2, 0:n], self.CD["cosk_a"][:, lo:hi], [], [r_tab])
            self.ld(tab[0:32, 3, 0:n], self.CD["sink_a"][:, lo:hi], [], [r_tab])
            for h in range(4):
                for (w_, r_w_, b) in ((wq, r_wq, 0), (wqp, r_wqp, 1)):
                    for k in range(2):
                        self.mm(self.bank(b, n)[0:96], w_[:, k, h * 96:(h + 1) * 96], cqn[:, k, 0:n], k == 0, k == 1, [r_w_, r_cqn], [self.r_ps[b]])
                self.tt(t1[0:96, 0:n], self.bank(0, n)[0:96], tab[0:96, 0, 0:n], ALU.mult, [self.r_ps[0], r_tab], [r_t1])
                self.tt(t2[0:96, 0:n], self.bank(1, n)[0:96], tab[0:96, 1, 0:n], ALU.mult, [self.r_ps[1], r_tab], [r_t2])
                self.tt(qT[0:96, h, lo:hi], t1[0:96, 0:n], t2[0:96, 0:n], ALU.add, [r_t1, r_t2], [r_qT])
                for k in range(2):
                    self.mm(self.bank(2, n)[0:64], wkv[:, k, h * 128:h * 128 + 64], cqn[:, 2 + k, 0:n], k == 0, k == 1, [r_wkv, r_cqn], [self.r_ps[2]])
                self.cp(kT[0:64, h, lo:hi], self.bank(2, n)[0:64], [self.r_ps[2]], [r_kT], eng="act")
            for (w_, r_w_, b) in ((wkr, r_wA, 3), (wAp, r_wAp, 4)):
                for k in range(KC):
                    self.mm(self.bank(b, n)[0:32], w_[:, k, :], self.hT[:, k, lo:hi], k == 0, k == KC - 1, [r_w_, self.r_hT], [self.r_ps[b]])
            self.tt(t1[0:32, 0:n], self.bank(3, n)[0:32], tab[0:32, 2, 0:n], ALU.mult, [self.r_ps[3], r_tab], [r_t1])
            self.tt(t2[0:32, 0:n], self.bank(4, n)[0:32], tab[0:32, 3, 0:n], ALU.mult, [self.r_ps[4], r_tab], [r_t2])
            self.tt(t1[0:32, 0:n], t1[0:32, 0:n], t2[0:32, 0:n], ALU.add, [r_t1, r_t2], [r_t1])
            for h in range(4):
                self.cp(kT[64:96, h, lo:hi], t1[0:32, 0:n], [r_t1], [r_kT])
            for ti in range(lo // 128, hi // 128):
                o = ti * 128 - lo
                for k in range(2):
                    self.mm(self.bank(5, 256).rearrange("p (h d) -> p h d", d=64), cqn[:, 2 + k, o:o + 128],
                            wkv[:, k, :].rearrange("p (h x) -> p h x", x=128)[:, :, 64:128], k == 0, k == 1, [r_cqn, r_wkv], [self.r_ps[5]])
                self.cp(Va[:, ti, :, 0:64], self.bank(5, 256).rearrange("p (h d) -> p h d", d=64), [self.r_ps[5]], [r_Va])
        q_tiles = list(range(CT, TT)) + ([] if last else list(range(CT)))
        items = []
        for n_, qi in enumerate(q_tiles):
            is_ctx = qi < CT
            nk = self.NCTX if is_ctx else T
            yt, r_yt = ytms[n_ % 2]
            for h in range(4):
                kparts = []
                c0 = 0
                while c0 < nk:
                    n = min(512, nk - c0)
                    kparts.append((kT[0:96, h, c0:c0 + n], r_kT, c0, None))
                    c0 += n
                vparts = [(Va[:, i, h, :], r_Va, i * 128) for i in range(nk // 128)]
                it = dict(q=qT[0:96, h, qi * 128:(qi + 1) * 128], r_q=r_qT, kparts=kparts, sink=None, vparts=vparts, scale=1.0,
                          out=yt[:, h * 64:(h + 1) * 64], r_out=r_yt)
                if h == 3:
                    it["after"] = (lambda yt=yt, r_yt=r_yt, qi=qi: self.store_y_tm(yt, r_yt, s, 0, qi, ybuf))
                items.append(it)
        self.attn_run(items)

    def branch_c(self, s, l, last):
        self.phase()
        T, TT, CT = self.T, self.TT, self.CT
        wC, r_wC = self.tile([128, KC, 512], BF16, "wC")
        wCp, r_wCp = self.tile([128, KC, 384], BF16, "wCp")
        tab, r_tab = self.tile([128, 2, 512], F32, "tabC")
        t1, r_t1 = self.tile([128, 512], F32, "t1C")
        t2, r_t2 = self.tile([128, 512], F32, "t2C")
        qT, r_qT = self.tile([128, 4, T], BF16, "qTC")
        kT, r_kT = self.tile([128, 2, T], BF16, "kTC")
        Va, r_Va = self.tile([128, TT, 2, 65], BF16, "VaC")
        sk8, r_sk8 = self.tile([128, 4], F32, "sk8")
        ytms = [self.tile([128, 256], BF16, "ytmC%d" % i) for i in range(2)]
        ybuf = self.tile([128, 2, 128], BF16, "yTC")
        self.load_w_cols(wC, r_wC, l, 1584, 2096)
        self.make_perm(wCp, wC[:, :, 0:384], 6, 64, 0, 64, r_wCp, r_wC, KC)
        self.ts(sk8, self.sink_b[:, l, :], 8.0, ALU.mult, [self.r_pv], [r_sk8])
        self.P.op("dve", lambda e: e.memset(Va, 1.0), [], [r_Va])
        for bi, (lo, hi) in enumerate(self.blocks):
            n = hi - lo
            self.ld(tab[0:64, 0, 0:n], self.CD["cos_c"][:, lo:hi], [], [r_tab])
            self.ld(tab[0:64, 1, 0:n], self.CD["sin_c"][:, lo:hi], [], [r_tab])
            for hh in range(6):
                for (w_, r_w_, b) in ((wC, r_wC, 0), (wCp, r_wCp, 1)):
                    for k in range(KC):
                        self.mm(self.bank(b, n)[0:64], w_[:, k, hh * 64:(hh + 1) * 64], self.hT[:, k, lo:hi], k == 0, k == KC - 1,
                                [r_w_, self.r_hT], [self.r_ps[b]])
                self.tt(t1[0:64, 0:n], self.bank(0, n)[0:64], tab[0:64, 0, 0:n], ALU.mult, [self.r_ps[0], r_tab], [r_t1])
                self.tt(t2[0:64, 0:n], self.bank(1, n)[0:64], tab[0:64, 1, 0:n], ALU.mult, [self.r_ps[1], r_tab], [r_t2])
                dst = qT[0:64, hh, lo:hi] if hh < 4 else kT[0:64, hh - 4, lo:hi]
                self.tt(dst, t1[0:64, 0:n], t2[0:64, 0:n], ALU.add, [r_t1, r_t2], [r_qT if hh < 4 else r_kT])
            for ti in range(lo // 128, hi // 128):
                for k in range(KC):
                    self.mm(self.bank(5, 128), self.hT[:, k, ti * 128:(ti + 1) * 128], wC[:, k, 384:512], k == 0, k == KC - 1,
                            [self.r_hT, r_wC], [self.r_ps[5]])
                self.cp(Va[:, ti, :, 0:64], self.bank(5, 128).rearrange("p (h d) -> p h d", d=64), [self.r_ps[5]], [r_Va])
        NQ = TT - CT
        q_tiles = list(range(CT, TT)) + ([] if last else list(range(CT)))
        NC_ = self.NCTX
        items = []
        for n_, qi in enumerate(q_tiles):
            is_ctx = qi < CT
            yt, r_yt = ytms[n_ % 2]
            for h in range(4):
                g = h // 2
                kparts = [(kT[0:64, g, 0:NC_], r_kT, 0, None)]
                vparts = [(Va[:, i, g, :], r_Va, i * 128) for i in range(CT)]
                if not is_ctx:
                    i = qi - CT
                    col = NC_
                    for (j, mk) in ((i - 1, "maskA"), (i, None), (i + 1, "maskB")):
                        if 0 <= j < NQ:
                            kparts.append((kT[0:64, g, NC_ + j * 128:NC_ + (j + 1) * 128], r_kT, col, self.C[mk] if mk else None))
                            vparts.append((Va[:, CT + j, g, :], r_Va, col))
                            col += 128
                it = dict(q=qT[0:64, h, qi * 128:(qi + 1) * 128], r_q=r_qT, kparts=kparts, sink=sk8[0:1, h:h + 1], vparts=vparts, scale=0.125,
                          out=yt[:, h * 64:(h + 1) * 64], r_out=r_yt)
                if h == 3:
                    it["after"] = (lambda yt=yt, r_yt=r_yt, qi=qi: self.store_y_tm(yt, r_yt, s, 2, qi, ybuf))
                items.append(it)
        dg = self.branch_d(s, l, last)
        self.attn_run(items, hook=lambda: next(dg, None))
        for _ in dg:
            pass

    def branch_d(self, s, l, last):
        T, TT, CT = self.T, self.TT, self.CT
        wD, r_wD = self.tile([128, KC, 256], BF16, "wD")
        ud, r_ud = self.tile([128, 2, T], BF16, "udT")
        uc, r_uc = self.tile([128, TT, 2, 256], BF16, "uc_tm")
        cn, r_cn = self.tile([128, 16, 512], BF16, "cn")
        sn, r_sn = self.tile([128, 16, 512], BF16, "sn")
        yo, r_yo = self.tile([128, 2, 512], BF16, "yoD")
        self.load_w_cols(wD, r_wD, l, 2096, 2352)
        yield
        for (lo, hi) in self.blocks:
            n = hi - lo
            for c in range(2):
                b = 2 + c
                for k in range(KC):
                    self.mm(self.bank(b, n), wD[:, k, c * 128:(c + 1) * 128], self.hT[:, k, lo:hi], k == 0, k == KC - 1,
                            [r_wD, self.r_hT], [self.r_ps[b]])
                self.cp(ud[:, c, lo:hi], self.bank(b, n), [self.r_ps[b]], [r_ud], eng="act" if c else "dve")
                yield
        for ti in range(TT):
            for j, m in enumerate(("bdc", "bds")):
                for c in range(2):
                    self.mm(self.bank(4, 128, j * 256 + c * 128), ud[:, c, ti * 128:(ti + 1) * 128], self.C[m], c == 0 and j == 0, True,
                            [r_ud, self.r_c], [self.r_ps[4]])
            self.cp(uc[:, ti], self.bank(4).rearrange("p (a b) -> p a b", a=2), [self.r_ps[4]], [r_uc])
            yield
        segs = [("lat", CT, TT, self.NCTX)] + ([] if last else [("ctx", 0, CT, 0)])
        for nm, t0, t1_, col0 in segs:
            N = (t1_ - t0) * 128
            ntl = t1_ - t0
            for kb in range(0, N, 512):
                n = min(512, N - kb)
                self.ld(cn[:, 0:ntl, 0:n], self.CD["cn_" + nm][:, kb:kb + n].rearrange("(a p) k -> p a k", p=128), [], [r_cn])
                self.ld(sn[:, 0:ntl, 0:n], self.CD["sn_" + nm][:, kb:kb + n].rearrange("(a p) k -> p a k", p=128), [], [r_sn])
                for c in range(2):
                    b = 2 + c
                    for a in range(ntl):
                        self.mm(self.bank(b, n), uc[:, t0 + a, 0, c * 128:(c + 1) * 128], cn[:, a, 0:n], a == 0, False, [r_uc, r_cn], [self.r_ps[b]])
                        self.mm(self.bank(b, n), uc[:, t0 + a, 1, c * 128:(c + 1) * 128], sn[:, a, 0:n], False, a == ntl - 1, [r_uc, r_sn], [self.r_ps[b]])
                        if a % 4 == 3:
                            yield
                    self.cp(yo[:, c, 0:n], self.bank(b, n), [self.r_ps[b]], [r_yo], eng="act" if c else "dve")
                self.ld(self.YS[s][768:1024, col0 + kb:col0 + kb + n].rearrange("(c p) n -> p c n", p=128), yo[:, :, 0:n], [r_yo], [self.r_ys[s]])
                yield

    def branch_b(self, s, l, last):
        self.phase()
        T, TT, CT = self.T, self.TT, self.CT
        wB, r_wB = self.tile([128, KC, 1040], BF16, "wB")
        araw, r_araw = self.tile([128, 514], F32, "arawB")
        c1, r_c1 = self.tile([128, 512], F32, "c1B")
        qk, r_qk = self.tile([128, 8, T], BF16, "qkB")
        ktm, r_ktm = self.tile([128, TT, 256], BF16, "ktmB")
        Va, r_Va = self.tile([128, TT, 4, 65], BF16, "VaB")
        og, r_og = self.tile([128, 256], F32, "ogB")
        G, r_G = self.tile([128, TT, 16], F32, "GB")
        hs, r_hs = self.tile([128, TT, 256], F32, "hsB")
        self.load_w_cols(wB, r_wB, l, 544, 1584)
        self.ld(self.wconvB[0:64], self.PR["wconvB"][l], [], [self.r_wcb])
        self.P.op("dve", lambda e: e.memset(Va, 1.0), [], [r_Va])
        self.P.op("dve", lambda e: e.memset(hs, 0.0), [], [r_hs])
        for (lo, hi) in self.blocks:
            n = hi - lo
            seg_lo, seg_hi = (0, self.NCTX) if lo < self.NCTX else (self.NCTX, T)
            for hh in range(8):
                ch, half = hh // 2, hh % 2
                c0 = hh * 64
                for k in range(KC):
                    self.mm(self.bank(0, n)[0:64], wB[:, k, c0:c0 + 64], self.hT[:, k, lo:hi], k == 0, k == KC - 1, [r_wB, self.r_hT], [self.r_ps[0]])
                self.P.op("dve", lambda e: e.memset(araw[0:64, :], 0.0), [], [r_araw])
                if lo > seg_lo:
                    for k in range(KC):
                        self.mm(self.bank(1, 1)[0:64], wB[:, k, c0:c0 + 64], self.hT[:, k, lo - 1:lo], k == 0, k == KC - 1, [r_wB, self.r_hT], [self.r_ps[1]])
                    self.cp(araw[0:64, 0:1], self.bank(1, 1)[0:64], [self.r_ps[1]], [r_araw])
                if hi < seg_hi:
                    for k in range(KC):
                        self.mm(self.bank(1, 1, 8)[0:64], wB[:, k, c0:c0 + 64], self.hT[:, k, hi:hi + 1], k == 0, k == KC - 1, [r_wB, self.r_hT], [self.r_ps[1]])
                    self.cp(araw[0:64, n + 1:n + 2], self.bank(1, 1, 8)[0:64], [self.r_ps[1]], [r_araw])
                self.cp(araw[0:64, 1:n + 1], self.bank(0, n)[0:64], [self.r_ps[0]], [r_araw], eng="act")
                wsl = self.wconvB[:, hh, :]
                self.ts(c1[0:64, 0:n], araw[0:64, 0:n], wsl[0:64, 0:1], ALU.mult, [r_araw, self.r_wcb], [r_c1])
                self.stt(c1[0:64, 0:n], araw[0:64, 1:n + 1], wsl[0:64, 1:2], c1[0:64, 0:n], ALU.mult, ALU.add, [r_araw, self.r_wcb, r_c1], [r_c1])
                self.stt(c1[0:64, 0:n], araw[0:64, 2:n + 2], wsl[0:64, 2:3], c1[0:64, 0:n], ALU.mult, ALU.add, [r_araw, self.r_wcb, r_c1], [r_c1])
                self.act(c1[0:64, 0:n], c1[0:64, 0:n], AF.Silu, [r_c1], [r_c1])
                self.ts(qk[0:64, hh, lo:hi], c1[0:64, 0:n], 1.0 if hh < 4 else 0.125, ALU.mult, [r_c1], [r_qk])
        Gt, r_Gt = self.tile([128, TT, 2, 4], F32, "GtB")
        for ti in range(TT):
            tsl = slice(ti * 128, (ti + 1) * 128)
            for k in range(KC):
                self.mm(self.bank(2, 16), self.hT[:, k, tsl], wB[:, k, 1024:1040], k == 0, k == KC - 1, [self.r_hT, r_wB], [self.r_ps[2]])
            self.tt(G[:, ti, :], self.bank(2, 16), self.bg_b[:, l, :], ALU.add, [self.r_ps[2], self.r_pv], [r_G])
            for k in range(KC):
                self.mm(self.bank(3, 256), self.hT[:, k, tsl], wB[:, k, 512:768], k == 0, k == KC - 1, [self.r_hT, r_wB], [self.r_ps[3]])
            self.cp(Va[:, ti, :, 0:64], self.bank(3, 256).rearrange("p (h d) -> p h d", d=64), [self.r_ps[3]], [r_Va])
            for h in range(4):
                self.tr(self.bank_bf(5)[:, h * 64:(h + 1) * 64], qk[0:64, 4 + h, tsl], self.C["ident_bf"][0:64, 0:64], [r_qk, self.r_c], [self.r_ps[5]])
            self.cp(ktm[:, ti, :], self.bank_bf(5)[:, 0:256], [self.r_ps[5]], [r_ktm])
        G5 = G.rearrange("p t (a b c) -> p t a b c", a=2, b=2)
        for d_ in range(2):
            fv = G5[:, :, d_, 1, :]
            self.act(Gt[:, :, d_, :], fv, AF.Exp, [r_G], [r_Gt], scale=-1.0)
            self.act(Gt[:, :, d_, :], Gt[:, :, d_, :], AF.Ln, [r_Gt], [r_Gt], bias=1.0)
            self.ts(fv, Gt[:, :, d_, :], -1.0, ALU.mult, [r_Gt], [r_G])
        B_TM, M_, NEGM, WIN, EMT, DEN, DAB, RR, WTM, DEC, MX, DENI = range(12)
        SX = []
        for d_ in range(2):
            X = {}
            for nm, shp, dt in (("diag", [128, 4, 128], F32), ("bBm", [128, 4, 128], F32), ("Wt", [128, 4, 128], F32),
                                ("Sb", [128, 4, 128], BF16), ("ST", [128, 4, 128], BF16), ("kw", [128, 4, 64], BF16),
                                ("sv", [128, 16, 4], F32), ("cm", [128, 8], F32), ("mst", [128, 4], F32), ("tmpi", [128, 4, 65], F32),
                                ("numh", [128, 4, 64], F32), ("Cst", [128, 4, 65], F32), ("Cbf", [128, 4, 65], BF16)):
                X[nm], X["r_" + nm] = self.tile(shp, dt, nm + "B%d" % d_)
            X["tri"] = self.C["tri_f" if d_ == 0 else "tri_b"]
            X["mneg"] = self.C["mneg_f" if d_ == 0 else "mneg_b"]
            X["esel"] = self.C["e_last" if d_ == 0 else "e_first"]
            X["b0"] = 4 * d_
            SX.append(X)
            self.P.op("dve", lambda e, t=X["Cst"]: e.memset(t, 0.0), [], [X["r_Cst"]])
            self.P.op("dve", lambda e, t=X["Cbf"]: e.memset(t, 0.0), [], [X["r_Cbf"]])
            self.P.op("dve", lambda e, t=X["mst"]: e.memset(t, 0.0), [], [X["r_mst"]])

        def chunk(d_, ti):
            X = SX[d_]
            diag, bBm, Wt, Sb, ST, kw, sv, cm, mst, tmpi, numh, Cst, Cbf = (X[k] for k in (
                "diag", "bBm", "Wt", "Sb", "ST", "kw", "sv", "cm", "mst", "tmpi", "numh", "Cst", "Cbf"))
            r_diag, r_bBm, r_Wt, r_Sb, r_ST, r_kw, r_sv, r_cm, r_mst, r_tmpi, r_numh, r_Cst, r_Cbf = (X["r_" + k] for k in (
                "diag", "bBm", "Wt", "Sb", "ST", "kw", "sv", "cm", "mst", "tmpi", "numh", "Cst", "Cbf"))
            b0 = X["b0"]
            bB_b, qk_b, st_b, ms_b = b0, b0 + 1, b0 + 2, b0 + 3
            r0, r1, r2, r3 = self.r_ps[bB_b], self.r_ps[qk_b], self.r_ps[st_b], self.r_ps[ms_b]
            cum_ps = self.bank(ms_b, 4)
            sel_ps = self.bank(ms_b, 8, 8)
            inter_ps = self.bank(ms_b, 260, 16)
            upd_ps = self.bank(ms_b, 260, 16)
            num_ps = self.bank(st_b, 256, 256)
            tsl = slice(ti * 128, (ti + 1) * 128)
            li = G[:, ti, d_ * 8:d_ * 8 + 4]
            lf = G[:, ti, d_ * 8 + 4:d_ * 8 + 8]
            self.mm(cum_ps, X["tri"], lf, True, True, [self.r_c, r_G], [r3])
            for h in range(4):
                self.mm(self.bank(qk_b, 128, h * 128), qk[0:64, h, tsl], qk[0:64, 4 + h, tsl], True, True, [r_qk], [r1])
            yield
            self.tt(sv[:, B_TM, :], li, cum_ps, ALU.subtract, [r_G, r3], [r_sv])
            for h in range(4):
                self.ts(diag[:, h, :], self.C["ident_f"], sv[:, B_TM, h:h + 1], ALU.mult, [self.r_c, r_sv], [r_diag])
            yield
            for h in range(4):
                self.mm(self.bank(bB_b, 128, h * 128), self.C["ones_f"], diag[:, h, :], True, True, [self.r_c, r_diag], [r0])
            yield
            self.tt(bBm, self.bank(bB_b).rearrange("p (h s) -> p h s", h=4), X["mneg"], ALU.add, [r0, self.r_c], [r_bBm])
            self.red(sv[:, MX, :], bBm, ALU.max, [r_bBm], [r_sv])
            self.tt(sv[:, M_, :], sv[:, MX, :], mst, ALU.max, [r_sv, r_mst], [r_sv])
            self.ts(sv[:, NEGM, :], sv[:, M_, :], -1.0, ALU.mult, [r_sv], [r_sv])
            self.tt(sv[:, WIN, :], mst, sv[:, M_, :], ALU.subtract, [r_mst, r_sv], [r_sv])
            self.tt(cm[:, 0:4], cum_ps, sv[:, M_, :], ALU.add, [r3, r_sv], [r_cm])
            self.cp(cm[:, 4:8], sv[:, M_, :], [r_sv], [r_cm])
            yield
            for h in range(4):
                self.act(Wt[:, h, :], bBm[:, h, :], AF.Exp, [r_bBm, r_sv], [r_Wt], bias=sv[:, NEGM, h:h + 1], scale=1.0)
            self.act(sv[:, WIN, :], sv[:, WIN, :], AF.Exp, [r_sv], [r_sv])
            self.act(sv[:, EMT, :], cm[:, 0:4], AF.Exp, [r_cm], [r_sv], scale=-1.0)
            self.mm(sel_ps, X["esel"], cm, True, True, [self.r_c, r_cm], [r3])
            yield
            self.tt(Sb, self.bank(qk_b).rearrange("p (h s) -> p h s", h=4), Wt, ALU.mult, [r1, r_Wt], [r_Sb])
            self.red(sv[:, DENI, :], Sb, ALU.add, [r_Sb], [r_sv])
            self.tt(sv[:, WTM, :], sv[:, B_TM, :], self.bank(ms_b, 4, 12), ALU.subtract, [r_sv, r3], [r_sv])
            self.tt(sv[:, DEC, :], mst, self.bank(ms_b, 4, 12), ALU.subtract, [r_mst, r3], [r_sv])
            self.cp(mst, self.bank(ms_b, 4, 8), [r3], [r_mst])
            yield
            for h in range(4):
                self.tr(self.bank_bf(st_b)[:, h * 128:(h + 1) * 128], Sb[:, h, :], self.C["ident_bf"], [r_Sb, self.r_c], [r2])
            for h in range(4):
                self.mm(self.bank(ms_b, 65, 16 + h * 65), qk[0:64, h, tsl], Cbf[0:64, h, :], True, True, [r_qk, r_Cbf], [r3])
            self.act(sv[:, WTM, :], sv[:, WTM, :], AF.Exp, [r_sv], [r_sv])
            self.act(sv[:, DEC, :], sv[:, DEC, :], AF.Exp, [r_sv], [r_sv])
            yield
            self.cp(ST, self.bank_bf(st_b)[:, 0:512].rearrange("p (h s) -> p h s", h=4), [r2], [r_ST])
            self.tt(tmpi, inter_ps.rearrange("p (h e) -> p h e", h=4), sv[:, WIN, :].unsqueeze(2).to_broadcast([128, 4, 65]), ALU.mult,
                    [r3, r_sv], [r_tmpi])
            self.tt(kw, ktm[:, ti, :].rearrange("p (h e) -> p h e", h=4), sv[:, WTM, :].unsqueeze(2).to_broadcast([128, 4, 64]), ALU.mult,
                    [r_ktm, r_sv], [r_kw])
            yield
            for h in range(4):
                self.mm(self.bank(st_b, 64, 256 + h * 64), ST[:, h, :], Va[:, ti, h, 0:64], True, True, [r_ST, r_Va], [r2])
            for h in range(4):
                self.mm(self.bank(ms_b, 65, 16 + h * 65)[0:64], kw[:, h, :], Va[:, ti, h, :], True, True, [r_kw, r_Va], [r3])
            yield
            self.tt(numh, tmpi[:, :, 0:64], num_ps.rearrange("p (h e) -> p h e", h=4), ALU.add, [r_tmpi, r2], [r_numh])
            self.tt(sv[:, DEN, :], tmpi[:, :, 64], sv[:, DENI, :], ALU.add, [r_tmpi, r_sv], [r_sv])
            self.ts(sv[:, DAB, :], sv[:, DEN, :], -1.0, ALU.mult, [r_sv], [r_sv])
            self.tt(sv[:, DAB, :], sv[:, DAB, :], sv[:, DEN, :], ALU.max, [r_sv], [r_sv])
            self.tt(sv[:, DAB, :], sv[:, DAB, :], sv[:, EMT, :], ALU.max, [r_sv], [r_sv])
            self.recip(sv[:, RR, :], sv[:, DAB, :], [r_sv], [r_sv])
            self.tt(numh, numh, sv[:, RR, :].unsqueeze(2).to_broadcast([128, 4, 64]), ALU.mult, [r_numh, r_sv], [r_numh])
            hsv = hs[:, ti, :].rearrange("p (h e) -> p h e", h=4)
            self.tt(hsv, hsv, numh, ALU.add, [r_hs, r_numh], [r_hs])
            self.tt(Cst[0:64], Cst[0:64], sv[0:64, DEC, :].unsqueeze(2).to_broadcast([64, 4, 65]), ALU.mult, [r_Cst, r_sv], [r_Cst])
            self.tt(Cst[0:64], Cst[0:64], upd_ps[0:64].rearrange("p (h e) -> p h e", h=4), ALU.add, [r_Cst, r3], [r_Cst])
            self.cp(Cbf[0:64], Cst[0:64], [r_Cst], [r_Cbf])
            yield

        orders = [list(range(TT)), list(range(CT - 1, -1, -1)) + list(range(TT - 1, CT - 1, -1))]
        for i in range(TT):
            gens = [chunk(0, orders[0][i]), chunk(1, orders[1][i])]
            alive = True
            while alive:
                alive = False
                for g in gens:
                    try:
                        next(g)
                        alive = True
                    except StopIteration:
                        pass
        ytm, r_ytm = self.tile([128, 256], BF16, "ytmB")
        ybuf = self.tile([128, 2, 128], BF16, "yTB")
        for ti in range(CT if last else 0, TT):
            tsl = slice(ti * 128, (ti + 1) * 128)
            for k in range(KC):
                self.mm(self.bank(4, 256), self.hT[:, k, tsl], wB[:, k, 768:1024], k == 0, k == KC - 1, [self.r_hT, r_wB], [self.r_ps[4]])
            self.act(og, self.bank(4, 256), AF.Sigmoid, [self.r_ps[4]], [r_og])
            self.tt(ytm, og, hs[:, ti, :], ALU.mult, [r_og, r_hs], [r_ytm])
            self.store_y_tm(ytm, r_ytm, s, 1, ti, ybuf)

    def post_norm_res(self, s, lo, hi, yT, r_yT, gp_idx, tl):
        sq, r_sq, rs, r_rs, tmp, r_tmp, xb, r_xb = tl
        n = hi - lo
        rsp, r_rsp = self.rstd_block(yT, r_yT, n, KC, D, sq, r_sq, rs, r_rs, 7)
        dv, r_dv = self.seg_dv(lo)
        for k in range(KC):
            self.tt(tmp[:, 0:n], yT[:, k, 0:n], rsp, ALU.mult, [r_yT, r_rsp], [r_tmp])
            self.stt(xb[:, k, 0:n], tmp[:, 0:n], dv[:, gp_idx, k:k + 1], xb[:, k, 0:n], ALU.mult, ALU.add, [r_tmp, r_dv, r_xb], [r_xb])
        self.ld(self.XRES[s][:, lo:hi].rearrange("(k p) n -> p k n", p=128), xb[:, :, 0:n], [r_xb], [self.r_xres[s]])

    def prefetch_x(self, s, lo, hi, tl):
        xb, r_xb = tl[6], tl[7]
        self.ld(xb[:, :, 0:hi - lo], self.XRES[s][:, lo:hi].rearrange("(k p) n -> p k n", p=128), [self.r_xres[s]], [r_xb])

    def pn_tiles(self):
        sq, r_sq = self.tile([128, KC, 512], BF16, "sqP")
        rs, r_rs = self.tile([128, 512], F32, "rsP")
        tmp, r_tmp = self.tile([128, 512], F32, "tmpP")
        xb, r_xb = self.tile([128, KC, 512], F32, "xbP")
        return (sq, r_sq, rs, r_rs, tmp, r_tmp, xb, r_xb)

    def merge(self, s, l, last):
        self.phase()
        ysb, r_ysb = self.tile([128, 8, 512], BF16, "ysb")
        wg = [self.tile([128, 4, KC, 128], BF16, "wg%d" % i) for i in range(2)]
        wbr = [self.tile([128, 4, 2, 128], BF16, "wbr%d" % i) for i in range(2)]
        sig, r_sig = self.tile([128, 512], F32, "sig")
        accf, r_accf = self.tile([128, 512], F32, "accf")
        tmpm, r_tmpm = self.tile([128, 512], F32, "tmpm")
        acc, r_acc = self.tile([128, KC, 512], BF16, "acc")
        wo, r_wo = self.tile([128, KC, D], BF16, "wo")
        yT, r_yT = self.tile([128, KC, 512], F32, "yTm")
        tl = self.pn_tiles()
        self.ld(wo, self.WO[l].rearrange("p (k n) -> p k n", k=KC), [self.r_wbf], [r_wo])
        it = 0
        for (lo, hi) in self.blocks:
            if last and lo < self.NCTX:
                continue
            n = hi - lo
            self.ld(ysb[:, :, 0:n], self.YS[s][:, lo:hi].rearrange("(k p) n -> p k n", p=128), [self.r_ys[s]], [r_ysb])
            self.prefetch_x(s, lo, hi, tl)
            for dc in range(KC):
                (wg_, r_wg), (wb_, r_wb) = wg[it % 2], wbr[it % 2]
                it += 1
                for br in range(4):
                    self.ld(wg_[:, br], self.WG[l, br, dc].rearrange("p (k n) -> p k n", k=KC), [self.r_wbf], [r_wg])
                    self.ld(wb_[:, br], self.WBR[l, br].rearrange("p (k n) -> p k n", k=2)[:, :, dc * 128:(dc + 1) * 128], [self.r_wbf], [r_wb])
                for br in range(4):
                    bg, bp = br % 2, 2 + br % 2
                    for k in range(KC):
                        self.mm(self.bank(bg, n), wg_[:, br, k, :], self.hT[:, k, lo:hi], k == 0, k == KC - 1, [r_wg, self.r_hT], [self.r_ps[bg]])
                    self.act(sig[:, 0:n], self.bank(bg, n), AF.Sigmoid, [self.r_ps[bg], self.r_pv], [r_sig], bias=self.pv["b_gate"][:, l, br, dc:dc + 1], scale=1.0)
                    for k in range(2):
                        self.mm(self.bank(bp, n), wb_[:, br, k, :], ysb[:, br * 2 + k, 0:n], k == 0, k == 1, [r_wb, r_ysb], [self.r_ps[bp]])
                    if br == 0:
                        self.tt(accf[:, 0:n], sig[:, 0:n], self.bank(bp, n), ALU.mult, [r_sig, self.r_ps[bp]], [r_accf])
                    else:
                        self.tt(tmpm[:, 0:n], sig[:, 0:n], self.bank(bp, n), ALU.mult, [r_sig, self.r_ps[bp]], [r_tmpm])
                        if br < 3:
                            self.tt(accf[:, 0:n], accf[:, 0:n], tmpm[:, 0:n], ALU.add, [r_accf, r_tmpm], [r_accf])
                        else:
                            self.tt(acc[:, dc, 0:n], accf[:, 0:n], tmpm[:, 0:n], ALU.add, [r_accf, r_tmpm], [r_acc])
            for d2 in range(KC):
                b = 4 + d2 % 2
                for k in range(KC):
                    self.mm(self.bank(b, n), wo[:, k, d2 * 128:(d2 + 1) * 128], acc[:, k, 0:n], k == 0, k == KC - 1, [r_wo, r_acc], [self.r_ps[b]])
                self.cp(yT[:, d2, 0:n], self.bank(b, n), [self.r_ps[b]], [r_yT], eng="act" if d2 % 2 else "dve")
            self.post_norm_res(s, lo, hi, yT, r_yT, 2, tl)

    def ffn(self, s, l, last):
        self.phase()
        T = self.T
        wd, r_wd = self.tile([128, FC, D], BF16, "wd")
        wu = [self.tile([128, KC, 256], BF16, "wu%d" % i) for i in range(2)]
        gT, r_gT = self.tile([128, FC, 512], BF16, "gT")
        a_sb, r_a = self.tile([128, 514], F32, "a_sb")
        c1, r_c1 = self.tile([128, 512], F32, "c1F")
        yT, r_yT = self.tile([128, KC, 512], F32, "yTf")
        tl = self.pn_tiles()
        self.ld(wd, self.WD[l].rearrange("p (f n) -> p f n", f=FC), [self.r_wbf], [r_wd])
        wcv, bcv = self.pv["w_ffn_conv"], self.pv["b_ffn_conv"]
        it = 0
        for (lo, hi) in self.blocks:
            if last and lo < self.NCTX:
                continue
            n = hi - lo
            seg_lo, seg_hi = (0, self.NCTX) if lo < self.NCTX else (self.NCTX, T)
            self.prefetch_x(s, lo, hi, tl)
            for fc in range(FC):
                w_, r_w = wu[it % 2]
                it += 1
                self.ld(w_, self.WU[l, fc].rearrange("p (k n) -> p k n", k=KC), [self.r_wbf], [r_w])
                ba, bv = fc % 2, 2 + fc % 2
                for k in range(KC):
                    self.mm(self.bank(ba, n), w_[:, k, 0:128], self.hT[:, k, lo:hi], k == 0, k == KC - 1, [r_w, self.r_hT], [self.r_ps[ba]])
                for k in range(KC):
                    self.mm(self.bank(bv, n), w_[:, k, 128:256], self.hT[:, k, lo:hi], k == 0, k == KC - 1, [r_w, self.r_hT], [self.r_ps[bv]])
                self.P.op("dve", lambda e: e.memset(a_sb[:, 0:1], 0.0), [], [r_a])
                self.P.op("dve", lambda e: e.memset(a_sb[:, n + 1:n + 2], 0.0), [], [r_a])
                if lo > seg_lo:
                    for k in range(KC):
                        self.mm(self.bank(6, 1), w_[:, k, 0:128], self.hT[:, k, lo - 1:lo], k == 0, k == KC - 1, [r_w, self.r_hT], [self.r_ps[6]])
                    self.cp(a_sb[:, 0:1], self.bank(6, 1), [self.r_ps[6]], [r_a])
                if hi < seg_hi:
                    for k in range(KC):
                        self.mm(self.bank(6, 1, 8), w_[:, k, 0:128], self.hT[:, k, hi:hi + 1], k == 0, k == KC - 1, [r_w, self.r_hT], [self.r_ps[6]])
                    self.cp(a_sb[:, n + 1:n + 2], self.bank(6, 1, 8), [self.r_ps[6]], [r_a])
                self.cp(a_sb[:, 1:n + 1], self.bank(ba, n), [self.r_ps[ba]], [r_a], eng="act")
                self.act(self.bank(ba, n), self.bank(ba, n), AF.Copy, [self.r_ps[ba], self.r_pv], [self.r_ps[ba]], scale=wcv[:, l, 1, fc:fc + 1])
                self.stt(self.bank(ba, n), a_sb[:, 0:n], wcv[:, l, 0, fc:fc + 1], self.bank(ba, n), ALU.mult, ALU.add, [r_a, self.r_pv, self.r_ps[ba]], [self.r_ps[ba]])
                self.stt(c1[:, 0:n], a_sb[:, 2:n + 2], wcv[:, l, 2, fc:fc + 1], self.bank(ba, n), ALU.mult, ALU.add, [r_a, self.r_pv, self.r_ps[ba]], [r_c1])
                self.act(c1[:, 0:n], c1[:, 0:n], AF.Silu, [r_c1, self.r_pv], [r_c1], bias=bcv[:, l, fc:fc + 1], scale=1.0)
                self.tt(gT[:, fc, 0:n], c1[:, 0:n], self.bank(bv, n), ALU.mult, [r_c1, self.r_ps[bv]], [r_gT])
            for d2 in range(KC):
                b = 4 + d2 % 2
                for fc in range(FC):
                    self.mm(self.bank(b, n), wd[:, fc, d2 * 128:(d2 + 1) * 128], gT[:, fc, 0:n], fc == 0, fc == FC - 1, [r_wd, r_gT], [self.r_ps[b]])
                self.cp(yT[:, d2, 0:n], self.bank(b, n), [self.r_ps[b]], [r_yT], eng="act" if d2 % 2 else "dve")
            self.post_norm_res(s, lo, hi, yT, r_yT, 5, tl)


NLAT_FULL, NCTX_FULL, DEPTH = 2048, 256, 2
_cache = {}


def run_device(inp, NLAT, NCTX, B, L, n_cores):
    NSEQ = B // n_cores
    consts = make_consts(NLAT, NCTX)
    key = (NLAT, NCTX, NSEQ, L)
    bld = Builder(NLAT, NCTX, NSEQ, L, consts)
    nc = bld.build()
    x = np.asarray(inp["x"], np.float32)
    ctx = np.asarray(inp["ctx"], np.float32)
    c = np.asarray(inp["c"], np.float32)
    c_ctx = np.asarray(inp["c_ctx"], np.float32)
    params = layout_params(inp, L)
    wml = np.asarray(inp["w_ml_conv"], np.float32)
    params["wconvB"] = np.ascontiguousarray(wml.reshape(L, 3, 8, 64).transpose(0, 3, 2, 1))
    shared = {k: np.ascontiguousarray(np.asarray(inp[k], np.float32)) for k in W_SHAPES}
    shared.update(params)
    shared.update(consts)
    in_maps = []
    for ci in range(n_cores):
        sl = slice(ci * NSEQ, (ci + 1) * NSEQ)
        cc = np.concatenate([c[sl], c_ctx[None]], 0)
        m = dict(shared)
        m["xT"] = np.ascontiguousarray(x[sl].transpose(0, 2, 1))
        m["ctxT"] = np.ascontiguousarray(ctx[sl].transpose(0, 2, 1))
        m["ccT"] = np.ascontiguousarray(cc.T.reshape(KC, 128, NSEQ + 1).transpose(1, 0, 2))
        in_maps.append(m)
    return nc, in_maps


def kernel(**inp):
    n_cores = 8
    nc, in_maps = run_device(inp, NLAT_FULL, NCTX_FULL, 16, DEPTH, n_cores)
    res = run_bass_kernel_spmd(nc, in_maps, core_ids=list(range(n_cores)))
    outs = [np.asarray(r["outT"]).transpose(0, 2, 1) for r in res.results]
    return np.ascontiguousarray(np.concatenate(outs, 0).astype(np.float32))
```

```python
from contextlib import ExitStack
import numpy as np
import ml_dtypes
import concourse.bass as bass
import concourse.mybir as mybir
from concourse.bass_utils import run_bass_kernel_spmd

F32 = mybir.dt.float32
BF16 = mybir.dt.bfloat16
AF = mybir.ActivationFunctionType
ALU = mybir.AluOpType
AX = mybir.AxisListType
ENG = ("pe", "act", "dve", "pool", "sp")
NBIG = -30000.0
D = 1024
KC = 8
DFF = 2816
FC = 22
EPS = 1e-6


class Res:
    __slots__ = ("name", "w", "re", "rd")

    def __init__(self, name=""):
        self.name = name
        self.w = None
        self.re = {}
        self.rd = []


class Prog:
    N_DMA_SEMS = 80

    def __init__(self, nc, stack):
        self.nc = nc
        self.esem = {e: stack.enter_context(nc.semaphore("es_" + e)) for e in ENG}
        self.dsem = [stack.enter_context(nc.semaphore("ds%d" % i)) for i in range(self.N_DMA_SEMS)]
        self.dval = [0] * self.N_DMA_SEMS
        self.dnext = 0
        self.cnt = {e: 0 for e in ENG}
        self.seen = {e: {} for e in ENG}
        self.q = {e: [] for e in ENG}
        self.n_ops = 0

    def _deps(self, reads, writes):
        deps = []
        for r in reads:
            if r.w is not None:
                deps.append(r.w)
        for w in writes:
            if w.w is not None:
                deps.append(w.w)
            for e, c in w.re.items():
                deps.append(("E", e, c))
            deps.extend(w.rd)
        return deps

    def _waits(self, eng, deps):
        seen = self.seen[eng]
        need = {}
        for kind, key, val in deps:
            k = (kind, key)
            if seen.get(k, 0) >= val:
                continue
            if kind == "E" and key == "pe" and eng == "pe":
                continue
            if need.get(k, 0) < val:
                need[k] = val
        out = []
        for k, val in need.items():
            seen[k] = val
            sem = self.esem[k[1]] if k[0] == "E" else self.dsem[k[1]]
            out.append((sem, val))
        return out

    def op(self, eng, fn, reads=(), writes=()):
        waits = self._waits(eng, self._deps(reads, writes))
        self.cnt[eng] += 1
        c = self.cnt[eng]
        tok = ("E", eng, c)
        self.q[eng].append((waits, fn, (self.esem[eng], 1)))
        for r in reads:
            if r.re.get(eng, 0) < c:
                r.re[eng] = c
        for w in writes:
            w.w = tok
            w.re = {}
            w.rd = []
        self.n_ops += 1
        return tok

    def dma(self, qeng, out, in_, reads=(), writes=(), **kw):
        deps = self._deps(reads, writes)
        i = self.dnext
        self.dnext = (self.dnext + 1) % self.N_DMA_SEMS
        prev = self.dval[i]
        if prev:
            deps.append(("D", i, prev))
        waits = self._waits(qeng, deps)
        self.dval[i] = prev + 16
        tok = ("D", i, prev + 16)
        self.q[qeng].append((waits, (lambda e, o=out, s=in_, k=kw: e.dma_start(out=o, in_=s, **k)),
                             (self.dsem[i], 16)))
        for r in reads:
            r.rd.append(tok)
        for w in writes:
            w.w = tok
            w.re = {}
            w.rd = []
        self.n_ops += 1
        return tok

    def barrier(self):
        deps = [("E", e, self.cnt[e]) for e in ENG if self.cnt[e]]
        deps += [("D", i, v) for i, v in enumerate(self.dval) if v]
        for e in ENG:
            waits = self._waits(e, deps)
            if waits:
                self.q[e].append((waits, None, None))

    def emit(self):
        nc = self.nc
        allsems = [self.esem[e] for e in ENG] + self.dsem
        with nc.Block("init") as b0:
            @b0.vector
            def _(v):
                for s in allsems:
                    v.sem_clear(s)
        with nc.Block("main") as blk:
            def run(e, name):
                for waits, fn, inc in self.q[name]:
                    for sem, val in waits:
                        e.wait_ge(sem, val)
                    if fn is not None:
                        fn(e).then_inc(inc[0], inc[1])

            @blk.tensor
            def _(e):
                run(e, "pe")

            @blk.scalar
            def _(e):
                run(e, "act")

            @blk.vector
            def _(e):
                run(e, "dve")

            @blk.gpsimd
            def _(e):
                run(e, "pool")

            @blk.sync
            def _(e):
                run(e, "sp")


def _rope_tab(rd, pos_row, pos_col, n_ctx):
    half = rd // 2
    nf = half // 2
    inv = 10000.0 ** (-np.arange(nf, dtype=np.float64) / nf)
    n = len(pos_row)
    cos = np.ones((rd, n_ctx + n), np.float64)
    sin = np.zeros((rd, n_ctx + n), np.float64)
    for r in range(rd):
        hh, rr = r // half, r % half
        idx = rr % nf
        pos = pos_row if hh == 0 else pos_col
        ang = pos.astype(np.float64) * inv[idx]
        ang = (pos.astype(np.float32) * np.float32(inv[idx]).astype(np.float32)).astype(np.float64)
        cos[r, n_ctx:] = np.cos(ang)
        sin[r, n_ctx:] = np.sin(ang)
    return cos, sin


def make_consts(NLAT, NCTX):
    bf = ml_dtypes.bfloat16
    T = NLAT + NCTX
    c = {}
    c["ident_bf"] = np.eye(128).astype(bf)
    c["ones_bf"] = np.ones((128, 128)).astype(bf)
    c["ident_f"] = np.eye(128, dtype=np.float32)
    c["ones_f"] = np.ones((128, 128), np.float32)
    s = np.arange(128)[:, None]
    t = np.arange(128)[None, :]
    c["tri_f"] = (s <= t).astype(np.float32)
    c["tri_b"] = (s >= t).astype(np.float32)
    mf = np.where(t <= s, 0.0, NBIG).astype(np.float32)
    mb = np.where(t >= s, 0.0, NBIG).astype(np.float32)
    c["mneg_f"] = np.repeat(mf[:, None, :], 4, axis=1).copy()
    c["mneg_b"] = np.repeat(mb[:, None, :], 4, axis=1).copy()
    el = np.zeros((128, 128), np.float32); el[127, :] = 1
    ef = np.zeros((128, 128), np.float32); ef[0, :] = 1
    c["e_last"] = el
    c["e_first"] = ef
    c["maskA"] = np.where(t >= s, 0.0, NBIG).astype(bf)
    c["maskB"] = np.where(t <= s, 0.0, NBIG).astype(bf)
    c["maskN"] = np.full((128, 128), NBIG).astype(bf)
    rows = NLAT // 64
    pr = np.repeat(np.arange(rows), 64)
    pc = np.tile(np.arange(64), rows)
    ca, sa = _rope_tab(32, pr, pc, NCTX)
    sc_a = 96.0 ** -0.5
    cosq = np.ones((96, T)); sinq = np.zeros((96, T))
    cosq[64:] = ca; sinq[64:] = sa
    c["cosq_a"] = (cosq * sc_a).astype(np.float32)
    c["sinq_a"] = (sinq * sc_a).astype(np.float32)
    c["cosk_a"] = ca.astype(np.float32)
    c["sink_a"] = sa.astype(np.float32)
    cc, sc = _rope_tab(64, pr, pc, NCTX)
    c["cos_c"] = cc.astype(np.float32)
    c["sin_c"] = sc.astype(np.float32)
    j = np.arange(64)
    Cc = np.cos(2 * np.pi * np.outer(j, j) / 64)
    Sc = np.sin(2 * np.pi * np.outer(j, j) / 64)
    z = np.zeros((64, 64))
    c["bdc"] = np.block([[Cc, z], [z, Cc]]).astype(bf)
    c["bds"] = (-np.block([[Sc, z], [z, Sc]])).astype(bf)
    for nm, N in (("lat", NLAT), ("ctx", NCTX)):
        n = np.arange(N)
        ph = (np.outer(n, n) % N).astype(np.float64) * (2 * np.pi / N)
        nrm = 1.0 / np.sqrt(N * 64.0)
        c["cn_" + nm] = (np.cos(ph) * nrm).astype(bf)
        c["sn_" + nm] = (np.sin(ph) * nrm).astype(bf)
    return c


CONST_DT = {"ident_bf": BF16, "ones_bf": BF16, "maskA": BF16, "maskB": BF16, "maskN": BF16, "bdc": BF16, "bds": BF16,
            "cn_lat": BF16, "sn_lat": BF16, "cn_ctx": BF16, "sn_ctx": BF16}

W_SHAPES = {
    "w_mod": (D, 6 * D), "w_in": (D, 2352), "w_uq": (256, 384), "w_ukv": (256, 512),
    "w_gate": (4, D, D), "w_branch": (4, 256, D), "w_out": (D, D), "w_up": (D, 2 * DFF), "w_down": (DFF, D),
}


def layout_params(inp, L):
    o = {}

    def fm(v, k):
        v = np.asarray(v, np.float32)
        lead = v.shape[:-1]
        return np.ascontiguousarray(np.moveaxis(v.reshape(lead + (k, 128)), -1, 0))

    o["b_mod"] = np.stack([fm(inp["b_mod"][l], 48) for l in range(L)])
    for nm in ("g_pre_mix", "g_post_mix", "g_pre_ffn", "g_post_ffn"):
        o[nm] = np.stack([fm(inp[nm][l], 8) for l in range(L)])
    o["g_qa"] = np.stack([fm(inp["g_qa"][l], 2) for l in range(L)])
    o["g_kva"] = np.stack([fm(inp["g_kva"][l], 2) for l in range(L)])
    o["b_gate"] = np.stack([fm(inp["b_gate"][l], 8) for l in range(L)])
    o["w_ffn_conv"] = np.stack([fm(inp["w_ffn_conv"][l], FC) for l in range(L)])
    o["b_ffn_conv"] = np.stack([fm(inp["b_ffn_conv"][l], FC) for l in range(L)])
    o["w_ml_conv"] = np.stack([fm(inp["w_ml_conv"][l], 4) for l in range(L)])
    o["b_ml_gates"] = np.asarray(inp["b_ml_gates"], np.float32).reshape(L, 1, 16)
    o["wg_sink"] = np.asarray(inp["wg_sink"], np.float32).reshape(L, 1, 4)
    return o


PARAM_SHAPES = lambda L: {
    "b_mod": (L, 128, 48), "g_pre_mix": (L, 128, 8), "g_post_mix": (L, 128, 8), "g_pre_ffn": (L, 128, 8),
    "g_post_ffn": (L, 128, 8), "g_qa": (L, 128, 2), "g_kva": (L, 128, 2), "b_gate": (L, 128, 4, 8),
    "w_ffn_conv": (L, 128, 3, FC), "b_ffn_conv": (L, 128, FC), "w_ml_conv": (L, 128, 3, 4),
    "b_ml_gates": (L, 1, 16), "wg_sink": (L, 1, 4), "wconvB": (L, 64, 8, 3),
}


class Builder:
    def __init__(self, NLAT, NCTX, NSEQ, L, consts, dbg=None):
        self.NLAT, self.NCTX, self.NSEQ, self.L = NLAT, NCTX, NSEQ, L
        self.T = T = NLAT + NCTX
        self.TT = T // 128
        self.CT = NCTX // 128
        self.blocks = [(0, NCTX)] + [(NCTX + i, min(NCTX + i + 512, T)) for i in range(0, NLAT, 512)]
        self.dbg = dbg
        nc = self.nc = bass.Bass("TRN2", target_bir_lowering=False)
        self.st = ExitStack()
        self.P = Prog(nc, self.st)
        di = lambda n, s, dt=F32: nc.dram_tensor(n, list(s), dt, kind="ExternalInput").ap()
        self.xT_in = di("xT", (NSEQ, D, NLAT))
        self.cT_in = di("ctxT", (NSEQ, D, NCTX))
        self.ccT = di("ccT", (128, KC, NSEQ + 1))
        self.W = {k: di(k, (L,) + v) for k, v in W_SHAPES.items()}
        self.PR = {k: di(k, v) for k, v in PARAM_SHAPES(L).items()}
        self.CD = {k: di(k, v.shape, CONST_DT.get(k, F32)) for k, v in consts.items()}
        self.out = nc.dram_tensor("outT", [NSEQ, D, NLAT], F32, kind="ExternalOutput").ap()
        ds = lambda n, s, dt: nc.dram_tensor(n, list(s), dt, kind="Internal").ap()
        self.XRES = ds("xres", (NSEQ, D, T), F32)
        self.YS = ds("ys", (NSEQ, D, T), BF16)
        self.WI = ds("wi_bf", (L, 128, KC * 2352), BF16)
        self.WG = ds("wg_bf", (L, 4, KC, 128, KC * 128), BF16)
        self.WBR = ds("wbr_bf", (L, 4, 128, 2 * D), BF16)
        self.WO = ds("wo_bf", (L, 128, KC * D), BF16)
        self.WU = ds("wu_bf", (L, FC, 128, KC * 256), BF16)
        self.WD = ds("wd_bf", (L, 128, FC * D), BF16)
        self.r_wbf = Res("wbf")
        self.r_xres = [Res("xres%d" % i) for i in range(NSEQ)]
        self.r_ys = [Res("ys%d" % i) for i in range(NSEQ)]
        self.r_out = Res("out")
        if dbg:
            self.dbg_out = {k: nc.dram_tensor("dbg_" + k, list(s), dt, kind="ExternalOutput").ap() for k, (s, dt) in dbg.items()}
        self.PS = nc.alloc_psum_tensor("PS", [128, 4096], F32)
        self.r_ps = [Res("ps%d" % i) for i in range(8)]
        self.ARENA_E = 98000
        self.arena = nc.alloc_sbuf_tensor("arena", [128, self.ARENA_E], BF16)
        self.a_off = 0
        self.a_base = 0
        self.phase_log = []

    def tile(self, shape, dt, name=""):
        n = int(np.prod(shape[1:]))
        ne = n * (2 if dt == F32 else 1)
        off = (self.a_off + 15) // 16 * 16
        assert off + ne <= self.ARENA_E, ("arena overflow", name, off, ne)
        self.a_off = off + ne
        ap = self.arena[0:shape[0], off:off + ne]
        if dt == F32:
            ap = ap.bitcast(F32)
        if len(shape) == 3:
            ap = ap.rearrange("p (a b) -> p a b", a=shape[1])
        elif len(shape) == 4:
            ap = ap.rearrange("p (a b c) -> p a b c", a=shape[1], b=shape[2])
        return ap, Res(name)

    def phase(self):
        self.P.barrier()
        self.a_off = self.a_base
        import sys
        self.phase_log.append((sys._getframe(1).f_code.co_name, dict(self.P.cnt)))

    def bank(self, b, n=512, lo=0):
        return self.PS[:, b * 512 + lo:b * 512 + lo + n]

    def bank_bf(self, b):
        return self.PS[:, b * 512:(b + 1) * 512].bitcast(BF16)

    def mm(self, out, lhsT, rhs, start, stop, reads, writes):
        self.P.op("pe", lambda e: e.matmul(out, lhsT, rhs, start=start, stop=stop, skip_group_check=True), reads, writes)

    def tr(self, out, in_, ident, reads, writes):
        self.P.op("pe", lambda e: e.transpose(out, in_, ident), reads, writes)

    def act(self, out, in_, func, reads, writes, bias=None, scale=None):
        kw = {}
        if bias is not None:
            kw["bias"] = bias
        if scale is not None:
            kw["scale"] = scale
        self.P.op("act", lambda e: e.activation(out=out, in_=in_, func=func, **kw), reads, writes)

    def tt(self, out, a, b, op, reads, writes, eng="dve"):
        self.P.op(eng, lambda e: e.tensor_tensor(out=out, in0=a, in1=b, op=op), reads, writes)

    def ts(self, out, a, s1, op0, reads, writes, s2=None, op1=None, eng="dve"):
        if op1 is None:
            self.P.op(eng, lambda e: e.tensor_scalar(out=out, in0=a, scalar1=s1, scalar2=None, op0=op0), reads, writes)
        else:
            self.P.op(eng, lambda e: e.tensor_scalar(out=out, in0=a, scalar1=s1, scalar2=s2, op0=op0, op1=op1), reads, writes)

    def stt(self, out, a, s, b, op0, op1, reads, writes):
        self.P.op("dve", lambda e: e.scalar_tensor_tensor(out=out, in0=a, scalar=s, in1=b, op0=op0, op1=op1), reads, writes)

    def cp(self, out, in_, reads, writes, eng="dve"):
        if eng == "act":
            self.P.op("act", lambda e: e.copy(out=out, in_=in_), reads, writes)
        else:
            self.P.op(eng, lambda e: e.tensor_copy(out=out, in_=in_), reads, writes)

    def red(self, out, in_, op, reads, writes):
        self.P.op("dve", lambda e: e.tensor_reduce(out=out, in_=in_, axis=AX.X, op=op), reads, writes)

    def recip(self, out, in_, reads, writes):
        self.P.op("dve", lambda e: e.reciprocal(out=out, in_=in_), reads, writes)

    def ld(self, out, in_, reads, writes, q="sp"):
        self.P.dma(q, out, in_, reads, writes)

    def prepass(self):
        SZ = 2560
        stf = [self.tile([128, SZ], F32, "stf%d" % i) for i in range(3)]
        stb = [self.tile([128, SZ], BF16, "stb%d" % i) for i in range(3)]
        pieces = []

        def piece(srcs, dst, a, b):
            pieces.append((srcs, dst, a, b))

        def views(j):
            srcs, dst, a, b = pieces[j]
            (f, r_f), (bt, r_b) = stf[j % 3], stb[j % 3]
            fv = f[:, 0:a * b].rearrange("p (a b) -> p a b", a=a)
            bv = bt[:, 0:a * b].rearrange("p (a b) -> p a b", a=a)
            return srcs, dst, fv, bv, r_f, r_b

        def emit_in(j):
            srcs, dst, fv, bv, r_f, r_b = views(j)
            for (c0, c1, src) in srcs:
                self.ld(fv[:, :, c0:c1], src, [], [r_f])

        def emit_rest(j):
            srcs, dst, fv, bv, r_f, r_b = views(j)
            self.cp(bv, fv, [r_f], [r_b], eng=("act", "dve", "pool")[j % 3])
            self.ld(dst, bv, [r_b], [self.r_wbf])

        for l in range(self.L):
            wi = self.W["w_in"][l].rearrange("(k p) n -> p k n", p=128)
            wid = self.WI[l].rearrange("p (k n) -> p k n", k=KC)
            for k in range(KC):
                piece([(0, 2352, wi[:, k:k + 1, :])], wid[:, k:k + 1, :], 1, 2352)
            for br in range(4):
                for dc in range(KC):
                    src = self.W["w_gate"][l][br][:, dc * 128:(dc + 1) * 128].rearrange("(k p) n -> p k n", p=128)
                    piece([(0, 128, src)], self.WG[l, br, dc].rearrange("p (k n) -> p k n", k=KC), KC, 128)
                src = self.W["w_branch"][l][br].rearrange("(k p) n -> p k n", p=128)
                piece([(0, D, src)], self.WBR[l, br].rearrange("p (k n) -> p k n", k=2), 2, D)
            wo = self.W["w_out"][l].rearrange("(k p) n -> p k n", p=128)
            wod = self.WO[l].rearrange("p (k n) -> p k n", k=KC)
            for k in range(0, KC, 2):
                piece([(0, D, wo[:, k:k + 2, :])], wod[:, k:k + 2, :], 2, D)
            for fc in range(FC):
                sa = self.W["w_up"][l][:, fc * 128:(fc + 1) * 128].rearrange("(k p) n -> p k n", p=128)
                sv = self.W["w_up"][l][:, DFF + fc * 128:DFF + (fc + 1) * 128].rearrange("(k p) n -> p k n", p=128)
                piece([(0, 128, sa), (128, 256, sv)], self.WU[l, fc].rearrange("p (k n) -> p k n", k=KC), KC, 256)
            wdn = self.W["w_down"][l].rearrange("(f p) n -> p f n", p=128)
            wdd = self.WD[l].rearrange("p (f n) -> p f n", f=FC)
            for f0 in range(0, FC, 2):
                piece([(0, D, wdn[:, f0:f0 + 2, :])], wdd[:, f0:f0 + 2, :], 2, D)
        npc = len(pieces)
        for j in range(npc + 2):
            if j < npc:
                emit_in(j)
            if j - 2 >= 0:
                emit_rest(j - 2)
            yield

    def build(self):
        P = self.P
        NSEQ, L, T = self.NSEQ, self.L, self.T
        C = {}
        self.C = C
        r_c = self.r_c = Res("consts")
        for k in ("ident_bf", "ones_bf", "ident_f", "ones_f", "tri_f", "tri_b", "e_last", "e_first", "maskA", "maskB", "maskN", "bdc", "bds"):
            C[k], _ = self.tile([128, 128], CONST_DT.get(k, F32), k)
            self.ld(C[k], self.CD[k], [], [r_c])
        for k in ("mneg_f", "mneg_b"):
            C[k], _ = self.tile([128, 4, 128], F32, k)
            self.ld(C[k], self.CD[k], [], [r_c])
        self.hT, self.r_hT = self.tile([128, KC, T], BF16, "hT")
        self.MOD, self.r_mod = self.tile([128, L, 48, NSEQ + 1], F32, "MOD")
        self.pv = {}
        self.r_pv = Res("pvec")
        for k, shp in PARAM_SHAPES(L).items():
            if shp[1] == 128:
                self.pv[k], _ = self.tile([128, L] + list(shp[2:]) if len(shp) > 2 else [128, L], F32, k)
                src = self.PR[k]
                if len(shp) == 3:
                    self.ld(self.pv[k], src.rearrange("l p a -> p l a"), [], [self.r_pv])
                else:
                    for l in range(L):
                        self.ld(self.pv[k][:, l], src[l], [], [self.r_pv])
        self.bg_b, _ = self.tile([128, L, 16], F32, "bgb")
        self.sink_b, _ = self.tile([128, L, 4], F32, "sinkb")
        for l in range(L):
            self.ld(self.bg_b[:, l, :], self.PR["b_ml_gates"][l].partition_broadcast(128), [], [self.r_pv])
            self.ld(self.sink_b[:, l, :], self.PR["wg_sink"][l].partition_broadcast(128), [], [self.r_pv])
        self.wconvB, _ = self.tile([128, 8, 3], F32, "wconvB")
        self.r_wcb = Res("wcb")
        self.dv, self.r_dv = self.tile([128, 6, KC], F32, "derived")
        self.dvc, self.r_dvc = self.tile([128, 6, KC], F32, "derivedc")
        self.a_base = self.a_off
        for s in range(NSEQ):
            self.ld(self.XRES[s][:, 0:self.NCTX], self.cT_in[s], [], [self.r_xres[s]])
            self.ld(self.XRES[s][:, self.NCTX:T], self.xT_in[s], [], [self.r_xres[s]])
        self.phase()
        pg = self.prepass()
        self.mod_phase(pg)
        for _ in pg:
            pass
        import os
        stop = int(os.environ.get("KSTOP", "99"))
        for s in range(NSEQ):
            for l in range(L):
                last = (l == L - 1)
                steps = [lambda: self.derive(l, s), lambda: self.norm_mod(s, 0), lambda: self.branch_a(s, l, last),
                         lambda: self.branch_c(s, l, last), lambda: None, lambda: self.branch_b(s, l, last),
                         lambda: self.merge(s, l, last), lambda: self.norm_mod(s, 3, skip_ctx=last), lambda: self.ffn(s, l, last)]
                for i, f in enumerate(steps):
                    if i < stop:
                        f()
            self.phase()
            self.ld(self.out[s], self.XRES[s][:, self.NCTX:T], [self.r_xres[s]], [self.r_out])
        P.barrier()
        P.emit()
        self.st.close()
        return self.nc

    def mod_phase(self, pg):
        next(pg, None)
        NJ = self.NSEQ + 1
        cc, r_cc = self.tile([128, KC, NJ], F32, "cc")
        sg, r_sg = self.tile([128, KC, NJ], F32, "sg")
        self.ld(cc, self.ccT, [], [r_cc])
        self.act(sg, cc, AF.Sigmoid, [r_cc], [r_sg])
        self.tt(cc, cc, sg, ALU.mult, [r_sg, r_cc], [r_cc])
        wm = [self.tile([128, KC, 512], F32, "wm%d" % i) for i in range(2)]
        n = 0
        for l in range(self.L):
            for g in range(12):
                w, r_w = wm[n % 2]
                n += 1
                self.ld(w, self.W["w_mod"][l][:, g * 512:(g + 1) * 512].rearrange("(k p) n -> p k n", p=128), [], [r_w])
                for _ in range(7):
                    next(pg, None)
                b = n % 2
                for j4 in range(4):
                    for k in range(KC):
                        self.mm(self.bank(b, NJ, j4 * 8), w[:, k, j4 * 128:(j4 + 1) * 128], cc[:, k, :], k == 0 and j4 == 0, k == KC - 1,
                                [r_w, r_cc], [self.r_ps[b]])
                for j4 in range(4):
                    ch = g * 4 + j4
                    self.ts(self.MOD[:, l, ch, :], self.bank(b, NJ, j4 * 8), self.pv["b_mod"][:, l, ch:ch + 1], ALU.add,
                            [self.r_ps[b], self.r_pv], [self.r_mod])

    def derive(self, l, s):
        for (dst, r_dst, j) in ((self.dv, self.r_dv, s), (self.dvc, self.r_dvc, self.NSEQ)):
            for half, gpre, gpost in ((0, "g_pre_mix", "g_post_mix"), (1, "g_pre_ffn", "g_post_ffn")):
                sh = self.MOD[:, l, half * 24 + 0:half * 24 + 8, j]
                sc = self.MOD[:, l, half * 24 + 8:half * 24 + 16, j]
                g = self.MOD[:, l, half * 24 + 16:half * 24 + 24, j]
                self.stt(dst[:, half * 3 + 0, :], sc, 1.0, self.pv[gpre][:, l, :], ALU.add, ALU.mult, [self.r_mod, self.r_pv], [r_dst])
                self.cp(dst[:, half * 3 + 1, :], sh, [self.r_mod], [r_dst])
                self.tt(dst[:, half * 3 + 2, :], g, self.pv[gpost][:, l, :], ALU.mult, [self.r_mod, self.r_pv], [r_dst])

    def conv_blocks(self, skip_ctx=False):
        out = []
        segs = ([] if skip_ctx else [(0, self.NCTX)]) + [(self.NCTX, self.T)]
        for (s0, s1) in segs:
            lo = s0
            while lo < s1:
                hi = min(lo + 510, s1)
                out.append((lo, hi, lo - (1 if lo > s0 else 0), hi + (1 if hi < s1 else 0)))
                lo = hi
        return out

    def seg_dv(self, lo):
        return (self.dvc, self.r_dvc) if lo < self.NCTX else (self.dv, self.r_dv)

    def rstd_block(self, src, r_src, n, nk, dim, sq, r_sq, rs, r_rs, bank):
        for k in range(nk):
            self.act(sq[:, k, 0:n], src[:, k, 0:n], AF.Square, [r_src], [r_sq])
        for k in range(nk):
            self.mm(self.bank(bank, n), self.C["ones_bf"], sq[:, k, 0:n], k == 0, k == nk - 1, [r_sq, self.r_c], [self.r_ps[bank]])
        self.ts(rs[:, 0:n], self.bank(bank, n), 1.0 / dim, ALU.mult, [self.r_ps[bank]], [r_rs], s2=EPS, op1=ALU.add)
        self.act(rs[:, 0:n], rs[:, 0:n], AF.Ln, [r_rs], [r_rs])
        self.act(self.bank(bank, n), rs[:, 0:n], AF.Exp, [r_rs], [self.r_ps[bank]], scale=-0.5)
        return self.bank(bank, n), self.r_ps[bank]

    def norm_mod(self, s, base, skip_ctx=False):
        self.phase()
        xb = [self.tile([128, KC, 512], F32, "xb%d" % i) for i in range(2)]
        sqs = [self.tile([128, KC, 512], BF16, "sq%d" % i) for i in range(2)]
        rss = [self.tile([128, 512], F32, "rs%d" % i) for i in range(2)]
        tmps = [self.tile([128, 512], F32, "tmp%d" % i) for i in range(2)]
        for bi, (lo, hi) in enumerate(self.blocks):
            if skip_ctx and lo < self.NCTX:
                continue
            n = hi - lo
            x, r_x = xb[bi % 2]
            (sq, r_sq), (rs, r_rs) = sqs[bi % 2], rss[bi % 2]
            self.ld(x[:, :, 0:n], self.XRES[s][:, lo:hi].rearrange("(k p) n -> p k n", p=128), [self.r_xres[s]], [r_x])
            rsp, r_rsp = self.rstd_block(x, r_x, n, KC, D, sq, r_sq, rs, r_rs, bi % 2)
            dv, r_dv = self.seg_dv(lo)
            for k in range(KC):
                tmp, r_tmp = tmps[k % 2]
                self.tt(tmp[:, 0:n], x[:, k, 0:n], rsp, ALU.mult, [r_x, r_rsp], [r_tmp])
                self.act(self.hT[:, k, lo:hi], tmp[:, 0:n], AF.Identity, [r_tmp, r_dv], [self.r_hT],
                         bias=dv[:, base + 1, k:k + 1], scale=dv[:, base + 0, k:k + 1])

    def load_w_cols(self, dst, r_dst, l, c0, c1):
        self.ld(dst, self.WI[l].rearrange("p (k n) -> p k n", k=KC)[:, :, c0:c1], [self.r_wbf], [r_dst])

    def make_perm(self, dst, src, nheads, hd, r0, rd, r_dst, r_src, nk):
        nf = rd // 4
        self.P.op("dve", lambda e: e.memset(dst, 0.0), [], [r_dst])
        for k in range(nk):
            for h in range(nheads):
                b = h * hd + r0
                for hh in range(2):
                    o = b + hh * 2 * nf
                    self.ts(dst[:, k, o:o + nf], src[:, k, o + nf:o + 2 * nf], -1.0, ALU.mult, [r_src], [r_dst])
                    self.cp(dst[:, k, o + nf:o + 2 * nf], src[:, k, o:o + nf], [r_src], [r_dst])

    def attn_scores(self, it, buf):
        Pm, r_Pm, sm, r_sm = buf
        kparts, sink, scale = it["kparts"], it["sink"], it["scale"]
        pc = it.get("pcol", 0)
        ncols = max(c0 + k.shape[-1] for (k, _, c0, _) in kparts)
        started = set()
        for (kT, r_k, c0, mask) in kparts:
            n = kT.shape[-1]
            b = (pc + c0) // 512
            assert (pc + c0 + n - 1) // 512 == b
            self.mm(self.PS[:, pc + c0:pc + c0 + n], it["q"], kT, b not in started, mask is None, [it["r_q"], r_k], [self.r_ps[b]])
            started.add(b)
            if mask is not None:
                self.mm(self.PS[:, pc + c0:pc + c0 + n], self.C["ident_bf"], mask, False, True, [self.r_c], [self.r_ps[b]])
        tot = ncols
        if sink is not None:
            b = (pc + ncols) // 512
            self.mm(self.PS[:, pc + ncols:pc + ncols + 1], self.C["ones_f"][0:1, :], sink, b not in started, True, [self.r_c, self.r_pv], [self.r_ps[b]])
            started.add(b)
            tot = ncols + 1
        banks = [self.r_ps[b] for b in sorted(started)]
        self.red(sm[:, 0:1], self.PS[:, pc:pc + tot], ALU.max, banks, [r_sm])
        self.ts(sm[:, 1:2], sm[:, 0:1], -scale, ALU.mult, [r_sm], [r_sm])
        it["ncols"] = ncols
        it["exp"] = (lambda: self.act(Pm[:, 0:tot], self.PS[:, pc:pc + tot], AF.Exp, banks + [r_sm], [r_Pm], bias=sm[:, 1:2], scale=scale))

    def attn_transposes(self, it, buf, PTt, r_PT):
        Pm, r_Pm, sm, r_sm = buf
        vparts = it["vparts"]
        nv = len(vparts)
        for i, (V, r_v, c0) in enumerate(vparts):
            tb = 5 + (i // 8) % 2
            slot = i % 8
            pt_ps = self.bank_bf(tb)[:, slot * 128:(slot + 1) * 128]
            self.tr(pt_ps, Pm[:, c0:c0 + 128], self.C["ident_bf"], [r_Pm, self.r_c], [self.r_ps[tb]])
            if slot == 7 or i == nv - 1:
                g0 = i - slot
                self.cp(PTt[:, g0:i + 1, :], self.bank_bf(tb)[:, 0:(slot + 1) * 128].rearrange("p (a b) -> p a b", b=128),
                        [self.r_ps[tb]], [r_PT], eng="act")

    def attn_pv(self, it, buf, PTt, r_PT):
        Pm, r_Pm, sm, r_sm = buf
        vparts, sink, ncols = it["vparts"], it["sink"], it["ncols"]
        nv = len(vparts)
        for i, (V, r_v, c0) in enumerate(vparts):
            self.mm(self.bank(7, 65), PTt[:, i, :], V, i == 0, i == nv - 1, [r_PT, r_v], [self.r_ps[7]])
        if sink is not None:
            self.tt(sm[:, 2:3], self.bank(7, 1, 64), Pm[:, ncols:ncols + 1], ALU.add, [self.r_ps[7], r_Pm], [r_sm])
            self.recip(sm[:, 3:4], sm[:, 2:3], [r_sm], [r_sm])
        else:
            self.recip(sm[:, 3:4], self.bank(7, 1, 64), [self.r_ps[7]], [r_sm])
        self.ts(it["out"], self.bank(7, 64), sm[:, 3:4], ALU.mult, [self.r_ps[7], r_sm], [it["r_out"]])
        if it.get("after"):
            it["after"]()

    def attn_run(self, items, pingpong=False, hook=None):
        if pingpong:
            for i, it in enumerate(items):
                it["pcol"] = (i % 2) * 1024
        bufs = []
        for j in range(2):
            Pm, r_Pm = self.tile([128, self.T + 128], BF16, "Pm%d" % j)
            sm, r_sm = self.tile([128, 4], F32, "sm%d" % j)
            bufs.append((Pm, r_Pm, sm, r_sm))
        PTt, r_PT = self.tile([128, self.TT, 128], BF16, "PT")
        n = len(items)
        if n == 0:
            return
        self.attn_scores(items[0], bufs[0])
        items[0]["exp"]()
        for i in range(n):
            if i + 1 < n:
                self.attn_scores(items[i + 1], bufs[(i + 1) % 2])
            self.attn_transposes(items[i], bufs[i % 2], PTt, r_PT)
            if i + 1 < n:
                items[i + 1]["exp"]()
            if hook is not None:
                hook()
            self.attn_pv(items[i], bufs[i % 2], PTt, r_PT)

    def store_y_tm(self, ytm, r_ytm, s, br, tile_i, ybuf):
        yT, r_yT = ybuf
        for c in range(2):
            self.tr(self.bank_bf(6)[:, c * 128:(c + 1) * 128], ytm[:, c * 128:(c + 1) * 128], self.C["ident_bf"], [r_ytm, self.r_c], [self.r_ps[6]])
        self.cp(yT, self.bank_bf(6)[:, 0:256].rearrange("p (a b) -> p a b", b=128), [self.r_ps[6]], [r_yT])
        self.ld(self.YS[s][br * 256:(br + 1) * 256, tile_i * 128:(tile_i + 1) * 128].rearrange("(c p) n -> p c n", p=128), yT,
                [r_yT], [self.r_ys[s]])

    def branch_a(self, s, l, last):
        self.phase()
        T, TT, CT = self.T, self.TT, self.CT
        wA, r_wA = self.tile([128, KC, 544], BF16, "wA")
        wAp, r_wAp = self.tile([128, KC, 32], BF16, "wAp")
        wq, r_wq = self.tile([128, 2, 384], BF16, "wq")
        wqf, r_wqf = self.tile([128, 2, 384], F32, "wqf")
        wqp, r_wqp = self.tile([128, 2, 384], BF16, "wqp")
        wkv, r_wkv = self.tile([128, 2, 512], BF16, "wkv")
        wkvf, r_wkvf = self.tile([128, 2, 512], F32, "wkvf")
        raw, r_raw = self.tile([128, 4, 512], F32, "raw")
        sq, r_sq = self.tile([128, 4, 512], BF16, "sqA")
        rs, r_rs = self.tile([128, 2, 512], F32, "rsA")
        cqn, r_cqn = self.tile([128, 4, 512], BF16, "cqn")
        tab, r_tab = self.tile([128, 4, 512], F32, "tabA")
        t1, r_t1 = self.tile([128, 512], F32, "t1A")
        t2, r_t2 = self.tile([128, 512], F32, "t2A")
        qT, r_qT = self.tile([128, 4, T], BF16, "qTA")
        kT, r_kT = self.tile([128, 4, T], BF16, "kTA")
        Va, r_Va = self.tile([128, TT, 4, 65], BF16, "VaA")
        ytms = [self.tile([128, 256], BF16, "ytmA%d" % i) for i in range(2)]
        ybuf = self.tile([128, 2, 128], BF16, "yTA")
        self.load_w_cols(wA, r_wA, l, 0, 544)
        self.ld(wqf, self.W["w_uq"][l].rearrange("(k p) n -> p k n", p=128), [], [r_wqf])
        self.ld(wkvf, self.W["w_ukv"][l].rearrange("(k p) n -> p k n", p=128), [], [r_wkvf])
        for k in range(2):
            self.ts(wq[:, k, :], wqf[:, k, :], self.pv["g_qa"][:, l, k:k + 1], ALU.mult, [r_wqf, self.r_pv], [r_wq])
            self.ts(wkv[:, k, :], wkvf[:, k, :], self.pv["g_kva"][:, l, k:k + 1], ALU.mult, [r_wkvf, self.r_pv], [r_wkv])
        self.make_perm(wqp, wq, 4, 96, 64, 32, r_wqp, r_wq, 2)
        wkr = wA[:, :, 512:544]
        self.make_perm(wAp, wkr, 1, 32, 0, 32, r_wAp, r_wA, KC)
        self.P.op("dve", lambda e: e.memset(Va, 1.0), [], [r_Va])
        for bi, (lo, hi) in enumerate(self.blocks):
            n = hi - lo
            for c in range(4):
                b = c % 2
                for k in range(KC):
                    self.mm(self.bank(b, n), wA[:, k, c * 128:(c + 1) * 128], self.hT[:, k, lo:hi], k == 0, k == KC - 1,
                            [r_wA, self.r_hT], [self.r_ps[b]])
                self.cp(raw[:, c, 0:n], self.bank(b, n), [self.r_ps[b]], [r_raw], eng="act")
            rp0 = self.rstd_block(raw[:, 0:2], r_raw, n, 2, 256, sq[:, 0:2], r_sq, rs[:, 0], r_rs, 6)
            rp1 = self.rstd_block(raw[:, 2:4], r_raw, n, 2, 256, sq[:, 2:4], r_sq, rs[:, 1], r_rs, 7)
            for c in range(4):
                rp, r_rp = (rp0, rp1)[c // 2]
                self.tt(cqn[:, c, 0:n], raw[:, c, 0:n], rp, ALU.mult, [r_raw, r_rp], [r_cqn])
            self.ld(tab[0:96, 0, 0:n], self.CD["cosq_a"][:, lo:hi], [], [r_tab])
            self.ld(tab[0:96, 1, 0:n], self.CD["sinq_a"][:, lo:hi], [], [r_tab])
            self.ld(tab[0:32, 2, 0:n], self.CD["cosk_a"][:, lo:hi], [], [r_tab])
            self.ld(tab[0:32, 3, 0:n], self.CD["sink_a"][:, lo:hi], [], [r_tab])
            for h in range(4):
                for (w_, r_w_, b) in ((wq, r_wq, 0), (wqp, r_wqp, 1)):
                    for k in range(2):
                        self.mm(self.bank(b, n)[0:96], w_[:, k, h * 96:(h + 1) * 96], cqn[:, k, 0:n], k == 0, k == 1, [r_w_, r_cqn], [self.r_ps[b]])
                self.tt(t1[0:96, 0:n], self.bank(0, n)[0:96], tab[0:96, 0, 0:n], ALU.mult, [self.r_ps[0], r_tab], [r_t1])
                self.tt(t2[0:96, 0:n], self.bank(1, n)[0:96], tab[0:96, 1, 0:n], ALU.mult, [self.r_ps[1], r_tab], [r_t2])
                self.tt(qT[0:96, h, lo:hi], t1[0:96, 0:n], t2[0:96, 0:n], ALU.add, [r_t1, r_t2], [r_qT])
                for k in range(2):
                    self.mm(self.bank(2, n)[0:64], wkv[:, k, h * 128:h * 128 + 64], cqn[:, 2 + k, 0:n], k == 0, k == 1, [r_wkv, r_cqn], [self.r_ps[2]])
                self.cp(kT[0:64, h, lo:hi], self.bank(2, n)[0:64], [self.r_ps[2]], [r_kT], eng="act")
            for (w_, r_w_, b) in ((wkr, r_wA, 3), (wAp, r_wAp, 4)):
                for k in range(KC):
                    self.mm(self.bank(b, n)[0:32], w_[:, k, :], self.hT[:, k, lo:hi], k == 0, k == KC - 1, [r_w_, self.r_hT], [self.r_ps[b]])
            self.tt(t1[0:32, 0:n], self.bank(3, n)[0:32], tab[0:32, 2, 0:n], ALU.mult, [self.r_ps[3], r_tab], [r_t1])
            self.tt(t2[0:32, 0:n], self.bank(4, n)[0:32], tab[0:32, 3, 0:n], ALU.mult, [self.r_ps[4], r_tab], [r_t2])
            self.tt(t1[0:32, 0:n], t1[0:32, 0:n], t2[0:32, 0:n], ALU.add, [r_t1, r_t2], [r_t1])
            for h in range(4):
                self.cp(kT[64:96, h, lo:hi], t1[0:32, 0:n], [r_t1], [r_kT])
            for ti in range(lo // 128, hi // 128):
                o = ti * 128 - lo
                for k in range(2):
                    self.mm(self.bank(5, 256).rearrange("p (h d) -> p h d", d=64), cqn[:, 2 + k, o:o + 128],
                            wkv[:, k, :].rearrange("p (h x) -> p h x", x=128)[:, :, 64:128], k == 0, k == 1, [r_cqn, r_wkv], [self.r_ps[5]])
                self.cp(Va[:, ti, :, 0:64], self.bank(5, 256).rearrange("p (h d) -> p h d", d=64), [self.r_ps[5]], [r_Va])
        q_tiles = list(range(CT, TT)) + ([] if last else list(range(CT)))
        items = []
        for n_, qi in enumerate(q_tiles):
            is_ctx = qi < CT
            nk = self.NCTX if is_ctx else T
            yt, r_yt = ytms[n_ % 2]
            for h in range(4):
                kparts = []
                c0 = 0
                while c0 < nk:
                    n = min(512, nk - c0)
                    kparts.append((kT[0:96, h, c0:c0 + n], r_kT, c0, None))
                    c0 += n
                vparts = [(Va[:, i, h, :], r_Va, i * 128) for i in range(nk // 128)]
                it = dict(q=qT[0:96, h, qi * 128:(qi + 1) * 128], r_q=r_qT, kparts=kparts, sink=None, vparts=vparts, scale=1.0,
                          out=yt[:, h * 64:(h + 1) * 64], r_out=r_yt)
                if h == 3:
                    it["after"] = (lambda yt=yt, r_yt=r_yt, qi=qi: self.store_y_tm(yt, r_yt, s, 0, qi, ybuf))
                items.append(it)
        self.attn_run(items)

    def branch_c(self, s, l, last):
        self.phase()
        T, TT, CT = self.T, self.TT, self.CT
        wC, r_wC = self.tile([128, KC, 512], BF16, "wC")
        wCp, r_wCp = self.tile([128, KC, 384], BF16, "wCp")
        tab, r_tab = self.tile([128, 2, 512], F32, "tabC")
        t1, r_t1 = self.tile([128, 512], F32, "t1C")
        t2, r_t2 = self.tile([128, 512], F32, "t2C")
        qT, r_qT = self.tile([128, 4, T], BF16, "qTC")
        kT, r_kT = self.tile([128, 2, T], BF16, "kTC")
        Va, r_Va = self.tile([128, TT, 2, 65], BF16, "VaC")
        sk8, r_sk8 = self.tile([128, 4], F32, "sk8")
        ytms = [self.tile([128, 256], BF16, "ytmC%d" % i) for i in range(2)]
        ybuf = self.tile([128, 2, 128], BF16, "yTC")
        self.load_w_cols(wC, r_wC, l, 1584, 2096)
        self.make_perm(wCp, wC[:, :, 0:384], 6, 64, 0, 64, r_wCp, r_wC, KC)
        self.ts(sk8, self.sink_b[:, l, :], 8.0, ALU.mult, [self.r_pv], [r_sk8])
        self.P.op("dve", lambda e: e.memset(Va, 1.0), [], [r_Va])
        for bi, (lo, hi) in enumerate(self.blocks):
            n = hi - lo
            self.ld(tab[0:64, 0, 0:n], self.CD["cos_c"][:, lo:hi], [], [r_tab])
            self.ld(tab[0:64, 1, 0:n], self.CD["sin_c"][:, lo:hi], [], [r_tab])
            for hh in range(6):
                for (w_, r_w_, b) in ((wC, r_wC, 0), (wCp, r_wCp, 1)):
                    for k in range(KC):
                        self.mm(self.bank(b, n)[0:64], w_[:, k, hh * 64:(hh + 1) * 64], self.hT[:, k, lo:hi], k == 0, k == KC - 1,
                                [r_w_, self.r_hT], [self.r_ps[b]])
                self.tt(t1[0:64, 0:n], self.bank(0, n)[0:64], tab[0:64, 0, 0:n], ALU.mult, [self.r_ps[0], r_tab], [r_t1])
                self.tt(t2[0:64, 0:n], self.bank(1, n)[0:64], tab[0:64, 1, 0:n], ALU.mult, [self.r_ps[1], r_tab], [r_t2])
                dst = qT[0:64, hh, lo:hi] if hh < 4 else kT[0:64, hh - 4, lo:hi]
                self.tt(dst, t1[0:64, 0:n], t2[0:64, 0:n], ALU.add, [r_t1, r_t2], [r_qT if hh < 4 else r_kT])
            for ti in range(lo // 128, hi // 128):
                for k in range(KC):
                    self.mm(self.bank(5, 128), self.hT[:, k, ti * 128:(ti + 1) * 128], wC[:, k, 384:512], k == 0, k == KC - 1,
                            [self.r_hT, r_wC], [self.r_ps[5]])
                self.cp(Va[:, ti, :, 0:64], self.bank(5, 128).rearrange("p (h d) -> p h d", d=64), [self.r_ps[5]], [r_Va])
        NQ = TT - CT
        q_tiles = list(range(CT, TT)) + ([] if last else list(range(CT)))
        NC_ = self.NCTX
        items = []
        for n_, qi in enumerate(q_tiles):
            is_ctx = qi < CT
            yt, r_yt = ytms[n_ % 2]
            for h in range(4):
                g = h // 2
                kparts = [(kT[0:64, g, 0:NC_], r_kT, 0, None)]
                vparts = [(Va[:, i, g, :], r_Va, i * 128) for i in range(CT)]
                if not is_ctx:
                    i = qi - CT
                    col = NC_
                    for (j, mk) in ((i - 1, "maskA"), (i, None), (i + 1, "maskB")):
                        if 0 <= j < NQ:
                            kparts.append((kT[0:64, g, NC_ + j * 128:NC_ + (j + 1) * 128], r_kT, col, self.C[mk] if mk else None))
                            vparts.append((Va[:, CT + j, g, :], r_Va, col))
                            col += 128
                it = dict(q=qT[0:64, h, qi * 128:(qi + 1) * 128], r_q=r_qT, kparts=kparts, sink=sk8[0:1, h:h + 1], vparts=vparts, scale=0.125,
                          out=yt[:, h * 64:(h + 1) * 64], r_out=r_yt)
                if h == 3:
                    it["after"] = (lambda yt=yt, r_yt=r_yt, qi=qi: self.store_y_tm(yt, r_yt, s, 2, qi, ybuf))
                items.append(it)
        dg = self.branch_d(s, l, last)
        self.attn_run(items, hook=lambda: next(dg, None))
        for _ in dg:
            pass

    def branch_d(self, s, l, last):
        T, TT, CT = self.T, self.TT, self.CT
        wD, r_wD = self.tile([128, KC, 256], BF16, "wD")
        ud, r_ud = self.tile([128, 2, T], BF16, "udT")
        uc, r_uc = self.tile([128, TT, 2, 256], BF16, "uc_tm")
        cn, r_cn = self.tile([128, 16, 512], BF16, "cn")
        sn, r_sn = self.tile([128, 16, 512], BF16, "sn")
        yo, r_yo = self.tile([128, 2, 512], BF16, "yoD")
        self.load_w_cols(wD, r_wD, l, 2096, 2352)
        yield
        for (lo, hi) in self.blocks:
            n = hi - lo
            for c in range(2):
                b = 2 + c
                for k in range(KC):
                    self.mm(self.bank(b, n), wD[:, k, c * 128:(c + 1) * 128], self.hT[:, k, lo:hi], k == 0, k == KC - 1,
                            [r_wD, self.r_hT], [self.r_ps[b]])
                self.cp(ud[:, c, lo:hi], self.bank(b, n), [self.r_ps[b]], [r_ud], eng="act" if c else "dve")
                yield
        for ti in range(TT):
            for j, m in enumerate(("bdc", "bds")):
                for c in range(2):
                    self.mm(self.bank(4, 128, j * 256 + c * 128), ud[:, c, ti * 128:(ti + 1) * 128], self.C[m], c == 0 and j == 0, True,
                            [r_ud, self.r_c], [self.r_ps[4]])
            self.cp(uc[:, ti], self.bank(4).rearrange("p (a b) -> p a b", a=2), [self.r_ps[4]], [r_uc])
            yield
        segs = [("lat", CT, TT, self.NCTX)] + ([] if last else [("ctx", 0, CT, 0)])
        for nm, t0, t1_, col0 in segs:
            N = (t1_ - t0) * 128
            ntl = t1_ - t0
            for kb in range(0, N, 512):
                n = min(512, N - kb)
                self.ld(cn[:, 0:ntl, 0:n], self.CD["cn_" + nm][:, kb:kb + n].rearrange("(a p) k -> p a k", p=128), [], [r_cn])
                self.ld(sn[:, 0:ntl, 0:n], self.CD["sn_" + nm][:, kb:kb + n].rearrange("(a p) k -> p a k", p=128), [], [r_sn])
                for c in range(2):
                    b = 2 + c
                    for a in range(ntl):
                        self.mm(self.bank(b, n), uc[:, t0 + a, 0, c * 128:(c + 1) * 128], cn[:, a, 0:n], a == 0, False, [r_uc, r_cn], [self.r_ps[b]])
                        self.mm(self.bank(b, n), uc[:, t0 + a, 1, c * 128:(c + 1) * 128], sn[:, a, 0:n], False, a == ntl - 1, [r_uc, r_sn], [self.r_ps[b]])
                        if a % 4 == 3:
                            yield
                    self.cp(yo[:, c, 0:n], self.bank(b, n), [self.r_ps[b]], [r_yo], eng="act" if c else "dve")
                self.ld(self.YS[s][768:1024, col0 + kb:col0 + kb + n].rearrange("(c p) n -> p c n", p=128), yo[:, :, 0:n], [r_yo], [self.r_ys[s]])
                yield

    def branch_b(self, s, l, last):
        self.phase()
        T, TT, CT = self.T, self.TT, self.CT
        wB, r_wB = self.tile([128, KC, 1040], BF16, "wB")
        araw, r_araw = self.tile([128, 514], F32, "arawB")
        c1, r_c1 = self.tile([128, 512], F32, "c1B")
        qk, r_qk = self.tile([128, 8, T], BF16, "qkB")
        ktm, r_ktm = self.tile([128, TT, 256], BF16, "ktmB")
        Va, r_Va = self.tile([128, TT, 4, 65], BF16, "VaB")
        og, r_og = self.tile([128, 256], F32, "ogB")
        G, r_G = self.tile([128, TT, 16], F32, "GB")
        hs, r_hs = self.tile([128, TT, 256], F32, "hsB")
        self.load_w_cols(wB, r_wB, l, 544, 1584)
        self.ld(self.wconvB[0:64], self.PR["wconvB"][l], [], [self.r_wcb])
        self.P.op("dve", lambda e: e.memset(Va, 1.0), [], [r_Va])
        self.P.op("dve", lambda e: e.memset(hs, 0.0), [], [r_hs])
        for (lo, hi, cl, ch) in self.conv_blocks():
            n, w, off = hi - lo, ch - cl, lo - cl
            for hh in range(8):
                c0 = hh * 64
                for k in range(KC):
                    self.mm(self.bank(hh % 2, w)[0:64], wB[:, k, c0:c0 + 64], self.hT[:, k, cl:ch], k == 0, k == KC - 1, [r_wB, self.r_hT], [self.r_ps[hh % 2]])
                self.P.op("dve", lambda e: e.memset(araw[0:64, :], 0.0), [], [r_araw])
                self.cp(araw[0:64, 1 - off:1 - off + w], self.bank(hh % 2, w)[0:64], [self.r_ps[hh % 2]], [r_araw], eng="act")
                wsl = self.wconvB[:, hh, :]
                self.ts(c1[0:64, 0:n], araw[0:64, 0:n], wsl[0:64, 0:1], ALU.mult, [r_araw, self.r_wcb], [r_c1])
                self.stt(c1[0:64, 0:n], araw[0:64, 1:n + 1], wsl[0:64, 1:2], c1[0:64, 0:n], ALU.mult, ALU.add, [r_araw, self.r_wcb, r_c1], [r_c1])
                self.stt(c1[0:64, 0:n], araw[0:64, 2:n + 2], wsl[0:64, 2:3], c1[0:64, 0:n], ALU.mult, ALU.add, [r_araw, self.r_wcb, r_c1], [r_c1])
                self.act(c1[0:64, 0:n], c1[0:64, 0:n], AF.Silu, [r_c1], [r_c1])
                self.ts(qk[0:64, hh, lo:hi], c1[0:64, 0:n], 1.0 if hh < 4 else 0.125, ALU.mult, [r_c1], [r_qk])
        Gt, r_Gt = self.tile([128, TT, 2, 4], F32, "GtB")
        for ti in range(TT):
            tsl = slice(ti * 128, (ti + 1) * 128)
            for k in range(KC):
                self.mm(self.bank(2, 16), self.hT[:, k, tsl], wB[:, k, 1024:1040], k == 0, k == KC - 1, [self.r_hT, r_wB], [self.r_ps[2]])
            self.tt(G[:, ti, :], self.bank(2, 16), self.bg_b[:, l, :], ALU.add, [self.r_ps[2], self.r_pv], [r_G])
            for k in range(KC):
                self.mm(self.bank(3, 256), self.hT[:, k, tsl], wB[:, k, 512:768], k == 0, k == KC - 1, [self.r_hT, r_wB], [self.r_ps[3]])
            self.cp(Va[:, ti, :, 0:64], self.bank(3, 256).rearrange("p (h d) -> p h d", d=64), [self.r_ps[3]], [r_Va])
            for h in range(4):
                self.tr(self.bank_bf(5)[:, h * 64:(h + 1) * 64], qk[0:64, 4 + h, tsl], self.C["ident_bf"][0:64, 0:64], [r_qk, self.r_c], [self.r_ps[5]])
            self.cp(ktm[:, ti, :], self.bank_bf(5)[:, 0:256], [self.r_ps[5]], [r_ktm])
        G5 = G.rearrange("p t (a b c) -> p t a b c", a=2, b=2)
        for d_ in range(2):
            fv = G5[:, :, d_, 1, :]
            self.act(Gt[:, :, d_, :], fv, AF.Exp, [r_G], [r_Gt], scale=-1.0)
            self.act(Gt[:, :, d_, :], Gt[:, :, d_, :], AF.Ln, [r_Gt], [r_Gt], bias=1.0)
            self.ts(fv, Gt[:, :, d_, :], -1.0, ALU.mult, [r_Gt], [r_G])
        B_TM, M_, NEGM, WIN, EMT, DEN, DAB, RR, WTM, DEC, MX, DENI = range(12)
        SX = []
        for d_ in range(2):
            X = {}
            for nm, shp, dt in (("diag", [128, 4, 128], F32), ("bBm", [128, 4, 128], F32), ("Wt", [128, 4, 128], F32),
                                ("Sb", [128, 4, 128], BF16), ("ST", [128, 4, 128], BF16), ("kw", [128, 4, 64], BF16),
                                ("sv", [128, 16, 4], F32), ("cm", [128, 8], F32), ("mst", [128, 4], F32), ("tmpi", [128, 4, 65], F32),
                                ("numh", [128, 4, 64], F32), ("Cst", [128, 4, 65], F32), ("Cbf", [128, 4, 65], BF16)):
                X[nm], X["r_" + nm] = self.tile(shp, dt, nm + "B%d" % d_)
            X["tri"] = self.C["tri_f" if d_ == 0 else "tri_b"]
            X["mneg"] = self.C["mneg_f" if d_ == 0 else "mneg_b"]
            X["esel"] = self.C["e_last" if d_ == 0 else "e_first"]
            X["b0"] = 4 * d_
            SX.append(X)
            self.P.op("dve", lambda e, t=X["Cst"]: e.memset(t, 0.0), [], [X["r_Cst"]])
            self.P.op("dve", lambda e, t=X["Cbf"]: e.memset(t, 0.0), [], [X["r_Cbf"]])
            self.P.op("dve", lambda e, t=X["mst"]: e.memset(t, 0.0), [], [X["r_mst"]])

        def chunk(d_, ti):
            X = SX[d_]
            diag, bBm, Wt, Sb, ST, kw, sv, cm, mst, tmpi, numh, Cst, Cbf = (X[k] for k in (
                "diag", "bBm", "Wt", "Sb", "ST", "kw", "sv", "cm", "mst", "tmpi", "numh", "Cst", "Cbf"))
            r_diag, r_bBm, r_Wt, r_Sb, r_ST, r_kw, r_sv, r_cm, r_mst, r_tmpi, r_numh, r_Cst, r_Cbf = (X["r_" + k] for k in (
                "diag", "bBm", "Wt", "Sb", "ST", "kw", "sv", "cm", "mst", "tmpi", "numh", "Cst", "Cbf"))
            b0 = X["b0"]
            bB_b, qk_b, st_b, ms_b = b0, b0 + 1, b0 + 2, b0 + 3
            r0, r1, r2, r3 = self.r_ps[bB_b], self.r_ps[qk_b], self.r_ps[st_b], self.r_ps[ms_b]
            cum_ps = self.bank(ms_b, 4)
            sel_ps = self.bank(ms_b, 8, 8)
            inter_ps = self.bank(ms_b, 260, 16)
            upd_ps = self.bank(ms_b, 260, 16)
            num_ps = self.bank(st_b, 256, 256)
            tsl = slice(ti * 128, (ti + 1) * 128)
            li = G[:, ti, d_ * 8:d_ * 8 + 4]
            lf = G[:, ti, d_ * 8 + 4:d_ * 8 + 8]
            self.mm(cum_ps, X["tri"], lf, True, True, [self.r_c, r_G], [r3])
            for h in range(4):
                self.mm(self.bank(qk_b, 128, h * 128), qk[0:64, h, tsl], qk[0:64, 4 + h, tsl], True, True, [r_qk], [r1])
            yield
            self.tt(sv[:, B_TM, :], li, cum_ps, ALU.subtract, [r_G, r3], [r_sv])
            for h in range(4):
                self.ts(diag[:, h, :], self.C["ident_f"], sv[:, B_TM, h:h + 1], ALU.mult, [self.r_c, r_sv], [r_diag])
            yield
            for h in range(4):
                self.mm(self.bank(bB_b, 128, h * 128), self.C["ones_f"], diag[:, h, :], True, True, [self.r_c, r_diag], [r0])
            yield
            self.tt(bBm, self.bank(bB_b).rearrange("p (h s) -> p h s", h=4), X["mneg"], ALU.add, [r0, self.r_c], [r_bBm])
            self.red(sv[:, MX, :], bBm, ALU.max, [r_bBm], [r_sv])
            self.tt(sv[:, M_, :], sv[:, MX, :], mst, ALU.max, [r_sv, r_mst], [r_sv])
            self.ts(sv[:, NEGM, :], sv[:, M_, :], -1.0, ALU.mult, [r_sv], [r_sv])
            self.tt(sv[:, WIN, :], mst, sv[:, M_, :], ALU.subtract, [r_mst, r_sv], [r_sv])
            self.tt(cm[:, 0:4], cum_ps, sv[:, M_, :], ALU.add, [r3, r_sv], [r_cm])
            self.cp(cm[:, 4:8], sv[:, M_, :], [r_sv], [r_cm])
            yield
            for h in range(4):
                self.act(Wt[:, h, :], bBm[:, h, :], AF.Exp, [r_bBm, r_sv], [r_Wt], bias=sv[:, NEGM, h:h + 1], scale=1.0)
            self.act(sv[:, WIN, :], sv[:, WIN, :], AF.Exp, [r_sv], [r_sv])
            self.act(sv[:, EMT, :], cm[:, 0:4], AF.Exp, [r_cm], [r_sv], scale=-1.0)
            self.mm(sel_ps, X["esel"], cm, True, True, [self.r_c, r_cm], [r3])
            yield
            self.tt(Sb, self.bank(qk_b).rearrange("p (h s) -> p h s", h=4), Wt, ALU.mult, [r1, r_Wt], [r_Sb])
            self.red(sv[:, DENI, :], Sb, ALU.add, [r_Sb], [r_sv])
            self.tt(sv[:, WTM, :], sv[:, B_TM, :], self.bank(ms_b, 4, 12), ALU.subtract, [r_sv, r3], [r_sv])
            self.tt(sv[:, DEC, :], mst, self.bank(ms_b, 4, 12), ALU.subtract, [r_mst, r3], [r_sv])
            self.cp(mst, self.bank(ms_b, 4, 8), [r3], [r_mst])
            yield
            for h in range(4):
                self.tr(self.bank_bf(st_b)[:, h * 128:(h + 1) * 128], Sb[:, h, :], self.C["ident_bf"], [r_Sb, self.r_c], [r2])
            for h in range(4):
                self.mm(self.bank(ms_b, 65, 16 + h * 65), qk[0:64, h, tsl], Cbf[0:64, h, :], True, True, [r_qk, r_Cbf], [r3])
            self.act(sv[:, WTM, :], sv[:, WTM, :], AF.Exp, [r_sv], [r_sv])
            self.act(sv[:, DEC, :], sv[:, DEC, :], AF.Exp, [r_sv], [r_sv])
            yield
            self.cp(ST, self.bank_bf(st_b)[:, 0:512].rearrange("p (h s) -> p h s", h=4), [r2], [r_ST])
            self.tt(tmpi, inter_ps.rearrange("p (h e) -> p h e", h=4), sv[:, WIN, :].unsqueeze(2).to_broadcast([128, 4, 65]), ALU.mult,
                    [r3, r_sv], [r_tmpi])
            self.tt(kw, ktm[:, ti, :].rearrange("p (h e) -> p h e", h=4), sv[:, WTM, :].unsqueeze(2).to_broadcast([128, 4, 64]), ALU.mult,
                    [r_ktm, r_sv], [r_kw])
            yield
            for h in range(4):
                self.mm(self.bank(st_b, 64, 256 + h * 64), ST[:, h, :], Va[:, ti, h, 0:64], True, True, [r_ST, r_Va], [r2])
            for h in range(4):
                self.mm(self.bank(ms_b, 65, 16 + h * 65)[0:64], kw[:, h, :], Va[:, ti, h, :], True, True, [r_kw, r_Va], [r3])
            yield
            self.tt(numh, tmpi[:, :, 0:64], num_ps.rearrange("p (h e) -> p h e", h=4), ALU.add, [r_tmpi, r2], [r_numh])
            self.tt(sv[:, DEN, :], tmpi[:, :, 64], sv[:, DENI, :], ALU.add, [r_tmpi, r_sv], [r_sv])
            self.ts(sv[:, DAB, :], sv[:, DEN, :], -1.0, ALU.mult, [r_sv], [r_sv])
            self.tt(sv[:, DAB, :], sv[:, DAB, :], sv[:, DEN, :], ALU.max, [r_sv], [r_sv])
            self.tt(sv[:, DAB, :], sv[:, DAB, :], sv[:, EMT, :], ALU.max, [r_sv], [r_sv])
            self.recip(sv[:, RR, :], sv[:, DAB, :], [r_sv], [r_sv])
            self.tt(numh, numh, sv[:, RR, :].unsqueeze(2).to_broadcast([128, 4, 64]), ALU.mult, [r_numh, r_sv], [r_numh])
            hsv = hs[:, ti, :].rearrange("p (h e) -> p h e", h=4)
            self.tt(hsv, hsv, numh, ALU.add, [r_hs, r_numh], [r_hs])
            self.tt(Cst[0:64], Cst[0:64], sv[0:64, DEC, :].unsqueeze(2).to_broadcast([64, 4, 65]), ALU.mult, [r_Cst, r_sv], [r_Cst])
            self.tt(Cst[0:64], Cst[0:64], upd_ps[0:64].rearrange("p (h e) -> p h e", h=4), ALU.add, [r_Cst, r3], [r_Cst])
            self.cp(Cbf[0:64], Cst[0:64], [r_Cst], [r_Cbf])
            yield

        orders = [list(range(TT)), list(range(CT - 1, -1, -1)) + list(range(TT - 1, CT - 1, -1))]
        for i in range(TT):
            gens = [chunk(0, orders[0][i]), chunk(1, orders[1][i])]
            alive = True
            while alive:
                alive = False
                for g in gens:
                    try:
                        next(g)
                        alive = True
                    except StopIteration:
                        pass
        ytm, r_ytm = self.tile([128, 256], BF16, "ytmB")
        ybuf = self.tile([128, 2, 128], BF16, "yTB")
        for ti in range(CT if last else 0, TT):
            tsl = slice(ti * 128, (ti + 1) * 128)
            for k in range(KC):
                self.mm(self.bank(4, 256), self.hT[:, k, tsl], wB[:, k, 768:1024], k == 0, k == KC - 1, [self.r_hT, r_wB], [self.r_ps[4]])
            self.act(og, self.bank(4, 256), AF.Sigmoid, [self.r_ps[4]], [r_og])
            self.tt(ytm, og, hs[:, ti, :], ALU.mult, [r_og, r_hs], [r_ytm])
            self.store_y_tm(ytm, r_ytm, s, 1, ti, ybuf)

    def post_norm_res(self, s, lo, hi, yT, r_yT, gp_idx, tl):
        sq, r_sq, rs, r_rs, tmp, r_tmp, xb, r_xb = tl
        n = hi - lo
        rsp, r_rsp = self.rstd_block(yT, r_yT, n, KC, D, sq, r_sq, rs, r_rs, 7)
        dv, r_dv = self.seg_dv(lo)
        for k in range(KC):
            self.tt(tmp[:, 0:n], yT[:, k, 0:n], rsp, ALU.mult, [r_yT, r_rsp], [r_tmp])
            self.stt(xb[:, k, 0:n], tmp[:, 0:n], dv[:, gp_idx, k:k + 1], xb[:, k, 0:n], ALU.mult, ALU.add, [r_tmp, r_dv, r_xb], [r_xb])
        self.ld(self.XRES[s][:, lo:hi].rearrange("(k p) n -> p k n", p=128), xb[:, :, 0:n], [r_xb], [self.r_xres[s]])

    def prefetch_x(self, s, lo, hi, tl):
        xb, r_xb = tl[6], tl[7]
        self.ld(xb[:, :, 0:hi - lo], self.XRES[s][:, lo:hi].rearrange("(k p) n -> p k n", p=128), [self.r_xres[s]], [r_xb])

    def pn_tiles(self):
        sq, r_sq = self.tile([128, KC, 512], BF16, "sqP")
        rs, r_rs = self.tile([128, 512], F32, "rsP")
        tmp, r_tmp = self.tile([128, 512], F32, "tmpP")
        xb, r_xb = self.tile([128, KC, 512], F32, "xbP")
        return (sq, r_sq, rs, r_rs, tmp, r_tmp, xb, r_xb)

    def merge(self, s, l, last):
        self.phase()
        ysb, r_ysb = self.tile([128, 8, 512], BF16, "ysb")
        wg = [self.tile([128, 4, KC, 128], BF16, "wg%d" % i) for i in range(2)]
        wbr = [self.tile([128, 4, 2, 128], BF16, "wbr%d" % i) for i in range(2)]
        sig, r_sig = self.tile([128, 512], F32, "sig")
        accf, r_accf = self.tile([128, 512], F32, "accf")
        tmpm, r_tmpm = self.tile([128, 512], F32, "tmpm")
        acc, r_acc = self.tile([128, KC, 512], BF16, "acc")
        wo, r_wo = self.tile([128, KC, D], BF16, "wo")
        yT, r_yT = self.tile([128, KC, 512], F32, "yTm")
        tl = self.pn_tiles()
        self.ld(wo, self.WO[l].rearrange("p (k n) -> p k n", k=KC), [self.r_wbf], [r_wo])
        it = 0
        for (lo, hi) in self.blocks:
            if last and lo < self.NCTX:
                continue
            n = hi - lo
            self.ld(ysb[:, :, 0:n], self.YS[s][:, lo:hi].rearrange("(k p) n -> p k n", p=128), [self.r_ys[s]], [r_ysb])
            self.prefetch_x(s, lo, hi, tl)
            for dc in range(KC):
                (wg_, r_wg), (wb_, r_wb) = wg[it % 2], wbr[it % 2]
                it += 1
                for br in range(4):
                    self.ld(wg_[:, br], self.WG[l, br, dc].rearrange("p (k n) -> p k n", k=KC), [self.r_wbf], [r_wg])
                    self.ld(wb_[:, br], self.WBR[l, br].rearrange("p (k n) -> p k n", k=2)[:, :, dc * 128:(dc + 1) * 128], [self.r_wbf], [r_wb])
                for br in range(4):
                    bg, bp = br % 2, 2 + br % 2
                    for k in range(KC):
                        self.mm(self.bank(bg, n), wg_[:, br, k, :], self.hT[:, k, lo:hi], k == 0, k == KC - 1, [r_wg, self.r_hT], [self.r_ps[bg]])
                    self.act(sig[:, 0:n], self.bank(bg, n), AF.Sigmoid, [self.r_ps[bg], self.r_pv], [r_sig], bias=self.pv["b_gate"][:, l, br, dc:dc + 1], scale=1.0)
                    for k in range(2):
                        self.mm(self.bank(bp, n), wb_[:, br, k, :], ysb[:, br * 2 + k, 0:n], k == 0, k == 1, [r_wb, r_ysb], [self.r_ps[bp]])
                    if br == 0:
                        self.tt(accf[:, 0:n], sig[:, 0:n], self.bank(bp, n), ALU.mult, [r_sig, self.r_ps[bp]], [r_accf])
                    else:
                        self.tt(tmpm[:, 0:n], sig[:, 0:n], self.bank(bp, n), ALU.mult, [r_sig, self.r_ps[bp]], [r_tmpm])
                        if br < 3:
                            self.tt(accf[:, 0:n], accf[:, 0:n], tmpm[:, 0:n], ALU.add, [r_accf, r_tmpm], [r_accf])
                        else:
                            self.tt(acc[:, dc, 0:n], accf[:, 0:n], tmpm[:, 0:n], ALU.add, [r_accf, r_tmpm], [r_acc])
            for d2 in range(KC):
                b = 4 + d2 % 2
                for k in range(KC):
                    self.mm(self.bank(b, n), wo[:, k, d2 * 128:(d2 + 1) * 128], acc[:, k, 0:n], k == 0, k == KC - 1, [r_wo, r_acc], [self.r_ps[b]])
                self.cp(yT[:, d2, 0:n], self.bank(b, n), [self.r_ps[b]], [r_yT], eng="act" if d2 % 2 else "dve")
            self.post_norm_res(s, lo, hi, yT, r_yT, 2, tl)

    def ffn(self, s, l, last):
        self.phase()
        T = self.T
        wd, r_wd = self.tile([128, FC, D], BF16, "wd")
        wu = [self.tile([128, KC, 256], BF16, "wu%d" % i) for i in range(2)]
        gT, r_gT = self.tile([128, FC, 512], BF16, "gT")
        a_sb, r_a = self.tile([128, 514], F32, "a_sb")
        c1, r_c1 = self.tile([128, 512], F32, "c1F")
        yT, r_yT = self.tile([128, KC, 512], F32, "yTf")
        tl = self.pn_tiles()
        self.ld(wd, self.WD[l].rearrange("p (f n) -> p f n", f=FC), [self.r_wbf], [r_wd])
        wcv, bcv = self.pv["w_ffn_conv"], self.pv["b_ffn_conv"]
        it = 0
        for (lo, hi, cl, ch) in self.conv_blocks(skip_ctx=last):
            n, w, off = hi - lo, ch - cl, lo - cl
            self.prefetch_x(s, lo, hi, tl)
            for fc in range(FC):
                w_, r_w = wu[it % 2]
                it += 1
                self.ld(w_, self.WU[l, fc].rearrange("p (k n) -> p k n", k=KC), [self.r_wbf], [r_w])
                ba, bv = fc % 2, 2 + fc % 2
                for k in range(KC):
                    self.mm(self.bank(ba, w), w_[:, k, 0:128], self.hT[:, k, cl:ch], k == 0, k == KC - 1, [r_w, self.r_hT], [self.r_ps[ba]])
                for k in range(KC):
                    self.mm(self.bank(bv, n), w_[:, k, 128:256], self.hT[:, k, lo:hi], k == 0, k == KC - 1, [r_w, self.r_hT], [self.r_ps[bv]])
                if off == 0:
                    self.P.op("dve", lambda e: e.memset(a_sb[:, 0:1], 0.0), [], [r_a])
                if ch == hi:
                    self.P.op("dve", lambda e, n=n: e.memset(a_sb[:, n + 1:n + 2], 0.0), [], [r_a])
                self.cp(a_sb[:, 1 - off:1 - off + w], self.bank(ba, w), [self.r_ps[ba]], [r_a], eng="act")
                ctr = self.bank(ba, n, off)
                self.act(ctr, ctr, AF.Copy, [self.r_ps[ba], self.r_pv], [self.r_ps[ba]], scale=wcv[:, l, 1, fc:fc + 1])
                self.stt(ctr, a_sb[:, 0:n], wcv[:, l, 0, fc:fc + 1], ctr, ALU.mult, ALU.add, [r_a, self.r_pv, self.r_ps[ba]], [self.r_ps[ba]])
                self.stt(c1[:, 0:n], a_sb[:, 2:n + 2], wcv[:, l, 2, fc:fc + 1], ctr, ALU.mult, ALU.add, [r_a, self.r_pv, self.r_ps[ba]], [r_c1])
                self.act(c1[:, 0:n], c1[:, 0:n], AF.Silu, [r_c1, self.r_pv], [r_c1], bias=bcv[:, l, fc:fc + 1], scale=1.0)
                self.tt(gT[:, fc, 0:n], c1[:, 0:n], self.bank(bv, n), ALU.mult, [r_c1, self.r_ps[bv]], [r_gT])
            for d2 in range(KC):
                b = 4 + d2 % 2
                for fc in range(FC):
                    self.mm(self.bank(b, n), wd[:, fc, d2 * 128:(d2 + 1) * 128], gT[:, fc, 0:n], fc == 0, fc == FC - 1, [r_wd, r_gT], [self.r_ps[b]])
                self.cp(yT[:, d2, 0:n], self.bank(b, n), [self.r_ps[b]], [r_yT], eng="act" if d2 % 2 else "dve")
            self.post_norm_res(s, lo, hi, yT, r_yT, 5, tl)


NLAT_FULL, NCTX_FULL, DEPTH = 2048, 256, 2
_cache = {}


def run_device(inp, NLAT, NCTX, B, L, n_cores):
    NSEQ = B // n_cores
    consts = make_consts(NLAT, NCTX)
    key = (NLAT, NCTX, NSEQ, L)
    bld = Builder(NLAT, NCTX, NSEQ, L, consts)
    nc = bld.build()
    x = np.asarray(inp["x"], np.float32)
    ctx = np.asarray(inp["ctx"], np.float32)
    c = np.asarray(inp["c"], np.float32)
    c_ctx = np.asarray(inp["c_ctx"], np.float32)
    params = layout_params(inp, L)
    wml = np.asarray(inp["w_ml_conv"], np.float32)
    params["wconvB"] = np.ascontiguousarray(wml.reshape(L, 3, 8, 64).transpose(0, 3, 2, 1))
    shared = {k: np.ascontiguousarray(np.asarray(inp[k], np.float32)) for k in W_SHAPES}
    shared.update(params)
    shared.update(consts)
    in_maps = []
    for ci in range(n_cores):
        sl = slice(ci * NSEQ, (ci + 1) * NSEQ)
        cc = np.concatenate([c[sl], c_ctx[None]], 0)
        m = dict(shared)
        m["xT"] = np.ascontiguousarray(x[sl].transpose(0, 2, 1))
        m["ctxT"] = np.ascontiguousarray(ctx[sl].transpose(0, 2, 1))
        m["ccT"] = np.ascontiguousarray(cc.T.reshape(KC, 128, NSEQ + 1).transpose(1, 0, 2))
        in_maps.append(m)
    return nc, in_maps


def kernel(**inp):
    n_cores = 8
    nc, in_maps = run_device(inp, NLAT_FULL, NCTX_FULL, 16, DEPTH, n_cores)
    res = run_bass_kernel_spmd(nc, in_maps, core_ids=list(range(n_cores)))
    outs = [np.asarray(r["outT"]).transpose(0, 2, 1) for r in res.results]
    return np.ascontiguousarray(np.concatenate(outs, 0).astype(np.float32))
```

```python
from contextlib import ExitStack
import numpy as np
import ml_dtypes
import concourse.bass as bass
import concourse.mybir as mybir
from concourse.bass_utils import run_bass_kernel_spmd

F32 = mybir.dt.float32
BF16 = mybir.dt.bfloat16
AF = mybir.ActivationFunctionType
ALU = mybir.AluOpType
AX = mybir.AxisListType
ENG = ("pe", "act", "dve", "pool", "sp")
NBIG = -30000.0
D = 1024
KC = 8
DFF = 2816
FC = 22
EPS = 1e-6


class Res:
    __slots__ = ("name", "w", "re", "rd")

    def __init__(self, name=""):
        self.name = name
        self.w = None
        self.re = {}
        self.rd = []


class Prog:
    N_DMA_SEMS = 80

    def __init__(self, nc, stack):
        self.nc = nc
        self.esem = {e: stack.enter_context(nc.semaphore("es_" + e)) for e in ENG}
        self.dsem = [stack.enter_context(nc.semaphore("ds%d" % i)) for i in range(self.N_DMA_SEMS)]
        self.dval = [0] * self.N_DMA_SEMS
        self.dnext = 0
        self.cnt = {e: 0 for e in ENG}
        self.seen = {e: {} for e in ENG}
        self.q = {e: [] for e in ENG}
        self.n_ops = 0

    def _deps(self, reads, writes):
        deps = []
        for r in reads:
            if r.w is not None:
                deps.append(r.w)
        for w in writes:
            if w.w is not None:
                deps.append(w.w)
            for e, c in w.re.items():
                deps.append(("E", e, c))
            deps.extend(w.rd)
        return deps

    def _waits(self, eng, deps):
        seen = self.seen[eng]
        need = {}
        for kind, key, val in deps:
            k = (kind, key)
            if seen.get(k, 0) >= val:
                continue
            if kind == "E" and key == "pe" and eng == "pe":
                continue
            if need.get(k, 0) < val:
                need[k] = val
        out = []
        for k, val in need.items():
            seen[k] = val
            sem = self.esem[k[1]] if k[0] == "E" else self.dsem[k[1]]
            out.append((sem, val))
        return out

    def op(self, eng, fn, reads=(), writes=()):
        waits = self._waits(eng, self._deps(reads, writes))
        self.cnt[eng] += 1
        c = self.cnt[eng]
        tok = ("E", eng, c)
        self.q[eng].append((waits, fn, (self.esem[eng], 1)))
        for r in reads:
            if r.re.get(eng, 0) < c:
                r.re[eng] = c
        for w in writes:
            w.w = tok
            w.re = {}
            w.rd = []
        self.n_ops += 1
        return tok

    def dma(self, qeng, out, in_, reads=(), writes=(), **kw):
        deps = self._deps(reads, writes)
        i = self.dnext
        self.dnext = (self.dnext + 1) % self.N_DMA_SEMS
        prev = self.dval[i]
        if prev:
            deps.append(("D", i, prev))
        waits = self._waits(qeng, deps)
        self.dval[i] = prev + 16
        tok = ("D", i, prev + 16)
        self.q[qeng].append((waits, (lambda e, o=out, s=in_, k=kw: e.dma_start(out=o, in_=s, **k)),
                             (self.dsem[i], 16)))
        for r in reads:
            r.rd.append(tok)
        for w in writes:
            w.w = tok
            w.re = {}
            w.rd = []
        self.n_ops += 1
        return tok

    def barrier(self):
        deps = [("E", e, self.cnt[e]) for e in ENG if self.cnt[e]]
        deps += [("D", i, v) for i, v in enumerate(self.dval) if v]
        for e in ENG:
            waits = self._waits(e, deps)
            if waits:
                self.q[e].append((waits, None, None))

    def emit(self):
        nc = self.nc
        allsems = [self.esem[e] for e in ENG] + self.dsem
        with nc.Block("init") as b0:
            @b0.vector
            def _(v):
                for s in allsems:
                    v.sem_clear(s)
        with nc.Block("main") as blk:
            def run(e, name):
                for waits, fn, inc in self.q[name]:
                    for sem, val in waits:
                        e.wait_ge(sem, val)
                    if fn is not None:
                        fn(e).then_inc(inc[0], inc[1])

            @blk.tensor
            def _(e):
                run(e, "pe")

            @blk.scalar
            def _(e):
                run(e, "act")

            @blk.vector
            def _(e):
                run(e, "dve")

            @blk.gpsimd
            def _(e):
                run(e, "pool")

            @blk.sync
            def _(e):
                run(e, "sp")


def _rope_tab(rd, pos_row, pos_col, n_ctx):
    half = rd // 2
    nf = half // 2
    inv = 10000.0 ** (-np.arange(nf, dtype=np.float64) / nf)
    n = len(pos_row)
    cos = np.ones((rd, n_ctx + n), np.float64)
    sin = np.zeros((rd, n_ctx + n), np.float64)
    for r in range(rd):
        hh, rr = r // half, r % half
        idx = rr % nf
        pos = pos_row if hh == 0 else pos_col
        ang = pos.astype(np.float64) * inv[idx]
        ang = (pos.astype(np.float32) * np.float32(inv[idx]).astype(np.float32)).astype(np.float64)
        cos[r, n_ctx:] = np.cos(ang)
        sin[r, n_ctx:] = np.sin(ang)
    return cos, sin


def make_consts(NLAT, NCTX):
    bf = ml_dtypes.bfloat16
    T = NLAT + NCTX
    c = {}
    c["ident_bf"] = np.eye(128).astype(bf)
    c["ones_bf"] = np.ones((128, 128)).astype(bf)
    c["ident_f"] = np.eye(128, dtype=np.float32)
    c["ones_f"] = np.ones((128, 128), np.float32)
    s = np.arange(128)[:, None]
    t = np.arange(128)[None, :]
    c["tri_f"] = (s <= t).astype(np.float32)
    c["tri_b"] = (s >= t).astype(np.float32)
    mf = np.where(t <= s, 0.0, NBIG).astype(np.float32)
    mb = np.where(t >= s, 0.0, NBIG).astype(np.float32)
    c["mneg_f"] = np.repeat(mf[:, None, :], 4, axis=1).copy()
    c["mneg_b"] = np.repeat(mb[:, None, :], 4, axis=1).copy()
    el = np.zeros((128, 128), np.float32); el[127, :] = 1
    ef = np.zeros((128, 128), np.float32); ef[0, :] = 1
    c["e_last"] = el
    c["e_first"] = ef
    c["maskA"] = np.where(t >= s, 0.0, NBIG).astype(bf)
    c["maskB"] = np.where(t <= s, 0.0, NBIG).astype(bf)
    c["maskN"] = np.full((128, 128), NBIG).astype(bf)
    rows = NLAT // 64
    pr = np.repeat(np.arange(rows), 64)
    pc = np.tile(np.arange(64), rows)
    ca, sa = _rope_tab(32, pr, pc, NCTX)
    sc_a = 96.0 ** -0.5
    cosq = np.ones((96, T)); sinq = np.zeros((96, T))
    cosq[64:] = ca; sinq[64:] = sa
    c["cosq_a"] = (cosq * sc_a).astype(np.float32)
    c["sinq_a"] = (sinq * sc_a).astype(np.float32)
    c["cosk_a"] = ca.astype(np.float32)
    c["sink_a"] = sa.astype(np.float32)
    cc, sc = _rope_tab(64, pr, pc, NCTX)
    c["cos_c"] = cc.astype(np.float32)
    c["sin_c"] = sc.astype(np.float32)
    j = np.arange(64)
    Cc = np.cos(2 * np.pi * np.outer(j, j) / 64)
    Sc = np.sin(2 * np.pi * np.outer(j, j) / 64)
    z = np.zeros((64, 64))
    c["bdc"] = np.block([[Cc, z], [z, Cc]]).astype(bf)
    c["bds"] = (-np.block([[Sc, z], [z, Sc]])).astype(bf)
    for nm, N in (("lat", NLAT), ("ctx", NCTX)):
        n = np.arange(N)
        ph = (np.outer(n, n) % N).astype(np.float64) * (2 * np.pi / N)
        nrm = 1.0 / np.sqrt(N * 64.0)
        c["cn_" + nm] = (np.cos(ph) * nrm).astype(bf)
        c["sn_" + nm] = (np.sin(ph) * nrm).astype(bf)
    return c


CONST_DT = {"ident_bf": BF16, "ones_bf": BF16, "maskA": BF16, "maskB": BF16, "maskN": BF16, "bdc": BF16, "bds": BF16,
            "cn_lat": BF16, "sn_lat": BF16, "cn_ctx": BF16, "sn_ctx": BF16}

W_SHAPES = {
    "w_mod": (D, 6 * D), "w_in": (D, 2352), "w_uq": (256, 384), "w_ukv": (256, 512),
    "w_gate": (4, D, D), "w_branch": (4, 256, D), "w_out": (D, D), "w_up": (D, 2 * DFF), "w_down": (DFF, D),
}


def layout_params(inp, L):
    o = {}

    def fm(v, k):
        v = np.asarray(v, np.float32)
        lead = v.shape[:-1]
        return np.ascontiguousarray(np.moveaxis(v.reshape(lead + (k, 128)), -1, 0))

    o["b_mod"] = np.stack([fm(inp["b_mod"][l], 48) for l in range(L)])
    for nm in ("g_pre_mix", "g_post_mix", "g_pre_ffn", "g_post_ffn"):
        o[nm] = np.stack([fm(inp[nm][l], 8) for l in range(L)])
    o["g_qa"] = np.stack([fm(inp["g_qa"][l], 2) for l in range(L)])
    o["g_kva"] = np.stack([fm(inp["g_kva"][l], 2) for l in range(L)])
    o["b_gate"] = np.stack([fm(inp["b_gate"][l], 8) for l in range(L)])
    o["w_ffn_conv"] = np.stack([fm(inp["w_ffn_conv"][l], FC) for l in range(L)])
    o["b_ffn_conv"] = np.stack([fm(inp["b_ffn_conv"][l], FC) for l in range(L)])
    o["w_ml_conv"] = np.stack([fm(inp["w_ml_conv"][l], 4) for l in range(L)])
    o["b_ml_gates"] = np.asarray(inp["b_ml_gates"], np.float32).reshape(L, 1, 16)
    o["wg_sink"] = np.asarray(inp["wg_sink"], np.float32).reshape(L, 1, 4)
    return o


PARAM_SHAPES = lambda L: {
    "b_mod": (L, 128, 48), "g_pre_mix": (L, 128, 8), "g_post_mix": (L, 128, 8), "g_pre_ffn": (L, 128, 8),
    "g_post_ffn": (L, 128, 8), "g_qa": (L, 128, 2), "g_kva": (L, 128, 2), "b_gate": (L, 128, 4, 8),
    "w_ffn_conv": (L, 128, 3, FC), "b_ffn_conv": (L, 128, FC), "w_ml_conv": (L, 128, 3, 4),
    "b_ml_gates": (L, 1, 16), "wg_sink": (L, 1, 4), "wconvB": (L, 64, 8, 3),
}


class Builder:
    def __init__(self, NLAT, NCTX, NSEQ, L, consts, dbg=None):
        self.NLAT, self.NCTX, self.NSEQ, self.L = NLAT, NCTX, NSEQ, L
        self.T = T = NLAT + NCTX
        self.TT = T // 128
        self.CT = NCTX // 128
        self.blocks = [(0, NCTX)] + [(NCTX + i, min(NCTX + i + 512, T)) for i in range(0, NLAT, 512)]
        self.dbg = dbg
        nc = self.nc = bass.Bass("TRN2", target_bir_lowering=False)
        self.st = ExitStack()
        self.P = Prog(nc, self.st)
        di = lambda n, s, dt=F32: nc.dram_tensor(n, list(s), dt, kind="ExternalInput").ap()
        self.xT_in = di("xT", (NSEQ, D, NLAT))
        self.cT_in = di("ctxT", (NSEQ, D, NCTX))
        self.ccT = di("ccT", (128, KC, NSEQ + 1))
        self.W = {k: di(k, (L,) + v) for k, v in W_SHAPES.items()}
        self.PR = {k: di(k, v) for k, v in PARAM_SHAPES(L).items()}
        self.CD = {k: di(k, v.shape, CONST_DT.get(k, F32)) for k, v in consts.items()}
        self.out = nc.dram_tensor("outT", [NSEQ, D, NLAT], F32, kind="ExternalOutput").ap()
        ds = lambda n, s, dt: nc.dram_tensor(n, list(s), dt, kind="Internal").ap()
        self.XRES = ds("xres", (NSEQ, D, T), F32)
        self.YS = ds("ys", (NSEQ, D, T), BF16)
        self.WI = ds("wi_bf", (L, 128, KC * 2352), BF16)
        self.WG = ds("wg_bf", (L, 4, KC, 128, KC * 128), BF16)
        self.WBR = ds("wbr_bf", (L, 4, 128, 2 * D), BF16)
        self.WO = ds("wo_bf", (L, 128, KC * D), BF16)
        self.WU = ds("wu_bf", (L, FC, 128, KC * 256), BF16)
        self.WD = ds("wd_bf", (L, 128, FC * D), BF16)
        self.r_wbf = Res("wbf")
        self.r_xres = [Res("xres%d" % i) for i in range(NSEQ)]
        self.r_ys = [Res("ys%d" % i) for i in range(NSEQ)]
        self.r_out = Res("out")
        if dbg:
            self.dbg_out = {k: nc.dram_tensor("dbg_" + k, list(s), dt, kind="ExternalOutput").ap() for k, (s, dt) in dbg.items()}
        self.PS = nc.alloc_psum_tensor("PS", [128, 4096], F32)
        self.r_ps = [Res("ps%d" % i) for i in range(8)]
        self.ARENA_E = 98000
        self.arena = nc.alloc_sbuf_tensor("arena", [128, self.ARENA_E], BF16)
        self.a_off = 0
        self.a_base = 0
        self.phase_log = []

    def tile(self, shape, dt, name=""):
        n = int(np.prod(shape[1:]))
        ne = n * (2 if dt == F32 else 1)
        off = (self.a_off + 15) // 16 * 16
        assert off + ne <= self.ARENA_E, ("arena overflow", name, off, ne)
        self.a_off = off + ne
        ap = self.arena[0:shape[0], off:off + ne]
        if dt == F32:
            ap = ap.bitcast(F32)
        if len(shape) == 3:
            ap = ap.rearrange("p (a b) -> p a b", a=shape[1])
        elif len(shape) == 4:
            ap = ap.rearrange("p (a b c) -> p a b c", a=shape[1], b=shape[2])
        return ap, Res(name)

    def phase(self):
        self.P.barrier()
        self.a_off = self.a_base
        import sys
        self.phase_log.append((sys._getframe(1).f_code.co_name, dict(self.P.cnt)))

    def bank(self, b, n=512, lo=0):
        return self.PS[:, b * 512 + lo:b * 512 + lo + n]

    def bank_bf(self, b):
        return self.PS[:, b * 512:(b + 1) * 512].bitcast(BF16)

    def mm(self, out, lhsT, rhs, start, stop, reads, writes):
        self.P.op("pe", lambda e: e.matmul(out, lhsT, rhs, start=start, stop=stop, skip_group_check=True), reads, writes)

    def tr(self, out, in_, ident, reads, writes):
        self.P.op("pe", lambda e: e.transpose(out, in_, ident), reads, writes)

    def act(self, out, in_, func, reads, writes, bias=None, scale=None):
        kw = {}
        if bias is not None:
            kw["bias"] = bias
        if scale is not None:
            kw["scale"] = scale
        self.P.op("act", lambda e: e.activation(out=out, in_=in_, func=func, **kw), reads, writes)

    def tt(self, out, a, b, op, reads, writes, eng="dve"):
        self.P.op(eng, lambda e: e.tensor_tensor(out=out, in0=a, in1=b, op=op), reads, writes)

    def ts(self, out, a, s1, op0, reads, writes, s2=None, op1=None, eng="dve"):
        if op1 is None:
            self.P.op(eng, lambda e: e.tensor_scalar(out=out, in0=a, scalar1=s1, scalar2=None, op0=op0), reads, writes)
        else:
            self.P.op(eng, lambda e: e.tensor_scalar(out=out, in0=a, scalar1=s1, scalar2=s2, op0=op0, op1=op1), reads, writes)

    def stt(self, out, a, s, b, op0, op1, reads, writes):
        self.P.op("dve", lambda e: e.scalar_tensor_tensor(out=out, in0=a, scalar=s, in1=b, op0=op0, op1=op1), reads, writes)

    def cp(self, out, in_, reads, writes, eng="dve"):
        if eng == "act":
            self.P.op("act", lambda e: e.copy(out=out, in_=in_), reads, writes)
        else:
            self.P.op(eng, lambda e: e.tensor_copy(out=out, in_=in_), reads, writes)

    def red(self, out, in_, op, reads, writes):
        self.P.op("dve", lambda e: e.tensor_reduce(out=out, in_=in_, axis=AX.X, op=op), reads, writes)

    def recip(self, out, in_, reads, writes):
        self.P.op("dve", lambda e: e.reciprocal(out=out, in_=in_), reads, writes)

    def ld(self, out, in_, reads, writes, q="sp"):
        self.P.dma(q, out, in_, reads, writes)

    def prepass(self):
        SZ = 2560
        stf = [self.tile([128, SZ], F32, "stf%d" % i) for i in range(3)]
        stb = [self.tile([128, SZ], BF16, "stb%d" % i) for i in range(3)]
        pieces = []

        def piece(srcs, dst, a, b):
            pieces.append((srcs, dst, a, b))

        def views(j):
            srcs, dst, a, b = pieces[j]
            (f, r_f), (bt, r_b) = stf[j % 3], stb[j % 3]
            fv = f[:, 0:a * b].rearrange("p (a b) -> p a b", a=a)
            bv = bt[:, 0:a * b].rearrange("p (a b) -> p a b", a=a)
            return srcs, dst, fv, bv, r_f, r_b

        def emit_in(j):
            srcs, dst, fv, bv, r_f, r_b = views(j)
            for (c0, c1, src) in srcs:
                self.ld(fv[:, :, c0:c1], src, [], [r_f])

        def emit_rest(j):
            srcs, dst, fv, bv, r_f, r_b = views(j)
            self.cp(bv, fv, [r_f], [r_b], eng=("act", "dve", "pool")[j % 3])
            self.ld(dst, bv, [r_b], [self.r_wbf])

        for l in range(self.L):
            wi = self.W["w_in"][l].rearrange("(k p) n -> p k n", p=128)
            wid = self.WI[l].rearrange("p (k n) -> p k n", k=KC)
            for k in range(KC):
                piece([(0, 2352, wi[:, k:k + 1, :])], wid[:, k:k + 1, :], 1, 2352)
            for br in range(4):
                for dc in range(KC):
                    src = self.W["w_gate"][l][br][:, dc * 128:(dc + 1) * 128].rearrange("(k p) n -> p k n", p=128)
                    piece([(0, 128, src)], self.WG[l, br, dc].rearrange("p (k n) -> p k n", k=KC), KC, 128)
                src = self.W["w_branch"][l][br].rearrange("(k p) n -> p k n", p=128)
                piece([(0, D, src)], self.WBR[l, br].rearrange("p (k n) -> p k n", k=2), 2, D)
            wo = self.W["w_out"][l].rearrange("(k p) n -> p k n", p=128)
            wod = self.WO[l].rearrange("p (k n) -> p k n", k=KC)
            for k in range(0, KC, 2):
                piece([(0, D, wo[:, k:k + 2, :])], wod[:, k:k + 2, :], 2, D)
            for fc in range(FC):
                sa = self.W["w_up"][l][:, fc * 128:(fc + 1) * 128].rearrange("(k p) n -> p k n", p=128)
                sv = self.W["w_up"][l][:, DFF + fc * 128:DFF + (fc + 1) * 128].rearrange("(k p) n -> p k n", p=128)
                piece([(0, 128, sa), (128, 256, sv)], self.WU[l, fc].rearrange("p (k n) -> p k n", k=KC), KC, 256)
            wdn = self.W["w_down"][l].rearrange("(f p) n -> p f n", p=128)
            wdd = self.WD[l].rearrange("p (f n) -> p f n", f=FC)
            for f0 in range(0, FC, 2):
                piece([(0, D, wdn[:, f0:f0 + 2, :])], wdd[:, f0:f0 + 2, :], 2, D)
        npc = len(pieces)
        for j in range(npc + 2):
            if j < npc:
                emit_in(j)
            if j - 2 >= 0:
                emit_rest(j - 2)
            yield

    def build(self):
        P = self.P
        NSEQ, L, T = self.NSEQ, self.L, self.T
        C = {}
        self.C = C
        r_c = self.r_c = Res("consts")
        for k in ("ident_bf", "ones_bf", "ident_f", "ones_f", "tri_f", "tri_b", "e_last", "e_first", "maskA", "maskB", "maskN", "bdc", "bds"):
            C[k], _ = self.tile([128, 128], CONST_DT.get(k, F32), k)
            self.ld(C[k], self.CD[k], [], [r_c])
        for k in ("mneg_f", "mneg_b"):
            C[k], _ = self.tile([128, 4, 128], F32, k)
            self.ld(C[k], self.CD[k], [], [r_c])
        self.hT, self.r_hT = self.tile([128, KC, T], BF16, "hT")
        self.MOD, self.r_mod = self.tile([128, L, 48, NSEQ + 1], F32, "MOD")
        self.pv = {}
        self.r_pv = Res("pvec")
        for k, shp in PARAM_SHAPES(L).items():
            if shp[1] == 128:
                self.pv[k], _ = self.tile([128, L] + list(shp[2:]) if len(shp) > 2 else [128, L], F32, k)
                src = self.PR[k]
                if len(shp) == 3:
                    self.ld(self.pv[k], src.rearrange("l p a -> p l a"), [], [self.r_pv])
                else:
                    for l in range(L):
                        self.ld(self.pv[k][:, l], src[l], [], [self.r_pv])
        self.bg_b, _ = self.tile([128, L, 16], F32, "bgb")
        self.sink_b, _ = self.tile([128, L, 4], F32, "sinkb")
        for l in range(L):
            self.ld(self.bg_b[:, l, :], self.PR["b_ml_gates"][l].partition_broadcast(128), [], [self.r_pv])
            self.ld(self.sink_b[:, l, :], self.PR["wg_sink"][l].partition_broadcast(128), [], [self.r_pv])
        self.wconvB, _ = self.tile([128, 8, 3], F32, "wconvB")
        self.r_wcb = Res("wcb")
        self.dv, self.r_dv = self.tile([128, 6, KC], F32, "derived")
        self.dvc, self.r_dvc = self.tile([128, 6, KC], F32, "derivedc")
        self.a_base = self.a_off
        for s in range(NSEQ):
            self.ld(self.XRES[s][:, 0:self.NCTX], self.cT_in[s], [], [self.r_xres[s]])
            self.ld(self.XRES[s][:, self.NCTX:T], self.xT_in[s], [], [self.r_xres[s]])
        self.phase()
        pg = self.prepass()
        self.mod_phase(pg)
        for _ in pg:
            pass
        import os
        stop = int(os.environ.get("KSTOP", "99"))
        for s in range(NSEQ):
            for l in range(L):
                last = (l == L - 1)
                steps = [lambda: self.derive(l, s), lambda: self.norm_mod(s, 0), lambda: self.branch_a(s, l, last),
                         lambda: self.branch_c(s, l, last), lambda: None, lambda: self.branch_b(s, l, last),
                         lambda: self.merge(s, l, last), lambda: self.norm_mod(s, 3, skip_ctx=last), lambda: self.ffn(s, l, last)]
                for i, f in enumerate(steps):
                    if i < stop:
                        f()
            self.phase()
            self.ld(self.out[s], self.XRES[s][:, self.NCTX:T], [self.r_xres[s]], [self.r_out])
        P.barrier()
        P.emit()
        self.st.close()
        return self.nc

    def mod_phase(self, pg):
        next(pg, None)
        NJ = self.NSEQ + 1
        cc, r_cc = self.tile([128, KC, NJ], F32, "cc")
        sg, r_sg = self.tile([128, KC, NJ], F32, "sg")
        self.ld(cc, self.ccT, [], [r_cc])
        self.act(sg, cc, AF.Sigmoid, [r_cc], [r_sg])
        self.tt(cc, cc, sg, ALU.mult, [r_sg, r_cc], [r_cc])
        wm = [self.tile([128, KC, 512], F32, "wm%d" % i) for i in range(2)]
        n = 0
        for l in range(self.L):
            for g in range(12):
                w, r_w = wm[n % 2]
                n += 1
                self.ld(w, self.W["w_mod"][l][:, g * 512:(g + 1) * 512].rearrange("(k p) n -> p k n", p=128), [], [r_w])
                for _ in range(7):
                    next(pg, None)
                b = n % 2
                for j4 in range(4):
                    for k in range(KC):
                        self.mm(self.bank(b, NJ, j4 * 8), w[:, k, j4 * 128:(j4 + 1) * 128], cc[:, k, :], k == 0 and j4 == 0, k == KC - 1,
                                [r_w, r_cc], [self.r_ps[b]])
                for j4 in range(4):
                    ch = g * 4 + j4
                    self.ts(self.MOD[:, l, ch, :], self.bank(b, NJ, j4 * 8), self.pv["b_mod"][:, l, ch:ch + 1], ALU.add,
                            [self.r_ps[b], self.r_pv], [self.r_mod])

    def derive(self, l, s):
        for (dst, r_dst, j) in ((self.dv, self.r_dv, s), (self.dvc, self.r_dvc, self.NSEQ)):
            for half, gpre, gpost in ((0, "g_pre_mix", "g_post_mix"), (1, "g_pre_ffn", "g_post_ffn")):
                sh = self.MOD[:, l, half * 24 + 0:half * 24 + 8, j]
                sc = self.MOD[:, l, half * 24 + 8:half * 24 + 16, j]
                g = self.MOD[:, l, half * 24 + 16:half * 24 + 24, j]
                self.stt(dst[:, half * 3 + 0, :], sc, 1.0, self.pv[gpre][:, l, :], ALU.add, ALU.mult, [self.r_mod, self.r_pv], [r_dst])
                self.cp(dst[:, half * 3 + 1, :], sh, [self.r_mod], [r_dst])
                self.tt(dst[:, half * 3 + 2, :], g, self.pv[gpost][:, l, :], ALU.mult, [self.r_mod, self.r_pv], [r_dst])

    def conv_blocks(self, skip_ctx=False):
        out = []
        segs = ([] if skip_ctx else [(0, self.NCTX)]) + [(self.NCTX, self.T)]
        for (s0, s1) in segs:
            lo = s0
            while lo < s1:
                hi = min(lo + 510, s1)
                out.append((lo, hi, lo - (1 if lo > s0 else 0), hi + (1 if hi < s1 else 0)))
                lo = hi
        return out

    def seg_dv(self, lo):
        return (self.dvc, self.r_dvc) if lo < self.NCTX else (self.dv, self.r_dv)

    def rstd_block(self, src, r_src, n, nk, dim, sq, r_sq, rs, r_rs, bank):
        for k in range(nk):
            self.act(sq[:, k, 0:n], src[:, k, 0:n], AF.Square, [r_src], [r_sq])
        for k in range(nk):
            self.mm(self.bank(bank, n), self.C["ones_bf"], sq[:, k, 0:n], k == 0, k == nk - 1, [r_sq, self.r_c], [self.r_ps[bank]])
        self.ts(rs[:, 0:n], self.bank(bank, n), 1.0 / dim, ALU.mult, [self.r_ps[bank]], [r_rs], s2=EPS, op1=ALU.add)
        self.act(rs[:, 0:n], rs[:, 0:n], AF.Ln, [r_rs], [r_rs])
        self.act(self.bank(bank, n), rs[:, 0:n], AF.Exp, [r_rs], [self.r_ps[bank]], scale=-0.5)
        return self.bank(bank, n), self.r_ps[bank]

    def norm_mod(self, s, base, skip_ctx=False):
        self.phase()
        xb = [self.tile([128, KC, 512], F32, "xb%d" % i) for i in range(2)]
        sqs = [self.tile([128, KC, 512], BF16, "sq%d" % i) for i in range(2)]
        rss = [self.tile([128, 512], F32, "rs%d" % i) for i in range(2)]
        tmps = [self.tile([128, 512], F32, "tmp%d" % i) for i in range(2)]
        for bi, (lo, hi) in enumerate(self.blocks):
            if skip_ctx and lo < self.NCTX:
                continue
            n = hi - lo
            x, r_x = xb[bi % 2]
            (sq, r_sq), (rs, r_rs) = sqs[bi % 2], rss[bi % 2]
            self.ld(x[:, :, 0:n], self.XRES[s][:, lo:hi].rearrange("(k p) n -> p k n", p=128), [self.r_xres[s]], [r_x])
            rsp, r_rsp = self.rstd_block(x, r_x, n, KC, D, sq, r_sq, rs, r_rs, bi % 2)
            dv, r_dv = self.seg_dv(lo)
            for k in range(KC):
                tmp, r_tmp = tmps[k % 2]
                self.tt(tmp[:, 0:n], x[:, k, 0:n], rsp, ALU.mult, [r_x, r_rsp], [r_tmp])
                self.act(self.hT[:, k, lo:hi], tmp[:, 0:n], AF.Identity, [r_tmp, r_dv], [self.r_hT],
                         bias=dv[:, base + 1, k:k + 1], scale=dv[:, base + 0, k:k + 1])

    def load_w_cols(self, dst, r_dst, l, c0, c1):
        self.ld(dst, self.WI[l].rearrange("p (k n) -> p k n", k=KC)[:, :, c0:c1], [self.r_wbf], [r_dst])

    def make_perm(self, dst, src, nheads, hd, r0, rd, r_dst, r_src, nk):
        nf = rd // 4
        self.P.op("dve", lambda e: e.memset(dst, 0.0), [], [r_dst])
        for k in range(nk):
            for h in range(nheads):
                b = h * hd + r0
                for hh in range(2):
                    o = b + hh * 2 * nf
                    self.ts(dst[:, k, o:o + nf], src[:, k, o + nf:o + 2 * nf], -1.0, ALU.mult, [r_src], [r_dst])
                    self.cp(dst[:, k, o + nf:o + 2 * nf], src[:, k, o:o + nf], [r_src], [r_dst])

    def attn_scores(self, it, buf):
        Pm, r_Pm, sm, r_sm = buf
        kparts, sink, scale = it["kparts"], it["sink"], it["scale"]
        pc = it.get("pcol", 0)
        ncols = max(c0 + k.shape[-1] for (k, _, c0, _) in kparts)
        started = set()
        for (kT, r_k, c0, mask) in kparts:
            n = kT.shape[-1]
            b = (pc + c0) // 512
            assert (pc + c0 + n - 1) // 512 == b
            self.mm(self.PS[:, pc + c0:pc + c0 + n], it["q"], kT, b not in started, mask is None, [it["r_q"], r_k], [self.r_ps[b]])
            started.add(b)
            if mask is not None:
                self.mm(self.PS[:, pc + c0:pc + c0 + n], self.C["ident_bf"], mask, False, True, [self.r_c], [self.r_ps[b]])
        tot = ncols
        if sink is not None:
            b = (pc + ncols) // 512
            self.mm(self.PS[:, pc + ncols:pc + ncols + 1], self.C["ones_f"][0:1, :], sink, b not in started, True, [self.r_c, self.r_pv], [self.r_ps[b]])
            started.add(b)
            tot = ncols + 1
        banks = [self.r_ps[b] for b in sorted(started)]
        self.red(sm[:, 0:1], self.PS[:, pc:pc + tot], ALU.max, banks, [r_sm])
        self.ts(sm[:, 1:2], sm[:, 0:1], -scale, ALU.mult, [r_sm], [r_sm])
        it["ncols"] = ncols
        it["exp"] = (lambda: self.act(Pm[:, 0:tot], self.PS[:, pc:pc + tot], AF.Exp, banks + [r_sm], [r_Pm], bias=sm[:, 1:2], scale=scale))

    def attn_transposes(self, it, buf, PTt, r_PT):
        Pm, r_Pm, sm, r_sm = buf
        vparts = it["vparts"]
        nv = len(vparts)
        for i, (V, r_v, c0) in enumerate(vparts):
            tb = 5 + (i // 8) % 2
            slot = i % 8
            pt_ps = self.bank_bf(tb)[:, slot * 128:(slot + 1) * 128]
            self.tr(pt_ps, Pm[:, c0:c0 + 128], self.C["ident_bf"], [r_Pm, self.r_c], [self.r_ps[tb]])
            if slot == 7 or i == nv - 1:
                g0 = i - slot
                self.cp(PTt[:, g0:i + 1, :], self.bank_bf(tb)[:, 0:(slot + 1) * 128].rearrange("p (a b) -> p a b", b=128),
                        [self.r_ps[tb]], [r_PT], eng="act")

    def attn_pv(self, it, buf, PTt, r_PT):
        Pm, r_Pm, sm, r_sm = buf
        vparts, sink, ncols = it["vparts"], it["sink"], it["ncols"]
        nv = len(vparts)
        for i, (V, r_v, c0) in enumerate(vparts):
            self.mm(self.bank(7, 65), PTt[:, i, :], V, i == 0, i == nv - 1, [r_PT, r_v], [self.r_ps[7]])
        if sink is not None:
            self.tt(sm[:, 2:3], self.bank(7, 1, 64), Pm[:, ncols:ncols + 1], ALU.add, [self.r_ps[7], r_Pm], [r_sm])
            self.recip(sm[:, 3:4], sm[:, 2:3], [r_sm], [r_sm])
        else:
            self.recip(sm[:, 3:4], self.bank(7, 1, 64), [self.r_ps[7]], [r_sm])
        self.ts(it["out"], self.bank(7, 64), sm[:, 3:4], ALU.mult, [self.r_ps[7], r_sm], [it["r_out"]])
        if it.get("after"):
            it["after"]()

    def attn_run(self, items, pingpong=False, hook=None):
        if pingpong:
            for i, it in enumerate(items):
                it["pcol"] = (i % 2) * 1024
        bufs = []
        for j in range(2):
            Pm, r_Pm = self.tile([128, self.T + 128], BF16, "Pm%d" % j)
            sm, r_sm = self.tile([128, 4], F32, "sm%d" % j)
            bufs.append((Pm, r_Pm, sm, r_sm))
        PTt, r_PT = self.tile([128, self.TT, 128], BF16, "PT")
        n = len(items)
        if n == 0:
            return
        self.attn_scores(items[0], bufs[0])
        items[0]["exp"]()
        for i in range(n):
            if i + 1 < n:
                self.attn_scores(items[i + 1], bufs[(i + 1) % 2])
            self.attn_transposes(items[i], bufs[i % 2], PTt, r_PT)
            if i + 1 < n:
                items[i + 1]["exp"]()
            if hook is not None:
                hook()
            self.attn_pv(items[i], bufs[i % 2], PTt, r_PT)

    def store_y_tm(self, ytm, r_ytm, s, br, tile_i, ybuf):
        yT, r_yT = ybuf
        for c in range(2):
            self.tr(self.bank_bf(6)[:, c * 128:(c + 1) * 128], ytm[:, c * 128:(c + 1) * 128], self.C["ident_bf"], [r_ytm, self.r_c], [self.r_ps[6]])
        self.cp(yT, self.bank_bf(6)[:, 0:256].rearrange("p (a b) -> p a b", b=128), [self.r_ps[6]], [r_yT])
        self.ld(self.YS[s][br * 256:(br + 1) * 256, tile_i * 128:(tile_i + 1) * 128].rearrange("(c p) n -> p c n", p=128), yT,
                [r_yT], [self.r_ys[s]])

    def branch_a(self, s, l, last):
        self.phase()
        T, TT, CT = self.T, self.TT, self.CT
        wA, r_wA = self.tile([128, KC, 544], BF16, "wA")
        wAp, r_wAp = self.tile([128, KC, 32], BF16, "wAp")
        wq, r_wq = self.tile([128, 2, 384], BF16, "wq")
        wqf, r_wqf = self.tile([128, 2, 384], F32, "wqf")
        wqp, r_wqp = self.tile([128, 2, 384], BF16, "wqp")
        wkv, r_wkv = self.tile([128, 2, 512], BF16, "wkv")
        wkvf, r_wkvf = self.tile([128, 2, 512], F32, "wkvf")
        raw, r_raw = self.tile([128, 4, 512], F32, "raw")
        sq, r_sq = self.tile([128, 4, 512], BF16, "sqA")
        rs, r_rs = self.tile([128, 2, 512], F32, "rsA")
        cqn, r_cqn = self.tile([128, 4, 512], BF16, "cqn")
        tab, r_tab = self.tile([128, 4, 512], F32, "tabA")
        t1, r_t1 = self.tile([128, 512], F32, "t1A")
        t2, r_t2 = self.tile([128, 512], F32, "t2A")
        qT, r_qT = self.tile([128, 4, T], BF16, "qTA")
        kT, r_kT = self.tile([128, 4, T], BF16, "kTA")
        Va, r_Va = self.tile([128, TT, 4, 65], BF16, "VaA")
        ytms = [self.tile([128, 256], BF16, "ytmA%d" % i) for i in range(2)]
        ybuf = self.tile([128, 2, 128], BF16, "yTA")
        self.load_w_cols(wA, r_wA, l, 0, 544)
        self.ld(wqf, self.W["w_uq"][l].rearrange("(k p) n -> p k n", p=128), [], [r_wqf])
        self.ld(wkvf, self.W["w_ukv"][l].rearrange("(k p) n -> p k n", p=128), [], [r_wkvf])
        for k in range(2):
            self.ts(wq[:, k, :], wqf[:, k, :], self.pv["g_qa"][:, l, k:k + 1], ALU.mult, [r_wqf, self.r_pv], [r_wq])
            self.ts(wkv[:, k, :], wkvf[:, k, :], self.pv["g_kva"][:, l, k:k + 1], ALU.mult, [r_wkvf, self.r_pv], [r_wkv])
        self.make_perm(wqp, wq, 4, 96, 64, 32, r_wqp, r_wq, 2)
        wkr = wA[:, :, 512:544]
        self.make_perm(wAp, wkr, 1, 32, 0, 32, r_wAp, r_wA, KC)
        self.P.op("dve", lambda e: e.memset(Va, 1.0), [], [r_Va])
        for bi, (lo, hi) in enumerate(self.blocks):
            n = hi - lo
            for c in range(4):
                b = c % 2
                for k in range(KC):
                    self.mm(self.bank(b, n), wA[:, k, c * 128:(c + 1) * 128], self.hT[:, k, lo:hi], k == 0, k == KC - 1,
                            [r_wA, self.r_hT], [self.r_ps[b]])
                self.cp(raw[:, c, 0:n], self.bank(b, n), [self.r_ps[b]], [r_raw], eng="act")
            rp0 = self.rstd_block(raw[:, 0:2], r_raw, n, 2, 256, sq[:, 0:2], r_sq, rs[:, 0], r_rs, 6)
            rp1 = self.rstd_block(raw[:, 2:4], r_raw, n, 2, 256, sq[:, 2:4], r_sq, rs[:, 1], r_rs, 7)
            for c in range(4):
                rp, r_rp = (rp0, rp1)[c // 2]
                self.tt(cqn[:, c, 0:n], raw[:, c, 0:n], rp, ALU.mult, [r_raw, r_rp], [r_cqn])
            self.ld(tab[0:96, 0, 0:n], self.CD["cosq_a"][:, lo:hi], [], [r_tab])
            self.ld(tab[0:96, 1, 0:n], self.CD["sinq_a"][:, lo:hi], [], [r_tab])
            self.ld(tab[0:32, 2, 0:n], self.CD["cosk_a"][:, lo:hi], [], [r_tab])
            self.ld(tab[0:32, 3, 0:n], self.CD["sink_a"][:, lo:hi], [], [r_tab])
            for h in range(4):
                for (w_, r_w_, b) in ((wq, r_wq, 0), (wqp, r_wqp, 1)):
                    for k in range(2):
                        self.mm(self.bank(b, n)[0:96], w_[:, k, h * 96:(h + 1) * 96], cqn[:, k, 0:n], k == 0, k == 1, [r_w_, r_cqn], [self.r_ps[b]])
                self.tt(t1[0:96, 0:n], self.bank(0, n)[0:96], tab[0:96, 0, 0:n], ALU.mult, [self.r_ps[0], r_tab], [r_t1])
                self.tt(t2[0:96, 0:n], self.bank(1, n)[0:96], tab[0:96, 1, 0:n], ALU.mult, [self.r_ps[1], r_tab], [r_t2])
                self.tt(qT[0:96, h, lo:hi], t1[0:96, 0:n], t2[0:96, 0:n], ALU.add, [r_t1, r_t2], [r_qT])
                for k in range(2):
                    self.mm(self.bank(2, n)[0:64], wkv[:, k, h * 128:h * 128 + 64], cqn[:, 2 + k, 0:n], k == 0, k == 1, [r_wkv, r_cqn], [self.r_ps[2]])
                self.cp(kT[0:64, h, lo:hi], self.bank(2, n)[0:64], [self.r_ps[2]], [r_kT], eng="act")
            for (w_, r_w_, b) in ((wkr, r_wA, 3), (wAp, r_wAp, 4)):
                for k in range(KC):
                    self.mm(self.bank(b, n)[0:32], w_[:, k, :], self.hT[:, k, lo:hi], k == 0, k == KC - 1, [r_w_, self.r_hT], [self.r_ps[b]])
            self.tt(t1[0:32, 0:n], self.bank(3, n)[0:32], tab[0:32, 2, 0:n], ALU.mult, [self.r_ps[3], r_tab], [r_t1])
            self.tt(t2[0:32, 0:n], self.bank(4, n)[0:32], tab[0:32, 3, 0:n], ALU.mult, [self.r_ps[4], r_tab], [r_t2])
            self.tt(t1[0:32, 0:n], t1[0:32, 0:n], t2[0:32, 0:n], ALU.add, [r_t1, r_t2], [r_t1])
            for h in range(4):
                self.cp(kT[64:96, h, lo:hi], t1[0:32, 0:n], [r_t1], [r_kT])
            for ti in range(lo // 128, hi // 128):
                o = ti * 128 - lo
                for k in range(2):
                    self.mm(self.bank(5, 256).rearrange("p (h d) -> p h d", d=64), cqn[:, 2 + k, o:o + 128],
                            wkv[:, k, :].rearrange("p (h x) -> p h x", x=128)[:, :, 64:128], k == 0, k == 1, [r_cqn, r_wkv], [self.r_ps[5]])
                self.cp(Va[:, ti, :, 0:64], self.bank(5, 256).rearrange("p (h d) -> p h d", d=64), [self.r_ps[5]], [r_Va])
        q_tiles = list(range(CT, TT)) + ([] if last else list(range(CT)))
        items = []
        for n_, qi in enumerate(q_tiles):
            is_ctx = qi < CT
            nk = self.NCTX if is_ctx else T
            yt, r_yt = ytms[n_ % 2]
            for h in range(4):
                kparts = []
                c0 = 0
                while c0 < nk:
                    n = min(512, nk - c0)
                    kparts.append((kT[0:96, h, c0:c0 + n], r_kT, c0, None))
                    c0 += n
                vparts = [(Va[:, i, h, :], r_Va, i * 128) for i in range(nk // 128)]
                it = dict(q=qT[0:96, h, qi * 128:(qi + 1) * 128], r_q=r_qT, kparts=kparts, sink=None, vparts=vparts, scale=1.0,
                          out=yt[:, h * 64:(h + 1) * 64], r_out=r_yt)
                if h == 3:
                    it["after"] = (lambda yt=yt, r_yt=r_yt, qi=qi: self.store_y_tm(yt, r_yt, s, 0, qi, ybuf))
                items.append(it)
        self.attn_run(items)

    def branch_c(self, s, l, last):
        self.phase()
        T, TT, CT = self.T, self.TT, self.CT
        wC, r_wC = self.tile([128, KC, 512], BF16, "wC")
        wCp, r_wCp = self.tile([128, KC, 384], BF16, "wCp")
        tab, r_tab = self.tile([128, 2, 512], F32, "tabC")
        t1, r_t1 = self.tile([128, 512], F32, "t1C")
        t2, r_t2 = self.tile([128, 512], F32, "t2C")
        qT, r_qT = self.tile([128, 4, T], BF16, "qTC")
        kT, r_kT = self.tile([128, 2, T], BF16, "kTC")
        Va, r_Va = self.tile([128, TT, 2, 65], BF16, "VaC")
        sk8, r_sk8 = self.tile([128, 4], F32, "sk8")
        ytms = [self.tile([128, 256], BF16, "ytmC%d" % i) for i in range(2)]
        ybuf = self.tile([128, 2, 128], BF16, "yTC")
        self.load_w_cols(wC, r_wC, l, 1584, 2096)
        self.make_perm(wCp, wC[:, :, 0:384], 6, 64, 0, 64, r_wCp, r_wC, KC)
        self.ts(sk8, self.sink_b[:, l, :], 8.0, ALU.mult, [self.r_pv], [r_sk8])
        self.P.op("dve", lambda e: e.memset(Va, 1.0), [], [r_Va])
        for bi, (lo, hi) in enumerate(self.blocks):
            n = hi - lo
            self.ld(tab[0:64, 0, 0:n], self.CD["cos_c"][:, lo:hi], [], [r_tab])
            self.ld(tab[0:64, 1, 0:n], self.CD["sin_c"][:, lo:hi], [], [r_tab])
            for hh in range(6):
                for (w_, r_w_, b) in ((wC, r_wC, 0), (wCp, r_wCp, 1)):
                    for k in range(KC):
                        self.mm(self.bank(b, n)[0:64], w_[:, k, hh * 64:(hh + 1) * 64], self.hT[:, k, lo:hi], k == 0, k == KC - 1,
                                [r_w_, self.r_hT], [self.r_ps[b]])
                self.tt(t1[0:64, 0:n], self.bank(0, n)[0:64], tab[0:64, 0, 0:n], ALU.mult, [self.r_ps[0], r_tab], [r_t1])
                self.tt(t2[0:64, 0:n], self.bank(1, n)[0:64], tab[0:64, 1, 0:n], ALU.mult, [self.r_ps[1], r_tab], [r_t2])
                dst = qT[0:64, hh, lo:hi] if hh < 4 else kT[0:64, hh - 4, lo:hi]
                self.tt(dst, t1[0:64, 0:n], t2[0:64, 0:n], ALU.add, [r_t1, r_t2], [r_qT if hh < 4 else r_kT])
            for ti in range(lo // 128, hi // 128):
                for k in range(KC):
                    self.mm(self.bank(5, 128), self.hT[:, k, ti * 128:(ti + 1) * 128], wC[:, k, 384:512], k == 0, k == KC - 1,
                            [self.r_hT, r_wC], [self.r_ps[5]])
                self.cp(Va[:, ti, :, 0:64], self.bank(5, 128).rearrange("p (h d) -> p h d", d=64), [self.r_ps[5]], [r_Va])
        NQ = TT - CT
        q_tiles = list(range(CT, TT)) + ([] if last else list(range(CT)))
        NC_ = self.NCTX
        items = []
        for n_, qi in enumerate(q_tiles):
            is_ctx = qi < CT
            yt, r_yt = ytms[n_ % 2]
            for h in range(4):
                g = h // 2
                kparts = [(kT[0:64, g, 0:NC_], r_kT, 0, None)]
                vparts = [(Va[:, i, g, :], r_Va, i * 128) for i in range(CT)]
                if not is_ctx:
                    i = qi - CT
                    col = NC_
                    for (j, mk) in ((i - 1, "maskA"), (i, None), (i + 1, "maskB")):
                        if 0 <= j < NQ:
                            kparts.append((kT[0:64, g, NC_ + j * 128:NC_ + (j + 1) * 128], r_kT, col, self.C[mk] if mk else None))
                            vparts.append((Va[:, CT + j, g, :], r_Va, col))
                            col += 128
                it = dict(q=qT[0:64, h, qi * 128:(qi + 1) * 128], r_q=r_qT, kparts=kparts, sink=sk8[0:1, h:h + 1], vparts=vparts, scale=0.125,
                          out=yt[:, h * 64:(h + 1) * 64], r_out=r_yt)
                if h == 3:
                    it["after"] = (lambda yt=yt, r_yt=r_yt, qi=qi: self.store_y_tm(yt, r_yt, s, 2, qi, ybuf))
                items.append(it)
        dg = self.branch_d(s, l, last)
        self.attn_run(items, hook=lambda: next(dg, None))
        for _ in dg:
            pass

    def branch_d(self, s, l, last):
        T, TT, CT = self.T, self.TT, self.CT
        wD, r_wD = self.tile([128, KC, 256], BF16, "wD")
        ud, r_ud = self.tile([128, 2, T], BF16, "udT")
        uc, r_uc = self.tile([128, TT, 2, 256], BF16, "uc_tm")
        cn, r_cn = self.tile([128, 16, 512], BF16, "cn")
        sn, r_sn = self.tile([128, 16, 512], BF16, "sn")
        yo, r_yo = self.tile([128, 2, 512], BF16, "yoD")
        self.load_w_cols(wD, r_wD, l, 2096, 2352)
        yield
        for (lo, hi) in self.blocks:
            n = hi - lo
            for c in range(2):
                b = 2 + c
                for k in range(KC):
                    self.mm(self.bank(b, n), wD[:, k, c * 128:(c + 1) * 128], self.hT[:, k, lo:hi], k == 0, k == KC - 1,
                            [r_wD, self.r_hT], [self.r_ps[b]])
                self.cp(ud[:, c, lo:hi], self.bank(b, n), [self.r_ps[b]], [r_ud], eng="act" if c else "dve")
                yield
        for ti in range(TT):
            for j, m in enumerate(("bdc", "bds")):
                for c in range(2):
                    self.mm(self.bank(4, 128, j * 256 + c * 128), ud[:, c, ti * 128:(ti + 1) * 128], self.C[m], c == 0 and j == 0, True,
                            [r_ud, self.r_c], [self.r_ps[4]])
            self.cp(uc[:, ti], self.bank(4).rearrange("p (a b) -> p a b", a=2), [self.r_ps[4]], [r_uc])
            yield
        segs = [("lat", CT, TT, self.NCTX)] + ([] if last else [("ctx", 0, CT, 0)])
        for nm, t0, t1_, col0 in segs:
            N = (t1_ - t0) * 128
            ntl = t1_ - t0
            for kb in range(0, N, 512):
                n = min(512, N - kb)
                self.ld(cn[:, 0:ntl, 0:n], self.CD["cn_" + nm][:, kb:kb + n].rearrange("(a p) k -> p a k", p=128), [], [r_cn])
                self.ld(sn[:, 0:ntl, 0:n], self.CD["sn_" + nm][:, kb:kb + n].rearrange("(a p) k -> p a k", p=128), [], [r_sn])
                for c in range(2):
                    b = 2 + c
                    for a in range(ntl):
                        self.mm(self.bank(b, n), uc[:, t0 + a, 0, c * 128:(c + 1) * 128], cn[:, a, 0:n], a == 0, False, [r_uc, r_cn], [self.r_ps[b]])
                        self.mm(self.bank(b, n), uc[:, t0 + a, 1, c * 128:(c + 1) * 128], sn[:, a, 0:n], False, a == ntl - 1, [r_uc, r_sn], [self.r_ps[b]])
                        if a % 4 == 3:
                            yield
                    self.cp(yo[:, c, 0:n], self.bank(b, n), [self.r_ps[b]], [r_yo], eng="act" if c else "dve")
                self.ld(self.YS[s][768:1024, col0 + kb:col0 + kb + n].rearrange("(c p) n -> p c n", p=128), yo[:, :, 0:n], [r_yo], [self.r_ys[s]])
                yield

    def branch_b(self, s, l, last):
        self.phase()
        T, TT, CT = self.T, self.TT, self.CT
        wB, r_wB = self.tile([128, KC, 1040], BF16, "wB")
        araw, r_araw = self.tile([128, 514], F32, "arawB")
        c1, r_c1 = self.tile([128, 512], F32, "c1B")
        qk, r_qk = self.tile([128, 8, T], BF16, "qkB")
        ktm, r_ktm = self.tile([128, TT, 256], BF16, "ktmB")
        Va, r_Va = self.tile([128, TT, 4, 65], BF16, "VaB")
        og, r_og = self.tile([128, 256], F32, "ogB")
        G, r_G = self.tile([128, TT, 16], F32, "GB")
        hs, r_hs = self.tile([128, TT, 256], F32, "hsB")
        self.load_w_cols(wB, r_wB, l, 544, 1584)
        self.ld(self.wconvB[0:64], self.PR["wconvB"][l], [], [self.r_wcb])
        self.P.op("dve", lambda e: e.memset(Va, 1.0), [], [r_Va])
        self.P.op("dve", lambda e: e.memset(hs, 0.0), [], [r_hs])
        for (lo, hi, cl, ch) in self.conv_blocks():
            n, w, off = hi - lo, ch - cl, lo - cl
            for hh in range(8):
                c0 = hh * 64
                for k in range(KC):
                    self.mm(self.bank(hh % 2, w)[0:64], wB[:, k, c0:c0 + 64], self.hT[:, k, cl:ch], k == 0, k == KC - 1, [r_wB, self.r_hT], [self.r_ps[hh % 2]])
                self.P.op("dve", lambda e: e.memset(araw[0:64, :], 0.0), [], [r_araw])
                self.cp(araw[0:64, 1 - off:1 - off + w], self.bank(hh % 2, w)[0:64], [self.r_ps[hh % 2]], [r_araw], eng="act")
                wsl = self.wconvB[:, hh, :]
                ctr = self.bank(hh % 2, n, off)[0:64]
                rb = self.r_ps[hh % 2]
                self.act(ctr, ctr, AF.Copy, [rb, self.r_wcb], [rb], scale=wsl[0:64, 1:2])
                self.stt(ctr, araw[0:64, 0:n], wsl[0:64, 0:1], ctr, ALU.mult, ALU.add, [r_araw, self.r_wcb, rb], [rb])
                self.stt(c1[0:64, 0:n], araw[0:64, 2:n + 2], wsl[0:64, 2:3], ctr, ALU.mult, ALU.add, [r_araw, self.r_wcb, rb], [r_c1])
                self.act(c1[0:64, 0:n], c1[0:64, 0:n], AF.Silu, [r_c1], [r_c1])
                self.ts(qk[0:64, hh, lo:hi], c1[0:64, 0:n], 1.0 if hh < 4 else 0.125, ALU.mult, [r_c1], [r_qk])
        Gt, r_Gt = self.tile([128, TT, 2, 4], F32, "GtB")
        for ti in range(TT):
            tsl = slice(ti * 128, (ti + 1) * 128)
            for k in range(KC):
                self.mm(self.bank(2, 16), self.hT[:, k, tsl], wB[:, k, 1024:1040], k == 0, k == KC - 1, [self.r_hT, r_wB], [self.r_ps[2]])
            self.tt(G[:, ti, :], self.bank(2, 16), self.bg_b[:, l, :], ALU.add, [self.r_ps[2], self.r_pv], [r_G])
            for k in range(KC):
                self.mm(self.bank(3, 256), self.hT[:, k, tsl], wB[:, k, 512:768], k == 0, k == KC - 1, [self.r_hT, r_wB], [self.r_ps[3]])
            self.cp(Va[:, ti, :, 0:64], self.bank(3, 256).rearrange("p (h d) -> p h d", d=64), [self.r_ps[3]], [r_Va])
            for h in range(4):
                self.tr(self.bank_bf(5)[:, h * 64:(h + 1) * 64], qk[0:64, 4 + h, tsl], self.C["ident_bf"][0:64, 0:64], [r_qk, self.r_c], [self.r_ps[5]])
            self.cp(ktm[:, ti, :], self.bank_bf(5)[:, 0:256], [self.r_ps[5]], [r_ktm])
        G5 = G.rearrange("p t (a b c) -> p t a b c", a=2, b=2)
        for d_ in range(2):
            fv = G5[:, :, d_, 1, :]
            self.act(Gt[:, :, d_, :], fv, AF.Exp, [r_G], [r_Gt], scale=-1.0)
            self.act(Gt[:, :, d_, :], Gt[:, :, d_, :], AF.Ln, [r_Gt], [r_Gt], bias=1.0)
            self.ts(fv, Gt[:, :, d_, :], -1.0, ALU.mult, [r_Gt], [r_G])
        B_TM, M_, NEGM, WIN, EMT, DEN, DAB, RR, WTM, DEC, MX, DENI = range(12)
        SX = []
        for d_ in range(2):
            X = {}
            for nm, shp, dt in (("diag", [128, 4, 128], F32), ("bBm", [128, 4, 128], F32), ("Wt", [128, 4, 128], F32),
                                ("Sb", [128, 4, 128], BF16), ("ST", [128, 4, 128], BF16), ("kw", [128, 4, 64], BF16),
                                ("sv", [128, 16, 4], F32), ("cm", [128, 8], F32), ("mst", [128, 4], F32), ("tmpi", [128, 4, 65], F32),
                                ("numh", [128, 4, 64], F32), ("Cst", [128, 4, 65], F32), ("Cbf", [128, 4, 65], BF16)):
                X[nm], X["r_" + nm] = self.tile(shp, dt, nm + "B%d" % d_)
            X["tri"] = self.C["tri_f" if d_ == 0 else "tri_b"]
            X["mneg"] = self.C["mneg_f" if d_ == 0 else "mneg_b"]
            X["esel"] = self.C["e_last" if d_ == 0 else "e_first"]
            X["b0"] = 4 * d_
            SX.append(X)
            self.P.op("dve", lambda e, t=X["Cst"]: e.memset(t, 0.0), [], [X["r_Cst"]])
            self.P.op("dve", lambda e, t=X["Cbf"]: e.memset(t, 0.0), [], [X["r_Cbf"]])
            self.P.op("dve", lambda e, t=X["mst"]: e.memset(t, 0.0), [], [X["r_mst"]])

        def chunk(d_, ti):
            X = SX[d_]
            diag, bBm, Wt, Sb, ST, kw, sv, cm, mst, tmpi, numh, Cst, Cbf = (X[k] for k in (
                "diag", "bBm", "Wt", "Sb", "ST", "kw", "sv", "cm", "mst", "tmpi", "numh", "Cst", "Cbf"))
            r_diag, r_bBm, r_Wt, r_Sb, r_ST, r_kw, r_sv, r_cm, r_mst, r_tmpi, r_numh, r_Cst, r_Cbf = (X["r_" + k] for k in (
                "diag", "bBm", "Wt", "Sb", "ST", "kw", "sv", "cm", "mst", "tmpi", "numh", "Cst", "Cbf"))
            b0 = X["b0"]
            bB_b, qk_b, st_b, ms_b = b0, b0 + 1, b0 + 2, b0 + 3
            r0, r1, r2, r3 = self.r_ps[bB_b], self.r_ps[qk_b], self.r_ps[st_b], self.r_ps[ms_b]
            cum_ps = self.bank(ms_b, 4)
            sel_ps = self.bank(ms_b, 8, 8)
            inter_ps = self.bank(ms_b, 260, 16)
            upd_ps = self.bank(ms_b, 260, 16)
            num_ps = self.bank(st_b, 256, 256)
            tsl = slice(ti * 128, (ti + 1) * 128)
            li = G[:, ti, d_ * 8:d_ * 8 + 4]
            lf = G[:, ti, d_ * 8 + 4:d_ * 8 + 8]
            self.mm(cum_ps, X["tri"], lf, True, True, [self.r_c, r_G], [r3])
            for h in range(4):
                self.mm(self.bank(qk_b, 128, h * 128), qk[0:64, h, tsl], qk[0:64, 4 + h, tsl], True, True, [r_qk], [r1])
            yield
            self.tt(sv[:, B_TM, :], li, cum_ps, ALU.subtract, [r_G, r3], [r_sv])
            for h in range(4):
                self.ts(diag[:, h, :], self.C["ident_f"], sv[:, B_TM, h:h + 1], ALU.mult, [self.r_c, r_sv], [r_diag])
            yield
            for h in range(4):
                self.mm(self.bank(bB_b, 128, h * 128), self.C["ones_f"], diag[:, h, :], True, True, [self.r_c, r_diag], [r0])
            yield
            self.tt(bBm, self.bank(bB_b).rearrange("p (h s) -> p h s", h=4), X["mneg"], ALU.add, [r0, self.r_c], [r_bBm])
            self.red(sv[:, MX, :], bBm, ALU.max, [r_bBm], [r_sv])
            self.tt(sv[:, M_, :], sv[:, MX, :], mst, ALU.max, [r_sv, r_mst], [r_sv])
            self.ts(sv[:, NEGM, :], sv[:, M_, :], -1.0, ALU.mult, [r_sv], [r_sv])
            self.tt(sv[:, WIN, :], mst, sv[:, M_, :], ALU.subtract, [r_mst, r_sv], [r_sv])
            self.tt(cm[:, 0:4], cum_ps, sv[:, M_, :], ALU.add, [r3, r_sv], [r_cm])
            self.cp(cm[:, 4:8], sv[:, M_, :], [r_sv], [r_cm])
            yield
            for h in range(4):
                self.act(Wt[:, h, :], bBm[:, h, :], AF.Exp, [r_bBm, r_sv], [r_Wt], bias=sv[:, NEGM, h:h + 1], scale=1.0)
            self.act(sv[:, WIN, :], sv[:, WIN, :], AF.Exp, [r_sv], [r_sv])
            self.act(sv[:, EMT, :], cm[:, 0:4], AF.Exp, [r_cm], [r_sv], scale=-1.0)
            self.mm(sel_ps, X["esel"], cm, True, True, [self.r_c, r_cm], [r3])
            yield
            self.tt(Sb, self.bank(qk_b).rearrange("p (h s) -> p h s", h=4), Wt, ALU.mult, [r1, r_Wt], [r_Sb])
            self.red(sv[:, DENI, :], Sb, ALU.add, [r_Sb], [r_sv])
            self.tt(sv[:, WTM, :], sv[:, B_TM, :], self.bank(ms_b, 4, 12), ALU.subtract, [r_sv, r3], [r_sv])
            self.tt(sv[:, DEC, :], mst, self.bank(ms_b, 4, 12), ALU.subtract, [r_mst, r3], [r_sv])
            self.cp(mst, self.bank(ms_b, 4, 8), [r3], [r_mst])
            yield
            for h in range(4):
                self.tr(self.bank_bf(st_b)[:, h * 128:(h + 1) * 128], Sb[:, h, :], self.C["ident_bf"], [r_Sb, self.r_c], [r2])
            for h in range(4):
                self.mm(self.bank(ms_b, 65, 16 + h * 65), qk[0:64, h, tsl], Cbf[0:64, h, :], True, True, [r_qk, r_Cbf], [r3])
            self.act(sv[:, WTM, :], sv[:, WTM, :], AF.Exp, [r_sv], [r_sv])
            self.act(sv[:, DEC, :], sv[:, DEC, :], AF.Exp, [r_sv], [r_sv])
            yield
            self.cp(ST, self.bank_bf(st_b)[:, 0:512].rearrange("p (h s) -> p h s", h=4), [r2], [r_ST])
            self.tt(tmpi, inter_ps.rearrange("p (h e) -> p h e", h=4), sv[:, WIN, :].unsqueeze(2).to_broadcast([128, 4, 65]), ALU.mult,
                    [r3, r_sv], [r_tmpi])
            self.tt(kw, ktm[:, ti, :].rearrange("p (h e) -> p h e", h=4), sv[:, WTM, :].unsqueeze(2).to_broadcast([128, 4, 64]), ALU.mult,
                    [r_ktm, r_sv], [r_kw])
            yield
            for h in range(4):
                self.mm(self.bank(st_b, 64, 256 + h * 64), ST[:, h, :], Va[:, ti, h, 0:64], True, True, [r_ST, r_Va], [r2])
            for h in range(4):
                self.mm(self.bank(ms_b, 65, 16 + h * 65)[0:64], kw[:, h, :], Va[:, ti, h, :], True, True, [r_kw, r_Va], [r3])
            yield
            self.tt(numh, tmpi[:, :, 0:64], num_ps.rearrange("p (h e) -> p h e", h=4), ALU.add, [r_tmpi, r2], [r_numh])
            self.tt(sv[:, DEN, :], tmpi[:, :, 64], sv[:, DENI, :], ALU.add, [r_tmpi, r_sv], [r_sv])
            self.ts(sv[:, DAB, :], sv[:, DEN, :], -1.0, ALU.mult, [r_sv], [r_sv])
            self.tt(sv[:, DAB, :], sv[:, DAB, :], sv[:, DEN, :], ALU.max, [r_sv], [r_sv])
            self.tt(sv[:, DAB, :], sv[:, DAB, :], sv[:, EMT, :], ALU.max, [r_sv], [r_sv])
            self.recip(sv[:, RR, :], sv[:, DAB, :], [r_sv], [r_sv])
            self.tt(numh, numh, sv[:, RR, :].unsqueeze(2).to_broadcast([128, 4, 64]), ALU.mult, [r_numh, r_sv], [r_numh])
            hsv = hs[:, ti, :].rearrange("p (h e) -> p h e", h=4)
            self.tt(hsv, hsv, numh, ALU.add, [r_hs, r_numh], [r_hs])
            self.tt(Cst[0:64], Cst[0:64], sv[0:64, DEC, :].unsqueeze(2).to_broadcast([64, 4, 65]), ALU.mult, [r_Cst, r_sv], [r_Cst])
            self.tt(Cst[0:64], Cst[0:64], upd_ps[0:64].rearrange("p (h e) -> p h e", h=4), ALU.add, [r_Cst, r3], [r_Cst])
            self.cp(Cbf[0:64], Cst[0:64], [r_Cst], [r_Cbf])
            yield

        orders = [list(range(TT)), list(range(CT - 1, -1, -1)) + list(range(TT - 1, CT - 1, -1))]
        for i in range(TT):
            gens = [chunk(0, orders[0][i]), chunk(1, orders[1][i])]
            alive = True
            while alive:
                alive = False
                for g in gens:
                    try:
                        next(g)
                        alive = True
                    except StopIteration:
                        pass
        ytm, r_ytm = self.tile([128, 256], BF16, "ytmB")
        ybuf = self.tile([128, 2, 128], BF16, "yTB")
        for ti in range(CT if last else 0, TT):
            tsl = slice(ti * 128, (ti + 1) * 128)
            for k in range(KC):
                self.mm(self.bank(4, 256), self.hT[:, k, tsl], wB[:, k, 768:1024], k == 0, k == KC - 1, [self.r_hT, r_wB], [self.r_ps[4]])
            self.act(og, self.bank(4, 256), AF.Sigmoid, [self.r_ps[4]], [r_og])
            self.tt(ytm, og, hs[:, ti, :], ALU.mult, [r_og, r_hs], [r_ytm])
            self.store_y_tm(ytm, r_ytm, s, 1, ti, ybuf)

    def post_norm_res(self, s, lo, hi, yT, r_yT, gp_idx, tl):
        sq, r_sq, rs, r_rs, tmp, r_tmp, xb, r_xb = tl
        n = hi - lo
        rsp, r_rsp = self.rstd_block(yT, r_yT, n, KC, D, sq, r_sq, rs, r_rs, 7)
        dv, r_dv = self.seg_dv(lo)
        for k in range(KC):
            self.tt(tmp[:, 0:n], yT[:, k, 0:n], rsp, ALU.mult, [r_yT, r_rsp], [r_tmp])
            self.stt(xb[:, k, 0:n], tmp[:, 0:n], dv[:, gp_idx, k:k + 1], xb[:, k, 0:n], ALU.mult, ALU.add, [r_tmp, r_dv, r_xb], [r_xb])
        self.ld(self.XRES[s][:, lo:hi].rearrange("(k p) n -> p k n", p=128), xb[:, :, 0:n], [r_xb], [self.r_xres[s]])

    def prefetch_x(self, s, lo, hi, tl):
        xb, r_xb = tl[6], tl[7]
        self.ld(xb[:, :, 0:hi - lo], self.XRES[s][:, lo:hi].rearrange("(k p) n -> p k n", p=128), [self.r_xres[s]], [r_xb])

    def pn_tiles(self):
        sq, r_sq = self.tile([128, KC, 512], BF16, "sqP")
        rs, r_rs = self.tile([128, 512], F32, "rsP")
        tmp, r_tmp = self.tile([128, 512], F32, "tmpP")
        xb, r_xb = self.tile([128, KC, 512], F32, "xbP")
        return (sq, r_sq, rs, r_rs, tmp, r_tmp, xb, r_xb)

    def merge(self, s, l, last):
        self.phase()
        ysb, r_ysb = self.tile([128, 8, 512], BF16, "ysb")
        wg = [self.tile([128, 4, KC, 128], BF16, "wg%d" % i) for i in range(2)]
        wbr = [self.tile([128, 4, 2, 128], BF16, "wbr%d" % i) for i in range(2)]
        sig, r_sig = self.tile([128, 512], F32, "sig")
        accf, r_accf = self.tile([128, 512], F32, "accf")
        tmpm, r_tmpm = self.tile([128, 512], F32, "tmpm")
        acc, r_acc = self.tile([128, KC, 512], BF16, "acc")
        wo, r_wo = self.tile([128, KC, D], BF16, "wo")
        yT, r_yT = self.tile([128, KC, 512], F32, "yTm")
        tl = self.pn_tiles()
        self.ld(wo, self.WO[l].rearrange("p (k n) -> p k n", k=KC), [self.r_wbf], [r_wo])
        it = 0
        for (lo, hi) in self.blocks:
            if last and lo < self.NCTX:
                continue
            n = hi - lo
            self.ld(ysb[:, :, 0:n], self.YS[s][:, lo:hi].rearrange("(k p) n -> p k n", p=128), [self.r_ys[s]], [r_ysb])
            self.prefetch_x(s, lo, hi, tl)
            for dc in range(KC):
                (wg_, r_wg), (wb_, r_wb) = wg[it % 2], wbr[it % 2]
                it += 1
                for br in range(4):
                    self.ld(wg_[:, br], self.WG[l, br, dc].rearrange("p (k n) -> p k n", k=KC), [self.r_wbf], [r_wg])
                    self.ld(wb_[:, br], self.WBR[l, br].rearrange("p (k n) -> p k n", k=2)[:, :, dc * 128:(dc + 1) * 128], [self.r_wbf], [r_wb])
                for br in range(4):
                    bg, bp = br % 2, 2 + br % 2
                    for k in range(KC):
                        self.mm(self.bank(bg, n), wg_[:, br, k, :], self.hT[:, k, lo:hi], k == 0, k == KC - 1, [r_wg, self.r_hT], [self.r_ps[bg]])
                    self.act(sig[:, 0:n], self.bank(bg, n), AF.Sigmoid, [self.r_ps[bg], self.r_pv], [r_sig], bias=self.pv["b_gate"][:, l, br, dc:dc + 1], scale=1.0)
                    for k in range(2):
                        self.mm(self.bank(bp, n), wb_[:, br, k, :], ysb[:, br * 2 + k, 0:n], k == 0, k == 1, [r_wb, r_ysb], [self.r_ps[bp]])
                    if br == 0:
                        self.tt(accf[:, 0:n], sig[:, 0:n], self.bank(bp, n), ALU.mult, [r_sig, self.r_ps[bp]], [r_accf])
                    else:
                        self.tt(tmpm[:, 0:n], sig[:, 0:n], self.bank(bp, n), ALU.mult, [r_sig, self.r_ps[bp]], [r_tmpm])
                        if br < 3:
                            self.tt(accf[:, 0:n], accf[:, 0:n], tmpm[:, 0:n], ALU.add, [r_accf, r_tmpm], [r_accf])
                        else:
                            self.tt(acc[:, dc, 0:n], accf[:, 0:n], tmpm[:, 0:n], ALU.add, [r_accf, r_tmpm], [r_acc])
            for d2 in range(KC):
                b = 4 + d2 % 2
                for k in range(KC):
                    self.mm(self.bank(b, n), wo[:, k, d2 * 128:(d2 + 1) * 128], acc[:, k, 0:n], k == 0, k == KC - 1, [r_wo, r_acc], [self.r_ps[b]])
                self.cp(yT[:, d2, 0:n], self.bank(b, n), [self.r_ps[b]], [r_yT], eng="act" if d2 % 2 else "dve")
            self.post_norm_res(s, lo, hi, yT, r_yT, 2, tl)

    def ffn(self, s, l, last):
        self.phase()
        T = self.T
        wd, r_wd = self.tile([128, FC, D], BF16, "wd")
        wu = [self.tile([128, KC, 256], BF16, "wu%d" % i) for i in range(2)]
        gT, r_gT = self.tile([128, FC, 512], BF16, "gT")
        a_sb, r_a = self.tile([128, 514], F32, "a_sb")
        c1, r_c1 = self.tile([128, 512], F32, "c1F")
        yT, r_yT = self.tile([128, KC, 512], F32, "yTf")
        tl = self.pn_tiles()
        self.ld(wd, self.WD[l].rearrange("p (f n) -> p f n", f=FC), [self.r_wbf], [r_wd])
        wcv, bcv = self.pv["w_ffn_conv"], self.pv["b_ffn_conv"]
        it = 0
        for (lo, hi, cl, ch) in self.conv_blocks(skip_ctx=last):
            n, w, off = hi - lo, ch - cl, lo - cl
            self.prefetch_x(s, lo, hi, tl)
            for fc in range(FC):
                w_, r_w = wu[it % 2]
                it += 1
                self.ld(w_, self.WU[l, fc].rearrange("p (k n) -> p k n", k=KC), [self.r_wbf], [r_w])
                ba, bv = fc % 2, 2 + fc % 2
                for k in range(KC):
                    self.mm(self.bank(ba, w), w_[:, k, 0:128], self.hT[:, k, cl:ch], k == 0, k == KC - 1, [r_w, self.r_hT], [self.r_ps[ba]])
                for k in range(KC):
                    self.mm(self.bank(bv, n), w_[:, k, 128:256], self.hT[:, k, lo:hi], k == 0, k == KC - 1, [r_w, self.r_hT], [self.r_ps[bv]])
                if off == 0:
                    self.P.op("dve", lambda e: e.memset(a_sb[:, 0:1], 0.0), [], [r_a])
                if ch == hi:
                    self.P.op("dve", lambda e, n=n: e.memset(a_sb[:, n + 1:n + 2], 0.0), [], [r_a])
                self.cp(a_sb[:, 1 - off:1 - off + w], self.bank(ba, w), [self.r_ps[ba]], [r_a], eng="act")
                ctr = self.bank(ba, n, off)
                self.act(ctr, ctr, AF.Copy, [self.r_ps[ba], self.r_pv], [self.r_ps[ba]], scale=wcv[:, l, 1, fc:fc + 1])
                self.stt(ctr, a_sb[:, 0:n], wcv[:, l, 0, fc:fc + 1], ctr, ALU.mult, ALU.add, [r_a, self.r_pv, self.r_ps[ba]], [self.r_ps[ba]])
                self.stt(c1[:, 0:n], a_sb[:, 2:n + 2], wcv[:, l, 2, fc:fc + 1], ctr, ALU.mult, ALU.add, [r_a, self.r_pv, self.r_ps[ba]], [r_c1])
                self.act(c1[:, 0:n], c1[:, 0:n], AF.Silu, [r_c1, self.r_pv], [r_c1], bias=bcv[:, l, fc:fc + 1], scale=1.0)
                self.tt(gT[:, fc, 0:n], c1[:, 0:n], self.bank(bv, n), ALU.mult, [r_c1, self.r_ps[bv]], [r_gT])
            for d2 in range(KC):
                b = 4 + d2 % 2
                for fc in range(FC):
                    self.mm(self.bank(b, n), wd[:, fc, d2 * 128:(d2 + 1) * 128], gT[:, fc, 0:n], fc == 0, fc == FC - 1, [r_wd, r_gT], [self.r_ps[b]])
                self.cp(yT[:, d2, 0:n], self.bank(b, n), [self.r_ps[b]], [r_yT], eng="act" if d2 % 2 else "dve")
            self.post_norm_res(s, lo, hi, yT, r_yT, 5, tl)


NLAT_FULL, NCTX_FULL, DEPTH = 2048, 256, 2
_cache = {}


def run_device(inp, NLAT, NCTX, B, L, n_cores):
    NSEQ = B // n_cores
    consts = make_consts(NLAT, NCTX)
    key = (NLAT, NCTX, NSEQ, L)
    bld = Builder(NLAT, NCTX, NSEQ, L, consts)
    nc = bld.build()
    x = np.asarray(inp["x"], np.float32)
    ctx = np.asarray(inp["ctx"], np.float32)
    c = np.asarray(inp["c"], np.float32)
    c_ctx = np.asarray(inp["c_ctx"], np.float32)
    params = layout_params(inp, L)
    wml = np.asarray(inp["w_ml_conv"], np.float32)
    params["wconvB"] = np.ascontiguousarray(wml.reshape(L, 3, 8, 64).transpose(0, 3, 2, 1))
    shared = {k: np.ascontiguousarray(np.asarray(inp[k], np.float32)) for k in W_SHAPES}
    shared.update(params)
    shared.update(consts)
    in_maps = []
    for ci in range(n_cores):
        sl = slice(ci * NSEQ, (ci + 1) * NSEQ)
        cc = np.concatenate([c[sl], c_ctx[None]], 0)
        m = dict(shared)
        m["xT"] = np.ascontiguousarray(x[sl].transpose(0, 2, 1))
        m["ctxT"] = np.ascontiguousarray(ctx[sl].transpose(0, 2, 1))
        m["ccT"] = np.ascontiguousarray(cc.T.reshape(KC, 128, NSEQ + 1).transpose(1, 0, 2))
        in_maps.append(m)
    return nc, in_maps


def kernel(**inp):
    n_cores = 8
    nc, in_maps = run_device(inp, NLAT_FULL, NCTX_FULL, 16, DEPTH, n_cores)
    res = run_bass_kernel_spmd(nc, in_maps, core_ids=list(range(n_cores)))
    outs = [np.asarray(r["outT"]).transpose(0, 2, 1) for r in res.results]
    return np.ascontiguousarray(np.concatenate(outs, 0).astype(np.float32))
```
